# Optimizing a Trainium2 kernel written in Bass

```python
import math
import jax, jax.numpy as jnp
from jax import lax
import numpy as np

D_MODEL = 1024
BATCH = 2
SEQ = 8192
DEPTH = 2

PLE_DIM = 256
NSA_HEADS = 8
NSA_KV_GROUPS = 2
NSA_HEAD_DIM = 64
NSA_HPG = NSA_HEADS // NSA_KV_GROUPS
NSA_Q_W = NSA_HEADS * NSA_HEAD_DIM
NSA_KV_W = NSA_KV_GROUPS * NSA_HEAD_DIM
CMP_BLOCK = 32
CMP_STRIDE = 16
CMP_HIDDEN = 256
SEL_BLOCK = 64
SEL_TOPN = 16
WINDOW = 512
Q_BLOCK = 128
FORCE_SCORE = 1e4
MASK_VALUE = -1e30
SSD_HEADS = 8
SSD_HEAD_DIM = 64
SSD_INNER = SSD_HEADS * SSD_HEAD_DIM
SSD_GROUPS = 2
SSD_STATE = 64
SSD_CHUNK = 128
SSD_XBC_W = SSD_INNER + 2 * SSD_GROUPS * SSD_STATE
CONV_WIDTH = 4
LRU_WIDTH = 512
LRU_BLOCKS = 8
LRU_BLOCK_DIM = LRU_WIDTH // LRU_BLOCKS
LRU_C = 8.0
N_EXPERTS = 16
N_EXPERT_GROUPS = 4
EXPERTS_PER_GROUP = N_EXPERTS // N_EXPERT_GROUPS
TOP_K = 2
D_EXPERT = 256
ALPHA = (2 * DEPTH) ** 0.25
BETA = (8 * DEPTH) ** -0.25
LN_EPS = 1e-5
RMS_EPS = 1e-5
IN_SIZES = (NSA_Q_W, NSA_KV_W, NSA_KV_W, NSA_KV_W, NSA_KV_W, NSA_KV_W, NSA_KV_W, NSA_HEADS * 3,
            SSD_INNER, SSD_XBC_W, SSD_HEADS, LRU_WIDTH, LRU_WIDTH, 3 * D_MODEL)
IN_WIDTH = sum(IN_SIZES)

kernel_name = 'hybrid_nsa_ssd_rglru_moe_block'


def layer_norm(x, g, b):
    xf = x.astype(jnp.float32)
    mu = jnp.mean(xf, -1, keepdims=True)
    xc = xf - mu
    var = jnp.mean(xc * xc, -1, keepdims=True)
    return (xc * lax.rsqrt(var + LN_EPS) * g.astype(jnp.float32) + b.astype(jnp.float32)).astype(x.dtype)


def rms_norm(x, w):
    xf = x.astype(jnp.float32)
    ms = jnp.mean(xf * xf, -1, keepdims=True)
    return (xf * lax.rsqrt(ms + RMS_EPS) * w.astype(jnp.float32)).astype(x.dtype)


def causal_depthwise_conv(x, w, b):
    width = w.shape[0]
    S = x.shape[1]
    xp = jnp.pad(x, ((0, 0), (width - 1, 0), (0, 0)))
    y = b
    for k in range(width):
        y = y + xp[:, k:k + S] * w[k]
    return y


def masked_softmax(s, mask):
    s = jnp.where(mask, s.astype(jnp.float32), MASK_VALUE)
    m = jnp.max(s, -1, keepdims=True)
    e = jnp.where(mask, jnp.exp(s - m), 0.0)
    return e / jnp.maximum(jnp.sum(e, -1, keepdims=True), 1e-30)


def nsa_compress(kv, pe, w1, w2):
    Bsz, S, G, dk = kv.shape
    n_cmp = (S - CMP_BLOCK) // CMP_STRIDE + 1
    idx = np.arange(n_cmp)[:, None] * CMP_STRIDE + np.arange(CMP_BLOCK)[None, :]
    blk = kv[:, idx] + pe[None, None, :, None, :]
    blk = jnp.transpose(blk, (0, 1, 3, 2, 4)).reshape(Bsz, n_cmp, G, CMP_BLOCK * dk)
    return jax.nn.gelu(blk @ w1) @ w2


def nsa_attention(q, k_cmp, v_cmp, k_sel, v_sel, k_win, v_win, gate_logits,
                  pe_k, w1_k, w2_k, pe_v, w1_v, w2_v):
    Bsz, S = q.shape[:2]
    G, Hg, dk = NSA_KV_GROUPS, NSA_HPG, NSA_HEAD_DIM
    scale = dk ** -0.5
    q = q.reshape(Bsz, S, G, Hg, dk)
    k_cmp, v_cmp, k_sel, v_sel, k_win, v_win = [a.reshape(Bsz, S, G, dk) for a in (k_cmp, v_cmp, k_sel, v_sel, k_win, v_win)]
    t = jnp.arange(S)
    kc = nsa_compress(k_cmp, pe_k, w1_k, w2_k)
    vc = nsa_compress(v_cmp, pe_v, w1_v, w2_v)
    n_cmp = kc.shape[1]
    s_cmp = jnp.einsum('bsgjd,bngd->bgjsn', q, kc) * scale
    cmp_mask = (jnp.arange(n_cmp) * CMP_STRIDE + CMP_BLOCK - 1)[None, :] <= t[:, None]
    p_cmp = masked_softmax(s_cmp, cmp_mask)
    o_cmp = jnp.einsum('bgjsn,bngd->bsgjd', p_cmp, vc)
    n_sel = S // SEL_BLOCK
    ratio = SEL_BLOCK // CMP_STRIDE
    strides = np.arange(n_cmp)[:, None] + np.arange(CMP_BLOCK // CMP_STRIDE)[None, :]
    overlap = jnp.sum(jax.nn.one_hot(strides // ratio, n_sel, dtype=jnp.float32), axis=1)
    imp = jnp.einsum('bgjsn,nm->bgsm', p_cmp, overlap)
    blk = jnp.arange(n_sel)
    cur = t // SEL_BLOCK
    forced = (blk[None, :] == 0) | (blk[None, :] == cur[:, None]) | (blk[None, :] == cur[:, None] - 1)
    causal_blk = blk[None, :] * SEL_BLOCK <= t[:, None]
    imp = jnp.where(forced, FORCE_SCORE, jnp.where(causal_blk, imp, -1.0))
    n_top = min(SEL_TOPN, n_sel)
    _, sel_idx = lax.top_k(imp, n_top)
    ks_blocks = k_sel.reshape(Bsz, n_sel, SEL_BLOCK, G, dk).transpose(0, 3, 1, 2, 4)
    vs_blocks = v_sel.reshape(Bsz, n_sel, SEL_BLOCK, G, dk).transpose(0, 3, 1, 2, 4)
    kw_pad = jnp.pad(k_win, ((0, 0), (WINDOW, 0), (0, 0), (0, 0)))
    vw_pad = jnp.pad(v_win, ((0, 0), (WINDOW, 0), (0, 0), (0, 0)))
    bi = jnp.arange(Bsz)[:, None, None, None]
    gi = jnp.arange(G)[None, :, None, None]
    n_qb = S // Q_BLOCK
    q_blocks = q.reshape(Bsz, n_qb, Q_BLOCK, G, Hg, dk).transpose(1, 0, 2, 3, 4, 5)
    idx_blocks = sel_idx.reshape(Bsz, G, n_qb, Q_BLOCK, n_top).transpose(2, 0, 1, 3, 4)
    win_len = WINDOW + Q_BLOCK

    def block_fn(args):
        qb, idxb, t0 = args
        tq = t0 + jnp.arange(Q_BLOCK)
        ksg = ks_blocks[bi, gi, idxb]
        vsg = vs_blocks[bi, gi, idxb]
        s = jnp.einsum('bqgjd,bgqkld->bgjqkl', qb, ksg) * scale
        s = s.reshape(Bsz, G, Hg, Q_BLOCK, n_top * SEL_BLOCK)
        pos = idxb[..., None] * SEL_BLOCK + jnp.arange(SEL_BLOCK)
        smask = (pos <= tq[None, None, :, None, None]).reshape(Bsz, G, 1, Q_BLOCK, n_top * SEL_BLOCK)
        ps = masked_softmax(s, smask)
        o_s = jnp.einsum('bgjqm,bgqmd->bqgjd', ps, vsg.reshape(Bsz, G, Q_BLOCK, n_top * SEL_BLOCK, dk))
        kw = lax.dynamic_slice_in_dim(kw_pad, t0, win_len, axis=1)
        vw = lax.dynamic_slice_in_dim(vw_pad, t0, win_len, axis=1)
        sw = jnp.einsum('bqgjd,bmgd->bgjqm', qb, kw) * scale
        kpos = t0 - WINDOW + jnp.arange(win_len)
        diff = tq[:, None] - kpos[None, :]
        wmask = (diff >= 0) & (diff < WINDOW) & (kpos[None, :] >= 0)
        pw = masked_softmax(sw, wmask)
        o_w = jnp.einsum('bgjqm,bmgd->bqgjd', pw, vw)
        return o_s, o_w

    o_slc, o_win = lax.map(block_fn, (q_blocks, idx_blocks, jnp.arange(n_qb) * Q_BLOCK))
    o_slc = o_slc.transpose(1, 0, 2, 3, 4, 5).reshape(Bsz, S, G, Hg, dk)
    o_win = o_win.transpose(1, 0, 2, 3, 4, 5).reshape(Bsz, S, G, Hg, dk)
    g = jax.nn.sigmoid(gate_logits.reshape(Bsz, S, G, Hg, 3))
    o = g[..., 0:1] * o_cmp + g[..., 1:2] * o_slc + g[..., 2:3] * o_win
    return o.reshape(Bsz, S, NSA_Q_W)


def segsum(a):
    T = a.shape[-1]
    ae = jnp.broadcast_to(a[..., None], a.shape + (T,))
    ae = jnp.where(jnp.tril(jnp.ones((T, T), bool), -1), ae, 0.0)
    cs = jnp.cumsum(ae, axis=-2)
    return jnp.where(jnp.tril(jnp.ones((T, T), bool)), cs, -jnp.inf)


def ssd_chunked(x, dt, A, Bm, Cm):
    Bsz, S, H, P = x.shape
    G, N = Bm.shape[2], Bm.shape[3]
    Hg = H // G
    L = SSD_CHUNK
    nc = S // L
    a = (dt * A).reshape(Bsz, nc, L, H).transpose(0, 3, 1, 2)
    a_cum = jnp.cumsum(a, axis=-1)
    X = (x * dt[..., None]).reshape(Bsz, nc, L, G, Hg, P)
    Bc = Bm.reshape(Bsz, nc, L, G, N)
    Cc = Cm.reshape(Bsz, nc, L, G, N)
    Lmat = jnp.exp(segsum(a)).reshape(Bsz, G, Hg, nc, L, L)
    CB = jnp.einsum('bclgn,bcsgn->bcgls', Cc, Bc)
    y_diag = jnp.einsum('bcgls,bgjcls,bcsgjp->bclgjp', CB, Lmat, X)
    decay_states = jnp.exp(a_cum[..., -1:] - a_cum).reshape(Bsz, G, Hg, nc, L)
    states = jnp.einsum('bclgn,bgjcl,bclgjp->bcgjpn', Bc, decay_states, X)
    a_chunk = jnp.pad(a_cum[..., -1], ((0, 0), (0, 0), (1, 0)))
    decay_chunk = jnp.exp(segsum(a_chunk)).reshape(Bsz, G, Hg, nc + 1, nc + 1)
    states0 = jnp.concatenate([jnp.zeros_like(states[:, :1]), states], axis=1)
    new_states = jnp.einsum('bgjzc,bcgjpn->bzgjpn', decay_chunk, states0)
    prev_states = new_states[:, :-1]
    decay_out = jnp.exp(a_cum).reshape(Bsz, G, Hg, nc, L)
    y_off = jnp.einsum('bclgn,bcgjpn,bgjcl->bclgjp', Cc, prev_states, decay_out)
    return (y_diag + y_off).reshape(Bsz, S, H, P)


def ssd_mixer(z, xbc, dt_raw, conv_w, conv_b, dt_bias, a_log, d_skip, norm_w):
    Bsz, S = z.shape[:2]
    xbc = jax.nn.silu(causal_depthwise_conv(xbc, conv_w, conv_b))
    xs, Bm, Cm = jnp.split(xbc, [SSD_INNER, SSD_INNER + SSD_GROUPS * SSD_STATE], axis=-1)
    xs = xs.reshape(Bsz, S, SSD_HEADS, SSD_HEAD_DIM)
    Bm = Bm.reshape(Bsz, S, SSD_GROUPS, SSD_STATE)
    Cm = Cm.reshape(Bsz, S, SSD_GROUPS, SSD_STATE)
    dt = jax.nn.softplus(dt_raw + dt_bias)
    A = -jnp.exp(a_log)
    y = ssd_chunked(xs, dt, A, Bm, Cm) + d_skip[:, None] * xs
    y = y.reshape(Bsz, S, SSD_INNER) * jax.nn.silu(z)
    return rms_norm(y, norm_w)


def lru_combine(c1, c2):
    a1, b1 = c1
    a2, b2 = c2
    return a1 * a2, a2 * b1 + b2


def rglru_mixer(xr, yr, conv_w, conv_b, wa, ba, wx, bx, lam):
    Bsz, S = xr.shape[:2]
    xr = causal_depthwise_conv(xr, conv_w, conv_b)
    xb = xr.reshape(Bsz, S, LRU_BLOCKS, LRU_BLOCK_DIM)
    r = jax.nn.sigmoid(jnp.einsum('bsnc,ncd->bsnd', xb, wa).reshape(Bsz, S, LRU_WIDTH) + ba)
    i = jax.nn.sigmoid(jnp.einsum('bsnc,ncd->bsnd', xb, wx).reshape(Bsz, S, LRU_WIDTH) + bx)
    log_a = -LRU_C * r * jax.nn.softplus(-lam)
    a = jnp.exp(log_a)
    b = jnp.sqrt(-jnp.expm1(2.0 * log_a)) * (i * xr)
    _, h = lax.associative_scan(lru_combine, (a, b), axis=1)
    return h * jax.nn.gelu(yr)


def moe(x, router_w, router_b, w_gate, w_up, w_down):
    Bsz, S, D = x.shape
    xt = x.reshape(-1, D)
    aff = jax.nn.sigmoid((xt @ router_w).astype(jnp.float32))
    sel = aff + router_b.astype(jnp.float32)
    grp_score = jnp.sum(lax.top_k(sel.reshape(-1, N_EXPERT_GROUPS, EXPERTS_PER_GROUP), TOP_K)[0], axis=-1)
    best = jnp.argmax(grp_score, axis=-1)
    in_grp = (jnp.arange(N_EXPERTS) // EXPERTS_PER_GROUP)[None, :] == best[:, None]
    _, top_idx = lax.top_k(jnp.where(in_grp, sel, -jnp.inf), TOP_K)
    w = jnp.take_along_axis(aff, top_idx, axis=-1)
    w = w / jnp.sum(w, -1, keepdims=True)
    gates = jnp.sum(jax.nn.one_hot(top_idx, N_EXPERTS, dtype=jnp.float32) * w[..., None], axis=1)
    h = jax.nn.silu(jnp.einsum('td,edf->etf', xt, w_gate)) * jnp.einsum('td,edf->etf', xt, w_up)
    h = h * gates.T[:, :, None].astype(h.dtype)
    return jnp.einsum('etf,efd->td', h, w_down).reshape(Bsz, S, D)


def setup_inputs(seed: int = 0) -> dict:
    key = jax.random.key(seed)
    ks = iter(jax.random.split(key, 48))

    def nrm(shape, scale):
        return jax.random.normal(next(ks), shape, jnp.float32) * scale

    def unif(shape, lo, hi):
        return jax.random.uniform(next(ks), shape, jnp.float32, lo, hi)

    dk = NSA_HEAD_DIM
    dt0 = jnp.exp(unif((DEPTH, SSD_HEADS), math.log(1e-3), math.log(1e-1)))
    a0 = unif((DEPTH, LRU_WIDTH), 0.9, 0.999) ** (1.0 / LRU_C)
    return {
        'x': nrm((BATCH, SEQ, D_MODEL), 1.0),
        'p': nrm((DEPTH, BATCH, SEQ, PLE_DIM), 1.0),
        'w_in': nrm((DEPTH, D_MODEL, IN_WIDTH), D_MODEL ** -0.5),
        'nsa_pe_k': nrm((DEPTH, CMP_BLOCK, dk), 0.1),
        'nsa_w1_k': nrm((DEPTH, CMP_BLOCK * dk, CMP_HIDDEN), (CMP_BLOCK * dk) ** -0.5),
        'nsa_w2_k': nrm((DEPTH, CMP_HIDDEN, dk), CMP_HIDDEN ** -0.5),
        'nsa_pe_v': nrm((DEPTH, CMP_BLOCK, dk), 0.1),
        'nsa_w1_v': nrm((DEPTH, CMP_BLOCK * dk, CMP_HIDDEN), (CMP_BLOCK * dk) ** -0.5),
        'nsa_w2_v': nrm((DEPTH, CMP_HIDDEN, dk), CMP_HIDDEN ** -0.5),
        'ssd_conv_w': nrm((DEPTH, CONV_WIDTH, SSD_XBC_W), CONV_WIDTH ** -0.5),
        'ssd_conv_b': nrm((DEPTH, SSD_XBC_W), 0.02),
        'ssd_dt_bias': dt0 + jnp.log(-jnp.expm1(-dt0)),
        'ssd_a_log': jnp.log(unif((DEPTH, SSD_HEADS), 1.0, 16.0)),
        'ssd_d': 1.0 + nrm((DEPTH, SSD_HEADS), 0.1),
        'ssd_norm_w': 1.0 + nrm((DEPTH, SSD_INNER), 0.1),
        'lru_conv_w': nrm((DEPTH, CONV_WIDTH, LRU_WIDTH), CONV_WIDTH ** -0.5),
        'lru_conv_b': nrm((DEPTH, LRU_WIDTH), 0.02),
        'lru_wa': nrm((DEPTH, LRU_BLOCKS, LRU_BLOCK_DIM, LRU_BLOCK_DIM), LRU_BLOCK_DIM ** -0.5),
        'lru_ba': nrm((DEPTH, LRU_WIDTH), 0.02),
        'lru_wx': nrm((DEPTH, LRU_BLOCKS, LRU_BLOCK_DIM, LRU_BLOCK_DIM), LRU_BLOCK_DIM ** -0.5),
        'lru_bx': nrm((DEPTH, LRU_WIDTH), 0.02),
        'lru_lambda': jnp.log(a0) - jnp.log1p(-a0),
        'proj_nsa': nrm((DEPTH, NSA_Q_W, D_MODEL), NSA_Q_W ** -0.5),
        'proj_ssd': nrm((DEPTH, SSD_INNER, D_MODEL), SSD_INNER ** -0.5),
        'proj_lru': nrm((DEPTH, LRU_WIDTH, D_MODEL), LRU_WIDTH ** -0.5),
        'w_out': nrm((DEPTH, D_MODEL, D_MODEL), D_MODEL ** -0.5 * BETA),
        'ln1_g': 1.0 + nrm((DEPTH, D_MODEL), 0.05),
        'ln1_b': nrm((DEPTH, D_MODEL), 0.02),
        'router_w': nrm((D_MODEL, N_EXPERTS), D_MODEL ** -0.5),
        'router_b': nrm((N_EXPERTS,), 0.01),
        'exp_w_gate': nrm((DEPTH, N_EXPERTS, D_MODEL, D_EXPERT), D_MODEL ** -0.5),
        'exp_w_up': nrm((DEPTH, N_EXPERTS, D_MODEL, D_EXPERT), D_MODEL ** -0.5),
        'exp_w_down': nrm((DEPTH, N_EXPERTS, D_EXPERT, D_MODEL), D_EXPERT ** -0.5 * BETA),
        'ple_w_gate': nrm((DEPTH, D_MODEL, D_MODEL), D_MODEL ** -0.5),
        'ple_w_proj': nrm((DEPTH, PLE_DIM, D_MODEL), PLE_DIM ** -0.5 * BETA),
        'ln2_g': 1.0 + nrm((DEPTH, D_MODEL), 0.05),
        'ln2_b': nrm((DEPTH, D_MODEL), 0.02),
    }


def reference(x, p, w_in, nsa_pe_k, nsa_w1_k, nsa_w2_k, nsa_pe_v, nsa_w1_v, nsa_w2_v,
              ssd_conv_w, ssd_conv_b, ssd_dt_bias, ssd_a_log, ssd_d, ssd_norm_w,
              lru_conv_w, lru_conv_b, lru_wa, lru_ba, lru_wx, lru_bx, lru_lambda,
              proj_nsa, proj_ssd, proj_lru, w_out, ln1_g, ln1_b,
              router_w, router_b, exp_w_gate, exp_w_up, exp_w_down,
              ple_w_gate, ple_w_proj, ln2_g, ln2_b):
    offsets = np.cumsum(IN_SIZES)[:-1].tolist()
    for i in range(DEPTH):
        proj = x @ w_in[i]
        (q, k_cmp, v_cmp, k_sel, v_sel, k_win, v_win, nsa_gate,
         ssd_z, ssd_xbc, ssd_dt, lru_x, lru_y, merge) = jnp.split(proj, offsets, axis=-1)
        o_nsa = nsa_attention(q, k_cmp, v_cmp, k_sel, v_sel, k_win, v_win, nsa_gate,
                              nsa_pe_k[i], nsa_w1_k[i], nsa_w2_k[i], nsa_pe_v[i], nsa_w1_v[i], nsa_w2_v[i])
        o_ssd = ssd_mixer(ssd_z, ssd_xbc, ssd_dt, ssd_conv_w[i], ssd_conv_b[i], ssd_dt_bias[i],
                          ssd_a_log[i], ssd_d[i], ssd_norm_w[i])
        o_lru = rglru_mixer(lru_x, lru_y, lru_conv_w[i], lru_conv_b[i], lru_wa[i], lru_ba[i],
                            lru_wx[i], lru_bx[i], lru_lambda[i])
        g_nsa, g_ssd, g_lru = jnp.split(jax.nn.sigmoid(merge), 3, axis=-1)
        mixed = g_nsa * (o_nsa @ proj_nsa[i]) + g_ssd * (o_ssd @ proj_ssd[i]) + g_lru * (o_lru @ proj_lru[i])
        x = layer_norm(ALPHA * x + mixed @ w_out[i], ln1_g[i], ln1_b[i])
        ple = jax.nn.sigmoid(x @ ple_w_gate[i]) * (p[i] @ ple_w_proj[i])
        y = moe(x, router_w, router_b, exp_w_gate[i], exp_w_up[i], exp_w_down[i])
        x = layer_norm(ALPHA * x + y + ple, ln2_g[i], ln2_b[i])
    return x
```

```python
import numpy as np
import contextlib
import concourse.bass as bass
import concourse.mybir as mybir
from concourse.bass_utils import run_bass_kernel_spmd

F32 = mybir.dt.float32
BF16 = mybir.dt.bfloat16
AF = mybir.ActivationFunctionType
ALU = mybir.AluOpType
AX = mybir.AxisListType

SAME_ENGINE_SYNC = True
SEM_ROT = 20000
NDMA = 6


class Res:
    __slots__ = ("w", "r", "name", "excl")

    def __init__(self, name="", excl=False):
        self.name = name
        self.w = None
        self.r = {}
        self.excl = excl


class KB:
    def __init__(self, nc, es):
        self.nc = nc
        self.es = es
        self.eng = dict(pe=nc.tensor, act=nc.scalar, dve=nc.vector, pool=nc.gpsimd, sp=nc.sync)
        self.sems = {}
        self.cnt = {}
        self.cur = {}
        self.seen = {e: {} for e in self.eng}
        self.nsem = 0
        self.ninst = 0
        for e in self.eng:
            self.cur[e] = self._new_sem("e_" + e)
        self.dma_pool = {q: [self._new_sem("d_%s%d" % (q, i)) for i in range(NDMA)] for q in ("sp", "pool", "act")}
        self.dma_idx = {q: 0 for q in self.dma_pool}
        self.uid = 0
        self.out_marks = []

    def _new_sem(self, name):
        self.nsem += 1
        key = "%s_%d" % (name, self.nsem)
        self.sems[key] = self.es.enter_context(self.nc.semaphore(key))
        self.cnt[key] = 0
        return key

    def sb(self, shape, dtype, name=None, es=None):
        self.uid += 1
        return (es or self.es).enter_context(self.nc.sbuf_tensor("%s_%d" % (name or "sb", self.uid), list(shape), dtype))

    def ps(self, shape, dtype=F32, name=None):
        self.uid += 1
        return self.es.enter_context(self.nc.psum_tensor("%s_%d" % (name or "ps", self.uid), list(shape), dtype))

    def dram(self, name, shape, dtype, kind):
        return self.nc.dram_tensor(name, list(shape), dtype, kind=kind).ap()

    def _deps(self, reads, writes):
        deps = {}
        for r in reads:
            if r.w is not None:
                k, v = r.w
                if deps.get(k, 0) < v:
                    deps[k] = v
            if r.excl:
                for k, v in r.r.items():
                    if deps.get(k, 0) < v:
                        deps[k] = v
        for w in writes:
            if w.w is not None:
                k, v = w.w
                if deps.get(k, 0) < v:
                    deps[k] = v
            for k, v in w.r.items():
                if deps.get(k, 0) < v:
                    deps[k] = v
        return deps

    def _wait(self, e, deps, skip_key=None):
        seen = self.seen[e]
        for k, v in deps.items():
            if k == skip_key:
                continue
            if seen.get(k, 0) < v:
                self.eng[e].wait_ge(self.sems[k], v)
                seen[k] = v
                self.ninst += 1

    def _mark(self, key, v, reads, writes):
        for r in reads:
            r.r[key] = v
        for w in writes:
            w.w = (key, v)
            w.r = {}

    def op(self, e, fn, reads=(), writes=()):
        deps = self._deps(reads, writes)
        key = self.cur[e]
        skip = key if (e == "pe" or not SAME_ENGINE_SYNC) else None
        self._wait(e, deps, skip)
        inst = fn(self.eng[e])
        self.cnt[key] += 1
        v = self.cnt[key]
        inst.then_inc(self.sems[key], 1)
        self.ninst += 1
        self._mark(key, v, reads, writes)
        if v >= SEM_ROT:
            self.cur[e] = self._new_sem("e_" + e)
        return inst

    def dma(self, q, out, in_, reads=(), writes=(), final=False, **kw):
        pool = self.dma_pool[q]
        key = pool[self.dma_idx[q] % len(pool)]
        self.dma_idx[q] += 1
        deps = self._deps(reads, writes)
        if self.cnt[key] > 0:
            deps[key] = max(deps.get(key, 0), self.cnt[key])
        self._wait(q, deps)
        inst = self.eng[q].dma_start(out=out, in_=in_, **kw)
        self.cnt[key] += 16
        v = self.cnt[key]
        inst.then_inc(self.sems[key], 16)
        self.ninst += 1
        self._mark(key, v, reads, writes)
        if final:
            self.out_marks.append((key, v))
        if v >= SEM_ROT:
            i = pool.index(key)
            pool[i] = self._new_sem("d_" + q)
        return inst

    def barrier(self):
        allc = {k: v for k, v in self.cnt.items() if v > 0}
        for e in self.eng:
            self._wait(e, dict(allc))

    def finish(self, out_res):
        deps = self._deps(out_res, ())
        for k, v in self.out_marks:
            if deps.get(k, 0) < v:
                deps[k] = v
        self._wait("sp", deps)


def _kb_gather(self, out, in2d, idx_ap, reads=(), writes=()):
    pool = self.dma_pool["pool"]
    key = pool[self.dma_idx["pool"] % len(pool)]
    self.dma_idx["pool"] += 1
    deps = self._deps(reads, writes)
    if self.cnt[key] > 0:
        deps[key] = max(deps.get(key, 0), self.cnt[key])
    self._wait("pool", deps)
    inst = self.nc.gpsimd.indirect_dma_start(out=out, out_offset=None, in_=in2d,
                                             in_offset=bass.IndirectOffsetOnAxis(ap=idx_ap, axis=0))
    self.cnt[key] += 16
    v = self.cnt[key]
    inst.then_inc(self.sems[key], 16)
    self.ninst += 1
    self._mark(key, v, reads, writes)
    if v >= SEM_ROT:
        pool[pool.index(key)] = self._new_sem("d_pool")
    return inst


def _kb_allgather(self, in_ap, out_ap, groups, writes=()):
    if not hasattr(self, "cc_key"):
        self.cc_key = self._new_sem("cc")
    deps = self._deps((), writes)
    self._wait("pool", deps)
    inst = self.nc.gpsimd.collective_compute("AllGather", ALU.bypass, replica_groups=groups, ins=[in_ap], outs=[out_ap])
    key = self.cc_key
    self.cnt[key] += 1
    v = self.cnt[key]
    inst.then_inc(self.sems[key], 1)
    self.ninst += 1
    self._mark(key, v, (), writes)
    return inst


KB.gather = _kb_gather
KB.allgather = _kb_allgather


S = 8192


class Rot:
    def __init__(self, kb, n, shape, dtype, name, psum=False, es=None):
        if psum:
            self.t = [(kb.ps(shape, dtype, name), Res(name, excl=True)) for _ in range(n)]
        else:
            self.t = [(kb.sb(shape, dtype, name, es=es), Res(name)) for _ in range(n)]
        self.i = 0

    def next(self):
        x = self.t[self.i % len(self.t)]
        self.i += 1
        return x


LEVEL = 9

S = 8192


def emit_conv(kb, eng, out, xin, vt, vr, c0, N, P, xr, outr):
    kb.op(eng, lambda e: e.tensor_scalar(out=out[:P, :], in0=xin[:P, 0:N], scalar1=vt[:P, c0:c0 + 1], scalar2=vt[:P, c0 + 4:c0 + 5],
                                         op0=ALU.mult, op1=ALU.add), reads=[xr, vr], writes=[outr])
    for k in range(1, 4):
        kb.op("dve", lambda e, k=k: e.scalar_tensor_tensor(out=out[:P, :], in0=xin[:P, k:k + N], scalar=vt[:P, c0 + k:c0 + k + 1], in1=out[:P, :],
                                                          op0=ALU.mult, op1=ALU.add), reads=[xr, vr, outr], writes=[outr])


def ssd_consts():
    k = np.arange(128)
    cst = np.zeros((128, 4, 128), np.float32)
    cst[:, 0, :] = (k[:, None] <= k[None, :])
    cst[:, 1, :] = (k[:, None] > k[None, :])
    cst[:, 2, :] = np.eye(128)
    cst[:, 3, :] = 1.0
    return cst


DBG = ''

S = 8192
QT = 512
NCMP = 511
f32 = np.float32


def nsa_consts(par=0):
    kk = np.arange(128)
    tq = np.arange(512)
    c = {}
    zeros = np.zeros((128, 512), f32); onesm = np.ones((128, 512), f32)

    def cm_full(dl):
        if dl < 0:
            return zeros
        if dl >= 5:
            return onesm
        return ((16 * kk[:, None] + 31 - tq[None, :]) <= 512 * dl).astype(f32)

    def cneg_full(dk):
        if dk < 0:
            return zeros
        if dk > 3:
            return -onesm
        return np.where(128 * dk + kk[:, None] <= tq[None, :], 0.0, -1.0).astype(f32)

    def wm_full(dk):
        if dk < -4 or dk > 3:
            return zeros
        diff = tq[None, :] - (128 * dk + kk[:, None])
        return ((diff >= 0) & (diff < 512)).astype(f32)

    cm = [cm_full(dl + par) for dl in range(-1, 5)]
    cneg = [cneg_full(dk - 4 * par) for dk in range(0, 8)]
    wm = [wm_full(dk - 4 * par) for dk in range(-4, 8)]
    c["nmask"] = np.ascontiguousarray(np.stack(cm + cneg + wm, axis=1))
    key = np.arange(S)
    c["nE0"] = (kk[:, None] == (key[None, :] // 64)).astype(f32)
    n = np.arange(512)
    ov = ((n[:, None] // 4) == kk[None, :]).astype(f32) + (((n[:, None] + 1) // 4) == kk[None, :]).astype(f32)
    ov[511] = 0
    ovl = ov.reshape(4, 128, 128).transpose(1, 0, 2)
    mm = np.arange(256) - 8 * par
    hi = (kk >= 64).astype(np.int64)
    C0 = (mm[None, :] <= 128 + hi[:, None]).astype(f32)
    Cm1 = C0 - 1.0
    F = np.where((mm[None, :] == 128 + hi[:, None]) | (mm[None, :] == 127 + hi[:, None]), 1e4, -1e30).astype(f32)
    ident = np.eye(128, dtype=f32)
    ones = np.ones((128, 128), f32)
    sel64 = np.zeros((128, 64), f32); sel64[64] = 1.0
    gsel = np.zeros((128, 12, 64), f32)
    for r in range(12):
        gsel[r, r, :] = 1.0
    c["nmisc"] = np.ascontiguousarray(np.concatenate(
        [ovl.reshape(128, 512), C0, Cm1, F, ident, ones, sel64, gsel.reshape(128, 768)], axis=1))
    return c


NMASK = 26
MISC_W = 512 + 768 + 128 + 128 + 64 + 768


D = 1024
TOK = 2048


CDBG = ''

D = 1024
TOK = 2048
NT = TOK // 512
ALPHA = 4 ** 0.25
OFF_Z = 512 + 768 + 24
OFF_MERGE = 6688 - 3072
f32 = np.float32
CW = 36 + 16 + 128 + 128 + 128 + 2048


def c_consts(d, layer):
    v = np.zeros((128, CW), f32)
    col = lambda a: a.reshape(-1, 128).T
    v[:, 0:8] = col(d["ln1_g"][layer]); v[:, 8:16] = col(d["ln1_b"][layer])
    v[:, 16:24] = col(d["ln2_g"][layer]); v[:, 24:32] = col(d["ln2_b"][layer])
    v[:, 32:36] = col(d["ssd_norm_w"][layer])
    v[:, 36:52] = d["router_b"][None, :]
    v[:, 52:180] = 1.0 / 1024
    v[:, 180:308] = 1.0 / 512
    v[:, 308:436] = np.eye(128)
    sel = np.zeros((128, 16, 128), f32)
    for e in range(16):
        sel[e, e, :] = 1.0
    v[:, 436:436 + 2048] = sel.reshape(128, 2048)
    return v


def emit_nsa2(kb, tiles, agF2k, agF512, agFr, gx, gxr, IX, peT, w1, w2, nmask, nE0, nmisc, onsaT, onsar, acc, pmb, gen, es):
    sb = lambda shape, dt, name: kb.sb(shape, dt, name, es=es)
    mk = sb([128, NMASK, 512], BF16, "nmask"); mkr = Res()
    kb.dma("pool", mk[:], nmask[:, :, :], writes=[mkr])
    E0 = sb([128, S], BF16, "nE0"); E0r = Res()
    kb.dma("pool", E0[:], nE0[:, :], writes=[E0r])
    mf = sb([128, MISC_W], F32, "nmiscf"); mfr = Res()
    kb.dma("sp", mf[:], nmisc[:, :], writes=[mfr])
    o = 0
    ovl_f = mf[:, 0:512]; o = 512
    C0 = mf[:, o:o + 256]; Cm1 = mf[:, o + 256:o + 512]; F4 = mf[:, o + 512:o + 768]; o += 768
    ident = mf[:, o:o + 128]; o += 128
    ones_f = mf[:, o:o + 128]; o += 128
    sel64 = mf[:, o:o + 64]; o += 64
    gsel = mf[:, o:o + 768]
    cb_ = sb([128, 512 + 128 + 128], BF16, "ncb"); cbr = Res()
    kb.op("dve", lambda e: e.tensor_copy(out=cb_[:, 0:512], in_=ovl_f), reads=[mfr], writes=[cbr])
    kb.op("dve", lambda e: e.tensor_copy(out=cb_[:, 512:640], in_=ident), reads=[mfr], writes=[cbr])
    kb.op("dve", lambda e: e.tensor_copy(out=cb_[:, 640:768], in_=ones_f), reads=[mfr], writes=[cbr])
    ovl = cb_[:, 0:512]; identb = cb_[:, 512:640]; onesb = cb_[:, 640:768]

    kall = sb([64, 2, S], BF16, "nk"); kr = [None, None, Res(), Res()]
    for i, nm in ((2, "ksel"), (3, "kwin")):
        for rp in range(4):
            kb.gather(kall[:, i - 2, rp * 2048:(rp + 1) * 2048], agF2k, gx[0:64, IX[nm + str(rp)]:IX[nm + str(rp)] + 1], reads=[gxr, agFr], writes=[kr[i]])
    va = sb([128, 2, 64, 65], BF16, "nva"); var = [Res() for _ in range(2)]
    for i in range(2):
        kb.op("pool", lambda e, i=i: e.memset(va[:, i, :, 64:65], 1.0), writes=[var[i]])

    kcT = sb([64, 512], BF16, "nkcT"); kcr = Res()
    vct = sb([128, 4, 65], BF16, "nvct"); vcr = Res()
    kb.op("pool", lambda e: e.memset(kcT[:], 0.0), writes=[kcr])
    kb.op("pool", lambda e: e.memset(vct[:], 0.0), writes=[vcr])
    kb.op("pool", lambda e: e.memset(vct[:, :, 64:65], 1.0), writes=[vcr])
    with contextlib.ExitStack() as esv:
        vT = kb.sb([64, 2, S], BF16, "nvT", es=esv); vTr = [Res(), Res()]
        for i, nm in ((0, "vsel"), (1, "vwin")):
            for rp in range(4):
                kb.gather(vT[:, i, rp * 2048:(rp + 1) * 2048], agF2k, gx[0:64, IX[nm + str(rp)]:IX[nm + str(rp)] + 1], reads=[gxr, agFr], writes=[vTr[i]])
            for k8 in range(8):
                pv8, pv8r = gen.next()
                pvb = pv8[:, 0:256].bitcast(BF16)
                for kk in range(8):
                    kt = k8 * 8 + kk
                    kb.op("pe", lambda e, kt=kt, kk=kk, i=i: e.transpose(pvb[:, kk * 64:(kk + 1) * 64], vT[:, i, kt * 128:(kt + 1) * 128], identb[0:64, 0:64]),
                          reads=[vTr[i], cbr], writes=[pv8r])
                kb.op("act", lambda e, k8=k8, i=i: e.copy(out=va[:, i, k8 * 8:(k8 + 1) * 8, 0:64], in_=pvb.rearrange("p (k d) -> p k d", k=8)), reads=[pv8r], writes=[var[i]])
        kb.barrier()
    with contextlib.ExitStack() as es2:
        sb2 = lambda shape, dt, name: kb.sb(shape, dt, name, es=es2)
        kcv = sb2([64, 2, S], BF16, "nkcv"); kr[0] = Res(); kr[1] = Res()
        for i, nm in ((0, "kcmp"), (1, "vcmp")):
            for rp in range(4):
                kb.gather(kcv[:, i, rp * 2048:(rp + 1) * 2048], agF2k, gx[0:64, IX[nm + str(rp)]:IX[nm + str(rp)] + 1], reads=[gxr, agFr], writes=[kr[i]])
        w1t = sb2([64, 32, 256], BF16, "nw1"); w1r = Res()
        w2t = sb2([128, 2, 64], BF16, "nw2"); w2r = Res()
        pet = sb2([64, 32], BF16, "npe"); per = Res()
        bias = sb2([128, 1], F32, "nbias"); biasr = Res()
        tmp3 = [(sb2([128, 512], F32, "nt"), Res(), sb2([128, 512], F32, "nu"), Res(), sb2([128, 512], BF16, "ngt"), Res()) for _ in range(2)]
        for which in range(2):
            for q4 in range(4):
                kb.dma("pool", w1t[:, q4 * 8:(q4 + 1) * 8, :], w1[which, q4 * 512:(q4 + 1) * 512, :].rearrange("(l d) j -> d l j", d=64), writes=[w1r])
            kb.dma("pool", w2t[:], w2[which, :, :].rearrange("(c p) d -> p c d", p=128), writes=[w2r])
            kb.dma("pool", pet[:], peT[which, :, :], writes=[per])
            src = kcv[:, which, :]
            gts = []
            for jc in range(2):
                ph, phr = gen.next()
                for l in range(32):
                    kb.op("pe", lambda e, l=l: e.matmul(ph[:, 0:NCMP], lhsT=w1t[:, l, jc * 128:(jc + 1) * 128],
                                                         rhs=src[:, l:l + 16 * (NCMP - 1) + 1:16], start=(l == 0), stop=(l == 31)),
                          reads=[w1r, kr[which]], writes=[phr])
                pbias, pbr = gen.next()
                for l in range(32):
                    kb.op("pe", lambda e, l=l: e.matmul(pbias[:, 0:1], lhsT=w1t[:, l, jc * 128:(jc + 1) * 128], rhs=pet[:, l:l + 1],
                                                         start=(l == 0), stop=(l == 31)), reads=[w1r, per], writes=[pbr])
                kb.op("dve", lambda e: e.tensor_copy(out=bias[:], in_=pbias[:, 0:1]), reads=[pbr], writes=[biasr])
                t, tr, u, ur, gt, gr = tmp3[jc]
                kb.op("act", lambda e: e.activation(out=t[:, 0:NCMP], in_=ph[:, 0:NCMP], func=AF.Identity, bias=bias[:, 0:1]), reads=[phr, biasr], writes=[tr])
                kb.op("dve", lambda e: e.tensor_tensor(out=u[:, 0:NCMP], in0=t[:, 0:NCMP], in1=t[:, 0:NCMP], op=ALU.mult), reads=[tr], writes=[ur])
                kb.op("dve", lambda e: e.tensor_scalar(out=u[:, 0:NCMP], in0=u[:, 0:NCMP], scalar1=0.044715, scalar2=1.0, op0=ALU.mult, op1=ALU.add), reads=[ur], writes=[ur])
                kb.op("dve", lambda e: e.tensor_tensor(out=u[:, 0:NCMP], in0=u[:, 0:NCMP], in1=t[:, 0:NCMP], op=ALU.mult), reads=[ur, tr], writes=[ur])
                kb.op("act", lambda e: e.activation(out=u[:, 0:NCMP], in_=u[:, 0:NCMP], func=AF.Sigmoid, scale=1.5957691216057308), reads=[ur], writes=[ur])
                kb.op("pool", lambda e: e.memset(gt[:, NCMP:512], 0.0), writes=[gr])
                kb.op("dve", lambda e: e.tensor_tensor(out=gt[:, 0:NCMP], in0=u[:, 0:NCMP], in1=t[:, 0:NCMP], op=ALU.mult), reads=[ur, tr], writes=[gr])
                gts.append((gt, gr))
            if which == 0:
                pk, pkr = gen.next()
                for jc in range(2):
                    kb.op("pe", lambda e, jc=jc: e.matmul(pk[0:64, 0:NCMP], lhsT=w2t[:, jc, :], rhs=gts[jc][0][:, 0:NCMP], start=(jc == 0), stop=(jc == 1)),
                          reads=[w2r, gts[jc][1]], writes=[pkr])
                kb.op("act", lambda e: e.copy(out=kcT[:, 0:NCMP], in_=pk[0:64, 0:NCMP]), reads=[pkr], writes=[kcr])
            else:
                for m in range(4):
                    pv, pvr = gen.next()
                    for jc in range(2):
                        kb.op("pe", lambda e, jc=jc, m=m: e.matmul(pv[:, 0:64], lhsT=gts[jc][0][:, m * 128:(m + 1) * 128], rhs=w2t[:, jc, :], start=(jc == 0), stop=(jc == 1)),
                              reads=[w2r, gts[jc][1]], writes=[pvr])
                    kb.op("act", lambda e, m=m: e.copy(out=vct[:, m, 0:64], in_=pv[:, 0:64]), reads=[pvr], writes=[vcr])
        kb.barrier()
    kmax = sb([128, 1], F32, "nkmax"); kmr = Res()
    kb.op("pool", lambda e: e.memset(kmax[:], 0.0), writes=[kmr])
    sq = Rot(kb, 2, [64, 512], BF16, "nsq", es=es)
    red = Rot(kb, 2, [128, 1], F32, "nred", es=es)

    def norm_max(src_ap, n, src_res, dst, dstr):
        s_, sr_ = sq.next()
        kb.op("pool", lambda e: e.tensor_tensor(out=s_[:, 0:n], in0=src_ap, in1=src_ap, op=ALU.mult), reads=src_res, writes=[sr_])
        pn, pnr = gen.next()
        kb.op("pe", lambda e: e.matmul(pn[:, 0:n], lhsT=onesb[0:64, :], rhs=s_[:, 0:n], start=True, stop=True), reads=[cbr, sr_], writes=[pnr])
        r_, rr_ = red.next()
        kb.op("dve", lambda e: e.reduce_max(out=r_[:], in_=pn[:, 0:n], axis=AX.X), reads=[pnr], writes=[rr_])
        kb.op("dve", lambda e: e.tensor_tensor(out=dst[:], in0=dst[:], in1=r_[:], op=ALU.max), reads=[rr_, dstr], writes=[dstr])

    norm_max(kcT[:, 0:512], 512, [kcr], kmax, kmr)
    for which in (2, 3):
        for tt in range(S // 512):
            norm_max(kall[:, which - 2, tt * 512:(tt + 1) * 512], 512, [kr[which]], kmax, kmr)

    qb = Rot(kb, 2, [64, 4, 512], BF16, "nq", es=es)
    gtl = Rot(kb, 2, [12, 512], F32, "ngate", es=es)
    gtbl = Rot(kb, 2, [12, 512], BF16, "ngateb", es=es)
    ebuf = Rot(kb, 4, [128, 512], BF16, "ne", es=es)
    pbuf = Rot(kb, 4, [128, 512], BF16, "np", es=es)
    pnbuf = Rot(kb, 4, [128, 512], BF16, "npn", es=es)
    rinv = Rot(kb, 2, [128, 512], F32, "nrinv", es=es)
    ocmp = Rot(kb, 1, [64, 4, 512], F32, "nocmp", es=es)
    impT = Rot(kb, 2, [128, 512], F32, "nimpT", es=es)
    selT = Rot(kb, 2, [128, 512], BF16, "nselT", es=es)
    v1b = Rot(kb, 2, [128, 128], F32, "nv1", es=es)
    v2b = Rot(kb, 2, [128, 128], F32, "nv2", es=es)
    m8a = Rot(kb, 2, [128, 8], F32, "nm8a", es=es)
    m8b = Rot(kb, 2, [128, 8], F32, "nm8b", es=es)
    smk = Rot(kb, 2, [128, 128], F32, "nsmk", es=es)
    accs = Rot(kb, 1, [65, 8, 512], F32, "naccs", es=es)
    rec = Rot(kb, 2, [64, 512], F32, "nrec", es=es)
    osb = Rot(kb, 2, [64, 512], F32, "nosb", es=es)
    negc = Rot(kb, 2, [128, 4], F32, "nnegc", es=es)
    qm = Rot(kb, 2, [128, 1], F32, "nqm", es=es)

    for ti, i in enumerate(tiles):
        t0 = i * QT
        q, qr = qb.next()
        for j in range(4):
            cq = IX["q%d_%d" % (ti, j)]
            kb.gather(q[:, j, :], agF512, gx[0:64, cq:cq + 1], reads=[gxr, agFr], writes=[qr])
        gtb_, gtbr = gtbl.next()
        cg = IX["gate%d" % ti]
        kb.gather(gtb_[:], agF512, gx[0:12, cg:cg + 1], reads=[gxr, agFr], writes=[gtbr])
        gt_, gtr = gtl.next()
        kb.op("act", lambda e: e.activation(out=gt_[:], in_=gtb_[:], func=AF.Sigmoid), reads=[gtbr], writes=[gtr])
        nc_, ncr = negc.next()
        for j in range(4):
            qm_, qmr = qm.next()
            kb.op("pool", lambda e: e.memset(qm_[:], 0.0), writes=[qmr])
            norm_max(q[:, j, :], 512, [qr], qm_, qmr)
            kb.op("dve", lambda e, j=j: e.tensor_tensor(out=nc_[:, j:j + 1], in0=qm_[:], in1=kmax[:], op=ALU.mult), reads=[qmr, kmr], writes=[ncr])
        kb.op("act", lambda e: e.activation(out=nc_[:], in_=nc_[:], func=AF.Sqrt, scale=1.05), reads=[ncr], writes=[ncr])
        kb.op("dve", lambda e: e.tensor_scalar(out=nc_[:], in0=nc_[:], scalar1=-0.125, scalar2=None, op0=ALU.mult), reads=[ncr], writes=[ncr])

        nch = min(4, (32 * (i + 1) + 31 + 127) // 128)
        oc, ocr = ocmp.next()
        pimp, pimpr = acc[0]
        for j in range(4):
            es_ = []
            psum_, psumr = acc[1]
            for m in range(nch):
                ps_, psr = gen.next()
                kb.op("pe", lambda e, m=m, j=j: e.matmul(ps_[:, :], lhsT=kcT[:, m * 128:(m + 1) * 128], rhs=q[:, j, :], start=True, stop=True),
                      reads=[kcr, qr], writes=[psr])
                e_, er = ebuf.next()
                kb.op("act", lambda e, j=j: e.activation(out=e_[:], in_=ps_[:, :], func=AF.Exp, scale=0.125, bias=nc_[:, j:j + 1]), reads=[psr, ncr], writes=[er])
                dl = i - 4 * m
                if dl <= 4:
                    kb.op("pool", lambda e, dl=dl: e.tensor_tensor(out=e_[:], in0=e_[:], in1=mk[:, dl + 1, :], op=ALU.mult), reads=[er, mkr], writes=[er])
                kb.op("pe", lambda e, m=m: e.matmul(psum_[:, :], lhsT=onesb, rhs=e_[:], start=(m == 0), stop=(m == nch - 1)), reads=[cbr, er], writes=[psumr])
                es_.append((e_, er))
            ri, rir = rinv.next()
            kb.op("dve", lambda e: e.tensor_scalar(out=ri[:], in0=psum_[:, :], scalar1=1e-30, scalar2=None, op0=ALU.max), reads=[psumr], writes=[rir])
            kb.op("dve", lambda e: e.reciprocal(out=ri[:], in_=ri[:]), reads=[rir], writes=[rir])
            po, por = acc[2]
            for m in range(nch):
                e_, er = es_[m]
                pn_, pnr_ = pnbuf.next()
                kb.op("dve", lambda e: e.tensor_tensor(out=pn_[:], in0=e_[:], in1=ri[:], op=ALU.mult), reads=[er, rir], writes=[pnr_])
                kb.op("pe", lambda e, m=m: e.matmul(po[0:64, :], lhsT=vct[:, m, 0:64], rhs=pn_[:], start=(m == 0), stop=(m == nch - 1)), reads=[vcr, pnr_], writes=[por])
                kb.op("pe", lambda e, m=m, j=j: e.matmul(pimp[:, :], lhsT=ovl[:, m * 128:(m + 1) * 128], rhs=pn_[:], start=(j == 0 and m == 0), stop=(j == 3 and m == nch - 1)),
                      reads=[cbr, pnr_], writes=[pimpr])
            kb.op("act", lambda e, j=j: e.copy(out=oc[:, j, :], in_=po[0:64, :]), reads=[por], writes=[ocr])
        it_, itr = impT.next()
        kb.op("act", lambda e: e.copy(out=it_[:], in_=pimp[:, :]), reads=[pimpr], writes=[itr])
        st_, str_ = selT.next()
        for k4 in range(4):
            ksub = 4 * i + k4
            ptr_, ptrr = gen.next()
            kb.op("pe", lambda e, k4=k4: e.transpose(ptr_[:, 0:128], it_[:, k4 * 128:(k4 + 1) * 128], ident), reads=[itr, mfr], writes=[ptrr])
            co = 128 - 2 * ksub
            v1, v1r = v1b.next()
            kb.op("dve", lambda e, co=co: e.tensor_tensor(out=v1[:], in0=ptr_[:, 0:128], in1=C0[:, co:co + 128], op=ALU.mult), reads=[ptrr, mfr], writes=[v1r])
            kb.op("dve", lambda e, co=co: e.tensor_tensor(out=v1[:], in0=v1[:], in1=Cm1[:, co:co + 128], op=ALU.add), reads=[v1r, mfr], writes=[v1r])
            kb.op("dve", lambda e, co=co: e.tensor_tensor(out=v1[:], in0=v1[:], in1=F4[:, co:co + 128], op=ALU.max), reads=[v1r, mfr], writes=[v1r])
            kb.op("dve", lambda e: e.memset(v1[:, 0:1], 1e4), reads=[], writes=[v1r])
            a8, a8r = m8a.next()
            kb.op("dve", lambda e: e.max(out=a8[:], in_=v1[:]), reads=[v1r], writes=[a8r])
            v2, v2r = v2b.next()
            kb.op("dve", lambda e: e.match_replace(out=v2[:], in_to_replace=a8[:], in_values=v1[:], imm_value=-1e30), reads=[a8r, v1r], writes=[v2r])
            b8, b8r = m8b.next()
            kb.op("dve", lambda e: e.max(out=b8[:], in_=v2[:]), reads=[v2r], writes=[b8r])
            sm, smr = smk.next()
            kb.op("dve", lambda e: e.tensor_scalar(out=sm[:], in0=v1[:], scalar1=b8[:, 7:8], scalar2=None, op0=ALU.is_ge), reads=[v1r, b8r], writes=[smr])
            if DBG == "sm" and k4 == 0 and ti == 0:
                kb.dma("sp", onsaT[0:128, 0:128], sm[:], reads=[smr], final=True)
                kb.dma("sp", onsaT[128:256, 0:128], v1[:], reads=[v1r], final=True)
                kb.dma("sp", onsaT[0:128, 128:136], a8[:], reads=[a8r], final=True)
                kb.dma("sp", onsaT[0:128, 136:144], b8[:], reads=[b8r], final=True)
                kb.dma("sp", onsaT[128:256, 128:256], v2[:], reads=[v2r], final=True)
            pt2, pt2r = gen.next()
            kb.op("pe", lambda e: e.transpose(pt2[:, 0:128], sm[:], ident), reads=[smr, mfr], writes=[pt2r])
            kb.op("act", lambda e, k4=k4: e.copy(out=st_[:, k4 * 128:(k4 + 1) * 128], in_=pt2[:, 0:128]), reads=[pt2r], writes=[str_])

        as_, asr = accs.next()
        for br in range(2):
            if br == 0:
                kts = list(range(0, min(64, 4 * i + 8)))
            else:
                kts = [kt for kt in range(4 * i - 4, min(64, 4 * i + 8)) if kt >= 0]
            ksrc = 2 + br
            for n_, kt in enumerate(kts):
                dk = kt - 4 * i
                if br == 0:
                    pm, pmr = pmb
                    kb.op("pe", lambda e, kt=kt, dk=dk: e.matmul(pm[:, :], lhsT=E0[:, kt * 128:(kt + 1) * 128], rhs=st_[:], start=True, stop=(dk < 0)),
                          reads=[E0r, str_], writes=[pmr])
                    if dk >= 0:
                        kb.op("pe", lambda e, dk=dk: e.matmul(pm[:, :], lhsT=identb, rhs=mk[:, 6 + dk, :], start=False, stop=True), reads=[cbr, mkr], writes=[pmr])
                for j in range(4):
                    ps_, psr = gen.next()
                    kb.op("pe", lambda e, kt=kt, j=j: e.matmul(ps_[:, :], lhsT=kall[:, br, kt * 128:(kt + 1) * 128], rhs=q[:, j, :], start=True, stop=True),
                          reads=[kr[ksrc], qr], writes=[psr])
                    e_, er = ebuf.next()
                    kb.op("act", lambda e, j=j: e.activation(out=e_[:], in_=ps_[:, :], func=AF.Exp, scale=0.125, bias=nc_[:, j:j + 1]), reads=[psr, ncr], writes=[er])
                    p_, pr_ = pbuf.next()
                    if br == 0:
                        kb.op("dve", lambda e: e.scalar_tensor_tensor(out=p_[:], in0=pm[:, :], scalar=0.0, in1=e_[:], op0=ALU.max, op1=ALU.mult),
                              reads=[pmr, er], writes=[pr_])
                    else:
                        kb.op("pool", lambda e, dk=dk: e.tensor_tensor(out=p_[:], in0=e_[:], in1=mk[:, 14 + dk + 4, :], op=ALU.mult), reads=[er, mkr], writes=[pr_])
                    if DBG == "pm" and br == 0 and j == 0 and kt == 1 and ti == 0:
                        dbt = sb([128, 512], F32, "dbt"); dbr = Res()
                        kb.op("dve", lambda e: e.tensor_copy(out=dbt[:], in_=pm[:, :]), reads=[pmr], writes=[dbr])
                        kb.dma("sp", onsaT[0:128, 0:512], dbt[:], reads=[dbr], final=True)
                        dbt2 = sb([128, 512], F32, "dbt2"); dbr2 = Res()
                        kb.op("dve", lambda e: e.tensor_copy(out=dbt2[:], in_=p_[:]), reads=[pr_], writes=[dbr2])
                        kb.dma("sp", onsaT[128:256, 0:512], dbt2[:], reads=[dbr2], final=True)
                    pa, par = acc[j]
                    kb.op("pe", lambda e, kt=kt, n_=n_: e.matmul(pa[0:65, :], lhsT=va[:, br, kt, :], rhs=p_[:], start=(n_ == 0), stop=(n_ == len(kts) - 1)),
                          reads=[var[br], pr_], writes=[par])
            for j in range(4):
                pa, par = acc[j]
                kb.op("act", lambda e, j=j, br=br: e.copy(out=as_[:, br * 4 + j, :], in_=pa[0:65, :]), reads=[par], writes=[asr])
        for j in range(4 if DBG not in ("sm", "pm") else 0):
            o_, or_ = osb.next()
            pg, pgr = gen.next()
            kb.op("pe", lambda e, j=j: e.matmul(pg[0:64, :], lhsT=gsel[0:12, (j * 3) * 64:(j * 3 + 1) * 64], rhs=gt_[:], start=True, stop=True), reads=[mfr, gtr], writes=[pgr])
            if DBG == "cmp":
                kb.op("dve", lambda e, j=j: e.tensor_copy(out=o_[:], in_=oc[:, j, :]), reads=[pgr, ocr], writes=[or_])
            elif DBG:
                kb.op("dve", lambda e, j=j: e.memset(o_[:], 0.0), reads=[pgr, ocr], writes=[or_])
            else:
                kb.op("dve", lambda e, j=j: e.tensor_tensor(out=o_[:], in0=pg[0:64, :], in1=oc[:, j, :], op=ALU.mult), reads=[pgr, ocr], writes=[or_])
            for br in range(2):
                if DBG == "cmp" or (DBG == "sel" and br == 1) or (DBG == "win" and br == 0):
                    continue
                psm, psmr = gen.next()
                kb.op("pe", lambda e, j=j, br=br: e.matmul(psm[0:64, :], lhsT=sel64[0:65, :], rhs=as_[:, br * 4 + j, :], start=True, stop=True), reads=[mfr, asr], writes=[psmr])
                rc, rcr = rec.next()
                kb.op("dve", lambda e: e.tensor_scalar(out=rc[:], in0=psm[0:64, :], scalar1=1e-30, scalar2=None, op0=ALU.max), reads=[psmr], writes=[rcr])
                kb.op("dve", lambda e: e.reciprocal(out=rc[:], in_=rc[:]), reads=[rcr], writes=[rcr])
                pg2, pg2r = gen.next()
                kb.op("pe", lambda e, j=j, br=br: e.matmul(pg2[0:64, :], lhsT=gsel[0:12, (j * 3 + 1 + br) * 64:(j * 3 + 2 + br) * 64], rhs=gt_[:], start=True, stop=True),
                      reads=[mfr, gtr], writes=[pg2r])
                if not DBG:
                    kb.op("dve", lambda e: e.tensor_tensor(out=rc[:], in0=pg2[0:64, :], in1=rc[:], op=ALU.mult), reads=[pg2r, rcr], writes=[rcr])
                kb.op("pool", lambda e, j=j, br=br: e.tensor_tensor(out=rc[:], in0=rc[:], in1=as_[0:64, br * 4 + j, :], op=ALU.mult), reads=[rcr, asr], writes=[rcr])
                kb.op("pool", lambda e: e.tensor_tensor(out=o_[:], in0=o_[:], in1=rc[:], op=ALU.add), reads=[rcr, or_], writes=[or_])
            kb.dma("pool", onsaT[j * 64:(j + 1) * 64, ti * QT:(ti + 1) * QT], o_[:], reads=[or_, onsar])


def emit_ssd2(kb, agF2k, agFr, agD2k, agDr, gx, gxr, IX, svec, srep, cst, yT_dst, yT_r, pbanks, pbf, es=None):
    N = 1024
    NCH = S // 128
    ct = kb.sb([128, 4, 128], F32, "scst", es=es); cr = Res()
    kb.dma("sp", ct[:], cst[:, :, :], writes=[cr])
    tri = ct[:, 0, :]; U = ct[:, 1, :]; ident = ct[:, 2, :]; ones = ct[:, 3, :]
    identb = kb.sb([128, 128], BF16, "sidb", es=es); idbr = Res()
    kb.op("dve", lambda e: e.tensor_copy(out=identb[:], in_=ident), reads=[cr], writes=[idbr])
    vt = kb.sb([128, 16], F32, "svec", es=es); vr = Res()
    kb.dma("sp", vt[:], svec[:, :], writes=[vr])
    rp = kb.sb([128, 8], F32, "srep", es=es); rpr = Res()
    kb.dma("sp", rp[:], srep[:, :], writes=[rpr])
    if LEVEL == -1: return
    dt = kb.sb([128, NCH, 2], F32, "sdt", es=es); dtr = Res()
    dtT = kb.sb([2, S], F32, "sdtT", es=es); dtTr = Res()
    for rq in range(4):
        cd = IX["dt%d" % rq]
        kb.gather(dtT[0:2, rq * 2048:(rq + 1) * 2048], agD2k, gx[0:2, cd:cd + 1], reads=[gxr, agDr], writes=[dtTr])
    pdt, pdtr = pbanks.next()
    for c in range(NCH):
        kb.op("pe", lambda e, c=c: e.transpose(pdt[:, c * 2:(c + 1) * 2], dtT[0:2, c * 128:(c + 1) * 128], ident[0:2, 0:2]), reads=[dtTr, cr], writes=[pdtr])
    kb.op("dve", lambda e: e.tensor_copy(out=dt[:].rearrange("p c h -> p (c h)"), in_=pdt[:, 0:NCH * 2]), reads=[pdtr], writes=[dtr])
    aa = kb.sb([128, NCH, 2], F32, "sa", es=es); aar = Res()
    An = kb.sb([128, 2], F32, "sAn", es=es); Anr = Res()
    kb.op("act", lambda e: e.activation(out=An[:], in_=rp[:, 2:4], func=AF.Exp), reads=[rpr], writes=[Anr])
    kb.op("dve", lambda e: e.tensor_scalar(out=An[:], in0=An[:], scalar1=-1.0, scalar2=None, op0=ALU.mult), reads=[Anr], writes=[Anr])
    for h in range(2):
        kb.op("dve", lambda e, h=h: e.tensor_scalar(out=dt[:, :, h], in0=dt[:, :, h], scalar1=rp[:, h:h + 1], scalar2=None, op0=ALU.add),
              reads=[dtr, rpr], writes=[dtr])
    kb.op("act", lambda e: e.activation(out=dt[:], in_=dt[:], func=AF.Exp), reads=[dtr], writes=[dtr])
    kb.op("act", lambda e: e.activation(out=dt[:], in_=dt[:], func=AF.Ln, bias=1.0), reads=[dtr], writes=[dtr])
    for h in range(2):
        kb.op("dve", lambda e, h=h: e.tensor_scalar(out=aa[:, :, h], in0=dt[:, :, h], scalar1=An[:, h:h + 1], scalar2=None, op0=ALU.mult),
              reads=[dtr, Anr], writes=[aar])
    if LEVEL == -2: return
    acum = kb.sb([128, NCH, 2], F32, "sacum", es=es); acr = Res()
    dout = kb.sb([128, NCH, 2], F32, "sdout", es=es); dor = Res()
    dst = kb.sb([128, NCH, 2], F32, "sdst", es=es); dsr = Res()
    dtot = kb.sb([128, NCH, 2], F32, "sdtot", es=es); dtor = Res()
    aflat = aa[:].rearrange("p c h -> p (c h)")
    p1, p1r = pbanks.next()
    kb.op("pe", lambda e: e.matmul(p1[:, 0:NCH * 2], lhsT=tri, rhs=aflat, start=True, stop=True), reads=[cr, aar], writes=[p1r])
    p2, p2r = pbanks.next()
    kb.op("pe", lambda e: e.matmul(p2[:, 0:NCH * 2], lhsT=ones, rhs=aflat, start=True, stop=True), reads=[cr, aar], writes=[p2r])
    fl = lambda t: t[:].rearrange("p c h -> p (c h)")
    kb.op("dve", lambda e: e.tensor_copy(out=fl(acum), in_=p1[:, 0:NCH * 2]), reads=[p1r], writes=[acr])
    kb.op("act", lambda e: e.activation(out=fl(dout), in_=p1[:, 0:NCH * 2], func=AF.Exp), reads=[p1r], writes=[dor])
    kb.op("act", lambda e: e.activation(out=fl(dtot), in_=p2[:, 0:NCH * 2], func=AF.Exp), reads=[p2r], writes=[dtor])
    kb.op("dve", lambda e: e.tensor_tensor(out=fl(dst), in0=p2[:, 0:NCH * 2], in1=fl(acum), op=ALU.subtract), reads=[p2r, acr], writes=[dsr])
    kb.op("act", lambda e: e.activation(out=fl(dst), in_=fl(dst), func=AF.Exp), reads=[dsr], writes=[dsr])

    if LEVEL == -3: return
    prev = [kb.sb([64, 64], F32, "sprev", es=es) for _ in range(2)]
    prevr = [Res() for _ in range(2)]
    prevb = [kb.sb([64, 64], BF16, "sprevb", es=es) for _ in range(2)]
    prevbr = [Res() for _ in range(2)]
    for h in range(2):
        kb.op("pool", lambda e, h=h: e.memset(prev[h][:], 0.0), writes=[prevr[h]])
        kb.op("pool", lambda e, h=h: e.memset(prevb[h][:], 0.0), writes=[prevbr[h]])

    xfull = kb.sb([128, S + 3], BF16, "sxfull", es=es); xfr = Res()
    bfull = kb.sb([64, S + 3], BF16, "sbfull", es=es); bfr = Res()
    cfull = kb.sb([64, S + 3], BF16, "scfull", es=es); cfr = Res()
    for (tl, rs, nm, P) in ((xfull, xfr, "sx", 128), (bfull, bfr, "sB", 64), (cfull, cfr, "sC", 64)):
        kb.op("pool", lambda e, tl=tl, P=P: e.memset(tl[:P, 0:3], 0.0), writes=[rs])
        for rq in range(4):
            cc_ = IX[nm + str(rq)]
            kb.gather(tl[:P, 3 + rq * 2048:3 + (rq + 1) * 2048], agF2k, gx[0:P, cc_:cc_ + 1], reads=[gxr, agFr], writes=[rs])
    yTb = Rot(kb, 2, [128, N], BF16, "syTb", es=es)
    xcv = Rot(kb, 2, [128, N], F32, "sxcv", es=es)
    bcv = Rot(kb, 2, [64, N], F32, "sbcv", es=es)
    ccv = Rot(kb, 2, [64, N], F32, "sccv", es=es)
    xs = Rot(kb, 2, [128, N], F32, "sxs", es=es)
    bs = Rot(kb, 2, [64, N], BF16, "sbs", es=es)
    cs = Rot(kb, 2, [64, N], BF16, "scs", es=es)
    xtm = Rot(kb, 2, [128, 128], F32, "sxtm", es=es)
    Xb = Rot(kb, 2, [128, 128], BF16, "sXb", es=es)
    Xd = Rot(kb, 2, [128, 128], BF16, "sXd", es=es)
    Btm = Rot(kb, 2, [128, 64], BF16, "sBtm", es=es)
    CBm = Rot(kb, 2, [128, 128], F32, "sCBm", es=es)
    lh = Rot(kb, 2, [128, 128], F32, "slh", es=es)
    EE = Rot(kb, 2, [128, 128], F32, "sE", es=es)
    MT = Rot(kb, 2, [128, 128], BF16, "sMT", es=es)
    yt = Rot(kb, 2, [128, 8, 128], F32, "syt", es=es)
    pbi = 0
    for j in range(S // N if LEVEL > 0 else 0):
        t0 = j * N
        xt, xr = xfull[:, t0:t0 + N + 3], xfr
        bt, br = bfull[:, t0:t0 + N + 3], bfr
        ctt, ctr = cfull[:, t0:t0 + N + 3], cfr
        xc, xcr = xcv.next(); bc, bcr = bcv.next(); cc, ccr = ccv.next()
        emit_conv(kb, "dve", xc, xt, vt, vr, 0, N, 128, xr, xcr)
        emit_conv(kb, "dve", bc, bt, vt, vr, 5, N, 64, br, bcr)
        emit_conv(kb, "dve", cc, ctt, vt, vr, 10, N, 64, ctr, ccr)
        xst, xsr = xs.next(); bst, bsr = bs.next(); cst_, csr = cs.next()
        kb.op("act", lambda e: e.activation(out=xst[:], in_=xc[:], func=AF.Silu), reads=[xcr], writes=[xsr])
        kb.op("act", lambda e: e.activation(out=bst[:], in_=bc[:], func=AF.Silu), reads=[bcr], writes=[bsr])
        kb.op("act", lambda e: e.activation(out=cst_[:], in_=cc[:], func=AF.Silu), reads=[ccr], writes=[csr])
        ytile, ytr = yt.next()
        for c in range(N // 128 if LEVEL > 1 else 0):
            gc = j * (N // 128) + c
            ts = slice(c * 128, (c + 1) * 128)
            pT, pTr = pbanks.next()
            kb.op("pe", lambda e: e.transpose(pT[:, 0:128], xst[:, ts], ident), reads=[xsr, cr], writes=[pTr])
            xm, xmr = xtm.next()
            kb.op("act", lambda e: e.copy(out=xm[:], in_=pT[:, 0:128]), reads=[pTr], writes=[xmr])
            xb, xbr = Xb.next()
            for h in range(2):
                hs = slice(h * 64, (h + 1) * 64)
                kb.op("dve", lambda e, hs=hs, h=h: e.tensor_scalar(out=xb[:, hs], in0=pT[:, hs], scalar1=dt[:, gc, h:h + 1], scalar2=None, op0=ALU.mult),
                      reads=[pTr, dtr], writes=[xbr])
            xd, xdr = Xd.next()
            for h in range(2):
                hs = slice(h * 64, (h + 1) * 64)
                kb.op("pool", lambda e, hs=hs, h=h: e.tensor_scalar(out=xd[:, hs], in0=xb[:, hs], scalar1=dst[:, gc, h:h + 1], scalar2=None, op0=ALU.mult),
                      reads=[xbr, dsr], writes=[xdr])
            if LEVEL < 3: continue
            pb_t, pb_r = pbf[pbi % len(pbf)]; pbi += 1
            kb.op("pe", lambda e: e.transpose(pb_t[:, 0:64], bst[:, ts], identb[0:64, 0:64]), reads=[bsr, idbr], writes=[pb_r])
            btm, btmr = Btm.next()
            kb.op("act", lambda e: e.copy(out=btm[:], in_=pb_t[:, 0:64]), reads=[pb_r], writes=[btmr])
            if LEVEL < 4: continue
            pcb, pcbr = pbanks.next()
            kb.op("pe", lambda e: e.matmul(pcb[:, 0:128], lhsT=bst[:, ts], rhs=cst_[:, ts], start=True, stop=True), reads=[bsr, csr], writes=[pcbr])
            cbm, cbmr = CBm.next()
            kb.op("dve", lambda e: e.tensor_tensor(out=cbm[:], in0=pcb[:, 0:128], in1=tri, op=ALU.mult), reads=[pcbr, cr], writes=[cbmr])
            for h in range(2 if LEVEL > 4 else 0):
                hs = slice(h * 64, (h + 1) * 64)
                l_, lr_ = lh.next()
                kb.op("pool", lambda e, h=h: e.tensor_scalar(out=l_[:], in0=U, scalar1=aa[:, gc, h:h + 1], scalar2=None, op0=ALU.mult),
                      reads=[cr, aar], writes=[lr_])
                pseg, psegr = pbanks.next()
                kb.op("pe", lambda e: e.matmul(pseg[:, 0:128], lhsT=l_[:], rhs=tri, start=True, stop=True), reads=[lr_, cr], writes=[psegr])
                E, Er = EE.next()
                kb.op("act", lambda e: e.activation(out=E[:], in_=pseg[:, 0:128], func=AF.Exp), reads=[psegr], writes=[Er])
                mt, mtr = MT.next()
                kb.op("dve", lambda e: e.tensor_tensor(out=mt[:], in0=E[:], in1=cbm[:], op=ALU.mult), reads=[Er, cbmr], writes=[mtr])
                py, pyr = pbanks.next()
                kb.op("pe", lambda e, hs=hs: e.matmul(py[:, 0:64], lhsT=mt[:], rhs=xb[:, hs], start=True, stop=True), reads=[mtr, xbr], writes=[pyr])
                po, por = pbanks.next()
                kb.op("pe", lambda e, h=h: e.matmul(po[:, 0:64], lhsT=cst_[:, ts], rhs=prevb[h][:], start=True, stop=True), reads=[csr, prevbr[h]], writes=[por])
                kb.op("act", lambda e, hs=hs: e.copy(out=ytile[:, c, hs], in_=py[:, 0:64]), reads=[pyr], writes=[ytr])
                kb.op("dve", lambda e, hs=hs, h=h: e.scalar_tensor_tensor(out=ytile[:, c, hs], in0=po[:, 0:64], scalar=dout[:, gc, h:h + 1], in1=ytile[:, c, hs],
                                                                          op0=ALU.mult, op1=ALU.add), reads=[por, dor, ytr], writes=[ytr])
                kb.op("dve", lambda e, hs=hs, h=h: e.scalar_tensor_tensor(out=ytile[:, c, hs], in0=xm[:, hs], scalar=rp[:, 4 + h:5 + h], in1=ytile[:, c, hs],
                                                                          op0=ALU.mult, op1=ALU.add), reads=[xmr, rpr, ytr], writes=[ytr])
                pst, pstr = pbanks.next()
                kb.op("pe", lambda e, hs=hs: e.matmul(pst[0:64, 0:64], lhsT=btm[:], rhs=xd[:, hs], start=True, stop=True), reads=[btmr, xdr], writes=[pstr])
                kb.op("dve", lambda e, h=h: e.scalar_tensor_tensor(out=prev[h][:], in0=prev[h][:], scalar=dtot[0:64, gc, h:h + 1], in1=pst[0:64, 0:64],
                                                                  op0=ALU.mult, op1=ALU.add), reads=[pstr, dtor, prevr[h]], writes=[prevr[h]])
                kb.op("act", lambda e, h=h: e.copy(out=prevb[h][:], in_=prev[h][:]), reads=[prevr[h]], writes=[prevbr[h]])
        yb_, ybr = yTb.next()
        for q2 in range(N // 512):
            pyt, pytr = pbanks.next()
            for c4 in range(4):
                c = q2 * 4 + c4
                kb.op("pe", lambda e, c=c, c4=c4: e.transpose(pyt[:, c4 * 128:(c4 + 1) * 128], ytile[:, c, :], ident), reads=[ytr, cr], writes=[pytr])
            kb.op("act", lambda e, q2=q2: e.copy(out=yb_[:, q2 * 512:(q2 + 1) * 512], in_=pyt[:, :]), reads=[pytr], writes=[ybr])
        kb.dma("sp", yT_dst[:, t0:t0 + N], yb_[:], reads=[ybr, yT_r])


def emit_lru2(kb, agF2k, agFr, gx, gxr, IX, vec, wab, wxb, outT, outr, pbanks, es=None):
    N = 1024
    vt = kb.sb([128, 8], F32, "lvec", es=es); vr = Res()
    wa = kb.sb([128, 128], F32, "lwa", es=es); war = Res()
    wx = kb.sb([128, 128], F32, "lwx", es=es); wxr = Res()
    kb.dma("sp", vt[:], vec[:, :], writes=[vr])
    kb.dma("sp", wa[:], wab[:, :], writes=[war])
    kb.dma("sp", wx[:], wxb[:, :], writes=[wxr])
    c1 = kb.sb([128, 1], F32, "lc1", es=es); c1r = Res()
    kb.op("act", lambda e: e.activation(out=c1[:], in_=vt[:, 7:8], func=AF.Exp, scale=-1.0), reads=[vr], writes=[c1r])
    kb.op("act", lambda e: e.activation(out=c1[:], in_=c1[:], func=AF.Ln, bias=1.0), reads=[c1r], writes=[c1r])
    kb.op("dve", lambda e: e.tensor_scalar(out=c1[:], in0=c1[:], scalar1=-8.0, scalar2=None, op0=ALU.mult), reads=[c1r], writes=[c1r])

    xfull = kb.sb([128, S + 3], BF16, "lxfull", es=es); xfr = Res()
    yfull = kb.sb([128, S], BF16, "lyfull", es=es); yfr = Res()
    kb.op("pool", lambda e: e.memset(xfull[:, 0:3], 0.0), writes=[xfr])
    for rp in range(4):
        c1_ = IX["lx%d" % rp]; c2_ = IX["ly%d" % rp]
        kb.gather(xfull[:, 3 + rp * 2048:3 + (rp + 1) * 2048], agF2k, gx[:, c1_:c1_ + 1], reads=[gxr, agFr], writes=[xfr])
        kb.gather(yfull[:, rp * 2048:(rp + 1) * 2048], agF2k, gx[:, c2_:c2_ + 1], reads=[gxr, agFr], writes=[yfr])
    obf = Rot(kb, 2, [128, N], BF16, "lob", es=es)
    xc = Rot(kb, 2, [128, N], F32, "lxc", es=es)
    rr = Rot(kb, 2, [128, N], F32, "lr", es=es)
    ii = Rot(kb, 2, [128, N], F32, "li", es=es)
    aa = Rot(kb, 2, [128, N], F32, "la", es=es)
    qq = Rot(kb, 2, [128, N], F32, "lq", es=es)
    hh = Rot(kb, 2, [128, N], F32, "lh", es=es)
    uu = Rot(kb, 2, [128, N], F32, "lu", es=es)
    hprev = None
    for j in range(S // N):
        t0 = j * N
        xt, xr = xfull[:, t0:t0 + N + 3], xfr
        yt, yr = yfull[:, t0:t0 + N], yfr
        ct, cr = xc.next()
        kb.op("dve", lambda e: e.tensor_scalar(out=ct[:], in0=xt[:, 0:N], scalar1=vt[:, 0:1], scalar2=vt[:, 4:5],
                                               op0=ALU.mult, op1=ALU.add), reads=[xr, vr], writes=[cr])
        for k in range(1, 4):
            kb.op("dve", lambda e, k=k: e.scalar_tensor_tensor(out=ct[:], in0=xt[:, k:k + N], scalar=vt[:, k:k + 1], in1=ct[:],
                                                              op0=ALU.mult, op1=ALU.add), reads=[xr, vr, cr], writes=[cr])
        rt, rres = rr.next()
        it, ires = ii.next()
        for hf in range(N // 512):
            sl = slice(hf * 512, (hf + 1) * 512)
            pa, par = pbanks.next()
            kb.op("pe", lambda e: e.matmul(pa[:, :], lhsT=wa[:], rhs=ct[:, sl], start=True, stop=True), reads=[war, cr], writes=[par])
            kb.op("act", lambda e: e.activation(out=rt[:, sl], in_=pa[:, :], func=AF.Sigmoid, bias=vt[:, 5:6]), reads=[par, vr], writes=[rres])
            px, pxr = pbanks.next()
            kb.op("pe", lambda e: e.matmul(px[:, :], lhsT=wx[:], rhs=ct[:, sl], start=True, stop=True), reads=[wxr, cr], writes=[pxr])
            kb.op("act", lambda e: e.activation(out=it[:, sl], in_=px[:, :], func=AF.Sigmoid, bias=vt[:, 6:7]), reads=[pxr, vr], writes=[ires])
        at, ar = aa.next()
        kb.op("act", lambda e: e.activation(out=at[:], in_=rt[:], func=AF.Exp, scale=c1[:, 0:1]), reads=[rres, c1r], writes=[ar])
        qt, qr = qq.next()
        kb.op("pool", lambda e: e.tensor_tensor(out=qt[:], in0=at[:], in1=at[:], op=ALU.mult), reads=[ar], writes=[qr])
        kb.op("pool", lambda e: e.tensor_scalar(out=qt[:], in0=qt[:], scalar1=-1.0, scalar2=1.0, op0=ALU.mult, op1=ALU.add), reads=[qr], writes=[qr])
        kb.op("act", lambda e: e.activation(out=qt[:], in_=qt[:], func=AF.Sqrt), reads=[qr], writes=[qr])
        kb.op("dve", lambda e: e.tensor_tensor(out=it[:], in0=it[:], in1=ct[:], op=ALU.mult), reads=[ires, cr], writes=[ires])
        kb.op("dve", lambda e: e.tensor_tensor(out=it[:], in0=it[:], in1=qt[:], op=ALU.mult), reads=[ires, qr], writes=[ires])
        ht, hr = hh.next()
        if hprev is None:
            kb.op("dve", lambda e: e.tensor_tensor_scan(out=ht[:], data0=at[:], data1=it[:], initial=0.0, op0=ALU.mult, op1=ALU.add),
                  reads=[ar, ires], writes=[hr])
        else:
            hp, hpr = hprev
            kb.op("dve", lambda e: e.tensor_tensor_scan(out=ht[:], data0=at[:], data1=it[:], initial=hp[:, N - 1:N], op0=ALU.mult, op1=ALU.add),
                  reads=[ar, ires, hpr], writes=[hr])
        hprev = (ht, hr)
        ut, ur = uu.next()
        kb.op("pool", lambda e: e.tensor_tensor(out=ut[:], in0=yt[:], in1=yt[:], op=ALU.mult), reads=[yr], writes=[ur])
        kb.op("pool", lambda e: e.tensor_scalar(out=ut[:], in0=ut[:], scalar1=0.044715, scalar2=1.0, op0=ALU.mult, op1=ALU.add), reads=[ur], writes=[ur])
        kb.op("pool", lambda e: e.tensor_tensor(out=ut[:], in0=ut[:], in1=yt[:], op=ALU.mult), reads=[ur, yr], writes=[ur])
        kb.op("act", lambda e: e.activation(out=ut[:], in_=ut[:], func=AF.Sigmoid, scale=1.5957691216057308), reads=[ur], writes=[ur])
        kb.op("pool", lambda e: e.tensor_tensor(out=ut[:], in0=ut[:], in1=yt[:], op=ALU.mult), reads=[ur, yr], writes=[ur])
        ob_, obr_ = obf.next()
        kb.op("pool", lambda e: e.tensor_tensor(out=ob_[:], in0=ut[:], in1=ht[:], op=ALU.mult), reads=[ur, hr], writes=[obr_])
        kb.dma("sp", outT[:, t0:t0 + N], ob_[:], reads=[obr_, outr])


def emit_C2(kb, xT, agN512, agNr, agY2k, agYr, agL2k, agLr, gx, gxr, IX, pT, w_in, pnsa, pssd, plru, w_out, pwg, pwp, rw, ewg, ewu, ewd, cst, x2T, x2r, final_out, banks, es, xTr=None):
    gen = Rot.__new__(Rot); gen.t = banks; gen.i = 0
    sb = lambda shape, dt, name: kb.sb(shape, dt, name, es=es)
    cv = sb([128, CW], F32, "cc"); cvr = Res()
    kb.dma("sp", cv[:], cst[:, :], writes=[cvr])
    ones1k = cv[:, 52:180]; ones512 = cv[:, 180:308]; ident = cv[:, 308:436]
    selE = lambda e: cv[0:16, 436 + e * 128:436 + (e + 1) * 128]
    rb = cv[:, 36:52]

    tmpf = Rot(kb, 3, [128, 512], F32, "ctf", es=es)
    stat = Rot(kb, 3, [128, 512], F32, "cstat", es=es)
    kb.uid += 1
    mixD = kb.dram("mixD%d" % kb.uid, [D, TOK], BF16, "Internal")
    mixDr = [Res() for _ in range(8)]
    wst_box = [None]

    def load_w(src_rows, nk, ncols):
        wt, wr = wst_box[0].next()
        kb.dma("pool", wt[:, 0:nk, 0:ncols], src_rows.rearrange("(c p) f -> p c f", p=128), writes=[wr])
        return wt, wr

    def ln_stats(src, srcr, nchunk, tt, ones_ap, eps, sq_eng="pool"):
        ts = slice(tt * 512, (tt + 1) * 512)
        pm_, pmr = gen.next()
        for c in range(nchunk):
            kb.op("pe", lambda e, c=c: e.matmul(pm_[:, :], lhsT=ones_ap, rhs=src[:, c, ts], start=(c == 0), stop=(c == nchunk - 1)), reads=[cvr, srcr[c]], writes=[pmr])
        pq_, pqr = gen.next()
        for c in range(nchunk):
            sq, sqr = tmpf.next()
            kb.op(sq_eng, lambda e, c=c: e.tensor_tensor(out=sq[:], in0=src[:, c, ts], in1=src[:, c, ts], op=ALU.mult), reads=[srcr[c]], writes=[sqr])
            kb.op("pe", lambda e, c=c: e.matmul(pq_[:, :], lhsT=ones_ap, rhs=sq[:], start=(c == 0), stop=(c == nchunk - 1)), reads=[cvr, sqr], writes=[pqr])
        mean, meanr = stat.next()
        kb.op("act", lambda e: e.copy(out=mean[:], in_=pm_[:, :]), reads=[pmr], writes=[meanr])
        rstd, rstdr = stat.next()
        return mean, meanr, pq_, pqr, rstd, rstdr

    with contextlib.ExitStack() as es1:
        sb1 = lambda shape, dt, name: kb.sb(shape, dt, name, es=es1)
        wst_box[0] = Rot.__new__(Rot); wst_box[0].i = 0
        wst_box[0].t = [(sb1([128, 8, 512], BF16, "cw"), Res()) for _ in range(3)]
        xbf = sb1([128, 8, TOK], BF16, "cxbf"); xbr = [Res() for _ in range(8)]
        for c in range(8):
            kb.dma("pool", xbf[:, c, :], xT[c * 128:(c + 1) * 128, :], reads=([xTr[c]] if xTr else []), writes=[xbr[c]])
        ob = [sb1([128, 4, TOK], BF16, "cob%d" % i) for i in range(3)]
        obr = [[Res() for _ in range(4)] for _ in range(3)]
        for c in range(4):
            for q4 in range(4):
                cn = IX["on%d_%d" % (c, q4)]
                kb.gather(ob[0][:, c, q4 * 512:(q4 + 1) * 512], agN512, gx[:, cn:cn + 1], reads=[gxr, agNr], writes=[obr[0][c]])
            cl = IX["l%d" % c]
            kb.gather(ob[2][:, c, :], agL2k, gx[:, cl:cl + 1], reads=[gxr, agLr], writes=[obr[2][c]])
        with contextlib.ExitStack() as es1b:
            yg = kb.sb([128, 4, TOK], F32, "cyg", es=es1b); ygr = [Res() for _ in range(4)]
            ygb = ob[1]; ygbr = obr[1]
            for c in range(4):
                cy = IX["y%d" % c]
                kb.gather(ygb[:, c, :], agY2k, gx[:, cy:cy + 1], reads=[gxr, agYr], writes=[ygbr[c]])
            wz, wzr = load_w(w_in[:, OFF_Z:OFF_Z + 512], 8, 512)
            for tt in range(NT):
                ts = slice(tt * 512, (tt + 1) * 512)
                for fc in range(4):
                    pz, pzr = gen.next()
                    for c in range(8):
                        kb.op("pe", lambda e, c=c: e.matmul(pz[:, :], lhsT=wz[:, c, fc * 128:(fc + 1) * 128], rhs=xbf[:, c, ts], start=(c == 0), stop=(c == 7)),
                              reads=[wzr, xbr[c]], writes=[pzr])
                    sz, szr = tmpf.next()
                    kb.op("act", lambda e: e.activation(out=sz[:], in_=pz[:, :], func=AF.Silu), reads=[pzr], writes=[szr])
                    kb.op("dve", lambda e, fc=fc: e.tensor_tensor(out=yg[:, fc, ts], in0=ygb[:, fc, ts], in1=sz[:], op=ALU.mult), reads=[szr, ygbr[fc]], writes=[ygr[fc]])
                pq_, pqr = gen.next()
                for c in range(4):
                    sq, sqr = tmpf.next()
                    kb.op("pool", lambda e, c=c: e.tensor_tensor(out=sq[:], in0=yg[:, c, ts], in1=yg[:, c, ts], op=ALU.mult), reads=[ygr[c]], writes=[sqr])
                    kb.op("pe", lambda e, c=c: e.matmul(pq_[:, :], lhsT=ones512, rhs=sq[:], start=(c == 0), stop=(c == 3)), reads=[cvr, sqr], writes=[pqr])
                rstd, rstdr = stat.next()
                kb.op("dve", lambda e: e.tensor_scalar(out=rstd[:], in0=pq_[:, :], scalar1=1e-5, scalar2=None, op0=ALU.add), reads=[pqr], writes=[rstdr])
                kb.op("act", lambda e: e.activation(out=rstd[:], in_=rstd[:], func=AF.Sqrt), reads=[rstdr], writes=[rstdr])
                kb.op("dve", lambda e: e.reciprocal(out=rstd[:], in_=rstd[:]), reads=[rstdr], writes=[rstdr])
                for c in range(4):
                    t1, t1r = tmpf.next()
                    kb.op("dve", lambda e, c=c: e.tensor_tensor(out=t1[:], in0=yg[:, c, ts], in1=rstd[:], op=ALU.mult), reads=[ygr[c], rstdr], writes=[t1r])
                    kb.op("act", lambda e, c=c: e.activation(out=ob[1][:, c, ts], in_=t1[:], func=AF.Copy, scale=cv[:, 32 + c:33 + c]), reads=[t1r, cvr], writes=[obr[1][c]])
            kb.barrier()
        if CDBG == "ossd":
            dbg = sb1([128, 4, TOK], F32, "cdbg"); dbgr = Res()
            for c in range(4):
                kb.op("dve", lambda e, c=c: e.tensor_copy(out=dbg[:, c, :], in_=ob[1][:, c, :]), reads=[obr[1][c]], writes=[dbgr])
                kb.dma("sp", x2T[c * 128:(c + 1) * 128, :], dbg[:, c, :], reads=[dbgr], final=True)
            kb.barrier()
            return
        projs = [pnsa, pssd, plru]
        mixh = sb1([128, 4, TOK], BF16, "cmixh"); mixhr = [Res() for _ in range(4)]
        for half in range(2):
            fs = slice(half * 512, (half + 1) * 512)
            for br in range(3):
                wp, wpr = load_w(projs[br][:, fs], 4, 512)
                wm, wmr = load_w(w_in[:, OFF_MERGE + br * 1024 + half * 512: OFF_MERGE + br * 1024 + (half + 1) * 512], 8, 512)
                for fc in range(4):
                    oc = half * 4 + fc
                    for tt in range(NT):
                        ts = slice(tt * 512, (tt + 1) * 512)
                        pg, pgr = gen.next()
                        for c in range(8):
                            kb.op("pe", lambda e, c=c: e.matmul(pg[:, :], lhsT=wm[:, c, fc * 128:(fc + 1) * 128], rhs=xbf[:, c, ts], start=(c == 0), stop=(c == 7)),
                                  reads=[wmr, xbr[c]], writes=[pgr])
                        pp, ppr = gen.next()
                        for c in range(4):
                            kb.op("pe", lambda e, c=c: e.matmul(pp[:, :], lhsT=wp[:, c, fc * 128:(fc + 1) * 128], rhs=ob[br][:, c, ts], start=(c == 0), stop=(c == 3)),
                                  reads=[wpr, obr[br][c]], writes=[ppr])
                        sg, sgr = tmpf.next()
                        kb.op("act", lambda e: e.activation(out=sg[:], in_=pg[:, :], func=AF.Sigmoid), reads=[pgr], writes=[sgr])
                        if br == 0:
                            kb.op("dve", lambda e: e.tensor_tensor(out=mixh[:, fc, ts], in0=pp[:, :], in1=sg[:], op=ALU.mult), reads=[ppr, sgr], writes=[mixhr[fc]])
                        else:
                            kb.op("dve", lambda e: e.tensor_tensor(out=sg[:], in0=pp[:, :], in1=sg[:], op=ALU.mult), reads=[ppr, sgr], writes=[sgr])
                            kb.op("pool", lambda e: e.tensor_tensor(out=mixh[:, fc, ts], in0=mixh[:, fc, ts], in1=sg[:], op=ALU.add), reads=[sgr, mixhr[fc]], writes=[mixhr[fc]])
            for fc in range(4):
                kb.dma("sp", mixD[(half * 4 + fc) * 128:(half * 4 + fc + 1) * 128, :], mixh[:, fc, :], reads=[mixhr[fc]], writes=[mixDr[half * 4 + fc]])
        kb.barrier()

    with contextlib.ExitStack() as es2:
        sb2 = lambda shape, dt, name: kb.sb(shape, dt, name, es=es2)
        r = sb2([128, 8, TOK], F32, "cr"); rr = [Res() for _ in range(8)]
        x1b = sb2([128, 8, TOK], BF16, "cx1b"); x1br = [Res() for _ in range(8)]
        for c in range(8):
            kb.dma("sp", r[:, c, :], xT[c * 128:(c + 1) * 128, :], reads=([xTr[c]] if xTr else []), writes=[rr[c]])
        gatesT = sb2([16, TOK], F32, "cgT"); gTr = Res()
        rwt = sb2([128, 8, 16], F32, "crw"); rwr = Res()
        kb.dma("sp", rwt[:], rw.rearrange("(c p) e -> p c e", p=128), writes=[rwr])
        pad = sb2([128, 4, 8], F32, "cpad"); padr = Res()
        kb.op("pool", lambda e: e.memset(pad[:], -1e30), writes=[padr])
        rt = Rot(kb, 2, [128, 16], F32, "crt", es=es2)
        rt2 = Rot(kb, 2, [128, 16], F32, "crt2", es=es2)
        t8 = Rot(kb, 2, [128, 4, 8], F32, "ct8", es=es2)
        sm4 = Rot(kb, 4, [128, 4], F32, "csm4", es=es2)
        sm1 = Rot(kb, 4, [128, 1], F32, "csm1", es=es2)
        t8b = Rot(kb, 2, [128, 8], F32, "ct8b", es=es2)
        es2a = contextlib.ExitStack()
        wst_box[0] = Rot.__new__(Rot); wst_box[0].i = 0
        wst_box[0].t = [(kb.sb([128, 8, 512], BF16, "cw2", es=es2a), Res()) for _ in range(2)]
        mixed = kb.sb([128, 8, TOK], BF16, "cmixed", es=es2a); mixr = [Res() for _ in range(8)]
        pb_ = kb.sb([128, 2, TOK], BF16, "cpb", es=es2a); pbr = [Res() for _ in range(2)]
        for c in range(8):
            kb.dma("sp", mixed[:, c, :], mixD[c * 128:(c + 1) * 128, :], reads=[mixDr[c]], writes=[mixr[c]])
        if CDBG == "mixed":
            for c in range(8):
                kb.op("dve", lambda e, c=c: e.tensor_copy(out=r[:, c, :], in_=mixed[:, c, :]), reads=[mixr[c], rr[c]], writes=[rr[c]])
                kb.dma("sp", x2T[c * 128:(c + 1) * 128, :], r[:, c, :], reads=[rr[c]], final=True)
            kb.barrier(); es2a.close()
            return
        for half in range(2):
            wo, wor = load_w(w_out[:, half * 512:(half + 1) * 512], 8, 512)
            for fc in range(4):
                oc = half * 4 + fc
                for tt in range(NT):
                    ts = slice(tt * 512, (tt + 1) * 512)
                    pu, pur = gen.next()
                    for c in range(8):
                        kb.op("pe", lambda e, c=c: e.matmul(pu[:, :], lhsT=wo[:, c, fc * 128:(fc + 1) * 128], rhs=mixed[:, c, ts], start=(c == 0), stop=(c == 7)),
                              reads=[wor, mixr[c]], writes=[pur])
                    kb.op("dve", lambda e: e.scalar_tensor_tensor(out=r[:, oc, ts], in0=r[:, oc, ts], scalar=ALPHA, in1=pu[:, :], op0=ALU.mult, op1=ALU.add),
                          reads=[pur, rr[oc]], writes=[rr[oc]])

        def layer_norm(src, srcr, gcol, bcol, dst_b, dst_br):
            for tt in range(NT):
                ts = slice(tt * 512, (tt + 1) * 512)
                mean, meanr, pq_, pqr, rstd, rstdr = ln_stats(src, srcr, 8, tt, ones1k, 1e-5)
                m2, m2r = stat.next()
                kb.op("pool", lambda e: e.tensor_tensor(out=m2[:], in0=mean[:], in1=mean[:], op=ALU.mult), reads=[meanr], writes=[m2r])
                kb.op("dve", lambda e: e.scalar_tensor_tensor(out=rstd[:], in0=pq_[:, :], scalar=1e-5, in1=m2[:], op0=ALU.add, op1=ALU.subtract), reads=[pqr, m2r], writes=[rstdr])
                kb.op("act", lambda e: e.activation(out=rstd[:], in_=rstd[:], func=AF.Sqrt), reads=[rstdr], writes=[rstdr])
                kb.op("dve", lambda e: e.reciprocal(out=rstd[:], in_=rstd[:]), reads=[rstdr], writes=[rstdr])
                for c in range(8):
                    kb.op("dve", lambda e, c=c: e.tensor_tensor(out=src[:, c, ts], in0=src[:, c, ts], in1=mean[:], op=ALU.subtract), reads=[meanr, srcr[c]], writes=[srcr[c]])
                    kb.op("pool", lambda e, c=c: e.tensor_tensor(out=src[:, c, ts], in0=src[:, c, ts], in1=rstd[:], op=ALU.mult), reads=[rstdr, srcr[c]], writes=[srcr[c]])
                    kb.op("dve", lambda e, c=c: e.tensor_scalar(out=src[:, c, ts], in0=src[:, c, ts], scalar1=cv[:, gcol + c:gcol + c + 1], scalar2=cv[:, bcol + c:bcol + c + 1],
                                                               op0=ALU.mult, op1=ALU.add), reads=[cvr, srcr[c]], writes=[srcr[c]])
                    if dst_b is not None:
                        kb.op("act", lambda e, c=c: e.copy(out=dst_b[:, c, ts], in_=src[:, c, ts]), reads=[srcr[c]], writes=[dst_br[c]])

        layer_norm(r, rr, 0, 8, x1b, x1br)
        if CDBG == "x1":
            for c in range(8):
                kb.dma("sp", x2T[c * 128:(c + 1) * 128, :], r[:, c, :], reads=[rr[c]], final=True)
            kb.barrier(); es2a.close()
            return

        for s_ in range(TOK // 128):
            ss = slice(s_ * 128, (s_ + 1) * 128)
            pl, plr = gen.next()
            for c in range(8):
                kb.op("pe", lambda e, c=c: e.matmul(pl[:, 0:16], lhsT=r[:, c, ss], rhs=rwt[:, c, :], start=(c == 0), stop=(c == 7)), reads=[rr[c], rwr], writes=[plr])
            aff, affr = rt.next()
            kb.op("act", lambda e: e.activation(out=aff[:], in_=pl[:, 0:16], func=AF.Sigmoid), reads=[plr], writes=[affr])
            sel, selr = rt2.next()
            kb.op("dve", lambda e: e.tensor_tensor(out=sel[:], in0=aff[:], in1=rb, op=ALU.add), reads=[affr, cvr], writes=[selr])
            kb.op("dve", lambda e: e.tensor_copy(out=pad[:, :, 0:4], in_=sel[:].rearrange("p (g k) -> p g k", g=4)), reads=[selr, padr], writes=[padr])
            tp, tpr = t8.next()
            for g in range(4):
                kb.op("dve", lambda e, g=g: e.max(out=tp[:, g, :], in_=pad[:, g, :]), reads=[padr], writes=[tpr])
            gs, gsr = sm4.next()
            kb.op("dve", lambda e: e.tensor_tensor(out=gs[:], in0=tp[:, :, 0], in1=tp[:, :, 1], op=ALU.add), reads=[tpr], writes=[gsr])
            gm, gmr = sm1.next()
            kb.op("dve", lambda e: e.reduce_max(out=gm[:], in_=gs[:], axis=AX.X), reads=[gsr], writes=[gmr])
            isb, isbr = sm4.next()
            kb.op("dve", lambda e: e.tensor_scalar(out=isb[:], in0=gs[:], scalar1=gm[:, 0:1], scalar2=None, op0=ALU.is_ge), reads=[gsr, gmr], writes=[isbr])
            off, offr = sm4.next()
            kb.op("dve", lambda e: e.tensor_scalar(out=off[:], in0=isb[:], scalar1=1e9, scalar2=-1e9, op0=ALU.mult, op1=ALU.add), reads=[isbr], writes=[offr])
            msk, mskr = rt2.next()
            for g in range(4):
                kb.op("dve", lambda e, g=g: e.tensor_scalar(out=msk[:, g * 4:(g + 1) * 4], in0=sel[:, g * 4:(g + 1) * 4], scalar1=isb[:, g:g + 1], scalar2=off[:, g:g + 1],
                                                            op0=ALU.mult, op1=ALU.add), reads=[selr, isbr, offr], writes=[mskr])
            tb, tbr = t8b.next()
            kb.op("dve", lambda e: e.max(out=tb[:], in_=msk[:]), reads=[mskr], writes=[tbr])
            kb.op("dve", lambda e: e.tensor_scalar(out=msk[:], in0=msk[:], scalar1=tb[:, 1:2], scalar2=None, op0=ALU.is_ge), reads=[mskr, tbr], writes=[mskr])
            kb.op("dve", lambda e: e.tensor_tensor(out=msk[:], in0=msk[:], in1=aff[:], op=ALU.mult), reads=[mskr, affr], writes=[mskr])
            ws, wsr = sm1.next()
            kb.op("dve", lambda e: e.reduce_sum(out=ws[:], in_=msk[:], axis=AX.X), reads=[mskr], writes=[wsr])
            kb.op("dve", lambda e: e.reciprocal(out=ws[:], in_=ws[:]), reads=[wsr], writes=[wsr])
            kb.op("dve", lambda e: e.tensor_scalar(out=msk[:], in0=msk[:], scalar1=ws[:, 0:1], scalar2=None, op0=ALU.mult), reads=[mskr, wsr], writes=[mskr])
            pt, ptr_ = gen.next()
            kb.op("pe", lambda e: e.transpose(pt[0:16, 0:128], msk[:], ident), reads=[mskr, cvr], writes=[ptr_])
            kb.op("act", lambda e: e.copy(out=gatesT[:, ss], in_=pt[0:16, 0:128]), reads=[ptr_], writes=[gTr])

        for c in range(2):
            kb.dma("pool", pb_[:, c, :], pT[c * 128:(c + 1) * 128, :], writes=[pbr[c]])
        for half in range(2):
            fs = slice(half * 512, (half + 1) * 512)
            wg_, wgr = load_w(pwg[:, fs], 8, 512)
            wp_, wpr = load_w(pwp[:, fs], 2, 512)
            for fc in range(4):
                oc = half * 4 + fc
                for tt in range(NT):
                    ts = slice(tt * 512, (tt + 1) * 512)
                    pg, pgr = gen.next()
                    for c in range(8):
                        kb.op("pe", lambda e, c=c: e.matmul(pg[:, :], lhsT=wg_[:, c, fc * 128:(fc + 1) * 128], rhs=x1b[:, c, ts], start=(c == 0), stop=(c == 7)),
                              reads=[wgr, x1br[c]], writes=[pgr])
                    pp, ppr = gen.next()
                    for c in range(2):
                        kb.op("pe", lambda e, c=c: e.matmul(pp[:, :], lhsT=wp_[:, c, fc * 128:(fc + 1) * 128], rhs=pb_[:, c, ts], start=(c == 0), stop=(c == 1)),
                              reads=[wpr, pbr[c]], writes=[ppr])
                    sg, sgr = tmpf.next()
                    kb.op("act", lambda e: e.activation(out=sg[:], in_=pg[:, :], func=AF.Sigmoid), reads=[pgr], writes=[sgr])
                    kb.op("dve", lambda e: e.tensor_tensor(out=sg[:], in0=pp[:, :], in1=sg[:], op=ALU.mult), reads=[ppr, sgr], writes=[sgr])
                    kb.op("dve", lambda e: e.scalar_tensor_tensor(out=r[:, oc, ts], in0=r[:, oc, ts], scalar=ALPHA, in1=sg[:], op0=ALU.mult, op1=ALU.add),
                          reads=[sgr, rr[oc]], writes=[rr[oc]])

        kb.barrier()
        es2a.close()
        if CDBG == "ple":
            for c in range(8):
                kb.dma("sp", x2T[c * 128:(c + 1) * 128, :], r[:, c, :], reads=[rr[c]], final=True)
            return
        ew = Rot(kb, 2, [128, 2, 3, 8, 256], BF16, "cew", es=es2)
        hb = Rot(kb, 2, [128, 4, 512], BF16, "chb", es=es2)
        for ep in range(8):
            wt, wr = ew.next()
            for m in range(2):
                e_ = ep * 2 + m
                kb.dma("pool", wt[:, m, 0, :, :], ewg[e_, :, :].rearrange("(c p) f -> p c f", p=128), writes=[wr])
                kb.dma("pool", wt[:, m, 1, :, :], ewu[e_, :, :].rearrange("(c p) f -> p c f", p=128), writes=[wr])
                kb.dma("pool", wt[:, m, 2, :, :].rearrange("p (c a) f -> p c (a f)", c=2), ewd[e_, :, :].rearrange("(c p) f -> p c f", p=128), writes=[wr])
            for tt in range(NT):
                ts = slice(tt * 512, (tt + 1) * 512)
                h, hr = hb.next()
                for m in range(2):
                    e_ = ep * 2 + m
                    pgb, pgbr = gen.next()
                    kb.op("pe", lambda e, e_=e_: e.matmul(pgb[:, :], lhsT=selE(e_), rhs=gatesT[:, ts], start=True, stop=True), reads=[cvr, gTr], writes=[pgbr])
                    gb, gbr = tmpf.next()
                    kb.op("act", lambda e: e.copy(out=gb[:], in_=pgb[:, :]), reads=[pgbr], writes=[gbr])
                    for fc in range(2):
                        pg, pgr = gen.next()
                        for c in range(8):
                            kb.op("pe", lambda e, c=c, m=m, fc=fc: e.matmul(pg[:, :], lhsT=wt[:, m, 0, c, fc * 128:(fc + 1) * 128], rhs=x1b[:, c, ts], start=(c == 0), stop=(c == 7)),
                                  reads=[wr, x1br[c]], writes=[pgr])
                        pu, pur = gen.next()
                        for c in range(8):
                            kb.op("pe", lambda e, c=c, m=m, fc=fc: e.matmul(pu[:, :], lhsT=wt[:, m, 1, c, fc * 128:(fc + 1) * 128], rhs=x1b[:, c, ts], start=(c == 0), stop=(c == 7)),
                                  reads=[wr, x1br[c]], writes=[pur])
                        sg, sgr = tmpf.next()
                        kb.op("act", lambda e: e.activation(out=sg[:], in_=pg[:, :], func=AF.Silu), reads=[pgr], writes=[sgr])
                        kb.op("dve", lambda e: e.tensor_tensor(out=sg[:], in0=pu[:, :], in1=sg[:], op=ALU.mult), reads=[pur, sgr], writes=[sgr])
                        kb.op("pool", lambda e, m=m, fc=fc: e.tensor_tensor(out=h[:, m * 2 + fc, :], in0=sg[:], in1=gb[:], op=ALU.mult), reads=[sgr, gbr], writes=[hr])
                for dc in range(8):
                    py, pyr = gen.next()
                    for m in range(2):
                        wd = wt[:, m, 2, :, :].rearrange("p (c a) f -> p c (a f)", c=2)
                        for fc in range(2):
                            kb.op("pe", lambda e, wd=wd, m=m, fc=fc: e.matmul(py[:, :], lhsT=wd[:, fc, dc * 128:(dc + 1) * 128], rhs=h[:, m * 2 + fc, :],
                                                                              start=(m == 0 and fc == 0), stop=(m == 1 and fc == 1)), reads=[wr, hr], writes=[pyr])
                    kb.op("dve", lambda e, dc=dc: e.tensor_tensor(out=r[:, dc, ts], in0=r[:, dc, ts], in1=py[:, :], op=ALU.add), reads=[pyr, rr[dc]], writes=[rr[dc]])

        if CDBG != "moe":
            layer_norm(r, rr, 16, 24, None, None)
        for c in range(8):
            kb.dma("sp", x2T[c * 128:(c + 1) * 128, :], r[:, c, :], reads=[rr[c]], writes=[x2r[c]], final=final_out)
        kb.barrier()


NF = 3104
NOM_TILES = [0, 2, 4, 6, 8, 10, 12, 14]
BATCH = 2
T_ALL = BATCH * S
NCORES = 8
U32 = mybir.dt.uint32


def idx_cols():
    names = []
    for nm in ("kcmp", "vcmp", "ksel", "vsel", "kwin", "vwin", "sx", "sB", "sC", "dt", "lx", "ly"):
        names += [nm + str(rp) for rp in range(4)]
    for k in range(8):
        names += ["q%d_%d" % (k, j) for j in range(4)]
        names.append("gate%d" % k)
    for c in range(4):
        names += ["on%d_%d" % (c, q4) for q4 in range(4)]
        names.append("y%d" % c)
        names.append("l%d" % c)
    return {n: i for i, n in enumerate(names)}


IX = idx_cols()
NIDX = len(IX)


def make_gidx(r):
    g, par = r // 2, r % 2
    t = np.zeros((128, NIDX), np.int64)
    p = np.arange(128)
    def rowF(rp, f):
        f = np.asarray(f)
        k = f // 256
        rows_k = np.where(k < 12, 256, 32)
        return k * 1024 + rp * rows_k + (f - 256 * k)
    rowN = lambda rank, f256: (f256 // 128) * 512 + rank * 128 + f256 % 128
    rowY = lambda rank, ch: (ch // 64) * 256 + rank * 64 + ch % 64
    for rp in range(4):
        for nm, f0 in (("kcmp", 512), ("vcmp", 640), ("ksel", 768), ("vsel", 896), ("kwin", 1024), ("vwin", 1152)):
            t[:, IX[nm + str(rp)]] = rowF(rp, f0 + 64 * g + p)
        t[:, IX["sx%d" % rp]] = rowF(rp, 1304 + 128 * r + p)
        t[:, IX["sB%d" % rp]] = rowF(rp, 1304 + 512 + 64 * g + p)
        t[:, IX["sC%d" % rp]] = rowF(rp, 1304 + 640 + 64 * g + p)
        t[:, IX["dt%d" % rp]] = rp * 8 + 2 * r + p
        t[:, IX["lx%d" % rp]] = rowF(rp, 2080 + 128 * r + p)
        t[:, IX["ly%d" % rp]] = rowF(rp, 2080 + 512 + 128 * r + p)
    for k in range(8):
        i = 2 * k + par
        rp, q4 = i // 4, i % 4
        for j in range(4):
            t[:, IX["q%d_%d" % (k, j)]] = rowF(rp, g * 256 + j * 64 + p) * 4 + q4
        t[:, IX["gate%d" % k]] = rowF(rp, 1280 + 12 * g + p) * 4 + q4
    for c in range(4):
        gg, f256 = c // 2, (c % 2) * 128 + p
        for q4 in range(4):
            i = 4 * r + q4
            t[:, IX["on%d_%d" % (c, q4)]] = rowN(2 * gg + i % 2, f256) * 8 + i // 2
        t[:, IX["y%d" % c]] = rowY(c, p) * 4 + r
        t[:, IX["l%d" % c]] = rowY(c, p) * 4 + r
    t = np.clip(t, 0, None)
    return t.astype(np.uint32)


def emit_A2(kb, xT, xTr, w, agF, agFr, agD, agDr, banks, es):
    xs = kb.sb([128, 8, TOK], BF16, "xs", es=es)
    xs_r = [Res("xs") for _ in range(8)]
    for c in range(8):
        kb.dma("pool", xs[:, c, :], xT[c * 128:(c + 1) * 128, :], reads=([xTr[c]] if xTr else []), writes=[xs_r[c]])
    FB = 512
    wbuf = [(kb.sb([128, 8, FB], BF16, "wb", es=es), Res("wb")) for _ in range(2)]
    obuf = [(kb.sb([128, 512], BF16, "ob", es=es), Res("ob")) for _ in range(4)]
    obf = [(kb.sb([8, 512], F32, "obd", es=es), Res("obd")) for _ in range(2)]
    wv = w.rearrange("(c p) f -> p c f", p=128)
    it = 0
    nb = 0
    for (c0, ncols, r0) in ((0, 1304, 0), (1816, 1800, 1304)):
        for fb in range((ncols + FB - 1) // FB):
            f0 = fb * FB
            fw_ = min(FB, ncols - f0)
            wt, wr = wbuf[nb % 2]
            nb += 1
            kb.dma("pool", wt[:, :, :fw_], wv[:, :, c0 + f0:c0 + f0 + fw_], writes=[wr])
            for fc in range((fw_ + 127) // 128):
                m = min(128, fw_ - fc * 128)
                for tt in range(TOK // 512):
                    pt, pr = banks[it % 4]
                    ot, orr = obuf[it % 4]
                    for c in range(8):
                        kb.op("pe", lambda e, c=c: e.matmul(pt[:m, :], lhsT=wt[:, c, fc * 128:fc * 128 + m], rhs=xs[:, c, tt * 512:(tt + 1) * 512],
                                                             start=(c == 0), stop=(c == 7)), reads=[wr, xs_r[c]], writes=[pr])
                    if it % 2 == 0:
                        kb.op("act", lambda e: e.copy(out=ot[:m, :], in_=pt[:m, :]), reads=[pr], writes=[orr])
                    else:
                        kb.op("dve", lambda e: e.tensor_copy(out=ot[:m, :], in_=pt[:m, :]), reads=[pr], writes=[orr])
                    row = r0 + f0 + fc * 128
                    kb.dma("sp", agF[row:row + m, tt * 512:(tt + 1) * 512], ot[:m, :], reads=[orr, agFr])
                    it += 1
    wt, wr = wbuf[nb % 2]
    kb.dma("pool", wt[:, :, 0:8], wv[:, :, 2584:2592], writes=[wr])
    for tt in range(TOK // 512):
        pt, pr = banks[it % 4]
        it += 1
        for c in range(8):
            kb.op("pe", lambda e, c=c: e.matmul(pt[0:8, :], lhsT=wt[:, c, 0:8], rhs=xs[:, c, tt * 512:(tt + 1) * 512], start=(c == 0), stop=(c == 7)),
                  reads=[wr, xs_r[c]], writes=[pr])
        od, odr = obf[tt % 2]
        kb.op("act", lambda e: e.copy(out=od[:, :], in_=pt[0:8, :]), reads=[pr], writes=[odr])
        kb.dma("sp", agD[:, tt * 512:(tt + 1) * 512], od[:, :], reads=[odr, agDr])


def build_fused(nlayers=2, groups=None, ncores=8, fstop=9):
    groups = groups or [[0, 1, 2, 3], [4, 5, 6, 7]]
    nc = bass.Bass("TRN2", target_bir_lowering=False)
    es = contextlib.ExitStack()
    with es:
        kb = KB(nc, es)
        I = lambda n, s, dt=F32: kb.dram(n, s, dt, "ExternalInput")
        xT0 = I("xT0", [D, TOK]); pT = I("pT", [2, 256, TOK]); gidx = I("gidx", [128, NIDX], U32)
        w_in = I("w_in", [2, D, 6688])
        peT = I("npeT", [2, 2, 64, 32]); w1 = I("nw1", [2, 2, 2048, 256]); w2 = I("nw2", [2, 2, 256, 64])
        nmask = I("nmask", [128, NMASK, 512]); nE0 = I("nE0", [128, S]); nmisc = I("nmisc", [128, MISC_W])
        svec = I("svec", [2, 128, 16]); srep = I("srep", [2, 128, 8]); scst = I("scst", [128, 4, 128])
        lvec = I("lvec", [2, 128, 8]); lwab = I("lwab", [2, 128, 128]); lwxb = I("lwxb", [2, 128, 128])
        pnsa = I("proj_nsa", [2, 512, D]); pssd = I("proj_ssd", [2, 512, D]); plru = I("proj_lru", [2, 512, D])
        w_out = I("w_out", [2, D, D]); pwg = I("ple_w_gate", [2, D, D]); pwp = I("ple_w_proj", [2, 256, D]); rw = I("router_w", [D, 16])
        ewg = I("exp_w_gate", [2, 16, D, 256]); ewu = I("exp_w_up", [2, 16, D, 256]); ewd = I("exp_w_down", [2, 16, 256, D])
        ccst = I("ccst", [2, 128, CW])
        x2T = kb.dram("x2T", [D, TOK], F32, "ExternalOutput")
        N_ = lambda n, s, dt: kb.dram(n, s, dt, "Internal")
        agF_in = N_("agF_in", [NF, TOK], BF16); agF_out = N_("agF_out", [4 * NF, TOK], BF16)
        agD_in = N_("agD_in", [8, TOK], F32); agD_out = N_("agD_out", [32, TOK], F32)
        nq = 8 * QT
        agN_in = N_("agN_in", [256, nq], BF16); agN_out = N_("agN_out", [1024, nq], BF16)
        agY_in = N_("agY_in", [128, S], BF16); agY_out = N_("agY_out", [512, S], BF16)
        agL_in = N_("agL_in", [128, S], BF16); agL_out = N_("agL_out", [512, S], BF16)
        xcur = N_("xcur", [D, TOK], F32)
        rF_in, rF_out, rD_in, rD_out = Res(), Res(), Res(), Res()
        rN_in, rN_out, rY_in, rY_out, rL_in, rL_out = Res(), Res(), Res(), Res(), Res(), Res()
        xcur_r = [Res() for _ in range(8)]
        agF512 = agF_out.rearrange("r (q t) -> (r q) t", t=512)
        agN512 = agN_out.rearrange("r (q t) -> (r q) t", t=512)
        agY2k = agY_out.rearrange("r (q t) -> (r q) t", t=2048)
        agL2k = agL_out.rearrange("r (q t) -> (r q) t", t=2048)
        gx = kb.sb([128, NIDX], U32, "gx"); gxr = Res()
        kb.dma("sp", gx[:], gidx[:, :], writes=[gxr])
        banks = [(kb.ps([128, 512], F32, "bank"), Res("bank", excl=True)) for _ in range(8)]
        rot = lambda items: _rot(items)
        for L in range(nlayers):
            xin = xT0 if L == 0 else xcur
            xin_r = None if L == 0 else xcur_r
            with contextlib.ExitStack() as sa:
                emit_A2(kb, xin, xin_r, w_in[L], agF_in, rF_in, agD_in, rD_in, banks, sa)
                kb.barrier()
            for k in range(13):
                rows = 256 if k < 12 else 32
                kb.allgather(agF_in[k * 256:k * 256 + rows, :], agF_out[k * 1024:k * 1024 + 4 * rows, :], groups, writes=[rF_in, rF_out])
            kb.allgather(agD_in[:, :], agD_out[:, :], groups, writes=[rD_in, rD_out])
            if fstop <= 1:
                break
            with contextlib.ExitStack() as s1:
                emit_nsa2(kb, NOM_TILES, agF_out, agF512, rF_out, gx, gxr, IX, peT[L], w1[L], w2[L], nmask, nE0, nmisc, agN_in, rN_in,
                          banks[0:4], banks[4], rot(banks[5:8]), s1)
                kb.barrier()
            for k in range(2):
                kb.allgather(agN_in[k * 128:(k + 1) * 128, :], agN_out[k * 512:(k + 1) * 512, :], groups, writes=[rN_in, rN_out])
            if fstop <= 2:
                break
            with contextlib.ExitStack() as s2:
                pbf = [(banks[6][0][:, 0:32].bitcast(BF16), banks[6][1]), (banks[7][0][:, 0:32].bitcast(BF16), banks[7][1])]
                emit_ssd2(kb, agF_out, rF_out, agD_out, rD_out, gx, gxr, IX, svec[L], srep[L], scst, agY_in, rY_in, rot(banks[0:6]), pbf, es=s2)
                kb.barrier()
            for k in range(2):
                kb.allgather(agY_in[k * 64:(k + 1) * 64, :], agY_out[k * 256:(k + 1) * 256, :], groups, writes=[rY_in, rY_out])
            if fstop <= 3:
                break
            with contextlib.ExitStack() as s3:
                emit_lru2(kb, agF_out, rF_out, gx, gxr, IX, lvec[L], lwab[L], lwxb[L], agL_in, rL_in, rot(banks[0:4]), es=s3)
                kb.barrier()
            for k in range(2):
                kb.allgather(agL_in[k * 64:(k + 1) * 64, :], agL_out[k * 256:(k + 1) * 256, :], groups, writes=[rL_in, rL_out])
            if fstop <= 4:
                break
            last = (L == nlayers - 1)
            with contextlib.ExitStack() as s4:
                emit_C2(kb, xin, agN512, rN_out, agY2k, rY_out, agL2k, rL_out, gx, gxr, IX, pT[L], w_in[L], pnsa[L], pssd[L], plru[L], w_out[L],
                        pwg[L], pwp[L], rw, ewg[L], ewu[L], ewd[L], ccst[L], x2T if last else xcur, xcur_r, last, banks, s4, xTr=xin_r)
                kb.barrier()
        kb.finish(())
        print("fused instructions", kb.ninst, "sems", kb.nsem)
    return nc


def _rot(items):
    r = Rot.__new__(Rot)
    r.t = list(items)
    r.i = 0
    return r


def fused_inputs(d, core):
    b, r = core // 4, core % 4
    g, par = r // 2, r % 2
    tok = slice(core * TOK, (core + 1) * TOK)
    x = d["x"].reshape(T_ALL, D)
    im = {"xT0": np.ascontiguousarray(x[tok].T),
          "pT": np.ascontiguousarray(np.stack([d["p"][L].reshape(T_ALL, 256)[tok].T for L in range(2)])),
          "gidx": make_gidx(r), "w_in": d["w_in"],
          "npeT": np.ascontiguousarray(np.stack([np.stack([d["nsa_pe_k"][L].T, d["nsa_pe_v"][L].T]) for L in range(2)])),
          "nw1": np.stack([np.stack([d["nsa_w1_k"][L], d["nsa_w1_v"][L]]) for L in range(2)]),
          "nw2": np.stack([np.stack([d["nsa_w2_k"][L], d["nsa_w2_v"][L]]) for L in range(2)]),
          "proj_nsa": d["proj_nsa"], "proj_ssd": d["proj_ssd"], "proj_lru": d["proj_lru"], "w_out": d["w_out"],
          "ple_w_gate": d["ple_w_gate"], "ple_w_proj": d["ple_w_proj"], "router_w": d["router_w"],
          "exp_w_gate": d["exp_w_gate"], "exp_w_up": d["exp_w_up"], "exp_w_down": d["exp_w_down"],
          "ccst": np.stack([c_consts(d, L) for L in range(2)]), "scst": ssd_consts()}
    im.update(nsa_consts(par))
    sv, sr, lv, la, lx_ = [], [], [], [], []
    for L in range(2):
        a_, b_ = ssd_vecs(d, L, r)
        sv.append(a_); sr.append(b_)
        a_, b_, c_ = lru_vecs(d, L, r)
        lv.append(a_); la.append(b_); lx_.append(c_)
    im["svec"] = np.stack(sv); im["srep"] = np.stack(sr); im["lvec"] = np.stack(lv); im["lwab"] = np.stack(la); im["lwxb"] = np.stack(lx_)
    return im


def ssd_vecs(d, layer, r):
    g = r // 2
    cw = d["ssd_conv_w"][layer]; cb = d["ssd_conv_b"][layer]
    v = np.zeros((128, 16), np.float32)
    xs_ = slice(128 * r, 128 * r + 128); Bs_ = slice(512 + 64 * g, 512 + 64 * g + 64); Cs_ = slice(640 + 64 * g, 640 + 64 * g + 64)
    v[:, 0:4] = cw[:, xs_].T; v[:, 4] = cb[xs_]
    v[:64, 5:9] = cw[:, Bs_].T; v[:64, 9] = cb[Bs_]
    v[:64, 10:14] = cw[:, Cs_].T; v[:64, 14] = cb[Cs_]
    rp = np.zeros((128, 8), np.float32)
    hh = slice(2 * r, 2 * r + 2)
    rp[:, 0:2] = d["ssd_dt_bias"][layer][hh][None, :]
    rp[:, 2:4] = d["ssd_a_log"][layer][hh][None, :]
    rp[:, 4:6] = d["ssd_d"][layer][hh][None, :]
    return v, rp


def lru_vecs(d, layer, r):
    ch = slice(128 * r, 128 * r + 128)
    v = np.zeros((128, 8), np.float32)
    v[:, 0:4] = d["lru_conv_w"][layer][:, ch].T
    v[:, 4] = d["lru_conv_b"][layer][ch]
    v[:, 5] = d["lru_ba"][layer][ch]
    v[:, 6] = d["lru_bx"][layer][ch]
    v[:, 7] = d["lru_lambda"][layer][ch]
    wab_ = np.zeros((128, 128), np.float32)
    wxb_ = np.zeros((128, 128), np.float32)
    for k in range(2):
        wab_[64 * k:64 * k + 64, 64 * k:64 * k + 64] = d["lru_wa"][layer][2 * r + k]
        wxb_[64 * k:64 * k + 64, 64 * k:64 * k + 64] = d["lru_wx"][layer][2 * r + k]
    return v, wab_, wxb_


def kernel(**inputs):
    d = {k: np.asarray(v) for k, v in inputs.items()}
    nc = build_fused()
    in_maps = [fused_inputs(d, c) for c in range(NCORES)]
    res = run_bass_kernel_spmd(nc, in_maps, core_ids=list(range(NCORES))).results
    x = np.concatenate([res[c]["x2T"].T for c in range(NCORES)], axis=0)
    return np.ascontiguousarray(x.reshape(BATCH, S, D).astype(np.float32))
```

```python
import numpy as np
import contextlib
import concourse.bass as bass
import concourse.mybir as mybir
from concourse.bass_utils import run_bass_kernel_spmd

F32 = mybir.dt.float32
BF16 = mybir.dt.bfloat16
AF = mybir.ActivationFunctionType
ALU = mybir.AluOpType
AX = mybir.AxisListType

SAME_ENGINE_SYNC = True
SEM_ROT = 20000
NDMA = 6


class Res:
    __slots__ = ("w", "r", "name", "excl")

    def __init__(self, name="", excl=False):
        self.name = name
        self.w = None
        self.r = {}
        self.excl = excl


class KB:
    def __init__(self, nc, es):
        self.nc = nc
        self.es = es
        self.eng = dict(pe=nc.tensor, act=nc.scalar, dve=nc.vector, pool=nc.gpsimd, sp=nc.sync)
        self.sems = {}
        self.cnt = {}
        self.cur = {}
        self.seen = {e: {} for e in self.eng}
        self.nsem = 0
        self.ninst = 0
        for e in self.eng:
            self.cur[e] = self._new_sem("e_" + e)
        self.dma_pool = {q: [self._new_sem("d_%s%d" % (q, i)) for i in range(NDMA)] for q in ("sp", "pool", "act")}
        self.dma_idx = {q: 0 for q in self.dma_pool}
        self.uid = 0
        self.out_marks = []

    def _new_sem(self, name):
        self.nsem += 1
        key = "%s_%d" % (name, self.nsem)
        self.sems[key] = self.es.enter_context(self.nc.semaphore(key))
        self.cnt[key] = 0
        return key

    def sb(self, shape, dtype, name=None, es=None):
        self.uid += 1
        return (es or self.es).enter_context(self.nc.sbuf_tensor("%s_%d" % (name or "sb", self.uid), list(shape), dtype))

    def ps(self, shape, dtype=F32, name=None):
        self.uid += 1
        return self.es.enter_context(self.nc.psum_tensor("%s_%d" % (name or "ps", self.uid), list(shape), dtype))

    def dram(self, name, shape, dtype, kind):
        return self.nc.dram_tensor(name, list(shape), dtype, kind=kind).ap()

    def _deps(self, reads, writes):
        deps = {}
        for r in reads:
            if r.w is not None:
                k, v = r.w
                if deps.get(k, 0) < v:
                    deps[k] = v
            if r.excl:
                for k, v in r.r.items():
                    if deps.get(k, 0) < v:
                        deps[k] = v
        for w in writes:
            if w.w is not None:
                k, v = w.w
                if deps.get(k, 0) < v:
                    deps[k] = v
            for k, v in w.r.items():
                if deps.get(k, 0) < v:
                    deps[k] = v
        return deps

    def _wait(self, e, deps, skip_key=None):
        seen = self.seen[e]
        for k, v in deps.items():
            if k == skip_key:
                continue
            if seen.get(k, 0) < v:
                self.eng[e].wait_ge(self.sems[k], v)
                seen[k] = v
                self.ninst += 1

    def _mark(self, key, v, reads, writes):
        for r in reads:
            r.r[key] = v
        for w in writes:
            w.w = (key, v)
            w.r = {}

    def op(self, e, fn, reads=(), writes=()):
        deps = self._deps(reads, writes)
        key = self.cur[e]
        skip = key if (e == "pe" or not SAME_ENGINE_SYNC) else None
        self._wait(e, deps, skip)
        inst = fn(self.eng[e])
        self.cnt[key] += 1
        v = self.cnt[key]
        inst.then_inc(self.sems[key], 1)
        self.ninst += 1
        self._mark(key, v, reads, writes)
        if v >= SEM_ROT:
            self.cur[e] = self._new_sem("e_" + e)
        return inst

    def dma(self, q, out, in_, reads=(), writes=(), final=False, **kw):
        pool = self.dma_pool[q]
        key = pool[self.dma_idx[q] % len(pool)]
        self.dma_idx[q] += 1
        deps = self._deps(reads, writes)
        if self.cnt[key] > 0:
            deps[key] = max(deps.get(key, 0), self.cnt[key])
        self._wait(q, deps)
        inst = self.eng[q].dma_start(out=out, in_=in_, **kw)
        self.cnt[key] += 16
        v = self.cnt[key]
        inst.then_inc(self.sems[key], 16)
        self.ninst += 1
        self._mark(key, v, reads, writes)
        if final:
            self.out_marks.append((key, v))
        if v >= SEM_ROT:
            i = pool.index(key)
            pool[i] = self._new_sem("d_" + q)
        return inst

    def barrier(self):
        allc = {k: v for k, v in self.cnt.items() if v > 0}
        for e in self.eng:
            self._wait(e, dict(allc))

    def finish(self, out_res):
        deps = self._deps(out_res, ())
        for k, v in self.out_marks:
            if deps.get(k, 0) < v:
                deps[k] = v
        self._wait("sp", deps)


def _kb_gather(self, out, in2d, idx_ap, reads=(), writes=()):
    pool = self.dma_pool["pool"]
    key = pool[self.dma_idx["pool"] % len(pool)]
    self.dma_idx["pool"] += 1
    deps = self._deps(reads, writes)
    if self.cnt[key] > 0:
        deps[key] = max(deps.get(key, 0), self.cnt[key])
    self._wait("pool", deps)
    inst = self.nc.gpsimd.indirect_dma_start(out=out, out_offset=None, in_=in2d,
                                             in_offset=bass.IndirectOffsetOnAxis(ap=idx_ap, axis=0))
    self.cnt[key] += 16
    v = self.cnt[key]
    inst.then_inc(self.sems[key], 16)
    self.ninst += 1
    self._mark(key, v, reads, writes)
    if v >= SEM_ROT:
        pool[pool.index(key)] = self._new_sem("d_pool")
    return inst


def _kb_allgather(self, in_ap, out_ap, groups, writes=()):
    if not hasattr(self, "cc_key"):
        self.cc_key = self._new_sem("cc")
    deps = self._deps((), writes)
    self._wait("pool", deps)
    inst = self.nc.gpsimd.collective_compute("AllGather", ALU.bypass, replica_groups=groups, ins=[in_ap], outs=[out_ap])
    key = self.cc_key
    self.cnt[key] += 1
    v = self.cnt[key]
    inst.then_inc(self.sems[key], 1)
    self.ninst += 1
    self._mark(key, v, (), writes)
    return inst


KB.gather = _kb_gather
KB.allgather = _kb_allgather


S = 8192


class Rot:
    def __init__(self, kb, n, shape, dtype, name, psum=False, es=None):
        if psum:
            self.t = [(kb.ps(shape, dtype, name), Res(name, excl=True)) for _ in range(n)]
        else:
            self.t = [(kb.sb(shape, dtype, name, es=es), Res(name)) for _ in range(n)]
        self.i = 0

    def next(self):
        x = self.t[self.i % len(self.t)]
        self.i += 1
        return x


LEVEL = 9

S = 8192


def emit_conv(kb, eng, out, xin, vt, vr, c0, N, P, xr, outr):
    kb.op(eng, lambda e: e.tensor_scalar(out=out[:P, :], in0=xin[:P, 0:N], scalar1=vt[:P, c0:c0 + 1], scalar2=vt[:P, c0 + 4:c0 + 5],
                                         op0=ALU.mult, op1=ALU.add), reads=[xr, vr], writes=[outr])
    for k in range(1, 4):
        kb.op("dve", lambda e, k=k: e.scalar_tensor_tensor(out=out[:P, :], in0=xin[:P, k:k + N], scalar=vt[:P, c0 + k:c0 + k + 1], in1=out[:P, :],
                                                          op0=ALU.mult, op1=ALU.add), reads=[xr, vr, outr], writes=[outr])


def ssd_consts():
    k = np.arange(128)
    cst = np.zeros((128, 4, 128), np.float32)
    cst[:, 0, :] = (k[:, None] <= k[None, :])
    cst[:, 1, :] = (k[:, None] > k[None, :])
    cst[:, 2, :] = np.eye(128)
    cst[:, 3, :] = 1.0
    return cst


DBG = ''

S = 8192
QT = 512
NCMP = 511
f32 = np.float32


def nsa_consts(par=0):
    kk = np.arange(128)
    tq = np.arange(512)
    c = {}
    zeros = np.zeros((128, 512), f32); onesm = np.ones((128, 512), f32)

    def cm_full(dl):
        if dl < 0:
            return zeros
        if dl >= 5:
            return onesm
        return ((16 * kk[:, None] + 31 - tq[None, :]) <= 512 * dl).astype(f32)

    def cneg_full(dk):
        if dk < 0:
            return zeros
        if dk > 3:
            return -onesm
        return np.where(128 * dk + kk[:, None] <= tq[None, :], 0.0, -1.0).astype(f32)

    def wm_full(dk):
        if dk < -4 or dk > 3:
            return zeros
        diff = tq[None, :] - (128 * dk + kk[:, None])
        return ((diff >= 0) & (diff < 512)).astype(f32)

    cm = [cm_full(dl + par) for dl in range(-1, 5)]
    cneg = [cneg_full(dk - 4 * par) for dk in range(0, 8)]
    wm = [wm_full(dk - 4 * par) for dk in range(-4, 8)]
    c["nmask"] = np.ascontiguousarray(np.stack(cm + cneg + wm, axis=1))
    key = np.arange(S)
    c["nE0"] = (kk[:, None] == (key[None, :] // 64)).astype(f32)
    n = np.arange(512)
    ov = ((n[:, None] // 4) == kk[None, :]).astype(f32) + (((n[:, None] + 1) // 4) == kk[None, :]).astype(f32)
    ov[511] = 0
    ovl = ov.reshape(4, 128, 128).transpose(1, 0, 2)
    mm = np.arange(256) - 8 * par
    hi = (kk >= 64).astype(np.int64)
    C0 = (mm[None, :] <= 128 + hi[:, None]).astype(f32)
    Cm1 = C0 - 1.0
    F = np.where((mm[None, :] == 128 + hi[:, None]) | (mm[None, :] == 127 + hi[:, None]), 1e4, -1e30).astype(f32)
    ident = np.eye(128, dtype=f32)
    ones = np.ones((128, 128), f32)
    sel64 = np.zeros((128, 64), f32); sel64[64] = 1.0
    gsel = np.zeros((128, 12, 64), f32)
    for r in range(12):
        gsel[r, r, :] = 1.0
    c["nmisc"] = np.ascontiguousarray(np.concatenate(
        [ovl.reshape(128, 512), C0, Cm1, F, ident, ones, sel64, gsel.reshape(128, 768)], axis=1))
    return c


NMASK = 26
MISC_W = 512 + 768 + 128 + 128 + 64 + 768


D = 1024
TOK = 2048


CDBG = ''

D = 1024
TOK = 2048
NT = TOK // 512
ALPHA = 4 ** 0.25
OFF_Z = 512 + 768 + 24
OFF_MERGE = 6688 - 3072
f32 = np.float32
CW = 36 + 16 + 128 + 128 + 128 + 2048


def c_consts(d, layer):
    v = np.zeros((128, CW), f32)
    col = lambda a: a.reshape(-1, 128).T
    v[:, 0:8] = col(d["ln1_g"][layer]); v[:, 8:16] = col(d["ln1_b"][layer])
    v[:, 16:24] = col(d["ln2_g"][layer]); v[:, 24:32] = col(d["ln2_b"][layer])
    v[:, 32:36] = col(d["ssd_norm_w"][layer])
    v[:, 36:52] = d["router_b"][None, :]
    v[:, 52:180] = 1.0 / 1024
    v[:, 180:308] = 1.0 / 512
    v[:, 308:436] = np.eye(128)
    sel = np.zeros((128, 16, 128), f32)
    for e in range(16):
        sel[e, e, :] = 1.0
    v[:, 436:436 + 2048] = sel.reshape(128, 2048)
    return v


def emit_nsa2(kb, tiles, agF2k, agF512, agFr, gx, gxr, IX, peT, w1, w2, nmask, nE0, nmisc, onsaT, onsar, acc, pmb, gen, es):
    sb = lambda shape, dt, name: kb.sb(shape, dt, name, es=es)
    mk = sb([128, NMASK, 512], BF16, "nmask"); mkr = Res()
    kb.dma("pool", mk[:], nmask[:, :, :], writes=[mkr])
    E0 = sb([128, S], BF16, "nE0"); E0r = Res()
    kb.dma("pool", E0[:], nE0[:, :], writes=[E0r])
    mf = sb([128, MISC_W], F32, "nmiscf"); mfr = Res()
    kb.dma("sp", mf[:], nmisc[:, :], writes=[mfr])
    o = 0
    ovl_f = mf[:, 0:512]; o = 512
    C0 = mf[:, o:o + 256]; Cm1 = mf[:, o + 256:o + 512]; F4 = mf[:, o + 512:o + 768]; o += 768
    ident = mf[:, o:o + 128]; o += 128
    ones_f = mf[:, o:o + 128]; o += 128
    sel64 = mf[:, o:o + 64]; o += 64
    gsel = mf[:, o:o + 768]
    cb_ = sb([128, 512 + 128 + 128], BF16, "ncb"); cbr = Res()
    kb.op("dve", lambda e: e.tensor_copy(out=cb_[:, 0:512], in_=ovl_f), reads=[mfr], writes=[cbr])
    kb.op("dve", lambda e: e.tensor_copy(out=cb_[:, 512:640], in_=ident), reads=[mfr], writes=[cbr])
    kb.op("dve", lambda e: e.tensor_copy(out=cb_[:, 640:768], in_=ones_f), reads=[mfr], writes=[cbr])
    ovl = cb_[:, 0:512]; identb = cb_[:, 512:640]; onesb = cb_[:, 640:768]

    kall = sb([64, 2, S], BF16, "nk"); kr = [None, None, Res(), Res()]
    for i, nm in ((2, "ksel"), (3, "kwin")):
        for rp in range(4):
            kb.gather(kall[:, i - 2, rp * 2048:(rp + 1) * 2048], agF2k, gx[0:64, IX[nm + str(rp)]:IX[nm + str(rp)] + 1], reads=[gxr, agFr], writes=[kr[i]])
    va = sb([128, 2, 64, 65], BF16, "nva"); var = [Res() for _ in range(2)]
    for i in range(2):
        kb.op("pool", lambda e, i=i: e.memset(va[:, i, :, 64:65], 1.0), writes=[var[i]])

    kcT = sb([64, 512], BF16, "nkcT"); kcr = Res()
    vct = sb([128, 4, 65], BF16, "nvct"); vcr = Res()
    kb.op("pool", lambda e: e.memset(kcT[:], 0.0), writes=[kcr])
    kb.op("pool", lambda e: e.memset(vct[:], 0.0), writes=[vcr])
    kb.op("pool", lambda e: e.memset(vct[:, :, 64:65], 1.0), writes=[vcr])
    with contextlib.ExitStack() as esv:
        vT = kb.sb([64, 2, S], BF16, "nvT", es=esv); vTr = [Res(), Res()]
        for i, nm in ((0, "vsel"), (1, "vwin")):
            for rp in range(4):
                kb.gather(vT[:, i, rp * 2048:(rp + 1) * 2048], agF2k, gx[0:64, IX[nm + str(rp)]:IX[nm + str(rp)] + 1], reads=[gxr, agFr], writes=[vTr[i]])
            for k8 in range(8):
                pv8, pv8r = gen.next()
                pvb = pv8[:, 0:256].bitcast(BF16)
                for kk in range(8):
                    kt = k8 * 8 + kk
                    kb.op("pe", lambda e, kt=kt, kk=kk, i=i: e.transpose(pvb[:, kk * 64:(kk + 1) * 64], vT[:, i, kt * 128:(kt + 1) * 128], identb[0:64, 0:64]),
                          reads=[vTr[i], cbr], writes=[pv8r])
                kb.op("act", lambda e, k8=k8, i=i: e.copy(out=va[:, i, k8 * 8:(k8 + 1) * 8, 0:64], in_=pvb.rearrange("p (k d) -> p k d", k=8)), reads=[pv8r], writes=[var[i]])
        kb.barrier()
    with contextlib.ExitStack() as es2:
        sb2 = lambda shape, dt, name: kb.sb(shape, dt, name, es=es2)
        kcv = sb2([64, 2, S], BF16, "nkcv"); kr[0] = Res(); kr[1] = Res()
        for i, nm in ((0, "kcmp"), (1, "vcmp")):
            for rp in range(4):
                kb.gather(kcv[:, i, rp * 2048:(rp + 1) * 2048], agF2k, gx[0:64, IX[nm + str(rp)]:IX[nm + str(rp)] + 1], reads=[gxr, agFr], writes=[kr[i]])
        w1t = sb2([64, 32, 256], BF16, "nw1"); w1r = Res()
        w2t = sb2([128, 2, 64], BF16, "nw2"); w2r = Res()
        pet = sb2([64, 32], BF16, "npe"); per = Res()
        bias = sb2([128, 1], F32, "nbias"); biasr = Res()
        tmp3 = [(sb2([128, 512], F32, "nt"), Res(), sb2([128, 512], F32, "nu"), Res(), sb2([128, 512], BF16, "ngt"), Res()) for _ in range(2)]
        for which in range(2):
            for q4 in range(4):
                kb.dma("pool", w1t[:, q4 * 8:(q4 + 1) * 8, :], w1[which, q4 * 512:(q4 + 1) * 512, :].rearrange("(l d) j -> d l j", d=64), writes=[w1r])
            kb.dma("pool", w2t[:], w2[which, :, :].rearrange("(c p) d -> p c d", p=128), writes=[w2r])
            kb.dma("pool", pet[:], peT[which, :, :], writes=[per])
            src = kcv[:, which, :]
            gts = []
            for jc in range(2):
                ph, phr = gen.next()
                for l in range(32):
                    kb.op("pe", lambda e, l=l: e.matmul(ph[:, 0:NCMP], lhsT=w1t[:, l, jc * 128:(jc + 1) * 128],
                                                         rhs=src[:, l:l + 16 * (NCMP - 1) + 1:16], start=(l == 0), stop=(l == 31)),
                          reads=[w1r, kr[which]], writes=[phr])
                pbias, pbr = gen.next()
                for l in range(32):
                    kb.op("pe", lambda e, l=l: e.matmul(pbias[:, 0:1], lhsT=w1t[:, l, jc * 128:(jc + 1) * 128], rhs=pet[:, l:l + 1],
                                                         start=(l == 0), stop=(l == 31)), reads=[w1r, per], writes=[pbr])
                kb.op("dve", lambda e: e.tensor_copy(out=bias[:], in_=pbias[:, 0:1]), reads=[pbr], writes=[biasr])
                t, tr, u, ur, gt, gr = tmp3[jc]
                kb.op("act", lambda e: e.activation(out=t[:, 0:NCMP], in_=ph[:, 0:NCMP], func=AF.Identity, bias=bias[:, 0:1]), reads=[phr, biasr], writes=[tr])
                kb.op("dve", lambda e: e.tensor_tensor(out=u[:, 0:NCMP], in0=t[:, 0:NCMP], in1=t[:, 0:NCMP], op=ALU.mult), reads=[tr], writes=[ur])
                kb.op("dve", lambda e: e.tensor_scalar(out=u[:, 0:NCMP], in0=u[:, 0:NCMP], scalar1=0.044715, scalar2=1.0, op0=ALU.mult, op1=ALU.add), reads=[ur], writes=[ur])
                kb.op("dve", lambda e: e.tensor_tensor(out=u[:, 0:NCMP], in0=u[:, 0:NCMP], in1=t[:, 0:NCMP], op=ALU.mult), reads=[ur, tr], writes=[ur])
                kb.op("act", lambda e: e.activation(out=u[:, 0:NCMP], in_=u[:, 0:NCMP], func=AF.Sigmoid, scale=1.5957691216057308), reads=[ur], writes=[ur])
                kb.op("pool", lambda e: e.memset(gt[:, NCMP:512], 0.0), writes=[gr])
                kb.op("dve", lambda e: e.tensor_tensor(out=gt[:, 0:NCMP], in0=u[:, 0:NCMP], in1=t[:, 0:NCMP], op=ALU.mult), reads=[ur, tr], writes=[gr])
                gts.append((gt, gr))
            if which == 0:
                pk, pkr = gen.next()
                for jc in range(2):
                    kb.op("pe", lambda e, jc=jc: e.matmul(pk[0:64, 0:NCMP], lhsT=w2t[:, jc, :], rhs=gts[jc][0][:, 0:NCMP], start=(jc == 0), stop=(jc == 1)),
                          reads=[w2r, gts[jc][1]], writes=[pkr])
                kb.op("act", lambda e: e.copy(out=kcT[:, 0:NCMP], in_=pk[0:64, 0:NCMP]), reads=[pkr], writes=[kcr])
            else:
                for m in range(4):
                    pv, pvr = gen.next()
                    for jc in range(2):
                        kb.op("pe", lambda e, jc=jc, m=m: e.matmul(pv[:, 0:64], lhsT=gts[jc][0][:, m * 128:(m + 1) * 128], rhs=w2t[:, jc, :], start=(jc == 0), stop=(jc == 1)),
                              reads=[w2r, gts[jc][1]], writes=[pvr])
                    kb.op("act", lambda e, m=m: e.copy(out=vct[:, m, 0:64], in_=pv[:, 0:64]), reads=[pvr], writes=[vcr])
        kb.barrier()
    kmax = sb([128, 1], F32, "nkmax"); kmr = Res()
    kb.op("pool", lambda e: e.memset(kmax[:], 0.0), writes=[kmr])
    sq = Rot(kb, 2, [64, 512], BF16, "nsq", es=es)
    red = Rot(kb, 2, [128, 1], F32, "nred", es=es)

    def norm_max(src_ap, n, src_res, dst, dstr):
        s_, sr_ = sq.next()
        kb.op("pool", lambda e: e.tensor_tensor(out=s_[:, 0:n], in0=src_ap, in1=src_ap, op=ALU.mult), reads=src_res, writes=[sr_])
        pn, pnr = gen.next()
        kb.op("pe", lambda e: e.matmul(pn[:, 0:n], lhsT=onesb[0:64, :], rhs=s_[:, 0:n], start=True, stop=True), reads=[cbr, sr_], writes=[pnr])
        r_, rr_ = red.next()
        kb.op("dve", lambda e: e.reduce_max(out=r_[:], in_=pn[:, 0:n], axis=AX.X), reads=[pnr], writes=[rr_])
        kb.op("dve", lambda e: e.tensor_tensor(out=dst[:], in0=dst[:], in1=r_[:], op=ALU.max), reads=[rr_, dstr], writes=[dstr])

    norm_max(kcT[:, 0:512], 512, [kcr], kmax, kmr)
    for which in (2, 3):
        for tt in range(S // 512):
            norm_max(kall[:, which - 2, tt * 512:(tt + 1) * 512], 512, [kr[which]], kmax, kmr)

    qb = Rot(kb, 2, [64, 4, 512], BF16, "nq", es=es)
    gtl = Rot(kb, 2, [12, 512], F32, "ngate", es=es)
    gtbl = Rot(kb, 2, [12, 512], BF16, "ngateb", es=es)
    ebuf = Rot(kb, 4, [128, 512], BF16, "ne", es=es)
    pbuf = Rot(kb, 4, [128, 512], BF16, "np", es=es)
    mskb = Rot(kb, 2, [128, 512], BF16, "nmsk", es=es)
    pnbuf = Rot(kb, 4, [128, 512], BF16, "npn", es=es)
    rinv = Rot(kb, 2, [128, 512], F32, "nrinv", es=es)
    ocmp = Rot(kb, 1, [64, 4, 512], F32, "nocmp", es=es)
    impT = Rot(kb, 2, [128, 512], F32, "nimpT", es=es)
    selT = Rot(kb, 2, [128, 512], BF16, "nselT", es=es)
    v1b = Rot(kb, 2, [128, 128], F32, "nv1", es=es)
    v2b = Rot(kb, 2, [128, 128], F32, "nv2", es=es)
    m8a = Rot(kb, 2, [128, 8], F32, "nm8a", es=es)
    m8b = Rot(kb, 2, [128, 8], F32, "nm8b", es=es)
    smk = Rot(kb, 2, [128, 128], F32, "nsmk", es=es)
    accs = Rot(kb, 1, [65, 8, 512], F32, "naccs", es=es)
    rec = Rot(kb, 2, [64, 512], F32, "nrec", es=es)
    osb = Rot(kb, 2, [64, 512], F32, "nosb", es=es)
    negc = Rot(kb, 2, [128, 4], F32, "nnegc", es=es)
    qm = Rot(kb, 2, [128, 1], F32, "nqm", es=es)

    for ti, i in enumerate(tiles):
        t0 = i * QT
        q, qr = qb.next()
        for j in range(4):
            cq = IX["q%d_%d" % (ti, j)]
            kb.gather(q[:, j, :], agF512, gx[0:64, cq:cq + 1], reads=[gxr, agFr], writes=[qr])
        gtb_, gtbr = gtbl.next()
        cg = IX["gate%d" % ti]
        kb.gather(gtb_[:], agF512, gx[0:12, cg:cg + 1], reads=[gxr, agFr], writes=[gtbr])
        gt_, gtr = gtl.next()
        kb.op("act", lambda e: e.activation(out=gt_[:], in_=gtb_[:], func=AF.Sigmoid), reads=[gtbr], writes=[gtr])
        nc_, ncr = negc.next()
        for j in range(4):
            qm_, qmr = qm.next()
            kb.op("pool", lambda e: e.memset(qm_[:], 0.0), writes=[qmr])
            norm_max(q[:, j, :], 512, [qr], qm_, qmr)
            kb.op("dve", lambda e, j=j: e.tensor_tensor(out=nc_[:, j:j + 1], in0=qm_[:], in1=kmax[:], op=ALU.mult), reads=[qmr, kmr], writes=[ncr])
        kb.op("act", lambda e: e.activation(out=nc_[:], in_=nc_[:], func=AF.Sqrt, scale=1.05), reads=[ncr], writes=[ncr])
        kb.op("dve", lambda e: e.tensor_scalar(out=nc_[:], in0=nc_[:], scalar1=-0.125, scalar2=None, op0=ALU.mult), reads=[ncr], writes=[ncr])

        nch = min(4, (32 * (i + 1) + 31 + 127) // 128)
        oc, ocr = ocmp.next()
        pimp, pimpr = acc[0]
        for j in range(4):
            es_ = []
            psum_, psumr = acc[1]
            for m in range(nch):
                ps_, psr = gen.next()
                kb.op("pe", lambda e, m=m, j=j: e.matmul(ps_[:, :], lhsT=kcT[:, m * 128:(m + 1) * 128], rhs=q[:, j, :], start=True, stop=True),
                      reads=[kcr, qr], writes=[psr])
                e_, er = ebuf.next()
                kb.op("act", lambda e, j=j: e.activation(out=e_[:], in_=ps_[:, :], func=AF.Exp, scale=0.125, bias=nc_[:, j:j + 1]), reads=[psr, ncr], writes=[er])
                dl = i - 4 * m
                if dl <= 4:
                    kb.op("pool", lambda e, dl=dl: e.tensor_tensor(out=e_[:], in0=e_[:], in1=mk[:, dl + 1, :], op=ALU.mult), reads=[er, mkr], writes=[er])
                kb.op("pe", lambda e, m=m: e.matmul(psum_[:, :], lhsT=onesb, rhs=e_[:], start=(m == 0), stop=(m == nch - 1)), reads=[cbr, er], writes=[psumr])
                es_.append((e_, er))
            ri, rir = rinv.next()
            kb.op("dve", lambda e: e.tensor_scalar(out=ri[:], in0=psum_[:, :], scalar1=1e-30, scalar2=None, op0=ALU.max), reads=[psumr], writes=[rir])
            kb.op("dve", lambda e: e.reciprocal(out=ri[:], in_=ri[:]), reads=[rir], writes=[rir])
            po, por = acc[2]
            for m in range(nch):
                e_, er = es_[m]
                pn_, pnr_ = pnbuf.next()
                kb.op("dve", lambda e: e.tensor_tensor(out=pn_[:], in0=e_[:], in1=ri[:], op=ALU.mult), reads=[er, rir], writes=[pnr_])
                kb.op("pe", lambda e, m=m: e.matmul(po[0:64, :], lhsT=vct[:, m, 0:64], rhs=pn_[:], start=(m == 0), stop=(m == nch - 1)), reads=[vcr, pnr_], writes=[por])
                kb.op("pe", lambda e, m=m, j=j: e.matmul(pimp[:, :], lhsT=ovl[:, m * 128:(m + 1) * 128], rhs=pn_[:], start=(j == 0 and m == 0), stop=(j == 3 and m == nch - 1)),
                      reads=[cbr, pnr_], writes=[pimpr])
            kb.op("act", lambda e, j=j: e.copy(out=oc[:, j, :], in_=po[0:64, :]), reads=[por], writes=[ocr])
        it_, itr = impT.next()
        kb.op("act", lambda e: e.copy(out=it_[:], in_=pimp[:, :]), reads=[pimpr], writes=[itr])
        st_, str_ = selT.next()
        for k4 in range(4):
            ksub = 4 * i + k4
            ptr_, ptrr = gen.next()
            kb.op("pe", lambda e, k4=k4: e.transpose(ptr_[:, 0:128], it_[:, k4 * 128:(k4 + 1) * 128], ident), reads=[itr, mfr], writes=[ptrr])
            co = 128 - 2 * ksub
            v1, v1r = v1b.next()
            kb.op("dve", lambda e, co=co: e.tensor_tensor(out=v1[:], in0=ptr_[:, 0:128], in1=C0[:, co:co + 128], op=ALU.mult), reads=[ptrr, mfr], writes=[v1r])
            kb.op("dve", lambda e, co=co: e.tensor_tensor(out=v1[:], in0=v1[:], in1=Cm1[:, co:co + 128], op=ALU.add), reads=[v1r, mfr], writes=[v1r])
            kb.op("dve", lambda e, co=co: e.tensor_tensor(out=v1[:], in0=v1[:], in1=F4[:, co:co + 128], op=ALU.max), reads=[v1r, mfr], writes=[v1r])
            kb.op("dve", lambda e: e.memset(v1[:, 0:1], 1e4), reads=[], writes=[v1r])
            a8, a8r = m8a.next()
            kb.op("dve", lambda e: e.max(out=a8[:], in_=v1[:]), reads=[v1r], writes=[a8r])
            v2, v2r = v2b.next()
            kb.op("dve", lambda e: e.match_replace(out=v2[:], in_to_replace=a8[:], in_values=v1[:], imm_value=-1e30), reads=[a8r, v1r], writes=[v2r])
            b8, b8r = m8b.next()
            kb.op("dve", lambda e: e.max(out=b8[:], in_=v2[:]), reads=[v2r], writes=[b8r])
            sm, smr = smk.next()
            kb.op("dve", lambda e: e.tensor_scalar(out=sm[:], in0=v1[:], scalar1=b8[:, 7:8], scalar2=None, op0=ALU.is_ge), reads=[v1r, b8r], writes=[smr])
            if DBG == "sm" and k4 == 0 and ti == 0:
                kb.dma("sp", onsaT[0:128, 0:128], sm[:], reads=[smr], final=True)
                kb.dma("sp", onsaT[128:256, 0:128], v1[:], reads=[v1r], final=True)
                kb.dma("sp", onsaT[0:128, 128:136], a8[:], reads=[a8r], final=True)
                kb.dma("sp", onsaT[0:128, 136:144], b8[:], reads=[b8r], final=True)
                kb.dma("sp", onsaT[128:256, 128:256], v2[:], reads=[v2r], final=True)
            pt2, pt2r = gen.next()
            kb.op("pe", lambda e: e.transpose(pt2[:, 0:128], sm[:], ident), reads=[smr, mfr], writes=[pt2r])
            kb.op("act", lambda e, k4=k4: e.copy(out=st_[:, k4 * 128:(k4 + 1) * 128], in_=pt2[:, 0:128]), reads=[pt2r], writes=[str_])

        as_, asr = accs.next()
        LA = 2
        for br in range(2):
            if br == 0:
                kts = list(range(0, min(64, 4 * i + 8)))
            else:
                kts = [kt for kt in range(4 * i - 4, min(64, 4 * i + 8)) if kt >= 0]
            ksrc = 2 + br
            pairs = [(n_, kt, j) for n_, kt in enumerate(kts) for j in range(4)]
            pend = []
            msk = None
            for idx in range(len(pairs) + LA):
                if idx < len(pairs):
                    n_, kt, j = pairs[idx]
                    dk = kt - 4 * i
                    if br == 0 and j == 0:
                        pm, pmr = pmb
                        kb.op("pe", lambda e, kt=kt, dk=dk: e.matmul(pm[:, :], lhsT=E0[:, kt * 128:(kt + 1) * 128], rhs=st_[:], start=True, stop=(dk < 0)),
                              reads=[E0r, str_], writes=[pmr])
                        if dk >= 0:
                            kb.op("pe", lambda e, dk=dk: e.matmul(pm[:, :], lhsT=identb, rhs=mk[:, 6 + dk, :], start=False, stop=True), reads=[cbr, mkr], writes=[pmr])
                        msk = mskb.next()
                        kb.op("act", lambda e, msk=msk: e.activation(out=msk[0][:], in_=pm[:, :], func=AF.Relu), reads=[pmr], writes=[msk[1]])
                    ps_, psr = gen.next()
                    kb.op("pe", lambda e, kt=kt, j=j, ps_=ps_: e.matmul(ps_[:, :], lhsT=kall[:, br, kt * 128:(kt + 1) * 128], rhs=q[:, j, :], start=True, stop=True),
                          reads=[kr[ksrc], qr], writes=[psr])
                    e_, er = ebuf.next()
                    kb.op("act", lambda e, j=j, e_=e_, ps_=ps_: e.activation(out=e_[:], in_=ps_[:, :], func=AF.Exp, scale=0.125, bias=nc_[:, j:j + 1]), reads=[psr, ncr], writes=[er])
                    p_, pr_ = pbuf.next()
                    eng = "dve" if j % 2 == 0 else "pool"
                    if br == 0:
                        kb.op(eng, lambda e, p_=p_, e_=e_, msk=msk: e.tensor_tensor(out=p_[:], in0=e_[:], in1=msk[0][:], op=ALU.mult), reads=[er, msk[1]], writes=[pr_])
                    else:
                        kb.op(eng, lambda e, dk=dk, p_=p_, e_=e_: e.tensor_tensor(out=p_[:], in0=e_[:], in1=mk[:, 14 + dk + 4, :], op=ALU.mult), reads=[er, mkr], writes=[pr_])
                    pend.append((p_, pr_, n_, kt, j))
                if idx >= LA:
                    p_, pr_, n_, kt, j = pend[idx - LA]
                    pa, par = acc[j]
                    kb.op("pe", lambda e, kt=kt, n_=n_, p_=p_, pa=pa: e.matmul(pa[0:65, :], lhsT=va[:, br, kt, :], rhs=p_[:], start=(n_ == 0), stop=(n_ == len(kts) - 1)),
                          reads=[var[br], pr_], writes=[par])
            for j in range(4):
                pa, par = acc[j]
                kb.op("act", lambda e, j=j, br=br: e.copy(out=as_[:, br * 4 + j, :], in_=pa[0:65, :]), reads=[par], writes=[asr])
        for j in range(4 if DBG not in ("sm", "pm") else 0):
            o_, or_ = osb.next()
            pg, pgr = gen.next()
            kb.op("pe", lambda e, j=j: e.matmul(pg[0:64, :], lhsT=gsel[0:12, (j * 3) * 64:(j * 3 + 1) * 64], rhs=gt_[:], start=True, stop=True), reads=[mfr, gtr], writes=[pgr])
            if DBG == "cmp":
                kb.op("dve", lambda e, j=j: e.tensor_copy(out=o_[:], in_=oc[:, j, :]), reads=[pgr, ocr], writes=[or_])
            elif DBG:
                kb.op("dve", lambda e, j=j: e.memset(o_[:], 0.0), reads=[pgr, ocr], writes=[or_])
            else:
                kb.op("dve", lambda e, j=j: e.tensor_tensor(out=o_[:], in0=pg[0:64, :], in1=oc[:, j, :], op=ALU.mult), reads=[pgr, ocr], writes=[or_])
            for br in range(2):
                if DBG == "cmp" or (DBG == "sel" and br == 1) or (DBG == "win" and br == 0):
                    continue
                psm, psmr = gen.next()
                kb.op("pe", lambda e, j=j, br=br: e.matmul(psm[0:64, :], lhsT=sel64[0:65, :], rhs=as_[:, br * 4 + j, :], start=True, stop=True), reads=[mfr, asr], writes=[psmr])
                rc, rcr = rec.next()
                kb.op("dve", lambda e: e.tensor_scalar(out=rc[:], in0=psm[0:64, :], scalar1=1e-30, scalar2=None, op0=ALU.max), reads=[psmr], writes=[rcr])
                kb.op("dve", lambda e: e.reciprocal(out=rc[:], in_=rc[:]), reads=[rcr], writes=[rcr])
                pg2, pg2r = gen.next()
                kb.op("pe", lambda e, j=j, br=br: e.matmul(pg2[0:64, :], lhsT=gsel[0:12, (j * 3 + 1 + br) * 64:(j * 3 + 2 + br) * 64], rhs=gt_[:], start=True, stop=True),
                      reads=[mfr, gtr], writes=[pg2r])
                if not DBG:
                    kb.op("dve", lambda e: e.tensor_tensor(out=rc[:], in0=pg2[0:64, :], in1=rc[:], op=ALU.mult), reads=[pg2r, rcr], writes=[rcr])
                kb.op("pool", lambda e, j=j, br=br: e.tensor_tensor(out=rc[:], in0=rc[:], in1=as_[0:64, br * 4 + j, :], op=ALU.mult), reads=[rcr, asr], writes=[rcr])
                kb.op("pool", lambda e: e.tensor_tensor(out=o_[:], in0=o_[:], in1=rc[:], op=ALU.add), reads=[rcr, or_], writes=[or_])
            kb.dma("pool", onsaT[j * 64:(j + 1) * 64, ti * QT:(ti + 1) * QT], o_[:], reads=[or_, onsar])


def emit_ssd2(kb, agF2k, agFr, agD2k, agDr, gx, gxr, IX, svec, srep, cst, yT_dst, yT_r, pbanks, pbf, es=None):
    N = 1024
    NCH = S // 128
    ct = kb.sb([128, 4, 128], F32, "scst", es=es); cr = Res()
    kb.dma("sp", ct[:], cst[:, :, :], writes=[cr])
    tri = ct[:, 0, :]; U = ct[:, 1, :]; ident = ct[:, 2, :]; ones = ct[:, 3, :]
    identb = kb.sb([128, 128], BF16, "sidb", es=es); idbr = Res()
    kb.op("dve", lambda e: e.tensor_copy(out=identb[:], in_=ident), reads=[cr], writes=[idbr])
    vt = kb.sb([128, 16], F32, "svec", es=es); vr = Res()
    kb.dma("sp", vt[:], svec[:, :], writes=[vr])
    rp = kb.sb([128, 8], F32, "srep", es=es); rpr = Res()
    kb.dma("sp", rp[:], srep[:, :], writes=[rpr])
    if LEVEL == -1: return
    dt = kb.sb([128, NCH, 2], F32, "sdt", es=es); dtr = Res()
    dtT = kb.sb([2, S], F32, "sdtT", es=es); dtTr = Res()
    for rq in range(4):
        cd = IX["dt%d" % rq]
        kb.gather(dtT[0:2, rq * 2048:(rq + 1) * 2048], agD2k, gx[0:2, cd:cd + 1], reads=[gxr, agDr], writes=[dtTr])
    pdt, pdtr = pbanks.next()
    for c in range(NCH):
        kb.op("pe", lambda e, c=c: e.transpose(pdt[:, c * 2:(c + 1) * 2], dtT[0:2, c * 128:(c + 1) * 128], ident[0:2, 0:2]), reads=[dtTr, cr], writes=[pdtr])
    kb.op("dve", lambda e: e.tensor_copy(out=dt[:].rearrange("p c h -> p (c h)"), in_=pdt[:, 0:NCH * 2]), reads=[pdtr], writes=[dtr])
    aa = kb.sb([128, NCH, 2], F32, "sa", es=es); aar = Res()
    An = kb.sb([128, 2], F32, "sAn", es=es); Anr = Res()
    kb.op("act", lambda e: e.activation(out=An[:], in_=rp[:, 2:4], func=AF.Exp), reads=[rpr], writes=[Anr])
    kb.op("dve", lambda e: e.tensor_scalar(out=An[:], in0=An[:], scalar1=-1.0, scalar2=None, op0=ALU.mult), reads=[Anr], writes=[Anr])
    for h in range(2):
        kb.op("dve", lambda e, h=h: e.tensor_scalar(out=dt[:, :, h], in0=dt[:, :, h], scalar1=rp[:, h:h + 1], scalar2=None, op0=ALU.add),
              reads=[dtr, rpr], writes=[dtr])
    kb.op("act", lambda e: e.activation(out=dt[:], in_=dt[:], func=AF.Exp), reads=[dtr], writes=[dtr])
    kb.op("act", lambda e: e.activation(out=dt[:], in_=dt[:], func=AF.Ln, bias=1.0), reads=[dtr], writes=[dtr])
    for h in range(2):
        kb.op("dve", lambda e, h=h: e.tensor_scalar(out=aa[:, :, h], in0=dt[:, :, h], scalar1=An[:, h:h + 1], scalar2=None, op0=ALU.mult),
              reads=[dtr, Anr], writes=[aar])
    if LEVEL == -2: return
    acum = kb.sb([128, NCH, 2], F32, "sacum", es=es); acr = Res()
    dout = kb.sb([128, NCH, 2], F32, "sdout", es=es); dor = Res()
    dst = kb.sb([128, NCH, 2], F32, "sdst", es=es); dsr = Res()
    dtot = kb.sb([128, NCH, 2], F32, "sdtot", es=es); dtor = Res()
    aflat = aa[:].rearrange("p c h -> p (c h)")
    p1, p1r = pbanks.next()
    kb.op("pe", lambda e: e.matmul(p1[:, 0:NCH * 2], lhsT=tri, rhs=aflat, start=True, stop=True), reads=[cr, aar], writes=[p1r])
    p2, p2r = pbanks.next()
    kb.op("pe", lambda e: e.matmul(p2[:, 0:NCH * 2], lhsT=ones, rhs=aflat, start=True, stop=True), reads=[cr, aar], writes=[p2r])
    fl = lambda t: t[:].rearrange("p c h -> p (c h)")
    kb.op("dve", lambda e: e.tensor_copy(out=fl(acum), in_=p1[:, 0:NCH * 2]), reads=[p1r], writes=[acr])
    kb.op("act", lambda e: e.activation(out=fl(dout), in_=p1[:, 0:NCH * 2], func=AF.Exp), reads=[p1r], writes=[dor])
    kb.op("act", lambda e: e.activation(out=fl(dtot), in_=p2[:, 0:NCH * 2], func=AF.Exp), reads=[p2r], writes=[dtor])
    kb.op("dve", lambda e: e.tensor_tensor(out=fl(dst), in0=p2[:, 0:NCH * 2], in1=fl(acum), op=ALU.subtract), reads=[p2r, acr], writes=[dsr])
    kb.op("act", lambda e: e.activation(out=fl(dst), in_=fl(dst), func=AF.Exp), reads=[dsr], writes=[dsr])

    if LEVEL == -3: return
    prev = [kb.sb([64, 64], F32, "sprev", es=es) for _ in range(2)]
    prevr = [Res() for _ in range(2)]
    prevb = [kb.sb([64, 64], BF16, "sprevb", es=es) for _ in range(2)]
    prevbr = [Res() for _ in range(2)]
    for h in range(2):
        kb.op("pool", lambda e, h=h: e.memset(prev[h][:], 0.0), writes=[prevr[h]])
        kb.op("pool", lambda e, h=h: e.memset(prevb[h][:], 0.0), writes=[prevbr[h]])

    xfull = kb.sb([128, S + 3], BF16, "sxfull", es=es); xfr = Res()
    bfull = kb.sb([64, S + 3], BF16, "sbfull", es=es); bfr = Res()
    cfull = kb.sb([64, S + 3], BF16, "scfull", es=es); cfr = Res()
    for (tl, rs, nm, P) in ((xfull, xfr, "sx", 128), (bfull, bfr, "sB", 64), (cfull, cfr, "sC", 64)):
        kb.op("pool", lambda e, tl=tl, P=P: e.memset(tl[:P, 0:3], 0.0), writes=[rs])
        for rq in range(4):
            cc_ = IX[nm + str(rq)]
            kb.gather(tl[:P, 3 + rq * 2048:3 + (rq + 1) * 2048], agF2k, gx[0:P, cc_:cc_ + 1], reads=[gxr, agFr], writes=[rs])
    yTb = Rot(kb, 2, [128, N], BF16, "syTb", es=es)
    xcv = Rot(kb, 2, [128, N], F32, "sxcv", es=es)
    bcv = Rot(kb, 2, [64, N], F32, "sbcv", es=es)
    ccv = Rot(kb, 2, [64, N], F32, "sccv", es=es)
    xs = Rot(kb, 2, [128, N], F32, "sxs", es=es)
    bs = Rot(kb, 2, [64, N], BF16, "sbs", es=es)
    cs = Rot(kb, 2, [64, N], BF16, "scs", es=es)
    xtm = Rot(kb, 2, [128, 128], F32, "sxtm", es=es)
    Xb = Rot(kb, 2, [128, 128], BF16, "sXb", es=es)
    Xd = Rot(kb, 2, [128, 128], BF16, "sXd", es=es)
    Btm = Rot(kb, 2, [128, 64], BF16, "sBtm", es=es)
    CBm = Rot(kb, 2, [128, 128], F32, "sCBm", es=es)
    lh = Rot(kb, 2, [128, 128], F32, "slh", es=es)
    EE = Rot(kb, 2, [128, 128], F32, "sE", es=es)
    MT = Rot(kb, 2, [128, 128], BF16, "sMT", es=es)
    yt = Rot(kb, 2, [128, 8, 128], F32, "syt", es=es)
    pbi = 0
    for j in range(S // N if LEVEL > 0 else 0):
        t0 = j * N
        xt, xr = xfull[:, t0:t0 + N + 3], xfr
        bt, br = bfull[:, t0:t0 + N + 3], bfr
        ctt, ctr = cfull[:, t0:t0 + N + 3], cfr
        xc, xcr = xcv.next(); bc, bcr = bcv.next(); cc, ccr = ccv.next()
        emit_conv(kb, "dve", xc, xt, vt, vr, 0, N, 128, xr, xcr)
        emit_conv(kb, "dve", bc, bt, vt, vr, 5, N, 64, br, bcr)
        emit_conv(kb, "dve", cc, ctt, vt, vr, 10, N, 64, ctr, ccr)
        xst, xsr = xs.next(); bst, bsr = bs.next(); cst_, csr = cs.next()
        kb.op("act", lambda e: e.activation(out=xst[:], in_=xc[:], func=AF.Silu), reads=[xcr], writes=[xsr])
        kb.op("act", lambda e: e.activation(out=bst[:], in_=bc[:], func=AF.Silu), reads=[bcr], writes=[bsr])
        kb.op("act", lambda e: e.activation(out=cst_[:], in_=cc[:], func=AF.Silu), reads=[ccr], writes=[csr])
        ytile, ytr = yt.next()
        for c in range(N // 128 if LEVEL > 1 else 0):
            gc = j * (N // 128) + c
            ts = slice(c * 128, (c + 1) * 128)
            pT, pTr = pbanks.next()
            kb.op("pe", lambda e: e.transpose(pT[:, 0:128], xst[:, ts], ident), reads=[xsr, cr], writes=[pTr])
            xm, xmr = xtm.next()
            kb.op("act", lambda e: e.copy(out=xm[:], in_=pT[:, 0:128]), reads=[pTr], writes=[xmr])
            xb, xbr = Xb.next()
            for h in range(2):
                hs = slice(h * 64, (h + 1) * 64)
                kb.op("dve", lambda e, hs=hs, h=h: e.tensor_scalar(out=xb[:, hs], in0=pT[:, hs], scalar1=dt[:, gc, h:h + 1], scalar2=None, op0=ALU.mult),
                      reads=[pTr, dtr], writes=[xbr])
            xd, xdr = Xd.next()
            for h in range(2):
                hs = slice(h * 64, (h + 1) * 64)
                kb.op("pool", lambda e, hs=hs, h=h: e.tensor_scalar(out=xd[:, hs], in0=xb[:, hs], scalar1=dst[:, gc, h:h + 1], scalar2=None, op0=ALU.mult),
                      reads=[xbr, dsr], writes=[xdr])
            if LEVEL < 3: continue
            pb_t, pb_r = pbf[pbi % len(pbf)]; pbi += 1
            kb.op("pe", lambda e: e.transpose(pb_t[:, 0:64], bst[:, ts], identb[0:64, 0:64]), reads=[bsr, idbr], writes=[pb_r])
            btm, btmr = Btm.next()
            kb.op("act", lambda e: e.copy(out=btm[:], in_=pb_t[:, 0:64]), reads=[pb_r], writes=[btmr])
            if LEVEL < 4: continue
            pcb, pcbr = pbanks.next()
            kb.op("pe", lambda e: e.matmul(pcb[:, 0:128], lhsT=bst[:, ts], rhs=cst_[:, ts], start=True, stop=True), reads=[bsr, csr], writes=[pcbr])
            cbm, cbmr = CBm.next()
            kb.op("dve", lambda e: e.tensor_tensor(out=cbm[:], in0=pcb[:, 0:128], in1=tri, op=ALU.mult), reads=[pcbr, cr], writes=[cbmr])
            for h in range(2 if LEVEL > 4 else 0):
                hs = slice(h * 64, (h + 1) * 64)
                l_, lr_ = lh.next()
                kb.op("pool", lambda e, h=h: e.tensor_scalar(out=l_[:], in0=U, scalar1=aa[:, gc, h:h + 1], scalar2=None, op0=ALU.mult),
                      reads=[cr, aar], writes=[lr_])
                pseg, psegr = pbanks.next()
                kb.op("pe", lambda e: e.matmul(pseg[:, 0:128], lhsT=l_[:], rhs=tri, start=True, stop=True), reads=[lr_, cr], writes=[psegr])
                E, Er = EE.next()
                kb.op("act", lambda e: e.activation(out=E[:], in_=pseg[:, 0:128], func=AF.Exp), reads=[psegr], writes=[Er])
                mt, mtr = MT.next()
                kb.op("dve", lambda e: e.tensor_tensor(out=mt[:], in0=E[:], in1=cbm[:], op=ALU.mult), reads=[Er, cbmr], writes=[mtr])
                py, pyr = pbanks.next()
                kb.op("pe", lambda e, hs=hs: e.matmul(py[:, 0:64], lhsT=mt[:], rhs=xb[:, hs], start=True, stop=True), reads=[mtr, xbr], writes=[pyr])
                po, por = pbanks.next()
                kb.op("pe", lambda e, h=h: e.matmul(po[:, 0:64], lhsT=cst_[:, ts], rhs=prevb[h][:], start=True, stop=True), reads=[csr, prevbr[h]], writes=[por])
                kb.op("act", lambda e, hs=hs: e.copy(out=ytile[:, c, hs], in_=py[:, 0:64]), reads=[pyr], writes=[ytr])
                kb.op("dve", lambda e, hs=hs, h=h: e.scalar_tensor_tensor(out=ytile[:, c, hs], in0=po[:, 0:64], scalar=dout[:, gc, h:h + 1], in1=ytile[:, c, hs],
                                                                          op0=ALU.mult, op1=ALU.add), reads=[por, dor, ytr], writes=[ytr])
                kb.op("dve", lambda e, hs=hs, h=h: e.scalar_tensor_tensor(out=ytile[:, c, hs], in0=xm[:, hs], scalar=rp[:, 4 + h:5 + h], in1=ytile[:, c, hs],
                                                                          op0=ALU.mult, op1=ALU.add), reads=[xmr, rpr, ytr], writes=[ytr])
                pst, pstr = pbanks.next()
                kb.op("pe", lambda e, hs=hs: e.matmul(pst[0:64, 0:64], lhsT=btm[:], rhs=xd[:, hs], start=True, stop=True), reads=[btmr, xdr], writes=[pstr])
                kb.op("dve", lambda e, h=h: e.scalar_tensor_tensor(out=prev[h][:], in0=prev[h][:], scalar=dtot[0:64, gc, h:h + 1], in1=pst[0:64, 0:64],
                                                                  op0=ALU.mult, op1=ALU.add), reads=[pstr, dtor, prevr[h]], writes=[prevr[h]])
                kb.op("act", lambda e, h=h: e.copy(out=prevb[h][:], in_=prev[h][:]), reads=[prevr[h]], writes=[prevbr[h]])
        yb_, ybr = yTb.next()
        for q2 in range(N // 512):
            pyt, pytr = pbanks.next()
            for c4 in range(4):
                c = q2 * 4 + c4
                kb.op("pe", lambda e, c=c, c4=c4: e.transpose(pyt[:, c4 * 128:(c4 + 1) * 128], ytile[:, c, :], ident), reads=[ytr, cr], writes=[pytr])
            kb.op("act", lambda e, q2=q2: e.copy(out=yb_[:, q2 * 512:(q2 + 1) * 512], in_=pyt[:, :]), reads=[pytr], writes=[ybr])
        kb.dma("sp", yT_dst[:, t0:t0 + N], yb_[:], reads=[ybr, yT_r])


def emit_lru2(kb, agF2k, agFr, gx, gxr, IX, vec, wab, wxb, outT, outr, pbanks, es=None):
    N = 1024
    vt = kb.sb([128, 8], F32, "lvec", es=es); vr = Res()
    wa = kb.sb([128, 128], F32, "lwa", es=es); war = Res()
    wx = kb.sb([128, 128], F32, "lwx", es=es); wxr = Res()
    kb.dma("sp", vt[:], vec[:, :], writes=[vr])
    kb.dma("sp", wa[:], wab[:, :], writes=[war])
    kb.dma("sp", wx[:], wxb[:, :], writes=[wxr])
    c1 = kb.sb([128, 1], F32, "lc1", es=es); c1r = Res()
    kb.op("act", lambda e: e.activation(out=c1[:], in_=vt[:, 7:8], func=AF.Exp, scale=-1.0), reads=[vr], writes=[c1r])
    kb.op("act", lambda e: e.activation(out=c1[:], in_=c1[:], func=AF.Ln, bias=1.0), reads=[c1r], writes=[c1r])
    kb.op("dve", lambda e: e.tensor_scalar(out=c1[:], in0=c1[:], scalar1=-8.0, scalar2=None, op0=ALU.mult), reads=[c1r], writes=[c1r])

    xfull = kb.sb([128, S + 3], BF16, "lxfull", es=es); xfr = Res()
    yfull = kb.sb([128, S], BF16, "lyfull", es=es); yfr = Res()
    kb.op("pool", lambda e: e.memset(xfull[:, 0:3], 0.0), writes=[xfr])
    for rp in range(4):
        c1_ = IX["lx%d" % rp]; c2_ = IX["ly%d" % rp]
        kb.gather(xfull[:, 3 + rp * 2048:3 + (rp + 1) * 2048], agF2k, gx[:, c1_:c1_ + 1], reads=[gxr, agFr], writes=[xfr])
        kb.gather(yfull[:, rp * 2048:(rp + 1) * 2048], agF2k, gx[:, c2_:c2_ + 1], reads=[gxr, agFr], writes=[yfr])
    obf = Rot(kb, 2, [128, N], BF16, "lob", es=es)
    xc = Rot(kb, 2, [128, N], F32, "lxc", es=es)
    rr = Rot(kb, 2, [128, N], F32, "lr", es=es)
    ii = Rot(kb, 2, [128, N], F32, "li", es=es)
    aa = Rot(kb, 2, [128, N], F32, "la", es=es)
    qq = Rot(kb, 2, [128, N], F32, "lq", es=es)
    hh = Rot(kb, 2, [128, N], F32, "lh", es=es)
    uu = Rot(kb, 2, [128, N], F32, "lu", es=es)
    hprev = None
    for j in range(S // N):
        t0 = j * N
        xt, xr = xfull[:, t0:t0 + N + 3], xfr
        yt, yr = yfull[:, t0:t0 + N], yfr
        ct, cr = xc.next()
        kb.op("dve", lambda e: e.tensor_scalar(out=ct[:], in0=xt[:, 0:N], scalar1=vt[:, 0:1], scalar2=vt[:, 4:5],
                                               op0=ALU.mult, op1=ALU.add), reads=[xr, vr], writes=[cr])
        for k in range(1, 4):
            kb.op("dve", lambda e, k=k: e.scalar_tensor_tensor(out=ct[:], in0=xt[:, k:k + N], scalar=vt[:, k:k + 1], in1=ct[:],
                                                              op0=ALU.mult, op1=ALU.add), reads=[xr, vr, cr], writes=[cr])
        rt, rres = rr.next()
        it, ires = ii.next()
        for hf in range(N // 512):
            sl = slice(hf * 512, (hf + 1) * 512)
            pa, par = pbanks.next()
            kb.op("pe", lambda e: e.matmul(pa[:, :], lhsT=wa[:], rhs=ct[:, sl], start=True, stop=True), reads=[war, cr], writes=[par])
            kb.op("act", lambda e: e.activation(out=rt[:, sl], in_=pa[:, :], func=AF.Sigmoid, bias=vt[:, 5:6]), reads=[par, vr], writes=[rres])
            px, pxr = pbanks.next()
            kb.op("pe", lambda e: e.matmul(px[:, :], lhsT=wx[:], rhs=ct[:, sl], start=True, stop=True), reads=[wxr, cr], writes=[pxr])
            kb.op("act", lambda e: e.activation(out=it[:, sl], in_=px[:, :], func=AF.Sigmoid, bias=vt[:, 6:7]), reads=[pxr, vr], writes=[ires])
        at, ar = aa.next()
        kb.op("act", lambda e: e.activation(out=at[:], in_=rt[:], func=AF.Exp, scale=c1[:, 0:1]), reads=[rres, c1r], writes=[ar])
        qt, qr = qq.next()
        kb.op("pool", lambda e: e.tensor_tensor(out=qt[:], in0=at[:], in1=at[:], op=ALU.mult), reads=[ar], writes=[qr])
        kb.op("pool", lambda e: e.tensor_scalar(out=qt[:], in0=qt[:], scalar1=-1.0, scalar2=1.0, op0=ALU.mult, op1=ALU.add), reads=[qr], writes=[qr])
        kb.op("act", lambda e: e.activation(out=qt[:], in_=qt[:], func=AF.Sqrt), reads=[qr], writes=[qr])
        kb.op("dve", lambda e: e.tensor_tensor(out=it[:], in0=it[:], in1=ct[:], op=ALU.mult), reads=[ires, cr], writes=[ires])
        kb.op("dve", lambda e: e.tensor_tensor(out=it[:], in0=it[:], in1=qt[:], op=ALU.mult), reads=[ires, qr], writes=[ires])
        ht, hr = hh.next()
        if hprev is None:
            kb.op("dve", lambda e: e.tensor_tensor_scan(out=ht[:], data0=at[:], data1=it[:], initial=0.0, op0=ALU.mult, op1=ALU.add),
                  reads=[ar, ires], writes=[hr])
        else:
            hp, hpr = hprev
            kb.op("dve", lambda e: e.tensor_tensor_scan(out=ht[:], data0=at[:], data1=it[:], initial=hp[:, N - 1:N], op0=ALU.mult, op1=ALU.add),
                  reads=[ar, ires, hpr], writes=[hr])
        hprev = (ht, hr)
        ut, ur = uu.next()
        kb.op("pool", lambda e: e.tensor_tensor(out=ut[:], in0=yt[:], in1=yt[:], op=ALU.mult), reads=[yr], writes=[ur])
        kb.op("pool", lambda e: e.tensor_scalar(out=ut[:], in0=ut[:], scalar1=0.044715, scalar2=1.0, op0=ALU.mult, op1=ALU.add), reads=[ur], writes=[ur])
        kb.op("pool", lambda e: e.tensor_tensor(out=ut[:], in0=ut[:], in1=yt[:], op=ALU.mult), reads=[ur, yr], writes=[ur])
        kb.op("act", lambda e: e.activation(out=ut[:], in_=ut[:], func=AF.Sigmoid, scale=1.5957691216057308), reads=[ur], writes=[ur])
        kb.op("pool", lambda e: e.tensor_tensor(out=ut[:], in0=ut[:], in1=yt[:], op=ALU.mult), reads=[ur, yr], writes=[ur])
        ob_, obr_ = obf.next()
        kb.op("pool", lambda e: e.tensor_tensor(out=ob_[:], in0=ut[:], in1=ht[:], op=ALU.mult), reads=[ur, hr], writes=[obr_])
        kb.dma("sp", outT[:, t0:t0 + N], ob_[:], reads=[obr_, outr])


def emit_C2(kb, xT, agN512, agNr, agY2k, agYr, agL2k, agLr, gx, gxr, IX, pT, w_in, pnsa, pssd, plru, w_out, pwg, pwp, rw, ewg, ewu, ewd, cst, x2T, x2r, final_out, banks, es, xTr=None):
    gen = Rot.__new__(Rot); gen.t = banks; gen.i = 0
    sb = lambda shape, dt, name: kb.sb(shape, dt, name, es=es)
    cv = sb([128, CW], F32, "cc"); cvr = Res()
    kb.dma("sp", cv[:], cst[:, :], writes=[cvr])
    ones1k = cv[:, 52:180]; ones512 = cv[:, 180:308]; ident = cv[:, 308:436]
    selE = lambda e: cv[0:16, 436 + e * 128:436 + (e + 1) * 128]
    rb = cv[:, 36:52]

    tmpf = Rot(kb, 4, [128, 512], F32, "ctf", es=es)
    stat = Rot(kb, 3, [128, 512], F32, "cstat", es=es)
    kb.uid += 1
    mixD = kb.dram("mixD%d" % kb.uid, [D, TOK], BF16, "Internal")
    mixDr = [Res() for _ in range(8)]
    wst_box = [None]

    def load_w(src_rows, nk, ncols):
        wt, wr = wst_box[0].next()
        kb.dma("pool", wt[:, 0:nk, 0:ncols], src_rows.rearrange("(c p) f -> p c f", p=128), writes=[wr])
        return wt, wr

    def ln_stats(src, srcr, nchunk, tt, ones_ap, eps, sq_eng="pool"):
        ts = slice(tt * 512, (tt + 1) * 512)
        pm_, pmr = gen.next()
        for c in range(nchunk):
            kb.op("pe", lambda e, c=c: e.matmul(pm_[:, :], lhsT=ones_ap, rhs=src[:, c, ts], start=(c == 0), stop=(c == nchunk - 1)), reads=[cvr, srcr[c]], writes=[pmr])
        pq_, pqr = gen.next()
        for c in range(nchunk):
            sq, sqr = tmpf.next()
            kb.op(sq_eng, lambda e, c=c: e.tensor_tensor(out=sq[:], in0=src[:, c, ts], in1=src[:, c, ts], op=ALU.mult), reads=[srcr[c]], writes=[sqr])
            kb.op("pe", lambda e, c=c: e.matmul(pq_[:, :], lhsT=ones_ap, rhs=sq[:], start=(c == 0), stop=(c == nchunk - 1)), reads=[cvr, sqr], writes=[pqr])
        mean, meanr = stat.next()
        kb.op("act", lambda e: e.copy(out=mean[:], in_=pm_[:, :]), reads=[pmr], writes=[meanr])
        rstd, rstdr = stat.next()
        return mean, meanr, pq_, pqr, rstd, rstdr

    with contextlib.ExitStack() as es1:
        sb1 = lambda shape, dt, name: kb.sb(shape, dt, name, es=es1)
        wst_box[0] = Rot.__new__(Rot); wst_box[0].i = 0
        wst_box[0].t = [(sb1([128, 8, 512], BF16, "cw"), Res()) for _ in range(3)]
        xbf = sb1([128, 8, TOK], BF16, "cxbf"); xbr = [Res() for _ in range(8)]
        for c in range(8):
            kb.dma("pool", xbf[:, c, :], xT[c * 128:(c + 1) * 128, :], reads=([xTr[c]] if xTr else []), writes=[xbr[c]])
        ob = [sb1([128, 4, TOK], BF16, "cob%d" % i) for i in range(3)]
        obr = [[Res() for _ in range(4)] for _ in range(3)]
        for c in range(4):
            for q4 in range(4):
                cn = IX["on%d_%d" % (c, q4)]
                kb.gather(ob[0][:, c, q4 * 512:(q4 + 1) * 512], agN512, gx[:, cn:cn + 1], reads=[gxr, agNr], writes=[obr[0][c]])
            cl = IX["l%d" % c]
            kb.gather(ob[2][:, c, :], agL2k, gx[:, cl:cl + 1], reads=[gxr, agLr], writes=[obr[2][c]])
        with contextlib.ExitStack() as es1b:
            yg = kb.sb([128, 4, TOK], F32, "cyg", es=es1b); ygr = [Res() for _ in range(4)]
            ygb = ob[1]; ygbr = obr[1]
            for c in range(4):
                cy = IX["y%d" % c]
                kb.gather(ygb[:, c, :], agY2k, gx[:, cy:cy + 1], reads=[gxr, agYr], writes=[ygbr[c]])
            wz, wzr = load_w(w_in[:, OFF_Z:OFF_Z + 512], 8, 512)
            for tt in range(NT):
                ts = slice(tt * 512, (tt + 1) * 512)
                for fc in range(4):
                    pz, pzr = gen.next()
                    for c in range(8):
                        kb.op("pe", lambda e, c=c: e.matmul(pz[:, :], lhsT=wz[:, c, fc * 128:(fc + 1) * 128], rhs=xbf[:, c, ts], start=(c == 0), stop=(c == 7)),
                              reads=[wzr, xbr[c]], writes=[pzr])
                    sz, szr = tmpf.next()
                    kb.op("act", lambda e: e.activation(out=sz[:], in_=pz[:, :], func=AF.Silu), reads=[pzr], writes=[szr])
                    kb.op("dve", lambda e, fc=fc: e.tensor_tensor(out=yg[:, fc, ts], in0=ygb[:, fc, ts], in1=sz[:], op=ALU.mult), reads=[szr, ygbr[fc]], writes=[ygr[fc]])
                pq_, pqr = gen.next()
                for c in range(4):
                    sq, sqr = tmpf.next()
                    kb.op("pool", lambda e, c=c: e.tensor_tensor(out=sq[:], in0=yg[:, c, ts], in1=yg[:, c, ts], op=ALU.mult), reads=[ygr[c]], writes=[sqr])
                    kb.op("pe", lambda e, c=c: e.matmul(pq_[:, :], lhsT=ones512, rhs=sq[:], start=(c == 0), stop=(c == 3)), reads=[cvr, sqr], writes=[pqr])
                rstd, rstdr = stat.next()
                kb.op("dve", lambda e: e.tensor_scalar(out=rstd[:], in0=pq_[:, :], scalar1=1e-5, scalar2=None, op0=ALU.add), reads=[pqr], writes=[rstdr])
                kb.op("act", lambda e: e.activation(out=rstd[:], in_=rstd[:], func=AF.Sqrt), reads=[rstdr], writes=[rstdr])
                kb.op("dve", lambda e: e.reciprocal(out=rstd[:], in_=rstd[:]), reads=[rstdr], writes=[rstdr])
                for c in range(4):
                    t1, t1r = tmpf.next()
                    kb.op("dve", lambda e, c=c: e.tensor_tensor(out=t1[:], in0=yg[:, c, ts], in1=rstd[:], op=ALU.mult), reads=[ygr[c], rstdr], writes=[t1r])
                    kb.op("act", lambda e, c=c: e.activation(out=ob[1][:, c, ts], in_=t1[:], func=AF.Copy, scale=cv[:, 32 + c:33 + c]), reads=[t1r, cvr], writes=[obr[1][c]])
            kb.barrier()
        if CDBG == "ossd":
            dbg = sb1([128, 4, TOK], F32, "cdbg"); dbgr = Res()
            for c in range(4):
                kb.op("dve", lambda e, c=c: e.tensor_copy(out=dbg[:, c, :], in_=ob[1][:, c, :]), reads=[obr[1][c]], writes=[dbgr])
                kb.dma("sp", x2T[c * 128:(c + 1) * 128, :], dbg[:, c, :], reads=[dbgr], final=True)
            kb.barrier()
            return
        projs = [pnsa, pssd, plru]
        mixh = sb1([128, 4, TOK], BF16, "cmixh"); mixhr = [Res() for _ in range(4)]
        for half in range(2):
            fs = slice(half * 512, (half + 1) * 512)
            for br in range(3):
                wp, wpr = load_w(projs[br][:, fs], 4, 512)
                wm, wmr = load_w(w_in[:, OFF_MERGE + br * 1024 + half * 512: OFF_MERGE + br * 1024 + (half + 1) * 512], 8, 512)
                for fc in range(4):
                    oc = half * 4 + fc
                    for tt in range(NT):
                        ts = slice(tt * 512, (tt + 1) * 512)
                        pg, pgr = gen.next()
                        for c in range(8):
                            kb.op("pe", lambda e, c=c: e.matmul(pg[:, :], lhsT=wm[:, c, fc * 128:(fc + 1) * 128], rhs=xbf[:, c, ts], start=(c == 0), stop=(c == 7)),
                                  reads=[wmr, xbr[c]], writes=[pgr])
                        pp, ppr = gen.next()
                        for c in range(4):
                            kb.op("pe", lambda e, c=c: e.matmul(pp[:, :], lhsT=wp[:, c, fc * 128:(fc + 1) * 128], rhs=ob[br][:, c, ts], start=(c == 0), stop=(c == 3)),
                                  reads=[wpr, obr[br][c]], writes=[ppr])
                        sg, sgr = tmpf.next()
                        kb.op("act", lambda e: e.activation(out=sg[:], in_=pg[:, :], func=AF.Sigmoid), reads=[pgr], writes=[sgr])
                        if br == 0:
                            kb.op("dve", lambda e: e.tensor_tensor(out=mixh[:, fc, ts], in0=pp[:, :], in1=sg[:], op=ALU.mult), reads=[ppr, sgr], writes=[mixhr[fc]])
                        else:
                            kb.op("dve", lambda e: e.tensor_tensor(out=sg[:], in0=pp[:, :], in1=sg[:], op=ALU.mult), reads=[ppr, sgr], writes=[sgr])
                            kb.op("pool", lambda e: e.tensor_tensor(out=mixh[:, fc, ts], in0=mixh[:, fc, ts], in1=sg[:], op=ALU.add), reads=[sgr, mixhr[fc]], writes=[mixhr[fc]])
            for fc in range(4):
                kb.dma("sp", mixD[(half * 4 + fc) * 128:(half * 4 + fc + 1) * 128, :], mixh[:, fc, :], reads=[mixhr[fc]], writes=[mixDr[half * 4 + fc]])
        kb.barrier()

    with contextlib.ExitStack() as es2:
        sb2 = lambda shape, dt, name: kb.sb(shape, dt, name, es=es2)
        r = sb2([128, 8, TOK], F32, "cr"); rr = [Res() for _ in range(8)]
        x1b = sb2([128, 8, TOK], BF16, "cx1b"); x1br = [Res() for _ in range(8)]
        for c in range(8):
            kb.dma("sp", r[:, c, :], xT[c * 128:(c + 1) * 128, :], reads=([xTr[c]] if xTr else []), writes=[rr[c]])
        gatesT = sb2([16, TOK], F32, "cgT"); gTr = Res()
        rwt = sb2([128, 8, 16], F32, "crw"); rwr = Res()
        kb.dma("sp", rwt[:], rw.rearrange("(c p) e -> p c e", p=128), writes=[rwr])
        pad = sb2([128, 4, 8], F32, "cpad"); padr = Res()
        kb.op("pool", lambda e: e.memset(pad[:], -1e30), writes=[padr])
        rt = Rot(kb, 2, [128, 16], F32, "crt", es=es2)
        rt2 = Rot(kb, 2, [128, 16], F32, "crt2", es=es2)
        t8 = Rot(kb, 2, [128, 4, 8], F32, "ct8", es=es2)
        sm4 = Rot(kb, 4, [128, 4], F32, "csm4", es=es2)
        sm1 = Rot(kb, 4, [128, 1], F32, "csm1", es=es2)
        t8b = Rot(kb, 2, [128, 8], F32, "ct8b", es=es2)
        es2a = contextlib.ExitStack()
        wst_box[0] = Rot.__new__(Rot); wst_box[0].i = 0
        wst_box[0].t = [(kb.sb([128, 8, 512], BF16, "cw2", es=es2a), Res()) for _ in range(2)]
        mixed = kb.sb([128, 8, TOK], BF16, "cmixed", es=es2a); mixr = [Res() for _ in range(8)]
        pb_ = kb.sb([128, 2, TOK], BF16, "cpb", es=es2a); pbr = [Res() for _ in range(2)]
        for c in range(8):
            kb.dma("sp", mixed[:, c, :], mixD[c * 128:(c + 1) * 128, :], reads=[mixDr[c]], writes=[mixr[c]])
        if CDBG == "mixed":
            for c in range(8):
                kb.op("dve", lambda e, c=c: e.tensor_copy(out=r[:, c, :], in_=mixed[:, c, :]), reads=[mixr[c], rr[c]], writes=[rr[c]])
                kb.dma("sp", x2T[c * 128:(c + 1) * 128, :], r[:, c, :], reads=[rr[c]], final=True)
            kb.barrier(); es2a.close()
            return
        for half in range(2):
            wo, wor = load_w(w_out[:, half * 512:(half + 1) * 512], 8, 512)
            for fc in range(4):
                oc = half * 4 + fc
                for tt in range(NT):
                    ts = slice(tt * 512, (tt + 1) * 512)
                    pu, pur = gen.next()
                    for c in range(8):
                        kb.op("pe", lambda e, c=c: e.matmul(pu[:, :], lhsT=wo[:, c, fc * 128:(fc + 1) * 128], rhs=mixed[:, c, ts], start=(c == 0), stop=(c == 7)),
                              reads=[wor, mixr[c]], writes=[pur])
                    kb.op("dve", lambda e: e.scalar_tensor_tensor(out=r[:, oc, ts], in0=r[:, oc, ts], scalar=ALPHA, in1=pu[:, :], op0=ALU.mult, op1=ALU.add),
                          reads=[pur, rr[oc]], writes=[rr[oc]])

        def layer_norm(src, srcr, gcol, bcol, dst_b, dst_br):
            for tt in range(NT):
                ts = slice(tt * 512, (tt + 1) * 512)
                mean, meanr, pq_, pqr, rstd, rstdr = ln_stats(src, srcr, 8, tt, ones1k, 1e-5)
                m2, m2r = stat.next()
                kb.op("pool", lambda e: e.tensor_tensor(out=m2[:], in0=mean[:], in1=mean[:], op=ALU.mult), reads=[meanr], writes=[m2r])
                kb.op("dve", lambda e: e.scalar_tensor_tensor(out=rstd[:], in0=pq_[:, :], scalar=1e-5, in1=m2[:], op0=ALU.add, op1=ALU.subtract), reads=[pqr, m2r], writes=[rstdr])
                kb.op("act", lambda e: e.activation(out=rstd[:], in_=rstd[:], func=AF.Sqrt), reads=[rstdr], writes=[rstdr])
                kb.op("dve", lambda e: e.reciprocal(out=rstd[:], in_=rstd[:]), reads=[rstdr], writes=[rstdr])
                for c in range(8):
                    kb.op("dve", lambda e, c=c: e.tensor_tensor(out=src[:, c, ts], in0=src[:, c, ts], in1=mean[:], op=ALU.subtract), reads=[meanr, srcr[c]], writes=[srcr[c]])
                    kb.op("pool", lambda e, c=c: e.tensor_tensor(out=src[:, c, ts], in0=src[:, c, ts], in1=rstd[:], op=ALU.mult), reads=[rstdr, srcr[c]], writes=[srcr[c]])
                    kb.op("dve", lambda e, c=c: e.tensor_scalar(out=src[:, c, ts], in0=src[:, c, ts], scalar1=cv[:, gcol + c:gcol + c + 1], scalar2=cv[:, bcol + c:bcol + c + 1],
                                                               op0=ALU.mult, op1=ALU.add), reads=[cvr, srcr[c]], writes=[srcr[c]])
                    if dst_b is not None:
                        kb.op("act", lambda e, c=c: e.copy(out=dst_b[:, c, ts], in_=src[:, c, ts]), reads=[srcr[c]], writes=[dst_br[c]])

        layer_norm(r, rr, 0, 8, x1b, x1br)
        if CDBG == "x1":
            for c in range(8):
                kb.dma("sp", x2T[c * 128:(c + 1) * 128, :], r[:, c, :], reads=[rr[c]], final=True)
            kb.barrier(); es2a.close()
            return

        for s_ in range(TOK // 128):
            ss = slice(s_ * 128, (s_ + 1) * 128)
            pl, plr = gen.next()
            for c in range(8):
                kb.op("pe", lambda e, c=c: e.matmul(pl[:, 0:16], lhsT=r[:, c, ss], rhs=rwt[:, c, :], start=(c == 0), stop=(c == 7)), reads=[rr[c], rwr], writes=[plr])
            aff, affr = rt.next()
            kb.op("act", lambda e: e.activation(out=aff[:], in_=pl[:, 0:16], func=AF.Sigmoid), reads=[plr], writes=[affr])
            sel, selr = rt2.next()
            kb.op("dve", lambda e: e.tensor_tensor(out=sel[:], in0=aff[:], in1=rb, op=ALU.add), reads=[affr, cvr], writes=[selr])
            kb.op("dve", lambda e: e.tensor_copy(out=pad[:, :, 0:4], in_=sel[:].rearrange("p (g k) -> p g k", g=4)), reads=[selr, padr], writes=[padr])
            tp, tpr = t8.next()
            for g in range(4):
                kb.op("dve", lambda e, g=g: e.max(out=tp[:, g, :], in_=pad[:, g, :]), reads=[padr], writes=[tpr])
            gs, gsr = sm4.next()
            kb.op("dve", lambda e: e.tensor_tensor(out=gs[:], in0=tp[:, :, 0], in1=tp[:, :, 1], op=ALU.add), reads=[tpr], writes=[gsr])
            gm, gmr = sm1.next()
            kb.op("dve", lambda e: e.reduce_max(out=gm[:], in_=gs[:], axis=AX.X), reads=[gsr], writes=[gmr])
            isb, isbr = sm4.next()
            kb.op("dve", lambda e: e.tensor_scalar(out=isb[:], in0=gs[:], scalar1=gm[:, 0:1], scalar2=None, op0=ALU.is_ge), reads=[gsr, gmr], writes=[isbr])
            off, offr = sm4.next()
            kb.op("dve", lambda e: e.tensor_scalar(out=off[:], in0=isb[:], scalar1=1e9, scalar2=-1e9, op0=ALU.mult, op1=ALU.add), reads=[isbr], writes=[offr])
            msk, mskr = rt2.next()
            for g in range(4):
                kb.op("dve", lambda e, g=g: e.tensor_scalar(out=msk[:, g * 4:(g + 1) * 4], in0=sel[:, g * 4:(g + 1) * 4], scalar1=isb[:, g:g + 1], scalar2=off[:, g:g + 1],
                                                            op0=ALU.mult, op1=ALU.add), reads=[selr, isbr, offr], writes=[mskr])
            tb, tbr = t8b.next()
            kb.op("dve", lambda e: e.max(out=tb[:], in_=msk[:]), reads=[mskr], writes=[tbr])
            kb.op("dve", lambda e: e.tensor_scalar(out=msk[:], in0=msk[:], scalar1=tb[:, 1:2], scalar2=None, op0=ALU.is_ge), reads=[mskr, tbr], writes=[mskr])
            kb.op("dve", lambda e: e.tensor_tensor(out=msk[:], in0=msk[:], in1=aff[:], op=ALU.mult), reads=[mskr, affr], writes=[mskr])
            ws, wsr = sm1.next()
            kb.op("dve", lambda e: e.reduce_sum(out=ws[:], in_=msk[:], axis=AX.X), reads=[mskr], writes=[wsr])
            kb.op("dve", lambda e: e.reciprocal(out=ws[:], in_=ws[:]), reads=[wsr], writes=[wsr])
            kb.op("dve", lambda e: e.tensor_scalar(out=msk[:], in0=msk[:], scalar1=ws[:, 0:1], scalar2=None, op0=ALU.mult), reads=[mskr, wsr], writes=[mskr])
            pt, ptr_ = gen.next()
            kb.op("pe", lambda e: e.transpose(pt[0:16, 0:128], msk[:], ident), reads=[mskr, cvr], writes=[ptr_])
            kb.op("act", lambda e: e.copy(out=gatesT[:, ss], in_=pt[0:16, 0:128]), reads=[ptr_], writes=[gTr])

        for c in range(2):
            kb.dma("pool", pb_[:, c, :], pT[c * 128:(c + 1) * 128, :], writes=[pbr[c]])
        for half in range(2):
            fs = slice(half * 512, (half + 1) * 512)
            wg_, wgr = load_w(pwg[:, fs], 8, 512)
            wp_, wpr = load_w(pwp[:, fs], 2, 512)
            for fc in range(4):
                oc = half * 4 + fc
                for tt in range(NT):
                    ts = slice(tt * 512, (tt + 1) * 512)
                    pg, pgr = gen.next()
                    for c in range(8):
                        kb.op("pe", lambda e, c=c: e.matmul(pg[:, :], lhsT=wg_[:, c, fc * 128:(fc + 1) * 128], rhs=x1b[:, c, ts], start=(c == 0), stop=(c == 7)),
                              reads=[wgr, x1br[c]], writes=[pgr])
                    pp, ppr = gen.next()
                    for c in range(2):
                        kb.op("pe", lambda e, c=c: e.matmul(pp[:, :], lhsT=wp_[:, c, fc * 128:(fc + 1) * 128], rhs=pb_[:, c, ts], start=(c == 0), stop=(c == 1)),
                              reads=[wpr, pbr[c]], writes=[ppr])
                    sg, sgr = tmpf.next()
                    kb.op("act", lambda e: e.activation(out=sg[:], in_=pg[:, :], func=AF.Sigmoid), reads=[pgr], writes=[sgr])
                    kb.op("dve", lambda e: e.tensor_tensor(out=sg[:], in0=pp[:, :], in1=sg[:], op=ALU.mult), reads=[ppr, sgr], writes=[sgr])
                    kb.op("dve", lambda e: e.scalar_tensor_tensor(out=r[:, oc, ts], in0=r[:, oc, ts], scalar=ALPHA, in1=sg[:], op0=ALU.mult, op1=ALU.add),
                          reads=[sgr, rr[oc]], writes=[rr[oc]])

        kb.barrier()
        es2a.close()
        if CDBG == "ple":
            for c in range(8):
                kb.dma("sp", x2T[c * 128:(c + 1) * 128, :], r[:, c, :], reads=[rr[c]], final=True)
            return
        ew = Rot(kb, 2, [128, 2, 3, 8, 256], BF16, "cew", es=es2)
        hb = Rot(kb, 2, [128, 4, 512], BF16, "chb", es=es2)
        steps = [(ep, tt) for ep in range(8) for tt in range(NT)]
        live = {}
        for idx in range(len(steps) + 1):
            if idx < len(steps):
                ep, tt = steps[idx]
                if tt == 0:
                    wt, wr = ew.next()
                    for m in range(2):
                        e_ = ep * 2 + m
                        kb.dma("pool", wt[:, m, 0, :, :], ewg[e_, :, :].rearrange("(c p) f -> p c f", p=128), writes=[wr])
                        kb.dma("pool", wt[:, m, 1, :, :], ewu[e_, :, :].rearrange("(c p) f -> p c f", p=128), writes=[wr])
                        kb.dma("pool", wt[:, m, 2, :, :].rearrange("p (c a) f -> p c (a f)", c=2), ewd[e_, :, :].rearrange("(c p) f -> p c f", p=128), writes=[wr])
                ts = slice(tt * 512, (tt + 1) * 512)
                h, hr = hb.next()
                for m in range(2):
                    e_ = ep * 2 + m
                    pgb, pgbr = gen.next()
                    kb.op("pe", lambda e, e_=e_, pgb=pgb, ts=ts: e.matmul(pgb[:, :], lhsT=selE(e_), rhs=gatesT[:, ts], start=True, stop=True), reads=[cvr, gTr], writes=[pgbr])
                    gb, gbr = tmpf.next()
                    kb.op("act", lambda e, gb=gb, pgb=pgb: e.copy(out=gb[:], in_=pgb[:, :]), reads=[pgbr], writes=[gbr])
                    for fc in range(2):
                        pg, pgr = gen.next()
                        for c in range(8):
                            kb.op("pe", lambda e, c=c, m=m, fc=fc, pg=pg, wt=wt, ts=ts: e.matmul(pg[:, :], lhsT=wt[:, m, 0, c, fc * 128:(fc + 1) * 128], rhs=x1b[:, c, ts], start=(c == 0), stop=(c == 7)),
                                  reads=[wr, x1br[c]], writes=[pgr])
                        pu, pur = gen.next()
                        for c in range(8):
                            kb.op("pe", lambda e, c=c, m=m, fc=fc, pu=pu, wt=wt, ts=ts: e.matmul(pu[:, :], lhsT=wt[:, m, 1, c, fc * 128:(fc + 1) * 128], rhs=x1b[:, c, ts], start=(c == 0), stop=(c == 7)),
                                  reads=[wr, x1br[c]], writes=[pur])
                        sg, sgr = tmpf.next()
                        kb.op("act", lambda e, sg=sg, pg=pg: e.activation(out=sg[:], in_=pg[:, :], func=AF.Silu), reads=[pgr], writes=[sgr])
                        kb.op("dve", lambda e, sg=sg, pu=pu: e.tensor_tensor(out=sg[:], in0=pu[:, :], in1=sg[:], op=ALU.mult), reads=[pur, sgr], writes=[sgr])
                        kb.op("pool", lambda e, m=m, fc=fc, h=h, sg=sg, gb=gb: e.tensor_tensor(out=h[:, m * 2 + fc, :], in0=sg[:], in1=gb[:], op=ALU.mult), reads=[sgr, gbr], writes=[hr])
                live[idx] = (wt, wr, h, hr, ts)
            if idx >= 1:
                wt_, wr_, h_, hr_, ts_ = live.pop(idx - 1)
                for dc in range(8):
                    py, pyr = gen.next()
                    for m in range(2):
                        wd = wt_[:, m, 2, :, :].rearrange("p (c a) f -> p c (a f)", c=2)
                        for fc in range(2):
                            kb.op("pe", lambda e, wd=wd, m=m, fc=fc, py=py, h_=h_, dc=dc: e.matmul(py[:, :], lhsT=wd[:, fc, dc * 128:(dc + 1) * 128], rhs=h_[:, m * 2 + fc, :],
                                                                                                  start=(m == 0 and fc == 0), stop=(m == 1 and fc == 1)), reads=[wr_, hr_], writes=[pyr])
                    kb.op("dve", lambda e, dc=dc, py=py, ts_=ts_: e.tensor_tensor(out=r[:, dc, ts_], in0=r[:, dc, ts_], in1=py[:, :], op=ALU.add), reads=[pyr, rr[dc]], writes=[rr[dc]])

        if CDBG != "moe":
            layer_norm(r, rr, 16, 24, None, None)
        for c in range(8):
            kb.dma("sp", x2T[c * 128:(c + 1) * 128, :], r[:, c, :], reads=[rr[c]], writes=[x2r[c]], final=final_out)
        kb.barrier()


NF = 3104
NOM_TILES = [0, 2, 4, 6, 8, 10, 12, 14]
BATCH = 2
T_ALL = BATCH * S
NCORES = 8
U32 = mybir.dt.uint32


def idx_cols():
    names = []
    for nm in ("kcmp", "vcmp", "ksel", "vsel", "kwin", "vwin", "sx", "sB", "sC", "dt", "lx", "ly"):
        names += [nm + str(rp) for rp in range(4)]
    for k in range(8):
        names += ["q%d_%d" % (k, j) for j in range(4)]
        names.append("gate%d" % k)
    for c in range(4):
        names += ["on%d_%d" % (c, q4) for q4 in range(4)]
        names.append("y%d" % c)
        names.append("l%d" % c)
    return {n: i for i, n in enumerate(names)}


IX = idx_cols()
NIDX = len(IX)


def make_gidx(r):
    g, par = r // 2, r % 2
    t = np.zeros((128, NIDX), np.int64)
    p = np.arange(128)
    def rowF(rp, f):
        f = np.asarray(f)
        k = f // 256
        rows_k = np.where(k < 12, 256, 32)
        return k * 1024 + rp * rows_k + (f - 256 * k)
    rowN = lambda rank, f256: (f256 // 128) * 512 + rank * 128 + f256 % 128
    rowY = lambda rank, ch: (ch // 64) * 256 + rank * 64 + ch % 64
    for rp in range(4):
        for nm, f0 in (("kcmp", 512), ("vcmp", 640), ("ksel", 768), ("vsel", 896), ("kwin", 1024), ("vwin", 1152)):
            t[:, IX[nm + str(rp)]] = rowF(rp, f0 + 64 * g + p)
        t[:, IX["sx%d" % rp]] = rowF(rp, 1304 + 128 * r + p)
        t[:, IX["sB%d" % rp]] = rowF(rp, 1304 + 512 + 64 * g + p)
        t[:, IX["sC%d" % rp]] = rowF(rp, 1304 + 640 + 64 * g + p)
        t[:, IX["dt%d" % rp]] = rp * 8 + 2 * r + p
        t[:, IX["lx%d" % rp]] = rowF(rp, 2080 + 128 * r + p)
        t[:, IX["ly%d" % rp]] = rowF(rp, 2080 + 512 + 128 * r + p)
    for k in range(8):
        i = 2 * k + par
        rp, q4 = i // 4, i % 4
        for j in range(4):
            t[:, IX["q%d_%d" % (k, j)]] = rowF(rp, g * 256 + j * 64 + p) * 4 + q4
        t[:, IX["gate%d" % k]] = rowF(rp, 1280 + 12 * g + p) * 4 + q4
    for c in range(4):
        gg, f256 = c // 2, (c % 2) * 128 + p
        for q4 in range(4):
            i = 4 * r + q4
            t[:, IX["on%d_%d" % (c, q4)]] = rowN(2 * gg + i % 2, f256) * 8 + i // 2
        t[:, IX["y%d" % c]] = rowY(c, p) * 4 + r
        t[:, IX["l%d" % c]] = rowY(c, p) * 4 + r
    t = np.clip(t, 0, None)
    return t.astype(np.uint32)


def emit_A2(kb, xT, xTr, w, agF, agFr, agD, agDr, banks, es):
    xs = kb.sb([128, 8, TOK], BF16, "xs", es=es)
    xs_r = [Res("xs") for _ in range(8)]
    for c in range(8):
        kb.dma("pool", xs[:, c, :], xT[c * 128:(c + 1) * 128, :], reads=([xTr[c]] if xTr else []), writes=[xs_r[c]])
    FB = 512
    wbuf = [(kb.sb([128, 8, FB], BF16, "wb", es=es), Res("wb")) for _ in range(2)]
    obuf = [(kb.sb([128, 512], BF16, "ob", es=es), Res("ob")) for _ in range(4)]
    obf = [(kb.sb([8, 512], F32, "obd", es=es), Res("obd")) for _ in range(2)]
    wv = w.rearrange("(c p) f -> p c f", p=128)
    it = 0
    nb = 0
    for (c0, ncols, r0) in ((0, 1304, 0), (1816, 1800, 1304)):
        for fb in range((ncols + FB - 1) // FB):
            f0 = fb * FB
            fw_ = min(FB, ncols - f0)
            wt, wr = wbuf[nb % 2]
            nb += 1
            kb.dma("pool", wt[:, :, :fw_], wv[:, :, c0 + f0:c0 + f0 + fw_], writes=[wr])
            for fc in range((fw_ + 127) // 128):
                m = min(128, fw_ - fc * 128)
                for tt in range(TOK // 512):
                    pt, pr = banks[it % 4]
                    ot, orr = obuf[it % 4]
                    for c in range(8):
                        kb.op("pe", lambda e, c=c: e.matmul(pt[:m, :], lhsT=wt[:, c, fc * 128:fc * 128 + m], rhs=xs[:, c, tt * 512:(tt + 1) * 512],
                                                             start=(c == 0), stop=(c == 7)), reads=[wr, xs_r[c]], writes=[pr])
                    if it % 2 == 0:
                        kb.op("act", lambda e: e.copy(out=ot[:m, :], in_=pt[:m, :]), reads=[pr], writes=[orr])
                    else:
                        kb.op("dve", lambda e: e.tensor_copy(out=ot[:m, :], in_=pt[:m, :]), reads=[pr], writes=[orr])
                    row = r0 + f0 + fc * 128
                    kb.dma("sp", agF[row:row + m, tt * 512:(tt + 1) * 512], ot[:m, :], reads=[orr, agFr])
                    it += 1
    wt, wr = wbuf[nb % 2]
    kb.dma("pool", wt[:, :, 0:8], wv[:, :, 2584:2592], writes=[wr])
    for tt in range(TOK // 512):
        pt, pr = banks[it % 4]
        it += 1
        for c in range(8):
            kb.op("pe", lambda e, c=c: e.matmul(pt[0:8, :], lhsT=wt[:, c, 0:8], rhs=xs[:, c, tt * 512:(tt + 1) * 512], start=(c == 0), stop=(c == 7)),
                  reads=[wr, xs_r[c]], writes=[pr])
        od, odr = obf[tt % 2]
        kb.op("act", lambda e: e.copy(out=od[:, :], in_=pt[0:8, :]), reads=[pr], writes=[odr])
        kb.dma("sp", agD[:, tt * 512:(tt + 1) * 512], od[:, :], reads=[odr, agDr])


def build_fused(nlayers=2, groups=None, ncores=8, fstop=9):
    groups = groups or [[0, 1, 2, 3], [4, 5, 6, 7]]
    nc = bass.Bass("TRN2", target_bir_lowering=False)
    es = contextlib.ExitStack()
    with es:
        kb = KB(nc, es)
        I = lambda n, s, dt=F32: kb.dram(n, s, dt, "ExternalInput")
        xT0 = I("xT0", [D, TOK]); pT = I("pT", [2, 256, TOK]); gidx = I("gidx", [128, NIDX], U32)
        w_in = I("w_in", [2, D, 6688])
        peT = I("npeT", [2, 2, 64, 32]); w1 = I("nw1", [2, 2, 2048, 256]); w2 = I("nw2", [2, 2, 256, 64])
        nmask = I("nmask", [128, NMASK, 512]); nE0 = I("nE0", [128, S]); nmisc = I("nmisc", [128, MISC_W])
        svec = I("svec", [2, 128, 16]); srep = I("srep", [2, 128, 8]); scst = I("scst", [128, 4, 128])
        lvec = I("lvec", [2, 128, 8]); lwab = I("lwab", [2, 128, 128]); lwxb = I("lwxb", [2, 128, 128])
        pnsa = I("proj_nsa", [2, 512, D]); pssd = I("proj_ssd", [2, 512, D]); plru = I("proj_lru", [2, 512, D])
        w_out = I("w_out", [2, D, D]); pwg = I("ple_w_gate", [2, D, D]); pwp = I("ple_w_proj", [2, 256, D]); rw = I("router_w", [D, 16])
        ewg = I("exp_w_gate", [2, 16, D, 256]); ewu = I("exp_w_up", [2, 16, D, 256]); ewd = I("exp_w_down", [2, 16, 256, D])
        ccst = I("ccst", [2, 128, CW])
        x2T = kb.dram("x2T", [D, TOK], F32, "ExternalOutput")
        N_ = lambda n, s, dt: kb.dram(n, s, dt, "Internal")
        agF_in = N_("agF_in", [NF, TOK], BF16); agF_out = N_("agF_out", [4 * NF, TOK], BF16)
        agD_in = N_("agD_in", [8, TOK], F32); agD_out = N_("agD_out", [32, TOK], F32)
        nq = 8 * QT
        agN_in = N_("agN_in", [256, nq], BF16); agN_out = N_("agN_out", [1024, nq], BF16)
        agY_in = N_("agY_in", [128, S], BF16); agY_out = N_("agY_out", [512, S], BF16)
        agL_in = N_("agL_in", [128, S], BF16); agL_out = N_("agL_out", [512, S], BF16)
        xcur = N_("xcur", [D, TOK], F32)
        rF_in, rF_out, rD_in, rD_out = Res(), Res(), Res(), Res()
        rN_in, rN_out, rY_in, rY_out, rL_in, rL_out = Res(), Res(), Res(), Res(), Res(), Res()
        xcur_r = [Res() for _ in range(8)]
        agF512 = agF_out.rearrange("r (q t) -> (r q) t", t=512)
        agN512 = agN_out.rearrange("r (q t) -> (r q) t", t=512)
        agY2k = agY_out.rearrange("r (q t) -> (r q) t", t=2048)
        agL2k = agL_out.rearrange("r (q t) -> (r q) t", t=2048)
        gx = kb.sb([128, NIDX], U32, "gx"); gxr = Res()
        kb.dma("sp", gx[:], gidx[:, :], writes=[gxr])
        banks = [(kb.ps([128, 512], F32, "bank"), Res("bank", excl=True)) for _ in range(8)]
        rot = lambda items: _rot(items)
        for L in range(nlayers):
            xin = xT0 if L == 0 else xcur
            xin_r = None if L == 0 else xcur_r
            with contextlib.ExitStack() as sa:
                emit_A2(kb, xin, xin_r, w_in[L], agF_in, rF_in, agD_in, rD_in, banks, sa)
                kb.barrier()
            for k in range(13):
                rows = 256 if k < 12 else 32
                kb.allgather(agF_in[k * 256:k * 256 + rows, :], agF_out[k * 1024:k * 1024 + 4 * rows, :], groups, writes=[rF_in, rF_out])
            kb.allgather(agD_in[:, :], agD_out[:, :], groups, writes=[rD_in, rD_out])
            if fstop <= 1:
                break
            with contextlib.ExitStack() as s1:
                emit_nsa2(kb, NOM_TILES, agF_out, agF512, rF_out, gx, gxr, IX, peT[L], w1[L], w2[L], nmask, nE0, nmisc, agN_in, rN_in,
                          banks[0:4], banks[4], rot(banks[5:8]), s1)
                kb.barrier()
            for k in range(2):
                kb.allgather(agN_in[k * 128:(k + 1) * 128, :], agN_out[k * 512:(k + 1) * 512, :], groups, writes=[rN_in, rN_out])
            if fstop <= 2:
                break
            with contextlib.ExitStack() as s2:
                pbf = [(banks[6][0][:, 0:32].bitcast(BF16), banks[6][1]), (banks[7][0][:, 0:32].bitcast(BF16), banks[7][1])]
                emit_ssd2(kb, agF_out, rF_out, agD_out, rD_out, gx, gxr, IX, svec[L], srep[L], scst, agY_in, rY_in, rot(banks[0:6]), pbf, es=s2)
                kb.barrier()
            for k in range(2):
                kb.allgather(agY_in[k * 64:(k + 1) * 64, :], agY_out[k * 256:(k + 1) * 256, :], groups, writes=[rY_in, rY_out])
            if fstop <= 3:
                break
            with contextlib.ExitStack() as s3:
                emit_lru2(kb, agF_out, rF_out, gx, gxr, IX, lvec[L], lwab[L], lwxb[L], agL_in, rL_in, rot(banks[0:4]), es=s3)
                kb.barrier()
            for k in range(2):
                kb.allgather(agL_in[k * 64:(k + 1) * 64, :], agL_out[k * 256:(k + 1) * 256, :], groups, writes=[rL_in, rL_out])
            if fstop <= 4:
                break
            last = (L == nlayers - 1)
            with contextlib.ExitStack() as s4:
                emit_C2(kb, xin, agN512, rN_out, agY2k, rY_out, agL2k, rL_out, gx, gxr, IX, pT[L], w_in[L], pnsa[L], pssd[L], plru[L], w_out[L],
                        pwg[L], pwp[L], rw, ewg[L], ewu[L], ewd[L], ccst[L], x2T if last else xcur, xcur_r, last, banks, s4, xTr=xin_r)
                kb.barrier()
        kb.finish(())
        print("fused instructions", kb.ninst, "sems", kb.nsem)
    return nc


def _rot(items):
    r = Rot.__new__(Rot)
    r.t = list(items)
    r.i = 0
    return r


def fused_inputs(d, core):
    b, r = core // 4, core % 4
    g, par = r // 2, r % 2
    tok = slice(core * TOK, (core + 1) * TOK)
    x = d["x"].reshape(T_ALL, D)
    im = {"xT0": np.ascontiguousarray(x[tok].T),
          "pT": np.ascontiguousarray(np.stack([d["p"][L].reshape(T_ALL, 256)[tok].T for L in range(2)])),
          "gidx": make_gidx(r), "w_in": d["w_in"],
          "npeT": np.ascontiguousarray(np.stack([np.stack([d["nsa_pe_k"][L].T, d["nsa_pe_v"][L].T]) for L in range(2)])),
          "nw1": np.stack([np.stack([d["nsa_w1_k"][L], d["nsa_w1_v"][L]]) for L in range(2)]),
          "nw2": np.stack([np.stack([d["nsa_w2_k"][L], d["nsa_w2_v"][L]]) for L in range(2)]),
          "proj_nsa": d["proj_nsa"], "proj_ssd": d["proj_ssd"], "proj_lru": d["proj_lru"], "w_out": d["w_out"],
          "ple_w_gate": d["ple_w_gate"], "ple_w_proj": d["ple_w_proj"], "router_w": d["router_w"],
          "exp_w_gate": d["exp_w_gate"], "exp_w_up": d["exp_w_up"], "exp_w_down": d["exp_w_down"],
          "ccst": np.stack([c_consts(d, L) for L in range(2)]), "scst": ssd_consts()}
    im.update(nsa_consts(par))
    sv, sr, lv, la, lx_ = [], [], [], [], []
    for L in range(2):
        a_, b_ = ssd_vecs(d, L, r)
        sv.append(a_); sr.append(b_)
        a_, b_, c_ = lru_vecs(d, L, r)
        lv.append(a_); la.append(b_); lx_.append(c_)
    im["svec"] = np.stack(sv); im["srep"] = np.stack(sr); im["lvec"] = np.stack(lv); im["lwab"] = np.stack(la); im["lwxb"] = np.stack(lx_)
    return im


def ssd_vecs(d, layer, r):
    g = r // 2
    cw = d["ssd_conv_w"][layer]; cb = d["ssd_conv_b"][layer]
    v = np.zeros((128, 16), np.float32)
    xs_ = slice(128 * r, 128 * r + 128); Bs_ = slice(512 + 64 * g, 512 + 64 * g + 64); Cs_ = slice(640 + 64 * g, 640 + 64 * g + 64)
    v[:, 0:4] = cw[:, xs_].T; v[:, 4] = cb[xs_]
    v[:64, 5:9] = cw[:, Bs_].T; v[:64, 9] = cb[Bs_]
    v[:64, 10:14] = cw[:, Cs_].T; v[:64, 14] = cb[Cs_]
    rp = np.zeros((128, 8), np.float32)
    hh = slice(2 * r, 2 * r + 2)
    rp[:, 0:2] = d["ssd_dt_bias"][layer][hh][None, :]
    rp[:, 2:4] = d["ssd_a_log"][layer][hh][None, :]
    rp[:, 4:6] = d["ssd_d"][layer][hh][None, :]
    return v, rp


def lru_vecs(d, layer, r):
    ch = slice(128 * r, 128 * r + 128)
    v = np.zeros((128, 8), np.float32)
    v[:, 0:4] = d["lru_conv_w"][layer][:, ch].T
    v[:, 4] = d["lru_conv_b"][layer][ch]
    v[:, 5] = d["lru_ba"][layer][ch]
    v[:, 6] = d["lru_bx"][layer][ch]
    v[:, 7] = d["lru_lambda"][layer][ch]
    wab_ = np.zeros((128, 128), np.float32)
    wxb_ = np.zeros((128, 128), np.float32)
    for k in range(2):
        wab_[64 * k:64 * k + 64, 64 * k:64 * k + 64] = d["lru_wa"][layer][2 * r + k]
        wxb_[64 * k:64 * k + 64, 64 * k:64 * k + 64] = d["lru_wx"][layer][2 * r + k]
    return v, wab_, wxb_


def kernel(**inputs):
    d = {k: np.asarray(v) for k, v in inputs.items()}
    nc = build_fused()
    in_maps = [fused_inputs(d, c) for c in range(NCORES)]
    res = run_bass_kernel_spmd(nc, in_maps, core_ids=list(range(NCORES))).results
    x = np.concatenate([res[c]["x2T"].T for c in range(NCORES)], axis=0)
    return np.ascontiguousarray(x.reshape(BATCH, S, D).astype(np.float32))
```

```python
import numpy as np
import contextlib
import concourse.bass as bass
import concourse.mybir as mybir
from concourse.bass_utils import run_bass_kernel_spmd

F32 = mybir.dt.float32
BF16 = mybir.dt.bfloat16
AF = mybir.ActivationFunctionType
ALU = mybir.AluOpType
AX = mybir.AxisListType

SAME_ENGINE_SYNC = True
SEM_ROT = 20000
NDMA = 6


class Res:
    __slots__ = ("w", "r", "name", "excl")

    def __init__(self, name="", excl=False):
        self.name = name
        self.w = None
        self.r = {}
        self.excl = excl


class KB:
    def __init__(self, nc, es):
        self.nc = nc
        self.es = es
        self.eng = dict(pe=nc.tensor, act=nc.scalar, dve=nc.vector, pool=nc.gpsimd, sp=nc.sync)
        self.sems = {}
        self.cnt = {}
        self.cur = {}
        self.seen = {e: {} for e in self.eng}
        self.nsem = 0
        self.ninst = 0
        for e in self.eng:
            self.cur[e] = self._new_sem("e_" + e)
        self.dma_pool = {q: [self._new_sem("d_%s%d" % (q, i)) for i in range(NDMA)] for q in ("sp", "pool", "act")}
        self.dma_idx = {q: 0 for q in self.dma_pool}
        self.uid = 0
        self.out_marks = []

    def _new_sem(self, name):
        self.nsem += 1
        key = "%s_%d" % (name, self.nsem)
        self.sems[key] = self.es.enter_context(self.nc.semaphore(key))
        self.cnt[key] = 0
        return key

    def sb(self, shape, dtype, name=None, es=None):
        self.uid += 1
        return (es or self.es).enter_context(self.nc.sbuf_tensor("%s_%d" % (name or "sb", self.uid), list(shape), dtype))

    def ps(self, shape, dtype=F32, name=None):
        self.uid += 1
        return self.es.enter_context(self.nc.psum_tensor("%s_%d" % (name or "ps", self.uid), list(shape), dtype))

    def dram(self, name, shape, dtype, kind):
        return self.nc.dram_tensor(name, list(shape), dtype, kind=kind).ap()

    def _deps(self, reads, writes):
        deps = {}
        for r in reads:
            if r.w is not None:
                k, v = r.w
                if deps.get(k, 0) < v:
                    deps[k] = v
            if r.excl:
                for k, v in r.r.items():
                    if deps.get(k, 0) < v:
                        deps[k] = v
        for w in writes:
            if w.w is not None:
                k, v = w.w
                if deps.get(k, 0) < v:
                    deps[k] = v
            for k, v in w.r.items():
                if deps.get(k, 0) < v:
                    deps[k] = v
        return deps

    def _wait(self, e, deps, skip_key=None):
        seen = self.seen[e]
        for k, v in deps.items():
            if k == skip_key:
                continue
            if seen.get(k, 0) < v:
                self.eng[e].wait_ge(self.sems[k], v)
                seen[k] = v
                self.ninst += 1

    def _mark(self, key, v, reads, writes):
        for r in reads:
            r.r[key] = v
        for w in writes:
            w.w = (key, v)
            w.r = {}

    def op(self, e, fn, reads=(), writes=()):
        deps = self._deps(reads, writes)
        key = self.cur[e]
        skip = key if (e == "pe" or not SAME_ENGINE_SYNC) else None
        self._wait(e, deps, skip)
        inst = fn(self.eng[e])
        self.cnt[key] += 1
        v = self.cnt[key]
        inst.then_inc(self.sems[key], 1)
        self.ninst += 1
        self._mark(key, v, reads, writes)
        if v >= SEM_ROT:
            self.cur[e] = self._new_sem("e_" + e)
        return inst

    def dma(self, q, out, in_, reads=(), writes=(), final=False, **kw):
        pool = self.dma_pool[q]
        key = pool[self.dma_idx[q] % len(pool)]
        self.dma_idx[q] += 1
        deps = self._deps(reads, writes)
        if self.cnt[key] > 0:
            deps[key] = max(deps.get(key, 0), self.cnt[key])
        self._wait(q, deps)
        inst = self.eng[q].dma_start(out=out, in_=in_, **kw)
        self.cnt[key] += 16
        v = self.cnt[key]
        inst.then_inc(self.sems[key], 16)
        self.ninst += 1
        self._mark(key, v, reads, writes)
        if final:
            self.out_marks.append((key, v))
        if v >= SEM_ROT:
            i = pool.index(key)
            pool[i] = self._new_sem("d_" + q)
        return inst

    def barrier(self):
        allc = {k: v for k, v in self.cnt.items() if v > 0}
        for e in self.eng:
            self._wait(e, dict(allc))

    def finish(self, out_res):
        deps = self._deps(out_res, ())
        for k, v in self.out_marks:
            if deps.get(k, 0) < v:
                deps[k] = v
        self._wait("sp", deps)


def _kb_gather(self, out, in2d, idx_ap, reads=(), writes=()):
    pool = self.dma_pool["pool"]
    key = pool[self.dma_idx["pool"] % len(pool)]
    self.dma_idx["pool"] += 1
    deps = self._deps(reads, writes)
    if self.cnt[key] > 0:
        deps[key] = max(deps.get(key, 0), self.cnt[key])
    self._wait("pool", deps)
    inst = self.nc.gpsimd.indirect_dma_start(out=out, out_offset=None, in_=in2d,
                                             in_offset=bass.IndirectOffsetOnAxis(ap=idx_ap, axis=0))
    self.cnt[key] += 16
    v = self.cnt[key]
    inst.then_inc(self.sems[key], 16)
    self.ninst += 1
    self._mark(key, v, reads, writes)
    if v >= SEM_ROT:
        pool[pool.index(key)] = self._new_sem("d_pool")
    return inst


def _kb_allgather(self, in_ap, out_ap, groups, writes=()):
    if not hasattr(self, "cc_key"):
        self.cc_key = self._new_sem("cc")
    deps = self._deps((), writes)
    self._wait("pool", deps)
    inst = self.nc.gpsimd.collective_compute("AllGather", ALU.bypass, replica_groups=groups, ins=[in_ap], outs=[out_ap])
    key = self.cc_key
    self.cnt[key] += 1
    v = self.cnt[key]
    inst.then_inc(self.sems[key], 1)
    self.ninst += 1
    self._mark(key, v, (), writes)
    return inst


KB.gather = _kb_gather
KB.allgather = _kb_allgather


S = 8192


class Rot:
    def __init__(self, kb, n, shape, dtype, name, psum=False, es=None):
        if psum:
            self.t = [(kb.ps(shape, dtype, name), Res(name, excl=True)) for _ in range(n)]
        else:
            self.t = [(kb.sb(shape, dtype, name, es=es), Res(name)) for _ in range(n)]
        self.i = 0

    def next(self):
        x = self.t[self.i % len(self.t)]
        self.i += 1
        return x


LEVEL = 9

S = 8192


def emit_conv(kb, eng, out, xin, vt, vr, c0, N, P, xr, outr):
    kb.op(eng, lambda e: e.tensor_scalar(out=out[:P, :], in0=xin[:P, 0:N], scalar1=vt[:P, c0:c0 + 1], scalar2=vt[:P, c0 + 4:c0 + 5],
                                         op0=ALU.mult, op1=ALU.add), reads=[xr, vr], writes=[outr])
    for k in range(1, 4):
        kb.op("dve", lambda e, k=k: e.scalar_tensor_tensor(out=out[:P, :], in0=xin[:P, k:k + N], scalar=vt[:P, c0 + k:c0 + k + 1], in1=out[:P, :],
                                                          op0=ALU.mult, op1=ALU.add), reads=[xr, vr, outr], writes=[outr])


def ssd_consts():
    k = np.arange(128)
    cst = np.zeros((128, 4, 128), np.float32)
    cst[:, 0, :] = (k[:, None] <= k[None, :])
    cst[:, 1, :] = (k[:, None] > k[None, :])
    cst[:, 2, :] = np.eye(128)
    cst[:, 3, :] = 1.0
    return cst


DBG = ''

S = 8192
QT = 512
NCMP = 511
BIGM = 1024.0
f32 = np.float32


def nsa_consts(par=0):
    kk = np.arange(128)
    tq = np.arange(512)
    c = {}
    zeros = np.zeros((128, 512), f32); onesm = np.ones((128, 512), f32)

    def cm_full(dl):
        if dl < 0:
            return zeros
        if dl >= 5:
            return onesm
        return ((16 * kk[:, None] + 31 - tq[None, :]) <= 512 * dl).astype(f32)

    def cneg_full(dk):
        if dk < 0:
            return zeros
        if dk > 3:
            return -onesm
        return np.where(128 * dk + kk[:, None] <= tq[None, :], 0.0, -1.0).astype(f32)

    def wm_full(dk):
        if dk < -4 or dk > 3:
            return zeros
        diff = tq[None, :] - (128 * dk + kk[:, None])
        return ((diff >= 0) & (diff < 512)).astype(f32)

    cm = [cm_full(dl + par) for dl in range(-1, 5)]
    cneg = [cneg_full(dk - 4 * par) for dk in range(0, 8)]
    wm = [wm_full(dk - 4 * par) for dk in range(-4, 8)]
    c["nmask"] = np.ascontiguousarray(np.stack(cm + cneg + wm, axis=1))
    key = np.arange(S)
    c["nE0"] = (kk[:, None] == (key[None, :] // 64)).astype(f32)
    n = np.arange(512)
    ov = ((n[:, None] // 4) == kk[None, :]).astype(f32) + (((n[:, None] + 1) // 4) == kk[None, :]).astype(f32)
    ov[511] = 0
    ovl = ov.reshape(4, 128, 128).transpose(1, 0, 2)
    mm = np.arange(256) - 8 * par
    hi = (kk >= 64).astype(np.int64)
    C0 = (mm[None, :] <= 128 + hi[:, None]).astype(f32)
    Cm1 = C0 - 1.0
    F = np.where((mm[None, :] == 128 + hi[:, None]) | (mm[None, :] == 127 + hi[:, None]), 1e4, -1e30).astype(f32)
    ident = np.eye(128, dtype=f32)
    ones = np.ones((128, 128), f32)
    sel64 = np.zeros((128, 64), f32); sel64[64] = 1.0
    gsel = np.zeros((128, 12, 64), f32)
    for r in range(12):
        gsel[r, r, :] = 1.0
    c["nmisc"] = np.ascontiguousarray(np.concatenate(
        [ovl.reshape(128, 512), C0, Cm1, F, ident, ones, sel64, gsel.reshape(128, 768)], axis=1))
    return c


NMASK = 26
MISC_W = 512 + 768 + 128 + 128 + 64 + 768


D = 1024
TOK = 2048


CDBG = ''

D = 1024
TOK = 2048
NT = TOK // 512
ALPHA = 4 ** 0.25
OFF_Z = 512 + 768 + 24
OFF_MERGE = 6688 - 3072
f32 = np.float32
CW = 36 + 16 + 128 + 128 + 128 + 2048


def c_consts(d, layer):
    v = np.zeros((128, CW), f32)
    col = lambda a: a.reshape(-1, 128).T
    v[:, 0:8] = col(d["ln1_g"][layer]); v[:, 8:16] = col(d["ln1_b"][layer])
    v[:, 16:24] = col(d["ln2_g"][layer]); v[:, 24:32] = col(d["ln2_b"][layer])
    v[:, 32:36] = col(d["ssd_norm_w"][layer])
    v[:, 36:52] = d["router_b"][None, :]
    v[:, 52:180] = 1.0 / 1024
    v[:, 180:308] = 1.0 / 512
    v[:, 308:436] = np.eye(128)
    sel = np.zeros((128, 16, 128), f32)
    for e in range(16):
        sel[e, e, :] = 1.0
    v[:, 436:436 + 2048] = sel.reshape(128, 2048)
    return v


def emit_nsa2(kb, tiles, agF2k, agF512, agFr, gx, gxr, IX, peT, w1, w2, nmask, nE0, nmisc, onsaT, onsar, acc, pmb, gen, es):
    sb = lambda shape, dt, name: kb.sb(shape, dt, name, es=es)
    mk = sb([128, NMASK, 512], BF16, "nmask"); mkr = Res()
    kb.dma("pool", mk[:], nmask[:, :, :], writes=[mkr])
    E0 = sb([128, S], BF16, "nE0"); E0r = Res()
    kb.dma("pool", E0[:], nE0[:, :], writes=[E0r])
    mf = sb([128, MISC_W], F32, "nmiscf"); mfr = Res()
    kb.dma("sp", mf[:], nmisc[:, :], writes=[mfr])
    o = 0
    ovl_f = mf[:, 0:512]; o = 512
    C0 = mf[:, o:o + 256]; Cm1 = mf[:, o + 256:o + 512]; F4 = mf[:, o + 512:o + 768]; o += 768
    ident = mf[:, o:o + 128]; o += 128
    ones_f = mf[:, o:o + 128]; o += 128
    sel64 = mf[:, o:o + 64]; o += 64
    gsel = mf[:, o:o + 768]
    cb_ = sb([128, 512 + 128 + 128], BF16, "ncb"); cbr = Res()
    kb.op("dve", lambda e: e.tensor_copy(out=cb_[:, 0:512], in_=ovl_f), reads=[mfr], writes=[cbr])
    kb.op("dve", lambda e: e.tensor_copy(out=cb_[:, 512:640], in_=ident), reads=[mfr], writes=[cbr])
    kb.op("dve", lambda e: e.tensor_copy(out=cb_[:, 640:768], in_=ones_f), reads=[mfr], writes=[cbr])
    ovl = cb_[:, 0:512]; identb = cb_[:, 512:640]; onesb = cb_[:, 640:768]

    kall = sb([128, 2, S], BF16, "nk"); kr = [None, None, Res(), Res()]
    kb.op("pool", lambda e: e.memset(kall[64:128, :, :], 0.0), writes=[kr[2], kr[3]])
    for i, nm in ((2, "ksel"), (3, "kwin")):
        for rp in range(4):
            kb.gather(kall[0:64, i - 2, rp * 2048:(rp + 1) * 2048], agF2k, gx[0:64, IX[nm + str(rp)]:IX[nm + str(rp)] + 1], reads=[gxr, agFr], writes=[kr[i]])
    va = sb([128, 2, 64, 128], BF16, "nva"); var = [Res() for _ in range(2)]
    for i in range(2):
        kb.op("pool", lambda e, i=i: e.memset(va[:, i, :, 64:128], 0.0), writes=[var[i]])
        kb.op("pool", lambda e, i=i: e.memset(va[:, i, :, 64:65], 1.0), writes=[var[i]])

    kcT = sb([64, 512], BF16, "nkcT"); kcr = Res()
    vct = sb([128, 4, 65], BF16, "nvct"); vcr = Res()
    kb.op("pool", lambda e: e.memset(kcT[:], 0.0), writes=[kcr])
    kb.op("pool", lambda e: e.memset(vct[:], 0.0), writes=[vcr])
    kb.op("pool", lambda e: e.memset(vct[:, :, 64:65], 1.0), writes=[vcr])
    with contextlib.ExitStack() as esv:
        vT = kb.sb([64, 2, S], BF16, "nvT", es=esv); vTr = [Res(), Res()]
        for i, nm in ((0, "vsel"), (1, "vwin")):
            for rp in range(4):
                kb.gather(vT[:, i, rp * 2048:(rp + 1) * 2048], agF2k, gx[0:64, IX[nm + str(rp)]:IX[nm + str(rp)] + 1], reads=[gxr, agFr], writes=[vTr[i]])
            for k8 in range(8):
                pv8, pv8r = gen.next()
                pvb = pv8[:, 0:256].bitcast(BF16)
                for kk in range(8):
                    kt = k8 * 8 + kk
                    kb.op("pe", lambda e, kt=kt, kk=kk, i=i: e.transpose(pvb[:, kk * 64:(kk + 1) * 64], vT[:, i, kt * 128:(kt + 1) * 128], identb[0:64, 0:64]),
                          reads=[vTr[i], cbr], writes=[pv8r])
                kb.op("act", lambda e, k8=k8, i=i: e.copy(out=va[:, i, k8 * 8:(k8 + 1) * 8, 0:64], in_=pvb.rearrange("p (k d) -> p k d", k=8)), reads=[pv8r], writes=[var[i]])
        kb.barrier()
    with contextlib.ExitStack() as es2:
        sb2 = lambda shape, dt, name: kb.sb(shape, dt, name, es=es2)
        kcv = sb2([64, 2, S], BF16, "nkcv"); kr[0] = Res(); kr[1] = Res()
        for i, nm in ((0, "kcmp"), (1, "vcmp")):
            for rp in range(4):
                kb.gather(kcv[:, i, rp * 2048:(rp + 1) * 2048], agF2k, gx[0:64, IX[nm + str(rp)]:IX[nm + str(rp)] + 1], reads=[gxr, agFr], writes=[kr[i]])
        w1t = sb2([64, 32, 256], BF16, "nw1"); w1r = Res()
        w2t = sb2([128, 2, 64], BF16, "nw2"); w2r = Res()
        pet = sb2([64, 32], BF16, "npe"); per = Res()
        bias = sb2([128, 1], F32, "nbias"); biasr = Res()
        tmp3 = [(sb2([128, 512], F32, "nt"), Res(), sb2([128, 512], F32, "nu"), Res(), sb2([128, 512], BF16, "ngt"), Res()) for _ in range(2)]
        for which in range(2):
            for q4 in range(4):
                kb.dma("pool", w1t[:, q4 * 8:(q4 + 1) * 8, :], w1[which, q4 * 512:(q4 + 1) * 512, :].rearrange("(l d) j -> d l j", d=64), writes=[w1r])
            kb.dma("pool", w2t[:], w2[which, :, :].rearrange("(c p) d -> p c d", p=128), writes=[w2r])
            kb.dma("pool", pet[:], peT[which, :, :], writes=[per])
            src = kcv[:, which, :]
            gts = []
            for jc in range(2):
                ph, phr = gen.next()
                for l in range(32):
                    kb.op("pe", lambda e, l=l: e.matmul(ph[:, 0:NCMP], lhsT=w1t[:, l, jc * 128:(jc + 1) * 128],
                                                         rhs=src[:, l:l + 16 * (NCMP - 1) + 1:16], start=(l == 0), stop=(l == 31)),
                          reads=[w1r, kr[which]], writes=[phr])
                pbias, pbr = gen.next()
                for l in range(32):
                    kb.op("pe", lambda e, l=l: e.matmul(pbias[:, 0:1], lhsT=w1t[:, l, jc * 128:(jc + 1) * 128], rhs=pet[:, l:l + 1],
                                                         start=(l == 0), stop=(l == 31)), reads=[w1r, per], writes=[pbr])
                kb.op("dve", lambda e: e.tensor_copy(out=bias[:], in_=pbias[:, 0:1]), reads=[pbr], writes=[biasr])
                t, tr, u, ur, gt, gr = tmp3[jc]
                kb.op("act", lambda e: e.activation(out=t[:, 0:NCMP], in_=ph[:, 0:NCMP], func=AF.Identity, bias=bias[:, 0:1]), reads=[phr, biasr], writes=[tr])
                kb.op("dve", lambda e: e.tensor_tensor(out=u[:, 0:NCMP], in0=t[:, 0:NCMP], in1=t[:, 0:NCMP], op=ALU.mult), reads=[tr], writes=[ur])
                kb.op("dve", lambda e: e.tensor_scalar(out=u[:, 0:NCMP], in0=u[:, 0:NCMP], scalar1=0.044715, scalar2=1.0, op0=ALU.mult, op1=ALU.add), reads=[ur], writes=[ur])
                kb.op("dve", lambda e: e.tensor_tensor(out=u[:, 0:NCMP], in0=u[:, 0:NCMP], in1=t[:, 0:NCMP], op=ALU.mult), reads=[ur, tr], writes=[ur])
                kb.op("act", lambda e: e.activation(out=u[:, 0:NCMP], in_=u[:, 0:NCMP], func=AF.Sigmoid, scale=1.5957691216057308), reads=[ur], writes=[ur])
                kb.op("pool", lambda e: e.memset(gt[:, NCMP:512], 0.0), writes=[gr])
                kb.op("dve", lambda e: e.tensor_tensor(out=gt[:, 0:NCMP], in0=u[:, 0:NCMP], in1=t[:, 0:NCMP], op=ALU.mult), reads=[ur, tr], writes=[gr])
                gts.append((gt, gr))
            if which == 0:
                pk, pkr = gen.next()
                for jc in range(2):
                    kb.op("pe", lambda e, jc=jc: e.matmul(pk[0:64, 0:NCMP], lhsT=w2t[:, jc, :], rhs=gts[jc][0][:, 0:NCMP], start=(jc == 0), stop=(jc == 1)),
                          reads=[w2r, gts[jc][1]], writes=[pkr])
                kb.op("act", lambda e: e.copy(out=kcT[:, 0:NCMP], in_=pk[0:64, 0:NCMP]), reads=[pkr], writes=[kcr])
            else:
                for m in range(4):
                    pv, pvr = gen.next()
                    for jc in range(2):
                        kb.op("pe", lambda e, jc=jc, m=m: e.matmul(pv[:, 0:64], lhsT=gts[jc][0][:, m * 128:(m + 1) * 128], rhs=w2t[:, jc, :], start=(jc == 0), stop=(jc == 1)),
                              reads=[w2r, gts[jc][1]], writes=[pvr])
                    kb.op("act", lambda e, m=m: e.copy(out=vct[:, m, 0:64], in_=pv[:, 0:64]), reads=[pvr], writes=[vcr])
        kb.barrier()
    kmax = sb([128, 1], F32, "nkmax"); kmr = Res()
    kb.op("pool", lambda e: e.memset(kmax[:], 0.0), writes=[kmr])
    sq = Rot(kb, 2, [64, 512], BF16, "nsq", es=es)
    red = Rot(kb, 2, [128, 1], F32, "nred", es=es)

    def norm_max(src_ap, n, src_res, dst, dstr):
        s_, sr_ = sq.next()
        kb.op("pool", lambda e: e.tensor_tensor(out=s_[:, 0:n], in0=src_ap, in1=src_ap, op=ALU.mult), reads=src_res, writes=[sr_])
        pn, pnr = gen.next()
        kb.op("pe", lambda e: e.matmul(pn[:, 0:n], lhsT=onesb[0:64, :], rhs=s_[:, 0:n], start=True, stop=True), reads=[cbr, sr_], writes=[pnr])
        r_, rr_ = red.next()
        kb.op("dve", lambda e: e.reduce_max(out=r_[:], in_=pn[:, 0:n], axis=AX.X), reads=[pnr], writes=[rr_])
        kb.op("dve", lambda e: e.tensor_tensor(out=dst[:], in0=dst[:], in1=r_[:], op=ALU.max), reads=[rr_, dstr], writes=[dstr])

    norm_max(kcT[:, 0:512], 512, [kcr], kmax, kmr)
    for which in (2, 3):
        for tt in range(S // 512):
            norm_max(kall[0:64, which - 2, tt * 512:(tt + 1) * 512], 512, [kr[which]], kmax, kmr)

    qb = Rot(kb, 2, [128, 4, 512], BF16, "nq", es=es)
    for q_, qr_ in qb.t:
        kb.op("pool", lambda e, q_=q_: e.memset(q_[64:128, :, :], 0.0), writes=[qr_])
    gtl = Rot(kb, 2, [12, 512], F32, "ngate", es=es)
    gtbl = Rot(kb, 2, [12, 512], BF16, "ngateb", es=es)
    ebuf = Rot(kb, 4, [128, 512], BF16, "ne", es=es)
    pbuf = Rot(kb, 4, [128, 512], BF16, "np", es=es)
    mskb = Rot(kb, 2, [128, 512], BF16, "nmsk", es=es)
    pnbuf = Rot(kb, 4, [128, 512], BF16, "npn", es=es)
    rinv = Rot(kb, 2, [128, 512], F32, "nrinv", es=es)
    ocmp = Rot(kb, 1, [64, 4, 512], F32, "nocmp", es=es)
    impT = Rot(kb, 2, [128, 512], F32, "nimpT", es=es)
    selT = Rot(kb, 2, [128, 512], BF16, "nselT", es=es)
    v1b = Rot(kb, 2, [128, 128], F32, "nv1", es=es)
    v2b = Rot(kb, 2, [128, 128], F32, "nv2", es=es)
    m8a = Rot(kb, 2, [128, 8], F32, "nm8a", es=es)
    m8b = Rot(kb, 2, [128, 8], F32, "nm8b", es=es)
    smk = Rot(kb, 2, [128, 128], F32, "nsmk", es=es)
    accs = Rot(kb, 1, [65, 8, 512], F32, "naccs", es=es)
    rec = Rot(kb, 2, [64, 512], F32, "nrec", es=es)
    osb = Rot(kb, 2, [64, 512], F32, "nosb", es=es)
    negc = Rot(kb, 2, [128, 4], F32, "nnegc", es=es)
    qm = Rot(kb, 2, [128, 1], F32, "nqm", es=es)

    for ti, i in enumerate(tiles):
        t0 = i * QT
        q, qr = qb.next()
        for j in range(4):
            cq = IX["q%d_%d" % (ti, j)]
            kb.gather(q[0:64, j, :], agF512, gx[0:64, cq:cq + 1], reads=[gxr, agFr], writes=[qr])
        gtb_, gtbr = gtbl.next()
        cg = IX["gate%d" % ti]
        kb.gather(gtb_[:], agF512, gx[0:12, cg:cg + 1], reads=[gxr, agFr], writes=[gtbr])
        gt_, gtr = gtl.next()
        kb.op("act", lambda e: e.activation(out=gt_[:], in_=gtb_[:], func=AF.Sigmoid), reads=[gtbr], writes=[gtr])
        nc_, ncr = negc.next()
        for j in range(4):
            qm_, qmr = qm.next()
            kb.op("pool", lambda e: e.memset(qm_[:], 0.0), writes=[qmr])
            norm_max(q[0:64, j, :], 512, [qr], qm_, qmr)
            kb.op("dve", lambda e, j=j: e.tensor_tensor(out=nc_[:, j:j + 1], in0=qm_[:], in1=kmax[:], op=ALU.mult), reads=[qmr, kmr], writes=[ncr])
        kb.op("act", lambda e: e.activation(out=nc_[:], in_=nc_[:], func=AF.Sqrt, scale=1.05), reads=[ncr], writes=[ncr])
        kb.op("dve", lambda e: e.tensor_scalar(out=nc_[:], in0=nc_[:], scalar1=-0.125, scalar2=None, op0=ALU.mult), reads=[ncr], writes=[ncr])

        nch = min(4, (32 * (i + 1) + 31 + 127) // 128)
        oc, ocr = ocmp.next()
        pimp, pimpr = acc[0]
        for j in range(4):
            es_ = []
            psum_, psumr = acc[1]
            for m in range(nch):
                ps_, psr = gen.next()
                kb.op("pe", lambda e, m=m, j=j: e.matmul(ps_[:, :], lhsT=kcT[:, m * 128:(m + 1) * 128], rhs=q[0:64, j, :], start=True, stop=True),
                      reads=[kcr, qr], writes=[psr])
                e_, er = ebuf.next()
                kb.op("act", lambda e, j=j: e.activation(out=e_[:], in_=ps_[:, :], func=AF.Exp, scale=0.125, bias=nc_[:, j:j + 1]), reads=[psr, ncr], writes=[er])
                dl = i - 4 * m
                if dl <= 4:
                    kb.op("pool", lambda e, dl=dl: e.tensor_tensor(out=e_[:], in0=e_[:], in1=mk[:, dl + 1, :], op=ALU.mult), reads=[er, mkr], writes=[er])
                kb.op("pe", lambda e, m=m: e.matmul(psum_[:, :], lhsT=onesb, rhs=e_[:], start=(m == 0), stop=(m == nch - 1)), reads=[cbr, er], writes=[psumr])
                es_.append((e_, er))
            ri, rir = rinv.next()
            kb.op("dve", lambda e: e.tensor_scalar(out=ri[:], in0=psum_[:, :], scalar1=1e-30, scalar2=None, op0=ALU.max), reads=[psumr], writes=[rir])
            kb.op("dve", lambda e: e.reciprocal(out=ri[:], in_=ri[:]), reads=[rir], writes=[rir])
            po, por = acc[2]
            for m in range(nch):
                e_, er = es_[m]
                pn_, pnr_ = pnbuf.next()
                kb.op("dve", lambda e: e.tensor_tensor(out=pn_[:], in0=e_[:], in1=ri[:], op=ALU.mult), reads=[er, rir], writes=[pnr_])
                kb.op("pe", lambda e, m=m: e.matmul(po[0:64, :], lhsT=vct[:, m, 0:64], rhs=pn_[:], start=(m == 0), stop=(m == nch - 1)), reads=[vcr, pnr_], writes=[por])
                kb.op("pe", lambda e, m=m, j=j: e.matmul(pimp[:, :], lhsT=ovl[:, m * 128:(m + 1) * 128], rhs=pn_[:], start=(j == 0 and m == 0), stop=(j == 3 and m == nch - 1)),
                      reads=[cbr, pnr_], writes=[pimpr])
            kb.op("act", lambda e, j=j: e.copy(out=oc[:, j, :], in_=po[0:64, :]), reads=[por], writes=[ocr])
        it_, itr = impT.next()
        kb.op("act", lambda e: e.copy(out=it_[:], in_=pimp[:, :]), reads=[pimpr], writes=[itr])
        st_, str_ = selT.next()
        for k4 in range(4):
            ksub = 4 * i + k4
            ptr_, ptrr = gen.next()
            kb.op("pe", lambda e, k4=k4: e.transpose(ptr_[:, 0:128], it_[:, k4 * 128:(k4 + 1) * 128], ident), reads=[itr, mfr], writes=[ptrr])
            co = 128 - 2 * ksub
            v1, v1r = v1b.next()
            kb.op("dve", lambda e, co=co: e.tensor_tensor(out=v1[:], in0=ptr_[:, 0:128], in1=C0[:, co:co + 128], op=ALU.mult), reads=[ptrr, mfr], writes=[v1r])
            kb.op("dve", lambda e, co=co: e.tensor_tensor(out=v1[:], in0=v1[:], in1=Cm1[:, co:co + 128], op=ALU.add), reads=[v1r, mfr], writes=[v1r])
            kb.op("dve", lambda e, co=co: e.tensor_tensor(out=v1[:], in0=v1[:], in1=F4[:, co:co + 128], op=ALU.max), reads=[v1r, mfr], writes=[v1r])
            kb.op("dve", lambda e: e.memset(v1[:, 0:1], 1e4), reads=[], writes=[v1r])
            a8, a8r = m8a.next()
            kb.op("dve", lambda e: e.max(out=a8[:], in_=v1[:]), reads=[v1r], writes=[a8r])
            v2, v2r = v2b.next()
            kb.op("dve", lambda e: e.match_replace(out=v2[:], in_to_replace=a8[:], in_values=v1[:], imm_value=-1e30), reads=[a8r, v1r], writes=[v2r])
            b8, b8r = m8b.next()
            kb.op("dve", lambda e: e.max(out=b8[:], in_=v2[:]), reads=[v2r], writes=[b8r])
            sm, smr = smk.next()
            kb.op("dve", lambda e: e.tensor_scalar(out=sm[:], in0=v1[:], scalar1=b8[:, 7:8], scalar2=None, op0=ALU.is_ge), reads=[v1r, b8r], writes=[smr])
            if DBG == "sm" and k4 == 0 and ti == 0:
                kb.dma("sp", onsaT[0:128, 0:128], sm[:], reads=[smr], final=True)
                kb.dma("sp", onsaT[128:256, 0:128], v1[:], reads=[v1r], final=True)
                kb.dma("sp", onsaT[0:128, 128:136], a8[:], reads=[a8r], final=True)
                kb.dma("sp", onsaT[0:128, 136:144], b8[:], reads=[b8r], final=True)
                kb.dma("sp", onsaT[128:256, 128:256], v2[:], reads=[v2r], final=True)
            pt2, pt2r = gen.next()
            kb.op("pe", lambda e: e.transpose(pt2[:, 0:128], sm[:], ident), reads=[smr, mfr], writes=[pt2r])
            kb.op("act", lambda e, k4=k4: e.copy(out=st_[:, k4 * 128:(k4 + 1) * 128], in_=pt2[:, 0:128]), reads=[pt2r], writes=[str_])

        as_, asr = accs.next()
        LA = 2
        for br in range(2):
            if br == 0:
                kts = list(range(0, min(64, 4 * i + 8)))
            else:
                kts = [kt for kt in range(4 * i - 4, min(64, 4 * i + 8)) if kt >= 0]
            ksrc = 2 + br
            pairs = [(n_, kt, j) for n_, kt in enumerate(kts) for j in range(4)]
            pend = []
            msk = None
            for idx in range(len(pairs) + LA):
                if idx < len(pairs):
                    n_, kt, j = pairs[idx]
                    dk = kt - 4 * i
                    if br == 0 and j == 0:
                        pm, pmr = pmb
                        kb.op("pe", lambda e, kt=kt, dk=dk: e.matmul(pm[:, :], lhsT=E0[:, kt * 128:(kt + 1) * 128], rhs=st_[:], start=True, stop=(dk < 0)),
                              reads=[E0r, str_], writes=[pmr])
                        if dk >= 0:
                            kb.op("pe", lambda e, dk=dk: e.matmul(pm[:, :], lhsT=identb, rhs=mk[:, 6 + dk, :], start=False, stop=True), reads=[cbr, mkr], writes=[pmr])
                        msk = mskb.next()
                        kb.op("act", lambda e, msk=msk: e.activation(out=msk[0][:], in_=pm[:, :], func=AF.Relu), reads=[pmr], writes=[msk[1]])
                    ps_, psr = gen.next()
                    kb.op("pe", lambda e, kt=kt, j=j, ps_=ps_: e.matmul(ps_[:, :], lhsT=kall[:, br, kt * 128:(kt + 1) * 128], rhs=q[:, j, :], start=True, stop=True),
                          reads=[kr[ksrc], qr], writes=[psr])
                    e_, er = ebuf.next()
                    kb.op("act", lambda e, j=j, e_=e_, ps_=ps_: e.activation(out=e_[:], in_=ps_[:, :], func=AF.Exp, scale=0.125, bias=nc_[:, j:j + 1]), reads=[psr, ncr], writes=[er])
                    p_, pr_ = pbuf.next()
                    eng = "dve"
                    if br == 0:
                        kb.op(eng, lambda e, p_=p_, e_=e_, msk=msk: e.tensor_tensor(out=p_[:], in0=e_[:], in1=msk[0][:], op=ALU.mult), reads=[er, msk[1]], writes=[pr_])
                    else:
                        kb.op(eng, lambda e, dk=dk, p_=p_, e_=e_: e.tensor_tensor(out=p_[:], in0=e_[:], in1=mk[:, 14 + dk + 4, :], op=ALU.mult), reads=[er, mkr], writes=[pr_])
                    pend.append((p_, pr_, n_, kt, j))
                if idx >= LA:
                    p_, pr_, n_, kt, j = pend[idx - LA]
                    pa, par = acc[j]
                    kb.op("pe", lambda e, kt=kt, n_=n_, p_=p_, pa=pa: e.matmul(pa[:, :], lhsT=va[:, br, kt, :], rhs=p_[:], start=(n_ == 0), stop=(n_ == len(kts) - 1)),
                          reads=[var[br], pr_], writes=[par])
            for j in range(4):
                pa, par = acc[j]
                kb.op("act", lambda e, j=j, br=br: e.copy(out=as_[:, br * 4 + j, :], in_=pa[0:65, :]), reads=[par], writes=[asr])
        for j in range(4 if DBG not in ("sm", "pm") else 0):
            o_, or_ = osb.next()
            pg, pgr = gen.next()
            kb.op("pe", lambda e, j=j: e.matmul(pg[0:64, :], lhsT=gsel[0:12, (j * 3) * 64:(j * 3 + 1) * 64], rhs=gt_[:], start=True, stop=True), reads=[mfr, gtr], writes=[pgr])
            if DBG == "cmp":
                kb.op("dve", lambda e, j=j: e.tensor_copy(out=o_[:], in_=oc[:, j, :]), reads=[pgr, ocr], writes=[or_])
            elif DBG:
                kb.op("dve", lambda e, j=j: e.memset(o_[:], 0.0), reads=[pgr, ocr], writes=[or_])
            else:
                kb.op("dve", lambda e, j=j: e.tensor_tensor(out=o_[:], in0=pg[0:64, :], in1=oc[:, j, :], op=ALU.mult), reads=[pgr, ocr], writes=[or_])
            for br in range(2):
                if DBG == "cmp" or (DBG == "sel" and br == 1) or (DBG == "win" and br == 0):
                    continue
                psm, psmr = gen.next()
                kb.op("pe", lambda e, j=j, br=br: e.matmul(psm[0:64, :], lhsT=sel64[0:65, :], rhs=as_[:, br * 4 + j, :], start=True, stop=True), reads=[mfr, asr], writes=[psmr])
                rc, rcr = rec.next()
                kb.op("dve", lambda e: e.tensor_scalar(out=rc[:], in0=psm[0:64, :], scalar1=1e-30, scalar2=None, op0=ALU.max), reads=[psmr], writes=[rcr])
                kb.op("dve", lambda e: e.reciprocal(out=rc[:], in_=rc[:]), reads=[rcr], writes=[rcr])
                pg2, pg2r = gen.next()
                kb.op("pe", lambda e, j=j, br=br: e.matmul(pg2[0:64, :], lhsT=gsel[0:12, (j * 3 + 1 + br) * 64:(j * 3 + 2 + br) * 64], rhs=gt_[:], start=True, stop=True),
                      reads=[mfr, gtr], writes=[pg2r])
                if not DBG:
                    kb.op("dve", lambda e: e.tensor_tensor(out=rc[:], in0=pg2[0:64, :], in1=rc[:], op=ALU.mult), reads=[pg2r, rcr], writes=[rcr])
                kb.op("pool", lambda e, j=j, br=br: e.tensor_tensor(out=rc[:], in0=rc[:], in1=as_[0:64, br * 4 + j, :], op=ALU.mult), reads=[rcr, asr], writes=[rcr])
                kb.op("pool", lambda e: e.tensor_tensor(out=o_[:], in0=o_[:], in1=rc[:], op=ALU.add), reads=[rcr, or_], writes=[or_])
            kb.dma("pool", onsaT[j * 64:(j + 1) * 64, ti * QT:(ti + 1) * QT], o_[:], reads=[or_, onsar])


def emit_ssd2(kb, agF2k, agFr, agD2k, agDr, gx, gxr, IX, svec, srep, cst, yT_dst, yT_r, pbanks, pbf, es=None):
    N = 1024
    NCH = S // 128
    ct = kb.sb([128, 4, 128], F32, "scst", es=es); cr = Res()
    kb.dma("sp", ct[:], cst[:, :, :], writes=[cr])
    tri = ct[:, 0, :]; U = ct[:, 1, :]; ident = ct[:, 2, :]; ones = ct[:, 3, :]
    identb = kb.sb([128, 128], BF16, "sidb", es=es); idbr = Res()
    kb.op("dve", lambda e: e.tensor_copy(out=identb[:], in_=ident), reads=[cr], writes=[idbr])
    vt = kb.sb([128, 16], F32, "svec", es=es); vr = Res()
    kb.dma("sp", vt[:], svec[:, :], writes=[vr])
    rp = kb.sb([128, 8], F32, "srep", es=es); rpr = Res()
    kb.dma("sp", rp[:], srep[:, :], writes=[rpr])
    if LEVEL == -1: return
    dt = kb.sb([128, NCH, 2], F32, "sdt", es=es); dtr = Res()
    dtT = kb.sb([2, S], F32, "sdtT", es=es); dtTr = Res()
    for rq in range(4):
        cd = IX["dt%d" % rq]
        kb.gather(dtT[0:2, rq * 2048:(rq + 1) * 2048], agD2k, gx[0:2, cd:cd + 1], reads=[gxr, agDr], writes=[dtTr])
    pdt, pdtr = pbanks.next()
    for c in range(NCH):
        kb.op("pe", lambda e, c=c: e.transpose(pdt[:, c * 2:(c + 1) * 2], dtT[0:2, c * 128:(c + 1) * 128], ident[0:2, 0:2]), reads=[dtTr, cr], writes=[pdtr])
    kb.op("dve", lambda e: e.tensor_copy(out=dt[:].rearrange("p c h -> p (c h)"), in_=pdt[:, 0:NCH * 2]), reads=[pdtr], writes=[dtr])
    aa = kb.sb([128, NCH, 2], F32, "sa", es=es); aar = Res()
    An = kb.sb([128, 2], F32, "sAn", es=es); Anr = Res()
    kb.op("act", lambda e: e.activation(out=An[:], in_=rp[:, 2:4], func=AF.Exp), reads=[rpr], writes=[Anr])
    kb.op("dve", lambda e: e.tensor_scalar(out=An[:], in0=An[:], scalar1=-1.0, scalar2=None, op0=ALU.mult), reads=[Anr], writes=[Anr])
    for h in range(2):
        kb.op("dve", lambda e, h=h: e.tensor_scalar(out=dt[:, :, h], in0=dt[:, :, h], scalar1=rp[:, h:h + 1], scalar2=None, op0=ALU.add),
              reads=[dtr, rpr], writes=[dtr])
    kb.op("act", lambda e: e.activation(out=dt[:], in_=dt[:], func=AF.Exp), reads=[dtr], writes=[dtr])
    kb.op("act", lambda e: e.activation(out=dt[:], in_=dt[:], func=AF.Ln, bias=1.0), reads=[dtr], writes=[dtr])
    for h in range(2):
        kb.op("dve", lambda e, h=h: e.tensor_scalar(out=aa[:, :, h], in0=dt[:, :, h], scalar1=An[:, h:h + 1], scalar2=None, op0=ALU.mult),
              reads=[dtr, Anr], writes=[aar])
    if LEVEL == -2: return
    acum = kb.sb([128, NCH, 2], F32, "sacum", es=es); acr = Res()
    dout = kb.sb([128, NCH, 2], F32, "sdout", es=es); dor = Res()
    dst = kb.sb([128, NCH, 2], F32, "sdst", es=es); dsr = Res()
    dtot = kb.sb([128, NCH, 2], F32, "sdtot", es=es); dtor = Res()
    aflat = aa[:].rearrange("p c h -> p (c h)")
    p1, p1r = pbanks.next()
    kb.op("pe", lambda e: e.matmul(p1[:, 0:NCH * 2], lhsT=tri, rhs=aflat, start=True, stop=True), reads=[cr, aar], writes=[p1r])
    p2, p2r = pbanks.next()
    kb.op("pe", lambda e: e.matmul(p2[:, 0:NCH * 2], lhsT=ones, rhs=aflat, start=True, stop=True), reads=[cr, aar], writes=[p2r])
    fl = lambda t: t[:].rearrange("p c h -> p (c h)")
    kb.op("dve", lambda e: e.tensor_copy(out=fl(acum), in_=p1[:, 0:NCH * 2]), reads=[p1r], writes=[acr])
    kb.op("act", lambda e: e.activation(out=fl(dout), in_=p1[:, 0:NCH * 2], func=AF.Exp), reads=[p1r], writes=[dor])
    kb.op("act", lambda e: e.activation(out=fl(dtot), in_=p2[:, 0:NCH * 2], func=AF.Exp), reads=[p2r], writes=[dtor])
    kb.op("dve", lambda e: e.tensor_tensor(out=fl(dst), in0=p2[:, 0:NCH * 2], in1=fl(acum), op=ALU.subtract), reads=[p2r, acr], writes=[dsr])
    kb.op("act", lambda e: e.activation(out=fl(dst), in_=fl(dst), func=AF.Exp), reads=[dsr], writes=[dsr])

    if LEVEL == -3: return
    prev = [kb.sb([64, 64], F32, "sprev", es=es) for _ in range(2)]
    prevr = [Res() for _ in range(2)]
    prevb = [kb.sb([64, 64], BF16, "sprevb", es=es) for _ in range(2)]
    prevbr = [Res() for _ in range(2)]
    for h in range(2):
        kb.op("pool", lambda e, h=h: e.memset(prev[h][:], 0.0), writes=[prevr[h]])
        kb.op("pool", lambda e, h=h: e.memset(prevb[h][:], 0.0), writes=[prevbr[h]])

    xfull = kb.sb([128, S + 3], BF16, "sxfull", es=es); xfr = Res()
    bfull = kb.sb([64, S + 3], BF16, "sbfull", es=es); bfr = Res()
    cfull = kb.sb([64, S + 3], BF16, "scfull", es=es); cfr = Res()
    for (tl, rs, nm, P) in ((xfull, xfr, "sx", 128), (bfull, bfr, "sB", 64), (cfull, cfr, "sC", 64)):
        kb.op("pool", lambda e, tl=tl, P=P: e.memset(tl[:P, 0:3], 0.0), writes=[rs])
        for rq in range(4):
            cc_ = IX[nm + str(rq)]
            kb.gather(tl[:P, 3 + rq * 2048:3 + (rq + 1) * 2048], agF2k, gx[0:P, cc_:cc_ + 1], reads=[gxr, agFr], writes=[rs])
    yTb = Rot(kb, 2, [128, N], BF16, "syTb", es=es)
    xcv = Rot(kb, 2, [128, N], F32, "sxcv", es=es)
    bcv = Rot(kb, 2, [64, N], F32, "sbcv", es=es)
    ccv = Rot(kb, 2, [64, N], F32, "sccv", es=es)
    xs = Rot(kb, 2, [128, N], F32, "sxs", es=es)
    bs = Rot(kb, 2, [64, N], BF16, "sbs", es=es)
    cs = Rot(kb, 2, [64, N], BF16, "scs", es=es)
    xtm = Rot(kb, 2, [128, 128], F32, "sxtm", es=es)
    Xb = Rot(kb, 2, [128, 128], BF16, "sXb", es=es)
    Xd = Rot(kb, 2, [128, 128], BF16, "sXd", es=es)
    Btm = Rot(kb, 2, [128, 64], BF16, "sBtm", es=es)
    CBm = Rot(kb, 2, [128, 128], F32, "sCBm", es=es)
    lh = Rot(kb, 2, [128, 128], F32, "slh", es=es)
    EE = Rot(kb, 2, [128, 128], F32, "sE", es=es)
    MT = Rot(kb, 2, [128, 128], BF16, "sMT", es=es)
    yt = Rot(kb, 2, [128, 8, 128], F32, "syt", es=es)
    pbi = 0
    for j in range(S // N if LEVEL > 0 else 0):
        t0 = j * N
        xt, xr = xfull[:, t0:t0 + N + 3], xfr
        bt, br = bfull[:, t0:t0 + N + 3], bfr
        ctt, ctr = cfull[:, t0:t0 + N + 3], cfr
        xc, xcr = xcv.next(); bc, bcr = bcv.next(); cc, ccr = ccv.next()
        emit_conv(kb, "dve", xc, xt, vt, vr, 0, N, 128, xr, xcr)
        emit_conv(kb, "dve", bc, bt, vt, vr, 5, N, 64, br, bcr)
        emit_conv(kb, "dve", cc, ctt, vt, vr, 10, N, 64, ctr, ccr)
        xst, xsr = xs.next(); bst, bsr = bs.next(); cst_, csr = cs.next()
        kb.op("act", lambda e: e.activation(out=xst[:], in_=xc[:], func=AF.Silu), reads=[xcr], writes=[xsr])
        kb.op("act", lambda e: e.activation(out=bst[:], in_=bc[:], func=AF.Silu), reads=[bcr], writes=[bsr])
        kb.op("act", lambda e: e.activation(out=cst_[:], in_=cc[:], func=AF.Silu), reads=[ccr], writes=[csr])
        ytile, ytr = yt.next()
        for c in range(N // 128 if LEVEL > 1 else 0):
            gc = j * (N // 128) + c
            ts = slice(c * 128, (c + 1) * 128)
            pT, pTr = pbanks.next()
            kb.op("pe", lambda e: e.transpose(pT[:, 0:128], xst[:, ts], ident), reads=[xsr, cr], writes=[pTr])
            xm, xmr = xtm.next()
            kb.op("act", lambda e: e.copy(out=xm[:], in_=pT[:, 0:128]), reads=[pTr], writes=[xmr])
            xb, xbr = Xb.next()
            for h in range(2):
                hs = slice(h * 64, (h + 1) * 64)
                kb.op("dve", lambda e, hs=hs, h=h: e.tensor_scalar(out=xb[:, hs], in0=pT[:, hs], scalar1=dt[:, gc, h:h + 1], scalar2=None, op0=ALU.mult),
                      reads=[pTr, dtr], writes=[xbr])
            xd, xdr = Xd.next()
            for h in range(2):
                hs = slice(h * 64, (h + 1) * 64)
                kb.op("pool", lambda e, hs=hs, h=h: e.tensor_scalar(out=xd[:, hs], in0=xb[:, hs], scalar1=dst[:, gc, h:h + 1], scalar2=None, op0=ALU.mult),
                      reads=[xbr, dsr], writes=[xdr])
            if LEVEL < 3: continue
            pb_t, pb_r = pbf[pbi % len(pbf)]; pbi += 1
            kb.op("pe", lambda e: e.transpose(pb_t[:, 0:64], bst[:, ts], identb[0:64, 0:64]), reads=[bsr, idbr], writes=[pb_r])
            btm, btmr = Btm.next()
            kb.op("act", lambda e: e.copy(out=btm[:], in_=pb_t[:, 0:64]), reads=[pb_r], writes=[btmr])
            if LEVEL < 4: continue
            pcb, pcbr = pbanks.next()
            kb.op("pe", lambda e: e.matmul(pcb[:, 0:128], lhsT=bst[:, ts], rhs=cst_[:, ts], start=True, stop=True), reads=[bsr, csr], writes=[pcbr])
            cbm, cbmr = CBm.next()
            kb.op("dve", lambda e: e.tensor_tensor(out=cbm[:], in0=pcb[:, 0:128], in1=tri, op=ALU.mult), reads=[pcbr, cr], writes=[cbmr])
            for h in range(2 if LEVEL > 4 else 0):
                hs = slice(h * 64, (h + 1) * 64)
                l_, lr_ = lh.next()
                kb.op("pool", lambda e, h=h: e.tensor_scalar(out=l_[:], in0=U, scalar1=aa[:, gc, h:h + 1], scalar2=None, op0=ALU.mult),
                      reads=[cr, aar], writes=[lr_])
                pseg, psegr = pbanks.next()
                kb.op("pe", lambda e: e.matmul(pseg[:, 0:128], lhsT=l_[:], rhs=tri, start=True, stop=True), reads=[lr_, cr], writes=[psegr])
                E, Er = EE.next()
                kb.op("act", lambda e: e.activation(out=E[:], in_=pseg[:, 0:128], func=AF.Exp), reads=[psegr], writes=[Er])
                mt, mtr = MT.next()
                kb.op("dve", lambda e: e.tensor_tensor(out=mt[:], in0=E[:], in1=cbm[:], op=ALU.mult), reads=[Er, cbmr], writes=[mtr])
                py, pyr = pbanks.next()
                kb.op("pe", lambda e, hs=hs: e.matmul(py[:, 0:64], lhsT=mt[:], rhs=xb[:, hs], start=True, stop=True), reads=[mtr, xbr], writes=[pyr])
                po, por = pbanks.next()
                kb.op("pe", lambda e, h=h: e.matmul(po[:, 0:64], lhsT=cst_[:, ts], rhs=prevb[h][:], start=True, stop=True), reads=[csr, prevbr[h]], writes=[por])
                kb.op("act", lambda e, hs=hs: e.copy(out=ytile[:, c, hs], in_=py[:, 0:64]), reads=[pyr], writes=[ytr])
                kb.op("dve", lambda e, hs=hs, h=h: e.scalar_tensor_tensor(out=ytile[:, c, hs], in0=po[:, 0:64], scalar=dout[:, gc, h:h + 1], in1=ytile[:, c, hs],
                                                                          op0=ALU.mult, op1=ALU.add), reads=[por, dor, ytr], writes=[ytr])
                kb.op("dve", lambda e, hs=hs, h=h: e.scalar_tensor_tensor(out=ytile[:, c, hs], in0=xm[:, hs], scalar=rp[:, 4 + h:5 + h], in1=ytile[:, c, hs],
                                                                          op0=ALU.mult, op1=ALU.add), reads=[xmr, rpr, ytr], writes=[ytr])
                pst, pstr = pbanks.next()
                kb.op("pe", lambda e, hs=hs: e.matmul(pst[0:64, 0:64], lhsT=btm[:], rhs=xd[:, hs], start=True, stop=True), reads=[btmr, xdr], writes=[pstr])
                kb.op("dve", lambda e, h=h: e.scalar_tensor_tensor(out=prev[h][:], in0=prev[h][:], scalar=dtot[0:64, gc, h:h + 1], in1=pst[0:64, 0:64],
                                                                  op0=ALU.mult, op1=ALU.add), reads=[pstr, dtor, prevr[h]], writes=[prevr[h]])
                kb.op("act", lambda e, h=h: e.copy(out=prevb[h][:], in_=prev[h][:]), reads=[prevr[h]], writes=[prevbr[h]])
        yb_, ybr = yTb.next()
        for q2 in range(N // 512):
            pyt, pytr = pbanks.next()
            for c4 in range(4):
                c = q2 * 4 + c4
                kb.op("pe", lambda e, c=c, c4=c4: e.transpose(pyt[:, c4 * 128:(c4 + 1) * 128], ytile[:, c, :], ident), reads=[ytr, cr], writes=[pytr])
            kb.op("act", lambda e, q2=q2: e.copy(out=yb_[:, q2 * 512:(q2 + 1) * 512], in_=pyt[:, :]), reads=[pytr], writes=[ybr])
        kb.dma("sp", yT_dst[:, t0:t0 + N], yb_[:], reads=[ybr, yT_r])


def emit_lru2(kb, agF2k, agFr, gx, gxr, IX, vec, wab, wxb, outT, outr, pbanks, es=None):
    N = 1024
    vt = kb.sb([128, 8], F32, "lvec", es=es); vr = Res()
    wa = kb.sb([128, 128], F32, "lwa", es=es); war = Res()
    wx = kb.sb([128, 128], F32, "lwx", es=es); wxr = Res()
    kb.dma("sp", vt[:], vec[:, :], writes=[vr])
    kb.dma("sp", wa[:], wab[:, :], writes=[war])
    kb.dma("sp", wx[:], wxb[:, :], writes=[wxr])
    c1 = kb.sb([128, 1], F32, "lc1", es=es); c1r = Res()
    kb.op("act", lambda e: e.activation(out=c1[:], in_=vt[:, 7:8], func=AF.Exp, scale=-1.0), reads=[vr], writes=[c1r])
    kb.op("act", lambda e: e.activation(out=c1[:], in_=c1[:], func=AF.Ln, bias=1.0), reads=[c1r], writes=[c1r])
    kb.op("dve", lambda e: e.tensor_scalar(out=c1[:], in0=c1[:], scalar1=-8.0, scalar2=None, op0=ALU.mult), reads=[c1r], writes=[c1r])

    xfull = kb.sb([128, S + 3], BF16, "lxfull", es=es); xfr = Res()
    yfull = kb.sb([128, S], BF16, "lyfull", es=es); yfr = Res()
    kb.op("pool", lambda e: e.memset(xfull[:, 0:3], 0.0), writes=[xfr])
    for rp in range(4):
        c1_ = IX["lx%d" % rp]; c2_ = IX["ly%d" % rp]
        kb.gather(xfull[:, 3 + rp * 2048:3 + (rp + 1) * 2048], agF2k, gx[:, c1_:c1_ + 1], reads=[gxr, agFr], writes=[xfr])
        kb.gather(yfull[:, rp * 2048:(rp + 1) * 2048], agF2k, gx[:, c2_:c2_ + 1], reads=[gxr, agFr], writes=[yfr])
    obf = Rot(kb, 2, [128, N], BF16, "lob", es=es)
    xc = Rot(kb, 2, [128, N], F32, "lxc", es=es)
    rr = Rot(kb, 2, [128, N], F32, "lr", es=es)
    ii = Rot(kb, 2, [128, N], F32, "li", es=es)
    aa = Rot(kb, 2, [128, N], F32, "la", es=es)
    qq = Rot(kb, 2, [128, N], F32, "lq", es=es)
    hh = Rot(kb, 2, [128, N], F32, "lh", es=es)
    uu = Rot(kb, 2, [128, N], F32, "lu", es=es)
    hprev = None
    for j in range(S // N):
        t0 = j * N
        xt, xr = xfull[:, t0:t0 + N + 3], xfr
        yt, yr = yfull[:, t0:t0 + N], yfr
        ct, cr = xc.next()
        kb.op("dve", lambda e: e.tensor_scalar(out=ct[:], in0=xt[:, 0:N], scalar1=vt[:, 0:1], scalar2=vt[:, 4:5],
                                               op0=ALU.mult, op1=ALU.add), reads=[xr, vr], writes=[cr])
        for k in range(1, 4):
            kb.op("dve", lambda e, k=k: e.scalar_tensor_tensor(out=ct[:], in0=xt[:, k:k + N], scalar=vt[:, k:k + 1], in1=ct[:],
                                                              op0=ALU.mult, op1=ALU.add), reads=[xr, vr, cr], writes=[cr])
        rt, rres = rr.next()
        it, ires = ii.next()
        for hf in range(N // 512):
            sl = slice(hf * 512, (hf + 1) * 512)
            pa, par = pbanks.next()
            kb.op("pe", lambda e: e.matmul(pa[:, :], lhsT=wa[:], rhs=ct[:, sl], start=True, stop=True), reads=[war, cr], writes=[par])
            kb.op("act", lambda e: e.activation(out=rt[:, sl], in_=pa[:, :], func=AF.Sigmoid, bias=vt[:, 5:6]), reads=[par, vr], writes=[rres])
            px, pxr = pbanks.next()
            kb.op("pe", lambda e: e.matmul(px[:, :], lhsT=wx[:], rhs=ct[:, sl], start=True, stop=True), reads=[wxr, cr], writes=[pxr])
            kb.op("act", lambda e: e.activation(out=it[:, sl], in_=px[:, :], func=AF.Sigmoid, bias=vt[:, 6:7]), reads=[pxr, vr], writes=[ires])
        at, ar = aa.next()
        kb.op("act", lambda e: e.activation(out=at[:], in_=rt[:], func=AF.Exp, scale=c1[:, 0:1]), reads=[rres, c1r], writes=[ar])
        qt, qr = qq.next()
        kb.op("pool", lambda e: e.tensor_tensor(out=qt[:], in0=at[:], in1=at[:], op=ALU.mult), reads=[ar], writes=[qr])
        kb.op("pool", lambda e: e.tensor_scalar(out=qt[:], in0=qt[:], scalar1=-1.0, scalar2=1.0, op0=ALU.mult, op1=ALU.add), reads=[qr], writes=[qr])
        kb.op("act", lambda e: e.activation(out=qt[:], in_=qt[:], func=AF.Sqrt), reads=[qr], writes=[qr])
        kb.op("dve", lambda e: e.tensor_tensor(out=it[:], in0=it[:], in1=ct[:], op=ALU.mult), reads=[ires, cr], writes=[ires])
        kb.op("dve", lambda e: e.tensor_tensor(out=it[:], in0=it[:], in1=qt[:], op=ALU.mult), reads=[ires, qr], writes=[ires])
        ht, hr = hh.next()
        if hprev is None:
            kb.op("dve", lambda e: e.tensor_tensor_scan(out=ht[:], data0=at[:], data1=it[:], initial=0.0, op0=ALU.mult, op1=ALU.add),
                  reads=[ar, ires], writes=[hr])
        else:
            hp, hpr = hprev
            kb.op("dve", lambda e: e.tensor_tensor_scan(out=ht[:], data0=at[:], data1=it[:], initial=hp[:, N - 1:N], op0=ALU.mult, op1=ALU.add),
                  reads=[ar, ires, hpr], writes=[hr])
        hprev = (ht, hr)
        ut, ur = uu.next()
        kb.op("pool", lambda e: e.tensor_tensor(out=ut[:], in0=yt[:], in1=yt[:], op=ALU.mult), reads=[yr], writes=[ur])
        kb.op("pool", lambda e: e.tensor_scalar(out=ut[:], in0=ut[:], scalar1=0.044715, scalar2=1.0, op0=ALU.mult, op1=ALU.add), reads=[ur], writes=[ur])
        kb.op("pool", lambda e: e.tensor_tensor(out=ut[:], in0=ut[:], in1=yt[:], op=ALU.mult), reads=[ur, yr], writes=[ur])
        kb.op("act", lambda e: e.activation(out=ut[:], in_=ut[:], func=AF.Sigmoid, scale=1.5957691216057308), reads=[ur], writes=[ur])
        kb.op("pool", lambda e: e.tensor_tensor(out=ut[:], in0=ut[:], in1=yt[:], op=ALU.mult), reads=[ur, yr], writes=[ur])
        ob_, obr_ = obf.next()
        kb.op("pool", lambda e: e.tensor_tensor(out=ob_[:], in0=ut[:], in1=ht[:], op=ALU.mult), reads=[ur, hr], writes=[obr_])
        kb.dma("sp", outT[:, t0:t0 + N], ob_[:], reads=[obr_, outr])


def emit_C2(kb, xT, agN512, agNr, agY2k, agYr, agL2k, agLr, gx, gxr, IX, pT, w_in, pnsa, pssd, plru, w_out, pwg, pwp, rw, ewg, ewu, ewd, cst, x2T, x2r, final_out, banks, es, xTr=None):
    gen = Rot.__new__(Rot); gen.t = banks; gen.i = 0
    sb = lambda shape, dt, name: kb.sb(shape, dt, name, es=es)
    cv = sb([128, CW], F32, "cc"); cvr = Res()
    kb.dma("sp", cv[:], cst[:, :], writes=[cvr])
    ones1k = cv[:, 52:180]; ones512 = cv[:, 180:308]; ident = cv[:, 308:436]
    selE = lambda e: cv[0:16, 436 + e * 128:436 + (e + 1) * 128]
    rb = cv[:, 36:52]

    tmpf = Rot(kb, 4, [128, 512], F32, "ctf", es=es)
    stat = Rot(kb, 3, [128, 512], F32, "cstat", es=es)
    kb.uid += 1
    mixD = kb.dram("mixD%d" % kb.uid, [D, TOK], BF16, "Internal")
    mixDr = [Res() for _ in range(8)]
    wst_box = [None]

    def load_w(src_rows, nk, ncols):
        wt, wr = wst_box[0].next()
        kb.dma("pool", wt[:, 0:nk, 0:ncols], src_rows.rearrange("(c p) f -> p c f", p=128), writes=[wr])
        return wt, wr

    def ln_stats(src, srcr, nchunk, tt, ones_ap, eps, sq_eng="pool"):
        ts = slice(tt * 512, (tt + 1) * 512)
        pm_, pmr = gen.next()
        for c in range(nchunk):
            kb.op("pe", lambda e, c=c: e.matmul(pm_[:, :], lhsT=ones_ap, rhs=src[:, c, ts], start=(c == 0), stop=(c == nchunk - 1)), reads=[cvr, srcr[c]], writes=[pmr])
        pq_, pqr = gen.next()
        for c in range(nchunk):
            sq, sqr = tmpf.next()
            kb.op(sq_eng, lambda e, c=c: e.tensor_tensor(out=sq[:], in0=src[:, c, ts], in1=src[:, c, ts], op=ALU.mult), reads=[srcr[c]], writes=[sqr])
            kb.op("pe", lambda e, c=c: e.matmul(pq_[:, :], lhsT=ones_ap, rhs=sq[:], start=(c == 0), stop=(c == nchunk - 1)), reads=[cvr, sqr], writes=[pqr])
        mean, meanr = stat.next()
        kb.op("act", lambda e: e.copy(out=mean[:], in_=pm_[:, :]), reads=[pmr], writes=[meanr])
        rstd, rstdr = stat.next()
        return mean, meanr, pq_, pqr, rstd, rstdr

    with contextlib.ExitStack() as es1:
        sb1 = lambda shape, dt, name: kb.sb(shape, dt, name, es=es1)
        wst_box[0] = Rot.__new__(Rot); wst_box[0].i = 0
        wst_box[0].t = [(sb1([128, 8, 512], BF16, "cw"), Res()) for _ in range(3)]
        xbf = sb1([128, 8, TOK], BF16, "cxbf"); xbr = [Res() for _ in range(8)]
        for c in range(8):
            kb.dma("pool", xbf[:, c, :], xT[c * 128:(c + 1) * 128, :], reads=([xTr[c]] if xTr else []), writes=[xbr[c]])
        ob = [sb1([128, 4, TOK], BF16, "cob%d" % i) for i in range(3)]
        obr = [[Res() for _ in range(4)] for _ in range(3)]
        for c in range(4):
            for q4 in range(4):
                cn = IX["on%d_%d" % (c, q4)]
                kb.gather(ob[0][:, c, q4 * 512:(q4 + 1) * 512], agN512, gx[:, cn:cn + 1], reads=[gxr, agNr], writes=[obr[0][c]])
            cl = IX["l%d" % c]
            kb.gather(ob[2][:, c, :], agL2k, gx[:, cl:cl + 1], reads=[gxr, agLr], writes=[obr[2][c]])
        with contextlib.ExitStack() as es1b:
            yg = kb.sb([128, 4, TOK], F32, "cyg", es=es1b); ygr = [Res() for _ in range(4)]
            ygb = ob[1]; ygbr = obr[1]
            for c in range(4):
                cy = IX["y%d" % c]
                kb.gather(ygb[:, c, :], agY2k, gx[:, cy:cy + 1], reads=[gxr, agYr], writes=[ygbr[c]])
            wz, wzr = load_w(w_in[:, OFF_Z:OFF_Z + 512], 8, 512)
            for tt in range(NT):
                ts = slice(tt * 512, (tt + 1) * 512)
                for fc in range(4):
                    pz, pzr = gen.next()
                    for c in range(8):
                        kb.op("pe", lambda e, c=c: e.matmul(pz[:, :], lhsT=wz[:, c, fc * 128:(fc + 1) * 128], rhs=xbf[:, c, ts], start=(c == 0), stop=(c == 7)),
                              reads=[wzr, xbr[c]], writes=[pzr])
                    sz, szr = tmpf.next()
                    kb.op("act", lambda e: e.activation(out=sz[:], in_=pz[:, :], func=AF.Silu), reads=[pzr], writes=[szr])
                    kb.op("dve", lambda e, fc=fc: e.tensor_tensor(out=yg[:, fc, ts], in0=ygb[:, fc, ts], in1=sz[:], op=ALU.mult), reads=[szr, ygbr[fc]], writes=[ygr[fc]])
                pq_, pqr = gen.next()
                for c in range(4):
                    sq, sqr = tmpf.next()
                    kb.op("pool", lambda e, c=c: e.tensor_tensor(out=sq[:], in0=yg[:, c, ts], in1=yg[:, c, ts], op=ALU.mult), reads=[ygr[c]], writes=[sqr])
                    kb.op("pe", lambda e, c=c: e.matmul(pq_[:, :], lhsT=ones512, rhs=sq[:], start=(c == 0), stop=(c == 3)), reads=[cvr, sqr], writes=[pqr])
                rstd, rstdr = stat.next()
                kb.op("dve", lambda e: e.tensor_scalar(out=rstd[:], in0=pq_[:, :], scalar1=1e-5, scalar2=None, op0=ALU.add), reads=[pqr], writes=[rstdr])
                kb.op("act", lambda e: e.activation(out=rstd[:], in_=rstd[:], func=AF.Sqrt), reads=[rstdr], writes=[rstdr])
                kb.op("dve", lambda e: e.reciprocal(out=rstd[:], in_=rstd[:]), reads=[rstdr], writes=[rstdr])
                for c in range(4):
                    t1, t1r = tmpf.next()
                    kb.op("dve", lambda e, c=c: e.tensor_tensor(out=t1[:], in0=yg[:, c, ts], in1=rstd[:], op=ALU.mult), reads=[ygr[c], rstdr], writes=[t1r])
                    kb.op("act", lambda e, c=c: e.activation(out=ob[1][:, c, ts], in_=t1[:], func=AF.Copy, scale=cv[:, 32 + c:33 + c]), reads=[t1r, cvr], writes=[obr[1][c]])
            kb.barrier()
        if CDBG == "ossd":
            dbg = sb1([128, 4, TOK], F32, "cdbg"); dbgr = Res()
            for c in range(4):
                kb.op("dve", lambda e, c=c: e.tensor_copy(out=dbg[:, c, :], in_=ob[1][:, c, :]), reads=[obr[1][c]], writes=[dbgr])
                kb.dma("sp", x2T[c * 128:(c + 1) * 128, :], dbg[:, c, :], reads=[dbgr], final=True)
            kb.barrier()
            return
        projs = [pnsa, pssd, plru]
        mixh = sb1([128, 4, TOK], BF16, "cmixh"); mixhr = [Res() for _ in range(4)]
        for half in range(2):
            fs = slice(half * 512, (half + 1) * 512)
            for br in range(3):
                wp, wpr = load_w(projs[br][:, fs], 4, 512)
                wm, wmr = load_w(w_in[:, OFF_MERGE + br * 1024 + half * 512: OFF_MERGE + br * 1024 + (half + 1) * 512], 8, 512)
                for fc in range(4):
                    oc = half * 4 + fc
                    for tt in range(NT):
                        ts = slice(tt * 512, (tt + 1) * 512)
                        pg, pgr = gen.next()
                        for c in range(8):
                            kb.op("pe", lambda e, c=c: e.matmul(pg[:, :], lhsT=wm[:, c, fc * 128:(fc + 1) * 128], rhs=xbf[:, c, ts], start=(c == 0), stop=(c == 7)),
                                  reads=[wmr, xbr[c]], writes=[pgr])
                        pp, ppr = gen.next()
                        for c in range(4):
                            kb.op("pe", lambda e, c=c: e.matmul(pp[:, :], lhsT=wp[:, c, fc * 128:(fc + 1) * 128], rhs=ob[br][:, c, ts], start=(c == 0), stop=(c == 3)),
                                  reads=[wpr, obr[br][c]], writes=[ppr])
                        sg, sgr = tmpf.next()
                        kb.op("act", lambda e: e.activation(out=sg[:], in_=pg[:, :], func=AF.Sigmoid), reads=[pgr], writes=[sgr])
                        if br == 0:
                            kb.op("dve", lambda e: e.tensor_tensor(out=mixh[:, fc, ts], in0=pp[:, :], in1=sg[:], op=ALU.mult), reads=[ppr, sgr], writes=[mixhr[fc]])
                        else:
                            kb.op("dve", lambda e: e.tensor_tensor(out=sg[:], in0=pp[:, :], in1=sg[:], op=ALU.mult), reads=[ppr, sgr], writes=[sgr])
                            kb.op("pool", lambda e: e.tensor_tensor(out=mixh[:, fc, ts], in0=mixh[:, fc, ts], in1=sg[:], op=ALU.add), reads=[sgr, mixhr[fc]], writes=[mixhr[fc]])
            for fc in range(4):
                kb.dma("sp", mixD[(half * 4 + fc) * 128:(half * 4 + fc + 1) * 128, :], mixh[:, fc, :], reads=[mixhr[fc]], writes=[mixDr[half * 4 + fc]])
        kb.barrier()

    with contextlib.ExitStack() as es2:
        sb2 = lambda shape, dt, name: kb.sb(shape, dt, name, es=es2)
        r = sb2([128, 8, TOK], F32, "cr"); rr = [Res() for _ in range(8)]
        x1b = sb2([128, 8, TOK], BF16, "cx1b"); x1br = [Res() for _ in range(8)]
        for c in range(8):
            kb.dma("sp", r[:, c, :], xT[c * 128:(c + 1) * 128, :], reads=([xTr[c]] if xTr else []), writes=[rr[c]])
        gatesT = sb2([16, TOK], F32, "cgT"); gTr = Res()
        rwt = sb2([128, 8, 16], F32, "crw"); rwr = Res()
        kb.dma("sp", rwt[:], rw.rearrange("(c p) e -> p c e", p=128), writes=[rwr])
        pad = sb2([128, 4, 8], F32, "cpad"); padr = Res()
        kb.op("pool", lambda e: e.memset(pad[:], -1e30), writes=[padr])
        rt = Rot(kb, 2, [128, 16], F32, "crt", es=es2)
        rt2 = Rot(kb, 2, [128, 16], F32, "crt2", es=es2)
        t8 = Rot(kb, 2, [128, 4, 8], F32, "ct8", es=es2)
        sm4 = Rot(kb, 4, [128, 4], F32, "csm4", es=es2)
        sm1 = Rot(kb, 4, [128, 1], F32, "csm1", es=es2)
        t8b = Rot(kb, 2, [128, 8], F32, "ct8b", es=es2)
        es2a = contextlib.ExitStack()
        wst_box[0] = Rot.__new__(Rot); wst_box[0].i = 0
        wst_box[0].t = [(kb.sb([128, 8, 512], BF16, "cw2", es=es2a), Res()) for _ in range(2)]
        mixed = kb.sb([128, 8, TOK], BF16, "cmixed", es=es2a); mixr = [Res() for _ in range(8)]
        pb_ = kb.sb([128, 2, TOK], BF16, "cpb", es=es2a); pbr = [Res() for _ in range(2)]
        for c in range(8):
            kb.dma("sp", mixed[:, c, :], mixD[c * 128:(c + 1) * 128, :], reads=[mixDr[c]], writes=[mixr[c]])
        if CDBG == "mixed":
            for c in range(8):
                kb.op("dve", lambda e, c=c: e.tensor_copy(out=r[:, c, :], in_=mixed[:, c, :]), reads=[mixr[c], rr[c]], writes=[rr[c]])
                kb.dma("sp", x2T[c * 128:(c + 1) * 128, :], r[:, c, :], reads=[rr[c]], final=True)
            kb.barrier(); es2a.close()
            return
        for half in range(2):
            wo, wor = load_w(w_out[:, half * 512:(half + 1) * 512], 8, 512)
            for fc in range(4):
                oc = half * 4 + fc
                for tt in range(NT):
                    ts = slice(tt * 512, (tt + 1) * 512)
                    pu, pur = gen.next()
                    for c in range(8):
                        kb.op("pe", lambda e, c=c: e.matmul(pu[:, :], lhsT=wo[:, c, fc * 128:(fc + 1) * 128], rhs=mixed[:, c, ts], start=(c == 0), stop=(c == 7)),
                              reads=[wor, mixr[c]], writes=[pur])
                    kb.op("dve", lambda e: e.scalar_tensor_tensor(out=r[:, oc, ts], in0=r[:, oc, ts], scalar=ALPHA, in1=pu[:, :], op0=ALU.mult, op1=ALU.add),
                          reads=[pur, rr[oc]], writes=[rr[oc]])

        def layer_norm(src, srcr, gcol, bcol, dst_b, dst_br):
            for tt in range(NT):
                ts = slice(tt * 512, (tt + 1) * 512)
                mean, meanr, pq_, pqr, rstd, rstdr = ln_stats(src, srcr, 8, tt, ones1k, 1e-5)
                m2, m2r = stat.next()
                kb.op("pool", lambda e: e.tensor_tensor(out=m2[:], in0=mean[:], in1=mean[:], op=ALU.mult), reads=[meanr], writes=[m2r])
                kb.op("dve", lambda e: e.scalar_tensor_tensor(out=rstd[:], in0=pq_[:, :], scalar=1e-5, in1=m2[:], op0=ALU.add, op1=ALU.subtract), reads=[pqr, m2r], writes=[rstdr])
                kb.op("act", lambda e: e.activation(out=rstd[:], in_=rstd[:], func=AF.Sqrt), reads=[rstdr], writes=[rstdr])
                kb.op("dve", lambda e: e.reciprocal(out=rstd[:], in_=rstd[:]), reads=[rstdr], writes=[rstdr])
                for c in range(8):
                    kb.op("dve", lambda e, c=c: e.tensor_tensor(out=src[:, c, ts], in0=src[:, c, ts], in1=mean[:], op=ALU.subtract), reads=[meanr, srcr[c]], writes=[srcr[c]])
                    kb.op("pool", lambda e, c=c: e.tensor_tensor(out=src[:, c, ts], in0=src[:, c, ts], in1=rstd[:], op=ALU.mult), reads=[rstdr, srcr[c]], writes=[srcr[c]])
                    kb.op("dve", lambda e, c=c: e.tensor_scalar(out=src[:, c, ts], in0=src[:, c, ts], scalar1=cv[:, gcol + c:gcol + c + 1], scalar2=cv[:, bcol + c:bcol + c + 1],
                                                               op0=ALU.mult, op1=ALU.add), reads=[cvr, srcr[c]], writes=[srcr[c]])
                    if dst_b is not None:
                        kb.op("act", lambda e, c=c: e.copy(out=dst_b[:, c, ts], in_=src[:, c, ts]), reads=[srcr[c]], writes=[dst_br[c]])

        layer_norm(r, rr, 0, 8, x1b, x1br)
        if CDBG == "x1":
            for c in range(8):
                kb.dma("sp", x2T[c * 128:(c + 1) * 128, :], r[:, c, :], reads=[rr[c]], final=True)
            kb.barrier(); es2a.close()
            return

        for s_ in range(TOK // 128):
            ss = slice(s_ * 128, (s_ + 1) * 128)
            pl, plr = gen.next()
            for c in range(8):
                kb.op("pe", lambda e, c=c: e.matmul(pl[:, 0:16], lhsT=r[:, c, ss], rhs=rwt[:, c, :], start=(c == 0), stop=(c == 7)), reads=[rr[c], rwr], writes=[plr])
            aff, affr = rt.next()
            kb.op("act", lambda e: e.activation(out=aff[:], in_=pl[:, 0:16], func=AF.Sigmoid), reads=[plr], writes=[affr])
            sel, selr = rt2.next()
            kb.op("dve", lambda e: e.tensor_tensor(out=sel[:], in0=aff[:], in1=rb, op=ALU.add), reads=[affr, cvr], writes=[selr])
            kb.op("dve", lambda e: e.tensor_copy(out=pad[:, :, 0:4], in_=sel[:].rearrange("p (g k) -> p g k", g=4)), reads=[selr, padr], writes=[padr])
            tp, tpr = t8.next()
            for g in range(4):
                kb.op("dve", lambda e, g=g: e.max(out=tp[:, g, :], in_=pad[:, g, :]), reads=[padr], writes=[tpr])
            gs, gsr = sm4.next()
            kb.op("dve", lambda e: e.tensor_tensor(out=gs[:], in0=tp[:, :, 0], in1=tp[:, :, 1], op=ALU.add), reads=[tpr], writes=[gsr])
            gm, gmr = sm1.next()
            kb.op("dve", lambda e: e.reduce_max(out=gm[:], in_=gs[:], axis=AX.X), reads=[gsr], writes=[gmr])
            isb, isbr = sm4.next()
            kb.op("dve", lambda e: e.tensor_scalar(out=isb[:], in0=gs[:], scalar1=gm[:, 0:1], scalar2=None, op0=ALU.is_ge), reads=[gsr, gmr], writes=[isbr])
            off, offr = sm4.next()
            kb.op("dve", lambda e: e.tensor_scalar(out=off[:], in0=isb[:], scalar1=1e9, scalar2=-1e9, op0=ALU.mult, op1=ALU.add), reads=[isbr], writes=[offr])
            msk, mskr = rt2.next()
            for g in range(4):
                kb.op("dve", lambda e, g=g: e.tensor_scalar(out=msk[:, g * 4:(g + 1) * 4], in0=sel[:, g * 4:(g + 1) * 4], scalar1=isb[:, g:g + 1], scalar2=off[:, g:g + 1],
                                                            op0=ALU.mult, op1=ALU.add), reads=[selr, isbr, offr], writes=[mskr])
            tb, tbr = t8b.next()
            kb.op("dve", lambda e: e.max(out=tb[:], in_=msk[:]), reads=[mskr], writes=[tbr])
            kb.op("dve", lambda e: e.tensor_scalar(out=msk[:], in0=msk[:], scalar1=tb[:, 1:2], scalar2=None, op0=ALU.is_ge), reads=[mskr, tbr], writes=[mskr])
            kb.op("dve", lambda e: e.tensor_tensor(out=msk[:], in0=msk[:], in1=aff[:], op=ALU.mult), reads=[mskr, affr], writes=[mskr])
            ws, wsr = sm1.next()
            kb.op("dve", lambda e: e.reduce_sum(out=ws[:], in_=msk[:], axis=AX.X), reads=[mskr], writes=[wsr])
            kb.op("dve", lambda e: e.reciprocal(out=ws[:], in_=ws[:]), reads=[wsr], writes=[wsr])
            kb.op("dve", lambda e: e.tensor_scalar(out=msk[:], in0=msk[:], scalar1=ws[:, 0:1], scalar2=None, op0=ALU.mult), reads=[mskr, wsr], writes=[mskr])
            pt, ptr_ = gen.next()
            kb.op("pe", lambda e: e.transpose(pt[0:16, 0:128], msk[:], ident), reads=[mskr, cvr], writes=[ptr_])
            kb.op("act", lambda e: e.copy(out=gatesT[:, ss], in_=pt[0:16, 0:128]), reads=[ptr_], writes=[gTr])

        for c in range(2):
            kb.dma("pool", pb_[:, c, :], pT[c * 128:(c + 1) * 128, :], writes=[pbr[c]])
        for half in range(2):
            fs = slice(half * 512, (half + 1) * 512)
            wg_, wgr = load_w(pwg[:, fs], 8, 512)
            wp_, wpr = load_w(pwp[:, fs], 2, 512)
            for fc in range(4):
                oc = half * 4 + fc
                for tt in range(NT):
                    ts = slice(tt * 512, (tt + 1) * 512)
                    pg, pgr = gen.next()
                    for c in range(8):
                        kb.op("pe", lambda e, c=c: e.matmul(pg[:, :], lhsT=wg_[:, c, fc * 128:(fc + 1) * 128], rhs=x1b[:, c, ts], start=(c == 0), stop=(c == 7)),
                              reads=[wgr, x1br[c]], writes=[pgr])
                    pp, ppr = gen.next()
                    for c in range(2):
                        kb.op("pe", lambda e, c=c: e.matmul(pp[:, :], lhsT=wp_[:, c, fc * 128:(fc + 1) * 128], rhs=pb_[:, c, ts], start=(c == 0), stop=(c == 1)),
                              reads=[wpr, pbr[c]], writes=[ppr])
                    sg, sgr = tmpf.next()
                    kb.op("act", lambda e: e.activation(out=sg[:], in_=pg[:, :], func=AF.Sigmoid), reads=[pgr], writes=[sgr])
                    kb.op("dve", lambda e: e.tensor_tensor(out=sg[:], in0=pp[:, :], in1=sg[:], op=ALU.mult), reads=[ppr, sgr], writes=[sgr])
                    kb.op("dve", lambda e: e.scalar_tensor_tensor(out=r[:, oc, ts], in0=r[:, oc, ts], scalar=ALPHA, in1=sg[:], op0=ALU.mult, op1=ALU.add),
                          reads=[sgr, rr[oc]], writes=[rr[oc]])

        kb.barrier()
        es2a.close()
        if CDBG == "ple":
            for c in range(8):
                kb.dma("sp", x2T[c * 128:(c + 1) * 128, :], r[:, c, :], reads=[rr[c]], final=True)
            return
        ew = Rot(kb, 2, [128, 2, 3, 8, 256], BF16, "cew", es=es2)
        hb = Rot(kb, 2, [128, 4, 512], BF16, "chb", es=es2)
        steps = [(ep, tt) for ep in range(8) for tt in range(NT)]
        live = {}
        wts = {}

        def load_pair(ep):
            wt, wr = ew.next()
            for m in range(2):
                e_ = ep * 2 + m
                kb.dma("pool", wt[:, m, 0, :, :], ewg[e_, :, :].rearrange("(c p) f -> p c f", p=128), writes=[wr])
                kb.dma("pool", wt[:, m, 1, :, :], ewu[e_, :, :].rearrange("(c p) f -> p c f", p=128), writes=[wr])
                kb.dma("pool", wt[:, m, 2, :, :].rearrange("p (c a) f -> p c (a f)", c=2), ewd[e_, :, :].rearrange("(c p) f -> p c f", p=128), writes=[wr])
            return wt, wr

        for idx in range(len(steps) + 1):
            if idx < len(steps):
                ep, tt = steps[idx]
                if tt == 0:
                    if ep == 0:
                        wts[0] = load_pair(0)
                    wt, wr = wts[ep]
                ts = slice(tt * 512, (tt + 1) * 512)
                h, hr = hb.next()
                for m in range(2):
                    e_ = ep * 2 + m
                    pgb, pgbr = gen.next()
                    kb.op("pe", lambda e, e_=e_, pgb=pgb, ts=ts: e.matmul(pgb[:, :], lhsT=selE(e_), rhs=gatesT[:, ts], start=True, stop=True), reads=[cvr, gTr], writes=[pgbr])
                    gb, gbr = tmpf.next()
                    kb.op("act", lambda e, gb=gb, pgb=pgb: e.copy(out=gb[:], in_=pgb[:, :]), reads=[pgbr], writes=[gbr])
                    for fc in range(2):
                        pg, pgr = gen.next()
                        for c in range(8):
                            kb.op("pe", lambda e, c=c, m=m, fc=fc, pg=pg, wt=wt, ts=ts: e.matmul(pg[:, :], lhsT=wt[:, m, 0, c, fc * 128:(fc + 1) * 128], rhs=x1b[:, c, ts], start=(c == 0), stop=(c == 7)),
                                  reads=[wr, x1br[c]], writes=[pgr])
                        pu, pur = gen.next()
                        for c in range(8):
                            kb.op("pe", lambda e, c=c, m=m, fc=fc, pu=pu, wt=wt, ts=ts: e.matmul(pu[:, :], lhsT=wt[:, m, 1, c, fc * 128:(fc + 1) * 128], rhs=x1b[:, c, ts], start=(c == 0), stop=(c == 7)),
                                  reads=[wr, x1br[c]], writes=[pur])
                        sg, sgr = tmpf.next()
                        kb.op("act", lambda e, sg=sg, pg=pg: e.activation(out=sg[:], in_=pg[:, :], func=AF.Silu), reads=[pgr], writes=[sgr])
                        kb.op("dve", lambda e, sg=sg, pu=pu: e.tensor_tensor(out=sg[:], in0=pu[:, :], in1=sg[:], op=ALU.mult), reads=[pur, sgr], writes=[sgr])
                        kb.op("pool", lambda e, m=m, fc=fc, h=h, sg=sg, gb=gb: e.tensor_tensor(out=h[:, m * 2 + fc, :], in0=sg[:], in1=gb[:], op=ALU.mult), reads=[sgr, gbr], writes=[hr])
                live[idx] = (wt, wr, h, hr, ts)
            if idx >= 1:
                wt_, wr_, h_, hr_, ts_ = live.pop(idx - 1)
                for dc in range(8):
                    py, pyr = gen.next()
                    for m in range(2):
                        wd = wt_[:, m, 2, :, :].rearrange("p (c a) f -> p c (a f)", c=2)
                        for fc in range(2):
                            kb.op("pe", lambda e, wd=wd, m=m, fc=fc, py=py, h_=h_, dc=dc: e.matmul(py[:, :], lhsT=wd[:, fc, dc * 128:(dc + 1) * 128], rhs=h_[:, m * 2 + fc, :],
                                                                                                  start=(m == 0 and fc == 0), stop=(m == 1 and fc == 1)), reads=[wr_, hr_], writes=[pyr])
                    kb.op("dve", lambda e, dc=dc, py=py, ts_=ts_: e.tensor_tensor(out=r[:, dc, ts_], in0=r[:, dc, ts_], in1=py[:, :], op=ALU.add), reads=[pyr, rr[dc]], writes=[rr[dc]])
            if idx < len(steps) and steps[idx][1] == 0 and steps[idx][0] + 1 < 8:
                wts[steps[idx][0] + 1] = load_pair(steps[idx][0] + 1)

        if CDBG != "moe":
            layer_norm(r, rr, 16, 24, None, None)
        for c in range(8):
            kb.dma("sp", x2T[c * 128:(c + 1) * 128, :], r[:, c, :], reads=[rr[c]], writes=[x2r[c]], final=final_out)
        kb.barrier()


NF = 3104
NOM_TILES = [0, 2, 4, 6, 8, 10, 12, 14]
BATCH = 2
T_ALL = BATCH * S
NCORES = 8
U32 = mybir.dt.uint32


def idx_cols():
    names = []
    for nm in ("kcmp", "vcmp", "ksel", "vsel", "kwin", "vwin", "sx", "sB", "sC", "dt", "lx", "ly"):
        names += [nm + str(rp) for rp in range(4)]
    for k in range(8):
        names += ["q%d_%d" % (k, j) for j in range(4)]
        names.append("gate%d" % k)
    for c in range(4):
        names += ["on%d_%d" % (c, q4) for q4 in range(4)]
        names.append("y%d" % c)
        names.append("l%d" % c)
    return {n: i for i, n in enumerate(names)}


IX = idx_cols()
NIDX = len(IX)


def make_gidx(r):
    g, par = r // 2, r % 2
    t = np.zeros((128, NIDX), np.int64)
    p = np.arange(128)
    def rowF(rp, f):
        f = np.asarray(f)
        k = f // 256
        rows_k = np.where(k < 12, 256, 32)
        return k * 1024 + rp * rows_k + (f - 256 * k)
    rowN = lambda rank, f256: (f256 // 128) * 512 + rank * 128 + f256 % 128
    rowY = lambda rank, ch: (ch // 64) * 256 + rank * 64 + ch % 64
    for rp in range(4):
        for nm, f0 in (("kcmp", 512), ("vcmp", 640), ("ksel", 768), ("vsel", 896), ("kwin", 1024), ("vwin", 1152)):
            t[:, IX[nm + str(rp)]] = rowF(rp, f0 + 64 * g + p)
        t[:, IX["sx%d" % rp]] = rowF(rp, 1304 + 128 * r + p)
        t[:, IX["sB%d" % rp]] = rowF(rp, 1304 + 512 + 64 * g + p)
        t[:, IX["sC%d" % rp]] = rowF(rp, 1304 + 640 + 64 * g + p)
        t[:, IX["dt%d" % rp]] = rp * 8 + 2 * r + p
        t[:, IX["lx%d" % rp]] = rowF(rp, 2080 + 128 * r + p)
        t[:, IX["ly%d" % rp]] = rowF(rp, 2080 + 512 + 128 * r + p)
    for k in range(8):
        i = 2 * k + par
        rp, q4 = i // 4, i % 4
        for j in range(4):
            t[:, IX["q%d_%d" % (k, j)]] = rowF(rp, g * 256 + j * 64 + p) * 4 + q4
        t[:, IX["gate%d" % k]] = rowF(rp, 1280 + 12 * g + p) * 4 + q4
    for c in range(4):
        gg, f256 = c // 2, (c % 2) * 128 + p
        for q4 in range(4):
            i = 4 * r + q4
            t[:, IX["on%d_%d" % (c, q4)]] = rowN(2 * gg + i % 2, f256) * 8 + i // 2
        t[:, IX["y%d" % c]] = rowY(c, p) * 4 + r
        t[:, IX["l%d" % c]] = rowY(c, p) * 4 + r
    t = np.clip(t, 0, None)
    return t.astype(np.uint32)


def emit_A2(kb, xT, xTr, w, agF, agFr, agD, agDr, banks, es):
    xs = kb.sb([128, 8, TOK], BF16, "xs", es=es)
    xs_r = [Res("xs") for _ in range(8)]
    for c in range(8):
        kb.dma("pool", xs[:, c, :], xT[c * 128:(c + 1) * 128, :], reads=([xTr[c]] if xTr else []), writes=[xs_r[c]])
    FB = 512
    wbuf = [(kb.sb([128, 8, FB], BF16, "wb", es=es), Res("wb")) for _ in range(2)]
    obuf = [(kb.sb([128, 512], BF16, "ob", es=es), Res("ob")) for _ in range(4)]
    obf = [(kb.sb([8, 512], F32, "obd", es=es), Res("obd")) for _ in range(2)]
    wv = w.rearrange("(c p) f -> p c f", p=128)
    it = 0
    nb = 0
    for (c0, ncols, r0) in ((0, 1304, 0), (1816, 1800, 1304)):
        for fb in range((ncols + FB - 1) // FB):
            f0 = fb * FB
            fw_ = min(FB, ncols - f0)
            wt, wr = wbuf[nb % 2]
            nb += 1
            kb.dma("pool", wt[:, :, :fw_], wv[:, :, c0 + f0:c0 + f0 + fw_], writes=[wr])
            for fc in range((fw_ + 127) // 128):
                m = min(128, fw_ - fc * 128)
                for tt in range(TOK // 512):
                    pt, pr = banks[it % 4]
                    ot, orr = obuf[it % 4]
                    for c in range(8):
                        kb.op("pe", lambda e, c=c: e.matmul(pt[:m, :], lhsT=wt[:, c, fc * 128:fc * 128 + m], rhs=xs[:, c, tt * 512:(tt + 1) * 512],
                                                             start=(c == 0), stop=(c == 7)), reads=[wr, xs_r[c]], writes=[pr])
                    if it % 2 == 0:
                        kb.op("act", lambda e: e.copy(out=ot[:m, :], in_=pt[:m, :]), reads=[pr], writes=[orr])
                    else:
                        kb.op("dve", lambda e: e.tensor_copy(out=ot[:m, :], in_=pt[:m, :]), reads=[pr], writes=[orr])
                    row = r0 + f0 + fc * 128
                    kb.dma("sp", agF[row:row + m, tt * 512:(tt + 1) * 512], ot[:m, :], reads=[orr, agFr])
                    it += 1
    wt, wr = wbuf[nb % 2]
    kb.dma("pool", wt[:, :, 0:8], wv[:, :, 2584:2592], writes=[wr])
    for tt in range(TOK // 512):
        pt, pr = banks[it % 4]
        it += 1
        for c in range(8):
            kb.op("pe", lambda e, c=c: e.matmul(pt[0:8, :], lhsT=wt[:, c, 0:8], rhs=xs[:, c, tt * 512:(tt + 1) * 512], start=(c == 0), stop=(c == 7)),
                  reads=[wr, xs_r[c]], writes=[pr])
        od, odr = obf[tt % 2]
        kb.op("act", lambda e: e.copy(out=od[:, :], in_=pt[0:8, :]), reads=[pr], writes=[odr])
        kb.dma("sp", agD[:, tt * 512:(tt + 1) * 512], od[:, :], reads=[odr, agDr])


def build_fused(nlayers=2, groups=None, ncores=8, fstop=9, ntiles=8):
    groups = groups or [[0, 1, 2, 3], [4, 5, 6, 7]]
    nc = bass.Bass("TRN2", target_bir_lowering=False)
    es = contextlib.ExitStack()
    with es:
        kb = KB(nc, es)
        I = lambda n, s, dt=F32: kb.dram(n, s, dt, "ExternalInput")
        xT0 = I("xT0", [D, TOK]); pT = I("pT", [2, 256, TOK]); gidx = I("gidx", [128, NIDX], U32)
        w_in = I("w_in", [2, D, 6688])
        peT = I("npeT", [2, 2, 64, 32]); w1 = I("nw1", [2, 2, 2048, 256]); w2 = I("nw2", [2, 2, 256, 64])
        nmask = I("nmask", [128, NMASK, 512]); nE0 = I("nE0", [128, S]); nmisc = I("nmisc", [128, MISC_W])
        svec = I("svec", [2, 128, 16]); srep = I("srep", [2, 128, 8]); scst = I("scst", [128, 4, 128])
        lvec = I("lvec", [2, 128, 8]); lwab = I("lwab", [2, 128, 128]); lwxb = I("lwxb", [2, 128, 128])
        pnsa = I("proj_nsa", [2, 512, D]); pssd = I("proj_ssd", [2, 512, D]); plru = I("proj_lru", [2, 512, D])
        w_out = I("w_out", [2, D, D]); pwg = I("ple_w_gate", [2, D, D]); pwp = I("ple_w_proj", [2, 256, D]); rw = I("router_w", [D, 16])
        ewg = I("exp_w_gate", [2, 16, D, 256]); ewu = I("exp_w_up", [2, 16, D, 256]); ewd = I("exp_w_down", [2, 16, 256, D])
        ccst = I("ccst", [2, 128, CW])
        x2T = kb.dram("x2T", [D, TOK], F32, "ExternalOutput")
        N_ = lambda n, s, dt: kb.dram(n, s, dt, "Internal")
        agF_in = N_("agF_in", [NF, TOK], BF16); agF_out = N_("agF_out", [4 * NF, TOK], BF16)
        agD_in = N_("agD_in", [8, TOK], F32); agD_out = N_("agD_out", [32, TOK], F32)
        nq = 8 * QT
        agN_in = N_("agN_in", [256, nq], BF16); agN_out = N_("agN_out", [1024, nq], BF16)
        agY_in = N_("agY_in", [128, S], BF16); agY_out = N_("agY_out", [512, S], BF16)
        agL_in = N_("agL_in", [128, S], BF16); agL_out = N_("agL_out", [512, S], BF16)
        xcur = N_("xcur", [D, TOK], F32)
        rF_in, rF_out, rD_in, rD_out = Res(), Res(), Res(), Res()
        rN_in, rN_out, rY_in, rY_out, rL_in, rL_out = Res(), Res(), Res(), Res(), Res(), Res()
        xcur_r = [Res() for _ in range(8)]
        agF512 = agF_out.rearrange("r (q t) -> (r q) t", t=512)
        agN512 = agN_out.rearrange("r (q t) -> (r q) t", t=512)
        agY2k = agY_out.rearrange("r (q t) -> (r q) t", t=2048)
        agL2k = agL_out.rearrange("r (q t) -> (r q) t", t=2048)
        gx = kb.sb([128, NIDX], U32, "gx"); gxr = Res()
        kb.dma("sp", gx[:], gidx[:, :], writes=[gxr])
        banks = [(kb.ps([128, 512], F32, "bank"), Res("bank", excl=True)) for _ in range(8)]
        rot = lambda items: _rot(items)
        for L in range(nlayers):
            xin = xT0 if L == 0 else xcur
            xin_r = None if L == 0 else xcur_r
            with contextlib.ExitStack() as sa:
                emit_A2(kb, xin, xin_r, w_in[L], agF_in, rF_in, agD_in, rD_in, banks, sa)
                kb.barrier()
            for k in range(13):
                rows = 256 if k < 12 else 32
                kb.allgather(agF_in[k * 256:k * 256 + rows, :], agF_out[k * 1024:k * 1024 + 4 * rows, :], groups, writes=[rF_in, rF_out])
            kb.allgather(agD_in[:, :], agD_out[:, :], groups, writes=[rD_in, rD_out])
            if fstop <= 1:
                break
            with contextlib.ExitStack() as s1:
                emit_nsa2(kb, NOM_TILES[:ntiles], agF_out, agF512, rF_out, gx, gxr, IX, peT[L], w1[L], w2[L], nmask, nE0, nmisc, agN_in, rN_in,
                          banks[0:4], banks[4], rot(banks[5:8]), s1)
                kb.barrier()
            for k in range(2):
                kb.allgather(agN_in[k * 128:(k + 1) * 128, :], agN_out[k * 512:(k + 1) * 512, :], groups, writes=[rN_in, rN_out])
            if fstop <= 2:
                break
            with contextlib.ExitStack() as s2:
                pbf = [(banks[6][0][:, 0:32].bitcast(BF16), banks[6][1]), (banks[7][0][:, 0:32].bitcast(BF16), banks[7][1])]
                emit_ssd2(kb, agF_out, rF_out, agD_out, rD_out, gx, gxr, IX, svec[L], srep[L], scst, agY_in, rY_in, rot(banks[0:6]), pbf, es=s2)
                kb.barrier()
            for k in range(2):
                kb.allgather(agY_in[k * 64:(k + 1) * 64, :], agY_out[k * 256:(k + 1) * 256, :], groups, writes=[rY_in, rY_out])
            if fstop <= 3:
                break
            with contextlib.ExitStack() as s3:
                emit_lru2(kb, agF_out, rF_out, gx, gxr, IX, lvec[L], lwab[L], lwxb[L], agL_in, rL_in, rot(banks[0:4]), es=s3)
                kb.barrier()
            for k in range(2):
                kb.allgather(agL_in[k * 64:(k + 1) * 64, :], agL_out[k * 256:(k + 1) * 256, :], groups, writes=[rL_in, rL_out])
            if fstop <= 4:
                break
            last = (L == nlayers - 1)
            with contextlib.ExitStack() as s4:
                emit_C2(kb, xin, agN512, rN_out, agY2k, rY_out, agL2k, rL_out, gx, gxr, IX, pT[L], w_in[L], pnsa[L], pssd[L], plru[L], w_out[L],
                        pwg[L], pwp[L], rw, ewg[L], ewu[L], ewd[L], ccst[L], x2T if last else xcur, xcur_r, last, banks, s4, xTr=xin_r)
                kb.barrier()
        kb.finish(())
        print("fused instructions", kb.ninst, "sems", kb.nsem)
    return nc


def _rot(items):
    r = Rot.__new__(Rot)
    r.t = list(items)
    r.i = 0
    return r


def fused_inputs(d, core):
    b, r = core // 4, core % 4
    g, par = r // 2, r % 2
    tok = slice(core * TOK, (core + 1) * TOK)
    x = d["x"].reshape(T_ALL, D)
    im = {"xT0": np.ascontiguousarray(x[tok].T),
          "pT": np.ascontiguousarray(np.stack([d["p"][L].reshape(T_ALL, 256)[tok].T for L in range(2)])),
          "gidx": make_gidx(r), "w_in": d["w_in"],
          "npeT": np.ascontiguousarray(np.stack([np.stack([d["nsa_pe_k"][L].T, d["nsa_pe_v"][L].T]) for L in range(2)])),
          "nw1": np.stack([np.stack([d["nsa_w1_k"][L], d["nsa_w1_v"][L]]) for L in range(2)]),
          "nw2": np.stack([np.stack([d["nsa_w2_k"][L], d["nsa_w2_v"][L]]) for L in range(2)]),
          "proj_nsa": d["proj_nsa"], "proj_ssd": d["proj_ssd"], "proj_lru": d["proj_lru"], "w_out": d["w_out"],
          "ple_w_gate": d["ple_w_gate"], "ple_w_proj": d["ple_w_proj"], "router_w": d["router_w"],
          "exp_w_gate": d["exp_w_gate"], "exp_w_up": d["exp_w_up"], "exp_w_down": d["exp_w_down"],
          "ccst": np.stack([c_consts(d, L) for L in range(2)]), "scst": ssd_consts()}
    im.update(nsa_consts(par))
    sv, sr, lv, la, lx_ = [], [], [], [], []
    for L in range(2):
        a_, b_ = ssd_vecs(d, L, r)
        sv.append(a_); sr.append(b_)
        a_, b_, c_ = lru_vecs(d, L, r)
        lv.append(a_); la.append(b_); lx_.append(c_)
    im["svec"] = np.stack(sv); im["srep"] = np.stack(sr); im["lvec"] = np.stack(lv); im["lwab"] = np.stack(la); im["lwxb"] = np.stack(lx_)
    return im


def ssd_vecs(d, layer, r):
    g = r // 2
    cw = d["ssd_conv_w"][layer]; cb = d["ssd_conv_b"][layer]
    v = np.zeros((128, 16), np.float32)
    xs_ = slice(128 * r, 128 * r + 128); Bs_ = slice(512 + 64 * g, 512 + 64 * g + 64); Cs_ = slice(640 + 64 * g, 640 + 64 * g + 64)
    v[:, 0:4] = cw[:, xs_].T; v[:, 4] = cb[xs_]
    v[:64, 5:9] = cw[:, Bs_].T; v[:64, 9] = cb[Bs_]
    v[:64, 10:14] = cw[:, Cs_].T; v[:64, 14] = cb[Cs_]
    rp = np.zeros((128, 8), np.float32)
    hh = slice(2 * r, 2 * r + 2)
    rp[:, 0:2] = d["ssd_dt_bias"][layer][hh][None, :]
    rp[:, 2:4] = d["ssd_a_log"][layer][hh][None, :]
    rp[:, 4:6] = d["ssd_d"][layer][hh][None, :]
    return v, rp


def lru_vecs(d, layer, r):
    ch = slice(128 * r, 128 * r + 128)
    v = np.zeros((128, 8), np.float32)
    v[:, 0:4] = d["lru_conv_w"][layer][:, ch].T
    v[:, 4] = d["lru_conv_b"][layer][ch]
    v[:, 5] = d["lru_ba"][layer][ch]
    v[:, 6] = d["lru_bx"][layer][ch]
    v[:, 7] = d["lru_lambda"][layer][ch]
    wab_ = np.zeros((128, 128), np.float32)
    wxb_ = np.zeros((128, 128), np.float32)
    for k in range(2):
        wab_[64 * k:64 * k + 64, 64 * k:64 * k + 64] = d["lru_wa"][layer][2 * r + k]
        wxb_[64 * k:64 * k + 64, 64 * k:64 * k + 64] = d["lru_wx"][layer][2 * r + k]
    return v, wab_, wxb_


def kernel(**inputs):
    d = {k: np.asarray(v) for k, v in inputs.items()}
    nc = build_fused()
    in_maps = [fused_inputs(d, c) for c in range(NCORES)]
    res = run_bass_kernel_spmd(nc, in_maps, core_ids=list(range(NCORES))).results
    x = np.concatenate([res[c]["x2T"].T for c in range(NCORES)], axis=0)
    return np.ascontiguousarray(x.reshape(BATCH, S, D).astype(np.float32))
```

```python
import numpy as np
import contextlib
import concourse.bass as bass
import concourse.mybir as mybir
from concourse.bass_utils import run_bass_kernel_spmd

F32 = mybir.dt.float32
BF16 = mybir.dt.bfloat16
AF = mybir.ActivationFunctionType
ALU = mybir.AluOpType
AX = mybir.AxisListType

SAME_ENGINE_SYNC = True
SEM_ROT = 20000
NDMA = 6


class Res:
    __slots__ = ("w", "r", "name", "excl")

    def __init__(self, name="", excl=False):
        self.name = name
        self.w = None
        self.r = {}
        self.excl = excl


class KB:
    def __init__(self, nc, es):
        self.nc = nc
        self.es = es
        self.eng = dict(pe=nc.tensor, act=nc.scalar, dve=nc.vector, pool=nc.gpsimd, sp=nc.sync)
        self.sems = {}
        self.cnt = {}
        self.cur = {}
        self.seen = {e: {} for e in self.eng}
        self.nsem = 0
        self.ninst = 0
        for e in self.eng:
            self.cur[e] = self._new_sem("e_" + e)
        self.dma_pool = {q: [self._new_sem("d_%s%d" % (q, i)) for i in range(NDMA)] for q in ("sp", "pool", "act")}
        self.dma_idx = {q: 0 for q in self.dma_pool}
        self.uid = 0
        self.out_marks = []

    def _new_sem(self, name):
        self.nsem += 1
        key = "%s_%d" % (name, self.nsem)
        self.sems[key] = self.es.enter_context(self.nc.semaphore(key))
        self.cnt[key] = 0
        return key

    def sb(self, shape, dtype, name=None, es=None):
        self.uid += 1
        return (es or self.es).enter_context(self.nc.sbuf_tensor("%s_%d" % (name or "sb", self.uid), list(shape), dtype))

    def ps(self, shape, dtype=F32, name=None):
        self.uid += 1
        return self.es.enter_context(self.nc.psum_tensor("%s_%d" % (name or "ps", self.uid), list(shape), dtype))

    def dram(self, name, shape, dtype, kind):
        return self.nc.dram_tensor(name, list(shape), dtype, kind=kind).ap()

    def _deps(self, reads, writes):
        deps = {}
        for r in reads:
            if r.w is not None:
                k, v = r.w
                if deps.get(k, 0) < v:
                    deps[k] = v
            if r.excl:
                for k, v in r.r.items():
                    if deps.get(k, 0) < v:
                        deps[k] = v
        for w in writes:
            if w.w is not None:
                k, v = w.w
                if deps.get(k, 0) < v:
                    deps[k] = v
            for k, v in w.r.items():
                if deps.get(k, 0) < v:
                    deps[k] = v
        return deps

    def _wait(self, e, deps, skip_key=None):
        seen = self.seen[e]
        for k, v in deps.items():
            if k == skip_key:
                continue
            if seen.get(k, 0) < v:
                self.eng[e].wait_ge(self.sems[k], v)
                seen[k] = v
                self.ninst += 1

    def _mark(self, key, v, reads, writes):
        for r in reads:
            r.r[key] = v
        for w in writes:
            w.w = (key, v)
            w.r = {}

    def op(self, e, fn, reads=(), writes=()):
        deps = self._deps(reads, writes)
        key = self.cur[e]
        skip = key if (e == "pe" or not SAME_ENGINE_SYNC) else None
        self._wait(e, deps, skip)
        inst = fn(self.eng[e])
        self.cnt[key] += 1
        v = self.cnt[key]
        inst.then_inc(self.sems[key], 1)
        self.ninst += 1
        self._mark(key, v, reads, writes)
        if v >= SEM_ROT:
            self.cur[e] = self._new_sem("e_" + e)
        return inst

    def dma(self, q, out, in_, reads=(), writes=(), final=False, **kw):
        pool = self.dma_pool[q]
        key = pool[self.dma_idx[q] % len(pool)]
        self.dma_idx[q] += 1
        deps = self._deps(reads, writes)
        if self.cnt[key] > 0:
            deps[key] = max(deps.get(key, 0), self.cnt[key])
        self._wait(q, deps)
        inst = self.eng[q].dma_start(out=out, in_=in_, **kw)
        self.cnt[key] += 16
        v = self.cnt[key]
        inst.then_inc(self.sems[key], 16)
        self.ninst += 1
        self._mark(key, v, reads, writes)
        if final:
            self.out_marks.append((key, v))
        if v >= SEM_ROT:
            i = pool.index(key)
            pool[i] = self._new_sem("d_" + q)
        return inst

    def barrier(self):
        allc = {k: v for k, v in self.cnt.items() if v > 0}
        for e in self.eng:
            self._wait(e, dict(allc))

    def finish(self, out_res):
        deps = self._deps(out_res, ())
        for k, v in self.out_marks:
            if deps.get(k, 0) < v:
                deps[k] = v
        self._wait("sp", deps)


def _kb_gather(self, out, in2d, idx_ap, reads=(), writes=()):
    pool = self.dma_pool["pool"]
    key = pool[self.dma_idx["pool"] % len(pool)]
    self.dma_idx["pool"] += 1
    deps = self._deps(reads, writes)
    if self.cnt[key] > 0:
        deps[key] = max(deps.get(key, 0), self.cnt[key])
    self._wait("pool", deps)
    inst = self.nc.gpsimd.indirect_dma_start(out=out, out_offset=None, in_=in2d,
                                             in_offset=bass.IndirectOffsetOnAxis(ap=idx_ap, axis=0))
    self.cnt[key] += 16
    v = self.cnt[key]
    inst.then_inc(self.sems[key], 16)
    self.ninst += 1
    self._mark(key, v, reads, writes)
    if v >= SEM_ROT:
        pool[pool.index(key)] = self._new_sem("d_pool")
    return inst


def _kb_allgather(self, in_ap, out_ap, groups, writes=()):
    if not hasattr(self, "cc_key"):
        self.cc_key = self._new_sem("cc")
    deps = self._deps((), writes)
    self._wait("pool", deps)
    inst = self.nc.gpsimd.collective_compute("AllGather", ALU.bypass, replica_groups=groups, ins=[in_ap], outs=[out_ap])
    key = self.cc_key
    self.cnt[key] += 1
    v = self.cnt[key]
    inst.then_inc(self.sems[key], 1)
    self.ninst += 1
    self._mark(key, v, (), writes)
    return inst


KB.gather = _kb_gather
KB.allgather = _kb_allgather


S = 8192


class Rot:
    def __init__(self, kb, n, shape, dtype, name, psum=False, es=None):
        if psum:
            self.t = [(kb.ps(shape, dtype, name), Res(name, excl=True)) for _ in range(n)]
        else:
            self.t = [(kb.sb(shape, dtype, name, es=es), Res(name)) for _ in range(n)]
        self.i = 0

    def next(self):
        x = self.t[self.i % len(self.t)]
        self.i += 1
        return x


LEVEL = 9

S = 8192


def emit_conv(kb, eng, out, xin, vt, vr, c0, N, P, xr, outr):
    kb.op(eng, lambda e: e.tensor_scalar(out=out[:P, :], in0=xin[:P, 0:N], scalar1=vt[:P, c0:c0 + 1], scalar2=vt[:P, c0 + 4:c0 + 5],
                                         op0=ALU.mult, op1=ALU.add), reads=[xr, vr], writes=[outr])
    for k in range(1, 4):
        kb.op("dve", lambda e, k=k: e.scalar_tensor_tensor(out=out[:P, :], in0=xin[:P, k:k + N], scalar=vt[:P, c0 + k:c0 + k + 1], in1=out[:P, :],
                                                          op0=ALU.mult, op1=ALU.add), reads=[xr, vr, outr], writes=[outr])


def ssd_consts():
    k = np.arange(128)
    cst = np.zeros((128, 4, 128), np.float32)
    cst[:, 0, :] = (k[:, None] <= k[None, :])
    cst[:, 1, :] = (k[:, None] > k[None, :])
    cst[:, 2, :] = np.eye(128)
    cst[:, 3, :] = 1.0
    return cst


DBG = ''

S = 8192
QT = 512
NCMP = 511
BIGM = 1024.0
f32 = np.float32


def nsa_consts(par=0):
    kk = np.arange(128)
    tq = np.arange(512)
    c = {}
    zeros = np.zeros((128, 512), f32); onesm = np.ones((128, 512), f32)

    def cm_full(dl):
        if dl < 0:
            return zeros
        if dl >= 5:
            return onesm
        return ((16 * kk[:, None] + 31 - tq[None, :]) <= 512 * dl).astype(f32)

    def cneg_full(dk):
        if dk < 0:
            return zeros
        if dk > 3:
            return -onesm
        return np.where(128 * dk + kk[:, None] <= tq[None, :], 0.0, -1.0).astype(f32)

    def wm_full(dk):
        if dk < -4 or dk > 3:
            return zeros
        diff = tq[None, :] - (128 * dk + kk[:, None])
        return ((diff >= 0) & (diff < 512)).astype(f32)

    cm = [cm_full(dl + par) for dl in range(-1, 5)]
    cneg = [cneg_full(dk - 4 * par) for dk in range(0, 8)]
    wm = [wm_full(dk - 4 * par) for dk in range(-4, 8)]
    c["nmask"] = np.ascontiguousarray(np.stack(cm + cneg + wm, axis=1))
    key = np.arange(S)
    c["nE0"] = (kk[:, None] == (key[None, :] // 64)).astype(f32)
    n = np.arange(512)
    ov = ((n[:, None] // 4) == kk[None, :]).astype(f32) + (((n[:, None] + 1) // 4) == kk[None, :]).astype(f32)
    ov[511] = 0
    ovl = ov.reshape(4, 128, 128).transpose(1, 0, 2)
    mm = np.arange(256) - 8 * par
    hi = (kk >= 64).astype(np.int64)
    C0 = (mm[None, :] <= 128 + hi[:, None]).astype(f32)
    Cm1 = C0 - 1.0
    F = np.where((mm[None, :] == 128 + hi[:, None]) | (mm[None, :] == 127 + hi[:, None]), 1e4, -1e30).astype(f32)
    ident = np.eye(128, dtype=f32)
    ones = np.ones((128, 128), f32)
    sel64 = np.zeros((128, 64), f32); sel64[64] = 1.0
    gsel = np.zeros((128, 12, 64), f32)
    for r in range(12):
        gsel[r, r, :] = 1.0
    c["nmisc"] = np.ascontiguousarray(np.concatenate(
        [ovl.reshape(128, 512), C0, Cm1, F, ident, ones, sel64, gsel.reshape(128, 768)], axis=1))
    return c


NMASK = 26
MISC_W = 512 + 768 + 128 + 128 + 64 + 768


D = 1024
TOK = 2048


CDBG = ''

D = 1024
TOK = 2048
NT = TOK // 512
ALPHA = 4 ** 0.25
OFF_Z = 512 + 768 + 24
OFF_MERGE = 6688 - 3072
f32 = np.float32
CW = 36 + 16 + 128 + 128 + 128 + 2048


def c_consts(d, layer):
    v = np.zeros((128, CW), f32)
    col = lambda a: a.reshape(-1, 128).T
    v[:, 0:8] = col(d["ln1_g"][layer]); v[:, 8:16] = col(d["ln1_b"][layer])
    v[:, 16:24] = col(d["ln2_g"][layer]); v[:, 24:32] = col(d["ln2_b"][layer])
    v[:, 32:36] = col(d["ssd_norm_w"][layer])
    v[:, 36:52] = d["router_b"][None, :]
    v[:, 52:180] = 1.0 / 1024
    v[:, 180:308] = 1.0 / 512
    v[:, 308:436] = np.eye(128)
    sel = np.zeros((128, 16, 128), f32)
    for e in range(16):
        sel[e, e, :] = 1.0
    v[:, 436:436 + 2048] = sel.reshape(128, 2048)
    return v


def emit_nsa2(kb, tiles, agF2k, agF512, agFr, gx, gxr, IX, peT, w1, w2, nmask, nE0, nmisc, onsaT, onsar, acc, pmb, gen, es):
    sb = lambda shape, dt, name: kb.sb(shape, dt, name, es=es)
    mk = sb([128, NMASK, 512], BF16, "nmask"); mkr = Res()
    kb.dma("pool", mk[:], nmask[:, :, :], writes=[mkr])
    E0 = sb([128, S], BF16, "nE0"); E0r = Res()
    kb.dma("pool", E0[:], nE0[:, :], writes=[E0r])
    mf = sb([128, MISC_W], F32, "nmiscf"); mfr = Res()
    kb.dma("sp", mf[:], nmisc[:, :], writes=[mfr])
    o = 0
    ovl_f = mf[:, 0:512]; o = 512
    C0 = mf[:, o:o + 256]; Cm1 = mf[:, o + 256:o + 512]; F4 = mf[:, o + 512:o + 768]; o += 768
    ident = mf[:, o:o + 128]; o += 128
    ones_f = mf[:, o:o + 128]; o += 128
    sel64 = mf[:, o:o + 64]; o += 64
    gsel = mf[:, o:o + 768]
    cb_ = sb([128, 512 + 128 + 128], BF16, "ncb"); cbr = Res()
    kb.op("dve", lambda e: e.tensor_copy(out=cb_[:, 0:512], in_=ovl_f), reads=[mfr], writes=[cbr])
    kb.op("dve", lambda e: e.tensor_copy(out=cb_[:, 512:640], in_=ident), reads=[mfr], writes=[cbr])
    kb.op("dve", lambda e: e.tensor_copy(out=cb_[:, 640:768], in_=ones_f), reads=[mfr], writes=[cbr])
    ovl = cb_[:, 0:512]; identb = cb_[:, 512:640]; onesb = cb_[:, 640:768]

    kall = sb([128, 2, S], BF16, "nk"); kr = [None, None, Res(), Res()]
    kb.op("pool", lambda e: e.memset(kall[64:128, :, :], 0.0), writes=[kr[2], kr[3]])
    for i, nm in ((2, "ksel"), (3, "kwin")):
        for rp in range(4):
            kb.gather(kall[0:64, i - 2, rp * 2048:(rp + 1) * 2048], agF2k, gx[0:64, IX[nm + str(rp)]:IX[nm + str(rp)] + 1], reads=[gxr, agFr], writes=[kr[i]])
    va = sb([128, 2, 64, 128], BF16, "nva"); var = [Res() for _ in range(2)]
    for i in range(2):
        kb.op("pool", lambda e, i=i: e.memset(va[:, i, :, 64:128], 0.0), writes=[var[i]])
        kb.op("pool", lambda e, i=i: e.memset(va[:, i, :, 64:65], 1.0), writes=[var[i]])

    kcT = sb([64, 512], BF16, "nkcT"); kcr = Res()
    vct = sb([128, 4, 65], BF16, "nvct"); vcr = Res()
    kb.op("pool", lambda e: e.memset(kcT[:], 0.0), writes=[kcr])
    kb.op("pool", lambda e: e.memset(vct[:], 0.0), writes=[vcr])
    kb.op("pool", lambda e: e.memset(vct[:, :, 64:65], 1.0), writes=[vcr])
    with contextlib.ExitStack() as esv:
        vT = kb.sb([64, 2, S], BF16, "nvT", es=esv); vTr = [Res(), Res()]
        for i, nm in ((0, "vsel"), (1, "vwin")):
            for rp in range(4):
                kb.gather(vT[:, i, rp * 2048:(rp + 1) * 2048], agF2k, gx[0:64, IX[nm + str(rp)]:IX[nm + str(rp)] + 1], reads=[gxr, agFr], writes=[vTr[i]])
            for k8 in range(8):
                pv8, pv8r = gen.next()
                pvb = pv8[:, 0:256].bitcast(BF16)
                for kk in range(8):
                    kt = k8 * 8 + kk
                    kb.op("pe", lambda e, kt=kt, kk=kk, i=i: e.transpose(pvb[:, kk * 64:(kk + 1) * 64], vT[:, i, kt * 128:(kt + 1) * 128], identb[0:64, 0:64]),
                          reads=[vTr[i], cbr], writes=[pv8r])
                kb.op("act", lambda e, k8=k8, i=i: e.copy(out=va[:, i, k8 * 8:(k8 + 1) * 8, 0:64], in_=pvb.rearrange("p (k d) -> p k d", k=8)), reads=[pv8r], writes=[var[i]])
        kb.barrier()
    with contextlib.ExitStack() as es2:
        sb2 = lambda shape, dt, name: kb.sb(shape, dt, name, es=es2)
        kcv = sb2([64, 2, S], BF16, "nkcv"); kr[0] = Res(); kr[1] = Res()
        for i, nm in ((0, "kcmp"), (1, "vcmp")):
            for rp in range(4):
                kb.gather(kcv[:, i, rp * 2048:(rp + 1) * 2048], agF2k, gx[0:64, IX[nm + str(rp)]:IX[nm + str(rp)] + 1], reads=[gxr, agFr], writes=[kr[i]])
        w1t = sb2([64, 32, 256], BF16, "nw1"); w1r = Res()
        w2t = sb2([128, 2, 64], BF16, "nw2"); w2r = Res()
        pet = sb2([64, 32], BF16, "npe"); per = Res()
        bias = sb2([128, 1], F32, "nbias"); biasr = Res()
        tmp3 = [(sb2([128, 512], F32, "nt"), Res(), sb2([128, 512], F32, "nu"), Res(), sb2([128, 512], BF16, "ngt"), Res()) for _ in range(2)]
        for which in range(2):
            for q4 in range(4):
                kb.dma("pool", w1t[:, q4 * 8:(q4 + 1) * 8, :], w1[which, q4 * 512:(q4 + 1) * 512, :].rearrange("(l d) j -> d l j", d=64), writes=[w1r])
            kb.dma("pool", w2t[:], w2[which, :, :].rearrange("(c p) d -> p c d", p=128), writes=[w2r])
            kb.dma("pool", pet[:], peT[which, :, :], writes=[per])
            src = kcv[:, which, :]
            gts = []
            for jc in range(2):
                ph, phr = gen.next()
                for l in range(32):
                    kb.op("pe", lambda e, l=l: e.matmul(ph[:, 0:NCMP], lhsT=w1t[:, l, jc * 128:(jc + 1) * 128],
                                                         rhs=src[:, l:l + 16 * (NCMP - 1) + 1:16], start=(l == 0), stop=(l == 31)),
                          reads=[w1r, kr[which]], writes=[phr])
                pbias, pbr = gen.next()
                for l in range(32):
                    kb.op("pe", lambda e, l=l: e.matmul(pbias[:, 0:1], lhsT=w1t[:, l, jc * 128:(jc + 1) * 128], rhs=pet[:, l:l + 1],
                                                         start=(l == 0), stop=(l == 31)), reads=[w1r, per], writes=[pbr])
                kb.op("dve", lambda e: e.tensor_copy(out=bias[:], in_=pbias[:, 0:1]), reads=[pbr], writes=[biasr])
                t, tr, u, ur, gt, gr = tmp3[jc]
                kb.op("act", lambda e: e.activation(out=t[:, 0:NCMP], in_=ph[:, 0:NCMP], func=AF.Identity, bias=bias[:, 0:1]), reads=[phr, biasr], writes=[tr])
                kb.op("dve", lambda e: e.tensor_tensor(out=u[:, 0:NCMP], in0=t[:, 0:NCMP], in1=t[:, 0:NCMP], op=ALU.mult), reads=[tr], writes=[ur])
                kb.op("dve", lambda e: e.tensor_scalar(out=u[:, 0:NCMP], in0=u[:, 0:NCMP], scalar1=0.044715, scalar2=1.0, op0=ALU.mult, op1=ALU.add), reads=[ur], writes=[ur])
                kb.op("dve", lambda e: e.tensor_tensor(out=u[:, 0:NCMP], in0=u[:, 0:NCMP], in1=t[:, 0:NCMP], op=ALU.mult), reads=[ur, tr], writes=[ur])
                kb.op("act", lambda e: e.activation(out=u[:, 0:NCMP], in_=u[:, 0:NCMP], func=AF.Sigmoid, scale=1.5957691216057308), reads=[ur], writes=[ur])
                kb.op("pool", lambda e: e.memset(gt[:, NCMP:512], 0.0), writes=[gr])
                kb.op("dve", lambda e: e.tensor_tensor(out=gt[:, 0:NCMP], in0=u[:, 0:NCMP], in1=t[:, 0:NCMP], op=ALU.mult), reads=[ur, tr], writes=[gr])
                gts.append((gt, gr))
            if which == 0:
                pk, pkr = gen.next()
                for jc in range(2):
                    kb.op("pe", lambda e, jc=jc: e.matmul(pk[0:64, 0:NCMP], lhsT=w2t[:, jc, :], rhs=gts[jc][0][:, 0:NCMP], start=(jc == 0), stop=(jc == 1)),
                          reads=[w2r, gts[jc][1]], writes=[pkr])
                kb.op("act", lambda e: e.copy(out=kcT[:, 0:NCMP], in_=pk[0:64, 0:NCMP]), reads=[pkr], writes=[kcr])
            else:
                for m in range(4):
                    pv, pvr = gen.next()
                    for jc in range(2):
                        kb.op("pe", lambda e, jc=jc, m=m: e.matmul(pv[:, 0:64], lhsT=gts[jc][0][:, m * 128:(m + 1) * 128], rhs=w2t[:, jc, :], start=(jc == 0), stop=(jc == 1)),
                              reads=[w2r, gts[jc][1]], writes=[pvr])
                    kb.op("act", lambda e, m=m: e.copy(out=vct[:, m, 0:64], in_=pv[:, 0:64]), reads=[pvr], writes=[vcr])
        kb.barrier()
    kmax = sb([128, 1], F32, "nkmax"); kmr = Res()
    kb.op("pool", lambda e: e.memset(kmax[:], 0.0), writes=[kmr])
    sq = Rot(kb, 2, [64, 512], BF16, "nsq", es=es)
    red = Rot(kb, 2, [128, 1], F32, "nred", es=es)

    def norm_max(src_ap, n, src_res, dst, dstr):
        s_, sr_ = sq.next()
        kb.op("pool", lambda e: e.tensor_tensor(out=s_[:, 0:n], in0=src_ap, in1=src_ap, op=ALU.mult), reads=src_res, writes=[sr_])
        pn, pnr = gen.next()
        kb.op("pe", lambda e: e.matmul(pn[:, 0:n], lhsT=onesb[0:64, :], rhs=s_[:, 0:n], start=True, stop=True), reads=[cbr, sr_], writes=[pnr])
        r_, rr_ = red.next()
        kb.op("dve", lambda e: e.reduce_max(out=r_[:], in_=pn[:, 0:n], axis=AX.X), reads=[pnr], writes=[rr_])
        kb.op("dve", lambda e: e.tensor_tensor(out=dst[:], in0=dst[:], in1=r_[:], op=ALU.max), reads=[rr_, dstr], writes=[dstr])

    norm_max(kcT[:, 0:512], 512, [kcr], kmax, kmr)
    for which in (2, 3):
        for tt in range(S // 512):
            norm_max(kall[0:64, which - 2, tt * 512:(tt + 1) * 512], 512, [kr[which]], kmax, kmr)

    qb = Rot(kb, 2, [128, 4, 512], BF16, "nq", es=es)
    for q_, qr_ in qb.t:
        kb.op("pool", lambda e, q_=q_: e.memset(q_[64:128, :, :], 0.0), writes=[qr_])
    gtl = Rot(kb, 2, [12, 512], F32, "ngate", es=es)
    gtbl = Rot(kb, 2, [12, 512], BF16, "ngateb", es=es)
    ebuf = Rot(kb, 4, [128, 512], BF16, "ne", es=es)
    pbuf = Rot(kb, 4, [128, 512], BF16, "np", es=es)
    mskb = Rot(kb, 2, [128, 512], BF16, "nmsk", es=es)
    pnbuf = Rot(kb, 4, [128, 512], BF16, "npn", es=es)
    rinv = Rot(kb, 2, [128, 512], F32, "nrinv", es=es)
    ocmp = Rot(kb, 1, [64, 4, 512], F32, "nocmp", es=es)
    impT = Rot(kb, 2, [128, 512], F32, "nimpT", es=es)
    selT = Rot(kb, 2, [128, 512], BF16, "nselT", es=es)
    v1b = Rot(kb, 2, [128, 128], F32, "nv1", es=es)
    v2b = Rot(kb, 2, [128, 128], F32, "nv2", es=es)
    m8a = Rot(kb, 2, [128, 8], F32, "nm8a", es=es)
    m8b = Rot(kb, 2, [128, 8], F32, "nm8b", es=es)
    smk = Rot(kb, 2, [128, 128], F32, "nsmk", es=es)
    accs = Rot(kb, 1, [65, 8, 512], F32, "naccs", es=es)
    rec = Rot(kb, 2, [64, 512], F32, "nrec", es=es)
    osb = Rot(kb, 2, [64, 512], F32, "nosb", es=es)
    negc = Rot(kb, 2, [128, 4], F32, "nnegc", es=es)
    qm = Rot(kb, 2, [128, 1], F32, "nqm", es=es)

    for ti, i in enumerate(tiles):
        t0 = i * QT
        q, qr = qb.next()
        for j in range(4):
            cq = IX["q%d_%d" % (ti, j)]
            kb.gather(q[0:64, j, :], agF512, gx[0:64, cq:cq + 1], reads=[gxr, agFr], writes=[qr])
        gtb_, gtbr = gtbl.next()
        cg = IX["gate%d" % ti]
        kb.gather(gtb_[:], agF512, gx[0:12, cg:cg + 1], reads=[gxr, agFr], writes=[gtbr])
        gt_, gtr = gtl.next()
        kb.op("act", lambda e: e.activation(out=gt_[:], in_=gtb_[:], func=AF.Sigmoid), reads=[gtbr], writes=[gtr])
        nc_, ncr = negc.next()
        for j in range(4):
            qm_, qmr = qm.next()
            kb.op("pool", lambda e: e.memset(qm_[:], 0.0), writes=[qmr])
            norm_max(q[0:64, j, :], 512, [qr], qm_, qmr)
            kb.op("dve", lambda e, j=j: e.tensor_tensor(out=nc_[:, j:j + 1], in0=qm_[:], in1=kmax[:], op=ALU.mult), reads=[qmr, kmr], writes=[ncr])
        kb.op("act", lambda e: e.activation(out=nc_[:], in_=nc_[:], func=AF.Sqrt, scale=1.05), reads=[ncr], writes=[ncr])
        kb.op("dve", lambda e: e.tensor_scalar(out=nc_[:], in0=nc_[:], scalar1=-0.125, scalar2=None, op0=ALU.mult), reads=[ncr], writes=[ncr])

        nch = min(4, (32 * (i + 1) + 31 + 127) // 128)
        oc, ocr = ocmp.next()
        pimp, pimpr = acc[0]
        for j in range(4):
            es_ = []
            psum_, psumr = acc[1]
            for m in range(nch):
                ps_, psr = gen.next()
                kb.op("pe", lambda e, m=m, j=j: e.matmul(ps_[:, :], lhsT=kcT[:, m * 128:(m + 1) * 128], rhs=q[0:64, j, :], start=True, stop=True),
                      reads=[kcr, qr], writes=[psr])
                e_, er = ebuf.next()
                kb.op("act", lambda e, j=j: e.activation(out=e_[:], in_=ps_[:, :], func=AF.Exp, scale=0.125, bias=nc_[:, j:j + 1]), reads=[psr, ncr], writes=[er])
                dl = i - 4 * m
                if dl <= 4:
                    kb.op("pool", lambda e, dl=dl: e.tensor_tensor(out=e_[:], in0=e_[:], in1=mk[:, dl + 1, :], op=ALU.mult), reads=[er, mkr], writes=[er])
                kb.op("pe", lambda e, m=m: e.matmul(psum_[:, :], lhsT=onesb, rhs=e_[:], start=(m == 0), stop=(m == nch - 1)), reads=[cbr, er], writes=[psumr])
                es_.append((e_, er))
            ri, rir = rinv.next()
            kb.op("dve", lambda e: e.tensor_scalar(out=ri[:], in0=psum_[:, :], scalar1=1e-30, scalar2=None, op0=ALU.max), reads=[psumr], writes=[rir])
            kb.op("dve", lambda e: e.reciprocal(out=ri[:], in_=ri[:]), reads=[rir], writes=[rir])
            po, por = acc[2]
            for m in range(nch):
                e_, er = es_[m]
                pn_, pnr_ = pnbuf.next()
                kb.op("dve", lambda e: e.tensor_tensor(out=pn_[:], in0=e_[:], in1=ri[:], op=ALU.mult), reads=[er, rir], writes=[pnr_])
                kb.op("pe", lambda e, m=m: e.matmul(po[0:64, :], lhsT=vct[:, m, 0:64], rhs=pn_[:], start=(m == 0), stop=(m == nch - 1)), reads=[vcr, pnr_], writes=[por])
                kb.op("pe", lambda e, m=m, j=j: e.matmul(pimp[:, :], lhsT=ovl[:, m * 128:(m + 1) * 128], rhs=pn_[:], start=(j == 0 and m == 0), stop=(j == 3 and m == nch - 1)),
                      reads=[cbr, pnr_], writes=[pimpr])
            kb.op("act", lambda e, j=j: e.copy(out=oc[:, j, :], in_=po[0:64, :]), reads=[por], writes=[ocr])
        it_, itr = impT.next()
        kb.op("act", lambda e: e.copy(out=it_[:], in_=pimp[:, :]), reads=[pimpr], writes=[itr])
        st_, str_ = selT.next()
        for k4 in range(4):
            ksub = 4 * i + k4
            ptr_, ptrr = gen.next()
            kb.op("pe", lambda e, k4=k4: e.transpose(ptr_[:, 0:128], it_[:, k4 * 128:(k4 + 1) * 128], ident), reads=[itr, mfr], writes=[ptrr])
            co = 128 - 2 * ksub
            v1, v1r = v1b.next()
            kb.op("dve", lambda e, co=co: e.tensor_tensor(out=v1[:], in0=ptr_[:, 0:128], in1=C0[:, co:co + 128], op=ALU.mult), reads=[ptrr, mfr], writes=[v1r])
            kb.op("dve", lambda e, co=co: e.tensor_tensor(out=v1[:], in0=v1[:], in1=Cm1[:, co:co + 128], op=ALU.add), reads=[v1r, mfr], writes=[v1r])
            kb.op("dve", lambda e, co=co: e.tensor_tensor(out=v1[:], in0=v1[:], in1=F4[:, co:co + 128], op=ALU.max), reads=[v1r, mfr], writes=[v1r])
            kb.op("dve", lambda e: e.memset(v1[:, 0:1], 1e4), reads=[], writes=[v1r])
            a8, a8r = m8a.next()
            kb.op("dve", lambda e: e.max(out=a8[:], in_=v1[:]), reads=[v1r], writes=[a8r])
            v2, v2r = v2b.next()
            kb.op("dve", lambda e: e.match_replace(out=v2[:], in_to_replace=a8[:], in_values=v1[:], imm_value=-1e30), reads=[a8r, v1r], writes=[v2r])
            b8, b8r = m8b.next()
            kb.op("dve", lambda e: e.max(out=b8[:], in_=v2[:]), reads=[v2r], writes=[b8r])
            sm, smr = smk.next()
            kb.op("dve", lambda e: e.tensor_scalar(out=sm[:], in0=v1[:], scalar1=b8[:, 7:8], scalar2=None, op0=ALU.is_ge), reads=[v1r, b8r], writes=[smr])
            if DBG == "sm" and k4 == 0 and ti == 0:
                kb.dma("sp", onsaT[0:128, 0:128], sm[:], reads=[smr], final=True)
                kb.dma("sp", onsaT[128:256, 0:128], v1[:], reads=[v1r], final=True)
                kb.dma("sp", onsaT[0:128, 128:136], a8[:], reads=[a8r], final=True)
                kb.dma("sp", onsaT[0:128, 136:144], b8[:], reads=[b8r], final=True)
                kb.dma("sp", onsaT[128:256, 128:256], v2[:], reads=[v2r], final=True)
            pt2, pt2r = gen.next()
            kb.op("pe", lambda e: e.transpose(pt2[:, 0:128], sm[:], ident), reads=[smr, mfr], writes=[pt2r])
            kb.op("act", lambda e, k4=k4: e.copy(out=st_[:, k4 * 128:(k4 + 1) * 128], in_=pt2[:, 0:128]), reads=[pt2r], writes=[str_])

        as_, asr = accs.next()
        LA = 2
        for br in range(2):
            if br == 0:
                kts = list(range(0, min(64, 4 * i + 8)))
            else:
                kts = [kt for kt in range(4 * i - 4, min(64, 4 * i + 8)) if kt >= 0]
            ksrc = 2 + br
            pairs = [(n_, kt, j) for n_, kt in enumerate(kts) for j in range(4)]
            pend = []
            msk = None
            for idx in range(len(pairs) + LA):
                if idx < len(pairs):
                    n_, kt, j = pairs[idx]
                    dk = kt - 4 * i
                    if br == 0 and j == 0:
                        pm, pmr = pmb
                        kb.op("pe", lambda e, kt=kt, dk=dk: e.matmul(pm[:, :], lhsT=E0[:, kt * 128:(kt + 1) * 128], rhs=st_[:], start=True, stop=(dk < 0)),
                              reads=[E0r, str_], writes=[pmr])
                        if dk >= 0:
                            kb.op("pe", lambda e, dk=dk: e.matmul(pm[:, :], lhsT=identb, rhs=mk[:, 6 + dk, :], start=False, stop=True), reads=[cbr, mkr], writes=[pmr])
                        msk = mskb.next()
                        kb.op("act", lambda e, msk=msk: e.activation(out=msk[0][:], in_=pm[:, :], func=AF.Relu), reads=[pmr], writes=[msk[1]])
                    ps_, psr = gen.next()
                    kb.op("pe", lambda e, kt=kt, j=j, ps_=ps_: e.matmul(ps_[:, :], lhsT=kall[:, br, kt * 128:(kt + 1) * 128], rhs=q[:, j, :], start=True, stop=True),
                          reads=[kr[ksrc], qr], writes=[psr])
                    e_, er = ebuf.next()
                    kb.op("act", lambda e, j=j, e_=e_, ps_=ps_: e.activation(out=e_[:], in_=ps_[:, :], func=AF.Exp, scale=0.125, bias=nc_[:, j:j + 1]), reads=[psr, ncr], writes=[er])
                    p_, pr_ = pbuf.next()
                    eng = "dve"
                    if br == 0:
                        kb.op(eng, lambda e, p_=p_, e_=e_, msk=msk: e.tensor_tensor(out=p_[:], in0=e_[:], in1=msk[0][:], op=ALU.mult), reads=[er, msk[1]], writes=[pr_])
                    else:
                        kb.op(eng, lambda e, dk=dk, p_=p_, e_=e_: e.tensor_tensor(out=p_[:], in0=e_[:], in1=mk[:, 14 + dk + 4, :], op=ALU.mult), reads=[er, mkr], writes=[pr_])
                    pend.append((p_, pr_, n_, kt, j))
                if idx >= LA:
                    p_, pr_, n_, kt, j = pend[idx - LA]
                    pa, par = acc[j]
                    kb.op("pe", lambda e, kt=kt, n_=n_, p_=p_, pa=pa: e.matmul(pa[:, :], lhsT=va[:, br, kt, :], rhs=p_[:], start=(n_ == 0), stop=(n_ == len(kts) - 1)),
                          reads=[var[br], pr_], writes=[par])
            for j in range(4):
                pa, par = acc[j]
                kb.op("act", lambda e, j=j, br=br: e.copy(out=as_[:, br * 4 + j, :], in_=pa[0:65, :]), reads=[par], writes=[asr])
        for j in range(4 if DBG not in ("sm", "pm") else 0):
            o_, or_ = osb.next()
            pg, pgr = gen.next()
            kb.op("pe", lambda e, j=j: e.matmul(pg[0:64, :], lhsT=gsel[0:12, (j * 3) * 64:(j * 3 + 1) * 64], rhs=gt_[:], start=True, stop=True), reads=[mfr, gtr], writes=[pgr])
            if DBG == "cmp":
                kb.op("dve", lambda e, j=j: e.tensor_copy(out=o_[:], in_=oc[:, j, :]), reads=[pgr, ocr], writes=[or_])
            elif DBG:
                kb.op("dve", lambda e, j=j: e.memset(o_[:], 0.0), reads=[pgr, ocr], writes=[or_])
            else:
                kb.op("dve", lambda e, j=j: e.tensor_tensor(out=o_[:], in0=pg[0:64, :], in1=oc[:, j, :], op=ALU.mult), reads=[pgr, ocr], writes=[or_])
            for br in range(2):
                if DBG == "cmp" or (DBG == "sel" and br == 1) or (DBG == "win" and br == 0):
                    continue
                psm, psmr = gen.next()
                kb.op("pe", lambda e, j=j, br=br: e.matmul(psm[0:64, :], lhsT=sel64[0:65, :], rhs=as_[:, br * 4 + j, :], start=True, stop=True), reads=[mfr, asr], writes=[psmr])
                rc, rcr = rec.next()
                kb.op("dve", lambda e: e.tensor_scalar(out=rc[:], in0=psm[0:64, :], scalar1=1e-30, scalar2=None, op0=ALU.max), reads=[psmr], writes=[rcr])
                kb.op("dve", lambda e: e.reciprocal(out=rc[:], in_=rc[:]), reads=[rcr], writes=[rcr])
                pg2, pg2r = gen.next()
                kb.op("pe", lambda e, j=j, br=br: e.matmul(pg2[0:64, :], lhsT=gsel[0:12, (j * 3 + 1 + br) * 64:(j * 3 + 2 + br) * 64], rhs=gt_[:], start=True, stop=True),
                      reads=[mfr, gtr], writes=[pg2r])
                if not DBG:
                    kb.op("dve", lambda e: e.tensor_tensor(out=rc[:], in0=pg2[0:64, :], in1=rc[:], op=ALU.mult), reads=[pg2r, rcr], writes=[rcr])
                kb.op("pool", lambda e, j=j, br=br: e.tensor_tensor(out=rc[:], in0=rc[:], in1=as_[0:64, br * 4 + j, :], op=ALU.mult), reads=[rcr, asr], writes=[rcr])
                kb.op("pool", lambda e: e.tensor_tensor(out=o_[:], in0=o_[:], in1=rc[:], op=ALU.add), reads=[rcr, or_], writes=[or_])
            kb.dma("pool", onsaT[j * 64:(j + 1) * 64, ti * QT:(ti + 1) * QT], o_[:], reads=[or_, onsar])


def emit_ssd2(kb, agF2k, agFr, agD2k, agDr, gx, gxr, IX, svec, srep, cst, yT_dst, yT_r, pbanks, pbf, es=None):
    N = 1024
    NCH = S // 128
    ct = kb.sb([128, 4, 128], F32, "scst", es=es); cr = Res()
    kb.dma("sp", ct[:], cst[:, :, :], writes=[cr])
    tri = ct[:, 0, :]; U = ct[:, 1, :]; ident = ct[:, 2, :]; ones = ct[:, 3, :]
    identb = kb.sb([128, 128], BF16, "sidb", es=es); idbr = Res()
    kb.op("dve", lambda e: e.tensor_copy(out=identb[:], in_=ident), reads=[cr], writes=[idbr])
    vt = kb.sb([128, 16], F32, "svec", es=es); vr = Res()
    kb.dma("sp", vt[:], svec[:, :], writes=[vr])
    rp = kb.sb([128, 8], F32, "srep", es=es); rpr = Res()
    kb.dma("sp", rp[:], srep[:, :], writes=[rpr])
    if LEVEL == -1: return
    dt = kb.sb([128, NCH, 2], F32, "sdt", es=es); dtr = Res()
    dtT = kb.sb([2, S], F32, "sdtT", es=es); dtTr = Res()
    for rq in range(4):
        cd = IX["dt%d" % rq]
        kb.gather(dtT[0:2, rq * 2048:(rq + 1) * 2048], agD2k, gx[0:2, cd:cd + 1], reads=[gxr, agDr], writes=[dtTr])
    pdt, pdtr = pbanks.next()
    for c in range(NCH):
        kb.op("pe", lambda e, c=c: e.transpose(pdt[:, c * 2:(c + 1) * 2], dtT[0:2, c * 128:(c + 1) * 128], ident[0:2, 0:2]), reads=[dtTr, cr], writes=[pdtr])
    kb.op("dve", lambda e: e.tensor_copy(out=dt[:].rearrange("p c h -> p (c h)"), in_=pdt[:, 0:NCH * 2]), reads=[pdtr], writes=[dtr])
    aa = kb.sb([128, NCH, 2], F32, "sa", es=es); aar = Res()
    An = kb.sb([128, 2], F32, "sAn", es=es); Anr = Res()
    kb.op("act", lambda e: e.activation(out=An[:], in_=rp[:, 2:4], func=AF.Exp), reads=[rpr], writes=[Anr])
    kb.op("dve", lambda e: e.tensor_scalar(out=An[:], in0=An[:], scalar1=-1.0, scalar2=None, op0=ALU.mult), reads=[Anr], writes=[Anr])
    for h in range(2):
        kb.op("dve", lambda e, h=h: e.tensor_scalar(out=dt[:, :, h], in0=dt[:, :, h], scalar1=rp[:, h:h + 1], scalar2=None, op0=ALU.add),
              reads=[dtr, rpr], writes=[dtr])
    kb.op("act", lambda e: e.activation(out=dt[:], in_=dt[:], func=AF.Exp), reads=[dtr], writes=[dtr])
    kb.op("act", lambda e: e.activation(out=dt[:], in_=dt[:], func=AF.Ln, bias=1.0), reads=[dtr], writes=[dtr])
    for h in range(2):
        kb.op("dve", lambda e, h=h: e.tensor_scalar(out=aa[:, :, h], in0=dt[:, :, h], scalar1=An[:, h:h + 1], scalar2=None, op0=ALU.mult),
              reads=[dtr, Anr], writes=[aar])
    if LEVEL == -2: return
    acum = kb.sb([128, NCH, 2], F32, "sacum", es=es); acr = Res()
    dout = kb.sb([128, NCH, 2], F32, "sdout", es=es); dor = Res()
    dst = kb.sb([128, NCH, 2], F32, "sdst", es=es); dsr = Res()
    dtot = kb.sb([128, NCH, 2], F32, "sdtot", es=es); dtor = Res()
    aflat = aa[:].rearrange("p c h -> p (c h)")
    p1, p1r = pbanks.next()
    kb.op("pe", lambda e: e.matmul(p1[:, 0:NCH * 2], lhsT=tri, rhs=aflat, start=True, stop=True), reads=[cr, aar], writes=[p1r])
    p2, p2r = pbanks.next()
    kb.op("pe", lambda e: e.matmul(p2[:, 0:NCH * 2], lhsT=ones, rhs=aflat, start=True, stop=True), reads=[cr, aar], writes=[p2r])
    fl = lambda t: t[:].rearrange("p c h -> p (c h)")
    kb.op("dve", lambda e: e.tensor_copy(out=fl(acum), in_=p1[:, 0:NCH * 2]), reads=[p1r], writes=[acr])
    kb.op("act", lambda e: e.activation(out=fl(dout), in_=p1[:, 0:NCH * 2], func=AF.Exp), reads=[p1r], writes=[dor])
    kb.op("act", lambda e: e.activation(out=fl(dtot), in_=p2[:, 0:NCH * 2], func=AF.Exp), reads=[p2r], writes=[dtor])
    kb.op("dve", lambda e: e.tensor_tensor(out=fl(dst), in0=p2[:, 0:NCH * 2], in1=fl(acum), op=ALU.subtract), reads=[p2r, acr], writes=[dsr])
    kb.op("act", lambda e: e.activation(out=fl(dst), in_=fl(dst), func=AF.Exp), reads=[dsr], writes=[dsr])

    if LEVEL == -3: return
    prev = [kb.sb([64, 64], F32, "sprev", es=es) for _ in range(2)]
    prevr = [Res() for _ in range(2)]
    prevb = [kb.sb([64, 64], BF16, "sprevb", es=es) for _ in range(2)]
    prevbr = [Res() for _ in range(2)]
    for h in range(2):
        kb.op("pool", lambda e, h=h: e.memset(prev[h][:], 0.0), writes=[prevr[h]])
        kb.op("pool", lambda e, h=h: e.memset(prevb[h][:], 0.0), writes=[prevbr[h]])

    xfull = kb.sb([128, S + 3], BF16, "sxfull", es=es); xfr = Res()
    bfull = kb.sb([64, S + 3], BF16, "sbfull", es=es); bfr = Res()
    cfull = kb.sb([64, S + 3], BF16, "scfull", es=es); cfr = Res()
    for (tl, rs, nm, P) in ((xfull, xfr, "sx", 128), (bfull, bfr, "sB", 64), (cfull, cfr, "sC", 64)):
        kb.op("pool", lambda e, tl=tl, P=P: e.memset(tl[:P, 0:3], 0.0), writes=[rs])
        for rq in range(4):
            cc_ = IX[nm + str(rq)]
            kb.gather(tl[:P, 3 + rq * 2048:3 + (rq + 1) * 2048], agF2k, gx[0:P, cc_:cc_ + 1], reads=[gxr, agFr], writes=[rs])
    yTb = Rot(kb, 2, [128, N], BF16, "syTb", es=es)
    xcv = Rot(kb, 2, [128, N], F32, "sxcv", es=es)
    bcv = Rot(kb, 2, [64, N], F32, "sbcv", es=es)
    ccv = Rot(kb, 2, [64, N], F32, "sccv", es=es)
    xs = Rot(kb, 2, [128, N], F32, "sxs", es=es)
    bs = Rot(kb, 2, [64, N], BF16, "sbs", es=es)
    cs = Rot(kb, 2, [64, N], BF16, "scs", es=es)
    xtm = Rot(kb, 2, [128, 128], F32, "sxtm", es=es)
    Xb = Rot(kb, 2, [128, 128], BF16, "sXb", es=es)
    Xd = Rot(kb, 2, [128, 128], BF16, "sXd", es=es)
    Btm = Rot(kb, 2, [128, 64], BF16, "sBtm", es=es)
    CBm = Rot(kb, 2, [128, 128], F32, "sCBm", es=es)
    lh = Rot(kb, 2, [128, 128], F32, "slh", es=es)
    EE = Rot(kb, 2, [128, 128], F32, "sE", es=es)
    MT = Rot(kb, 2, [128, 128], BF16, "sMT", es=es)
    yt = Rot(kb, 2, [128, 8, 128], F32, "syt", es=es)
    pbi = 0
    for j in range(S // N if LEVEL > 0 else 0):
        t0 = j * N
        xt, xr = xfull[:, t0:t0 + N + 3], xfr
        bt, br = bfull[:, t0:t0 + N + 3], bfr
        ctt, ctr = cfull[:, t0:t0 + N + 3], cfr
        xc, xcr = xcv.next(); bc, bcr = bcv.next(); cc, ccr = ccv.next()
        emit_conv(kb, "dve", xc, xt, vt, vr, 0, N, 128, xr, xcr)
        emit_conv(kb, "dve", bc, bt, vt, vr, 5, N, 64, br, bcr)
        emit_conv(kb, "dve", cc, ctt, vt, vr, 10, N, 64, ctr, ccr)
        xst, xsr = xs.next(); bst, bsr = bs.next(); cst_, csr = cs.next()
        kb.op("act", lambda e: e.activation(out=xst[:], in_=xc[:], func=AF.Silu), reads=[xcr], writes=[xsr])
        kb.op("act", lambda e: e.activation(out=bst[:], in_=bc[:], func=AF.Silu), reads=[bcr], writes=[bsr])
        kb.op("act", lambda e: e.activation(out=cst_[:], in_=cc[:], func=AF.Silu), reads=[ccr], writes=[csr])
        ytile, ytr = yt.next()
        for c in range(N // 128 if LEVEL > 1 else 0):
            gc = j * (N // 128) + c
            ts = slice(c * 128, (c + 1) * 128)
            pT, pTr = pbanks.next()
            kb.op("pe", lambda e: e.transpose(pT[:, 0:128], xst[:, ts], ident), reads=[xsr, cr], writes=[pTr])
            xm, xmr = xtm.next()
            kb.op("act", lambda e: e.copy(out=xm[:], in_=pT[:, 0:128]), reads=[pTr], writes=[xmr])
            xb, xbr = Xb.next()
            for h in range(2):
                hs = slice(h * 64, (h + 1) * 64)
                kb.op("dve", lambda e, hs=hs, h=h: e.tensor_scalar(out=xb[:, hs], in0=pT[:, hs], scalar1=dt[:, gc, h:h + 1], scalar2=None, op0=ALU.mult),
                      reads=[pTr, dtr], writes=[xbr])
            xd, xdr = Xd.next()
            for h in range(2):
                hs = slice(h * 64, (h + 1) * 64)
                kb.op("pool", lambda e, hs=hs, h=h: e.tensor_scalar(out=xd[:, hs], in0=xb[:, hs], scalar1=dst[:, gc, h:h + 1], scalar2=None, op0=ALU.mult),
                      reads=[xbr, dsr], writes=[xdr])
            if LEVEL < 3: continue
            pb_t, pb_r = pbf[pbi % len(pbf)]; pbi += 1
            kb.op("pe", lambda e: e.transpose(pb_t[:, 0:64], bst[:, ts], identb[0:64, 0:64]), reads=[bsr, idbr], writes=[pb_r])
            btm, btmr = Btm.next()
            kb.op("act", lambda e: e.copy(out=btm[:], in_=pb_t[:, 0:64]), reads=[pb_r], writes=[btmr])
            if LEVEL < 4: continue
            pcb, pcbr = pbanks.next()
            kb.op("pe", lambda e: e.matmul(pcb[:, 0:128], lhsT=bst[:, ts], rhs=cst_[:, ts], start=True, stop=True), reads=[bsr, csr], writes=[pcbr])
            cbm, cbmr = CBm.next()
            kb.op("dve", lambda e: e.tensor_tensor(out=cbm[:], in0=pcb[:, 0:128], in1=tri, op=ALU.mult), reads=[pcbr, cr], writes=[cbmr])
            for h in range(2 if LEVEL > 4 else 0):
                hs = slice(h * 64, (h + 1) * 64)
                l_, lr_ = lh.next()
                kb.op("dve", lambda e, h=h: e.tensor_scalar(out=l_[:], in0=U, scalar1=aa[:, gc, h:h + 1], scalar2=None, op0=ALU.mult),
                      reads=[cr, aar], writes=[lr_])
                pseg, psegr = pbanks.next()
                kb.op("pe", lambda e: e.matmul(pseg[:, 0:128], lhsT=l_[:], rhs=tri, start=True, stop=True), reads=[lr_, cr], writes=[psegr])
                E, Er = EE.next()
                kb.op("act", lambda e: e.activation(out=E[:], in_=pseg[:, 0:128], func=AF.Exp), reads=[psegr], writes=[Er])
                mt, mtr = MT.next()
                kb.op("dve", lambda e: e.tensor_tensor(out=mt[:], in0=E[:], in1=cbm[:], op=ALU.mult), reads=[Er, cbmr], writes=[mtr])
                py, pyr = pbanks.next()
                kb.op("pe", lambda e, hs=hs: e.matmul(py[:, 0:64], lhsT=mt[:], rhs=xb[:, hs], start=True, stop=True), reads=[mtr, xbr], writes=[pyr])
                po, por = pbanks.next()
                kb.op("pe", lambda e, h=h: e.matmul(po[:, 0:64], lhsT=cst_[:, ts], rhs=prevb[h][:], start=True, stop=True), reads=[csr, prevbr[h]], writes=[por])
                kb.op("act", lambda e, hs=hs: e.copy(out=ytile[:, c, hs], in_=py[:, 0:64]), reads=[pyr], writes=[ytr])
                kb.op("dve", lambda e, hs=hs, h=h: e.scalar_tensor_tensor(out=ytile[:, c, hs], in0=po[:, 0:64], scalar=dout[:, gc, h:h + 1], in1=ytile[:, c, hs],
                                                                          op0=ALU.mult, op1=ALU.add), reads=[por, dor, ytr], writes=[ytr])
                kb.op("dve", lambda e, hs=hs, h=h: e.scalar_tensor_tensor(out=ytile[:, c, hs], in0=xm[:, hs], scalar=rp[:, 4 + h:5 + h], in1=ytile[:, c, hs],
                                                                          op0=ALU.mult, op1=ALU.add), reads=[xmr, rpr, ytr], writes=[ytr])
                pst, pstr = pbanks.next()
                kb.op("pe", lambda e, hs=hs: e.matmul(pst[0:64, 0:64], lhsT=btm[:], rhs=xd[:, hs], start=True, stop=True), reads=[btmr, xdr], writes=[pstr])
                kb.op("dve", lambda e, h=h: e.scalar_tensor_tensor(out=prev[h][:], in0=prev[h][:], scalar=dtot[0:64, gc, h:h + 1], in1=pst[0:64, 0:64],
                                                                  op0=ALU.mult, op1=ALU.add), reads=[pstr, dtor, prevr[h]], writes=[prevr[h]])
                kb.op("act", lambda e, h=h: e.copy(out=prevb[h][:], in_=prev[h][:]), reads=[prevr[h]], writes=[prevbr[h]])
        yb_, ybr = yTb.next()
        for q2 in range(N // 512):
            pyt, pytr = pbanks.next()
            for c4 in range(4):
                c = q2 * 4 + c4
                kb.op("pe", lambda e, c=c, c4=c4: e.transpose(pyt[:, c4 * 128:(c4 + 1) * 128], ytile[:, c, :], ident), reads=[ytr, cr], writes=[pytr])
            kb.op("act", lambda e, q2=q2: e.copy(out=yb_[:, q2 * 512:(q2 + 1) * 512], in_=pyt[:, :]), reads=[pytr], writes=[ybr])
        kb.dma("sp", yT_dst[:, t0:t0 + N], yb_[:], reads=[ybr, yT_r])


def emit_lru2(kb, agF2k, agFr, gx, gxr, IX, vec, wab, wxb, outT, outr, pbanks, es=None):
    N = 1024
    vt = kb.sb([128, 8], F32, "lvec", es=es); vr = Res()
    wa = kb.sb([128, 128], F32, "lwa", es=es); war = Res()
    wx = kb.sb([128, 128], F32, "lwx", es=es); wxr = Res()
    kb.dma("sp", vt[:], vec[:, :], writes=[vr])
    kb.dma("sp", wa[:], wab[:, :], writes=[war])
    kb.dma("sp", wx[:], wxb[:, :], writes=[wxr])
    c1 = kb.sb([128, 1], F32, "lc1", es=es); c1r = Res()
    kb.op("act", lambda e: e.activation(out=c1[:], in_=vt[:, 7:8], func=AF.Exp, scale=-1.0), reads=[vr], writes=[c1r])
    kb.op("act", lambda e: e.activation(out=c1[:], in_=c1[:], func=AF.Ln, bias=1.0), reads=[c1r], writes=[c1r])
    kb.op("dve", lambda e: e.tensor_scalar(out=c1[:], in0=c1[:], scalar1=-8.0, scalar2=None, op0=ALU.mult), reads=[c1r], writes=[c1r])

    xfull = kb.sb([128, S + 3], BF16, "lxfull", es=es); xfr = Res()
    yfull = kb.sb([128, S], BF16, "lyfull", es=es); yfr = Res()
    kb.op("pool", lambda e: e.memset(xfull[:, 0:3], 0.0), writes=[xfr])
    for rp in range(4):
        c1_ = IX["lx%d" % rp]; c2_ = IX["ly%d" % rp]
        kb.gather(xfull[:, 3 + rp * 2048:3 + (rp + 1) * 2048], agF2k, gx[:, c1_:c1_ + 1], reads=[gxr, agFr], writes=[xfr])
        kb.gather(yfull[:, rp * 2048:(rp + 1) * 2048], agF2k, gx[:, c2_:c2_ + 1], reads=[gxr, agFr], writes=[yfr])
    obf = Rot(kb, 2, [128, N], BF16, "lob", es=es)
    xc = Rot(kb, 2, [128, N], F32, "lxc", es=es)
    rr = Rot(kb, 2, [128, N], F32, "lr", es=es)
    ii = Rot(kb, 2, [128, N], F32, "li", es=es)
    aa = Rot(kb, 2, [128, N], F32, "la", es=es)
    qq = Rot(kb, 2, [128, N], F32, "lq", es=es)
    hh = Rot(kb, 2, [128, N], F32, "lh", es=es)
    uu = Rot(kb, 2, [128, N], F32, "lu", es=es)
    hprev = None
    for j in range(S // N):
        t0 = j * N
        xt, xr = xfull[:, t0:t0 + N + 3], xfr
        yt, yr = yfull[:, t0:t0 + N], yfr
        ct, cr = xc.next()
        kb.op("dve", lambda e: e.tensor_scalar(out=ct[:], in0=xt[:, 0:N], scalar1=vt[:, 0:1], scalar2=vt[:, 4:5],
                                               op0=ALU.mult, op1=ALU.add), reads=[xr, vr], writes=[cr])
        for k in range(1, 4):
            kb.op("dve", lambda e, k=k: e.scalar_tensor_tensor(out=ct[:], in0=xt[:, k:k + N], scalar=vt[:, k:k + 1], in1=ct[:],
                                                              op0=ALU.mult, op1=ALU.add), reads=[xr, vr, cr], writes=[cr])
        rt, rres = rr.next()
        it, ires = ii.next()
        for hf in range(N // 512):
            sl = slice(hf * 512, (hf + 1) * 512)
            pa, par = pbanks.next()
            kb.op("pe", lambda e: e.matmul(pa[:, :], lhsT=wa[:], rhs=ct[:, sl], start=True, stop=True), reads=[war, cr], writes=[par])
            kb.op("act", lambda e: e.activation(out=rt[:, sl], in_=pa[:, :], func=AF.Sigmoid, bias=vt[:, 5:6]), reads=[par, vr], writes=[rres])
            px, pxr = pbanks.next()
            kb.op("pe", lambda e: e.matmul(px[:, :], lhsT=wx[:], rhs=ct[:, sl], start=True, stop=True), reads=[wxr, cr], writes=[pxr])
            kb.op("act", lambda e: e.activation(out=it[:, sl], in_=px[:, :], func=AF.Sigmoid, bias=vt[:, 6:7]), reads=[pxr, vr], writes=[ires])
        at, ar = aa.next()
        kb.op("act", lambda e: e.activation(out=at[:], in_=rt[:], func=AF.Exp, scale=c1[:, 0:1]), reads=[rres, c1r], writes=[ar])
        qt, qr = qq.next()
        kb.op("pool", lambda e: e.tensor_tensor(out=qt[:], in0=at[:], in1=at[:], op=ALU.mult), reads=[ar], writes=[qr])
        kb.op("pool", lambda e: e.tensor_scalar(out=qt[:], in0=qt[:], scalar1=-1.0, scalar2=1.0, op0=ALU.mult, op1=ALU.add), reads=[qr], writes=[qr])
        kb.op("act", lambda e: e.activation(out=qt[:], in_=qt[:], func=AF.Sqrt), reads=[qr], writes=[qr])
        kb.op("dve", lambda e: e.tensor_tensor(out=it[:], in0=it[:], in1=ct[:], op=ALU.mult), reads=[ires, cr], writes=[ires])
        kb.op("dve", lambda e: e.tensor_tensor(out=it[:], in0=it[:], in1=qt[:], op=ALU.mult), reads=[ires, qr], writes=[ires])
        ht, hr = hh.next()
        if hprev is None:
            kb.op("dve", lambda e: e.tensor_tensor_scan(out=ht[:], data0=at[:], data1=it[:], initial=0.0, op0=ALU.mult, op1=ALU.add),
                  reads=[ar, ires], writes=[hr])
        else:
            hp, hpr = hprev
            kb.op("dve", lambda e: e.tensor_tensor_scan(out=ht[:], data0=at[:], data1=it[:], initial=hp[:, N - 1:N], op0=ALU.mult, op1=ALU.add),
                  reads=[ar, ires, hpr], writes=[hr])
        hprev = (ht, hr)
        ut, ur = uu.next()
        kb.op("pool", lambda e: e.tensor_tensor(out=ut[:], in0=yt[:], in1=yt[:], op=ALU.mult), reads=[yr], writes=[ur])
        kb.op("pool", lambda e: e.tensor_scalar(out=ut[:], in0=ut[:], scalar1=0.044715, scalar2=1.0, op0=ALU.mult, op1=ALU.add), reads=[ur], writes=[ur])
        kb.op("pool", lambda e: e.tensor_tensor(out=ut[:], in0=ut[:], in1=yt[:], op=ALU.mult), reads=[ur, yr], writes=[ur])
        kb.op("act", lambda e: e.activation(out=ut[:], in_=ut[:], func=AF.Sigmoid, scale=1.5957691216057308), reads=[ur], writes=[ur])
        kb.op("pool", lambda e: e.tensor_tensor(out=ut[:], in0=ut[:], in1=yt[:], op=ALU.mult), reads=[ur, yr], writes=[ur])
        ob_, obr_ = obf.next()
        kb.op("pool", lambda e: e.tensor_tensor(out=ob_[:], in0=ut[:], in1=ht[:], op=ALU.mult), reads=[ur, hr], writes=[obr_])
        kb.dma("sp", outT[:, t0:t0 + N], ob_[:], reads=[obr_, outr])


def emit_C2(kb, xT, agN512, agNr, agY2k, agYr, agL2k, agLr, gx, gxr, IX, pT, w_in, pnsa, pssd, plru, w_out, pwg, pwp, rw, ewg, ewu, ewd, cst, x2T, x2r, final_out, banks, es, xTr=None):
    gen = Rot.__new__(Rot); gen.t = banks; gen.i = 0
    sb = lambda shape, dt, name: kb.sb(shape, dt, name, es=es)
    cv = sb([128, CW], F32, "cc"); cvr = Res()
    kb.dma("sp", cv[:], cst[:, :], writes=[cvr])
    ones1k = cv[:, 52:180]; ones512 = cv[:, 180:308]; ident = cv[:, 308:436]
    selE = lambda e: cv[0:16, 436 + e * 128:436 + (e + 1) * 128]
    rb = cv[:, 36:52]

    tmpf = Rot(kb, 4, [128, 512], F32, "ctf", es=es)
    stat = Rot(kb, 3, [128, 512], F32, "cstat", es=es)
    kb.uid += 1
    mixD = kb.dram("mixD%d" % kb.uid, [D, TOK], BF16, "Internal")
    mixDr = [Res() for _ in range(8)]
    wst_box = [None]

    def load_w(src_rows, nk, ncols):
        wt, wr = wst_box[0].next()
        kb.dma("pool", wt[:, 0:nk, 0:ncols], src_rows.rearrange("(c p) f -> p c f", p=128), writes=[wr])
        return wt, wr

    def ln_stats(src, srcr, nchunk, tt, ones_ap, eps, sq_eng="pool"):
        ts = slice(tt * 512, (tt + 1) * 512)
        pm_, pmr = gen.next()
        for c in range(nchunk):
            kb.op("pe", lambda e, c=c: e.matmul(pm_[:, :], lhsT=ones_ap, rhs=src[:, c, ts], start=(c == 0), stop=(c == nchunk - 1)), reads=[cvr, srcr[c]], writes=[pmr])
        pq_, pqr = gen.next()
        for c in range(nchunk):
            sq, sqr = tmpf.next()
            kb.op(sq_eng, lambda e, c=c: e.tensor_tensor(out=sq[:], in0=src[:, c, ts], in1=src[:, c, ts], op=ALU.mult), reads=[srcr[c]], writes=[sqr])
            kb.op("pe", lambda e, c=c: e.matmul(pq_[:, :], lhsT=ones_ap, rhs=sq[:], start=(c == 0), stop=(c == nchunk - 1)), reads=[cvr, sqr], writes=[pqr])
        mean, meanr = stat.next()
        kb.op("act", lambda e: e.copy(out=mean[:], in_=pm_[:, :]), reads=[pmr], writes=[meanr])
        rstd, rstdr = stat.next()
        return mean, meanr, pq_, pqr, rstd, rstdr

    with contextlib.ExitStack() as es1:
        sb1 = lambda shape, dt, name: kb.sb(shape, dt, name, es=es1)
        wst_box[0] = Rot.__new__(Rot); wst_box[0].i = 0
        wst_box[0].t = [(sb1([128, 8, 512], BF16, "cw"), Res()) for _ in range(3)]
        xbf = sb1([128, 8, TOK], BF16, "cxbf"); xbr = [Res() for _ in range(8)]
        for c in range(8):
            kb.dma("pool", xbf[:, c, :], xT[c * 128:(c + 1) * 128, :], reads=([xTr[c]] if xTr else []), writes=[xbr[c]])
        ob = [sb1([128, 4, TOK], BF16, "cob%d" % i) for i in range(3)]
        obr = [[Res() for _ in range(4)] for _ in range(3)]
        for c in range(4):
            for q4 in range(4):
                cn = IX["on%d_%d" % (c, q4)]
                kb.gather(ob[0][:, c, q4 * 512:(q4 + 1) * 512], agN512, gx[:, cn:cn + 1], reads=[gxr, agNr], writes=[obr[0][c]])
            cl = IX["l%d" % c]
            kb.gather(ob[2][:, c, :], agL2k, gx[:, cl:cl + 1], reads=[gxr, agLr], writes=[obr[2][c]])
        with contextlib.ExitStack() as es1b:
            yg = kb.sb([128, 4, TOK], F32, "cyg", es=es1b); ygr = [Res() for _ in range(4)]
            ygb = ob[1]; ygbr = obr[1]
            for c in range(4):
                cy = IX["y%d" % c]
                kb.gather(ygb[:, c, :], agY2k, gx[:, cy:cy + 1], reads=[gxr, agYr], writes=[ygbr[c]])
            wz, wzr = load_w(w_in[:, OFF_Z:OFF_Z + 512], 8, 512)
            for tt in range(NT):
                ts = slice(tt * 512, (tt + 1) * 512)
                for fc in range(4):
                    pz, pzr = gen.next()
                    for c in range(8):
                        kb.op("pe", lambda e, c=c: e.matmul(pz[:, :], lhsT=wz[:, c, fc * 128:(fc + 1) * 128], rhs=xbf[:, c, ts], start=(c == 0), stop=(c == 7)),
                              reads=[wzr, xbr[c]], writes=[pzr])
                    sz, szr = tmpf.next()
                    kb.op("act", lambda e: e.activation(out=sz[:], in_=pz[:, :], func=AF.Silu), reads=[pzr], writes=[szr])
                    kb.op("dve", lambda e, fc=fc: e.tensor_tensor(out=yg[:, fc, ts], in0=ygb[:, fc, ts], in1=sz[:], op=ALU.mult), reads=[szr, ygbr[fc]], writes=[ygr[fc]])
                pq_, pqr = gen.next()
                for c in range(4):
                    sq, sqr = tmpf.next()
                    kb.op("pool", lambda e, c=c: e.tensor_tensor(out=sq[:], in0=yg[:, c, ts], in1=yg[:, c, ts], op=ALU.mult), reads=[ygr[c]], writes=[sqr])
                    kb.op("pe", lambda e, c=c: e.matmul(pq_[:, :], lhsT=ones512, rhs=sq[:], start=(c == 0), stop=(c == 3)), reads=[cvr, sqr], writes=[pqr])
                rstd, rstdr = stat.next()
                kb.op("dve", lambda e: e.tensor_scalar(out=rstd[:], in0=pq_[:, :], scalar1=1e-5, scalar2=None, op0=ALU.add), reads=[pqr], writes=[rstdr])
                kb.op("act", lambda e: e.activation(out=rstd[:], in_=rstd[:], func=AF.Sqrt), reads=[rstdr], writes=[rstdr])
                kb.op("dve", lambda e: e.reciprocal(out=rstd[:], in_=rstd[:]), reads=[rstdr], writes=[rstdr])
                for c in range(4):
                    t1, t1r = tmpf.next()
                    kb.op("dve", lambda e, c=c: e.tensor_tensor(out=t1[:], in0=yg[:, c, ts], in1=rstd[:], op=ALU.mult), reads=[ygr[c], rstdr], writes=[t1r])
                    kb.op("act", lambda e, c=c: e.activation(out=ob[1][:, c, ts], in_=t1[:], func=AF.Copy, scale=cv[:, 32 + c:33 + c]), reads=[t1r, cvr], writes=[obr[1][c]])
            kb.barrier()
        if CDBG == "ossd":
            dbg = sb1([128, 4, TOK], F32, "cdbg"); dbgr = Res()
            for c in range(4):
                kb.op("dve", lambda e, c=c: e.tensor_copy(out=dbg[:, c, :], in_=ob[1][:, c, :]), reads=[obr[1][c]], writes=[dbgr])
                kb.dma("sp", x2T[c * 128:(c + 1) * 128, :], dbg[:, c, :], reads=[dbgr], final=True)
            kb.barrier()
            return
        projs = [pnsa, pssd, plru]
        mixh = sb1([128, 4, TOK], BF16, "cmixh"); mixhr = [Res() for _ in range(4)]
        for half in range(2):
            fs = slice(half * 512, (half + 1) * 512)
            for br in range(3):
                wp, wpr = load_w(projs[br][:, fs], 4, 512)
                wm, wmr = load_w(w_in[:, OFF_MERGE + br * 1024 + half * 512: OFF_MERGE + br * 1024 + (half + 1) * 512], 8, 512)
                for fc in range(4):
                    oc = half * 4 + fc
                    for tt in range(NT):
                        ts = slice(tt * 512, (tt + 1) * 512)
                        pg, pgr = gen.next()
                        for c in range(8):
                            kb.op("pe", lambda e, c=c: e.matmul(pg[:, :], lhsT=wm[:, c, fc * 128:(fc + 1) * 128], rhs=xbf[:, c, ts], start=(c == 0), stop=(c == 7)),
                                  reads=[wmr, xbr[c]], writes=[pgr])
                        pp, ppr = gen.next()
                        for c in range(4):
                            kb.op("pe", lambda e, c=c: e.matmul(pp[:, :], lhsT=wp[:, c, fc * 128:(fc + 1) * 128], rhs=ob[br][:, c, ts], start=(c == 0), stop=(c == 3)),
                                  reads=[wpr, obr[br][c]], writes=[ppr])
                        sg, sgr = tmpf.next()
                        kb.op("act", lambda e: e.activation(out=sg[:], in_=pg[:, :], func=AF.Sigmoid), reads=[pgr], writes=[sgr])
                        if br == 0:
                            kb.op("dve", lambda e: e.tensor_tensor(out=mixh[:, fc, ts], in0=pp[:, :], in1=sg[:], op=ALU.mult), reads=[ppr, sgr], writes=[mixhr[fc]])
                        else:
                            kb.op("dve", lambda e: e.tensor_tensor(out=sg[:], in0=pp[:, :], in1=sg[:], op=ALU.mult), reads=[ppr, sgr], writes=[sgr])
                            kb.op("pool", lambda e: e.tensor_tensor(out=mixh[:, fc, ts], in0=mixh[:, fc, ts], in1=sg[:], op=ALU.add), reads=[sgr, mixhr[fc]], writes=[mixhr[fc]])
            for fc in range(4):
                kb.dma("sp", mixD[(half * 4 + fc) * 128:(half * 4 + fc + 1) * 128, :], mixh[:, fc, :], reads=[mixhr[fc]], writes=[mixDr[half * 4 + fc]])
        kb.barrier()

    with contextlib.ExitStack() as es2:
        sb2 = lambda shape, dt, name: kb.sb(shape, dt, name, es=es2)
        r = sb2([128, 8, TOK], F32, "cr"); rr = [Res() for _ in range(8)]
        x1b = sb2([128, 8, TOK], BF16, "cx1b"); x1br = [Res() for _ in range(8)]
        for c in range(8):
            kb.dma("sp", r[:, c, :], xT[c * 128:(c + 1) * 128, :], reads=([xTr[c]] if xTr else []), writes=[rr[c]])
        gatesT = sb2([16, TOK], F32, "cgT"); gTr = Res()
        rwt = sb2([128, 8, 16], F32, "crw"); rwr = Res()
        kb.dma("sp", rwt[:], rw.rearrange("(c p) e -> p c e", p=128), writes=[rwr])
        pad = sb2([128, 4, 8], F32, "cpad"); padr = Res()
        kb.op("pool", lambda e: e.memset(pad[:], -1e30), writes=[padr])
        rt = Rot(kb, 2, [128, 16], F32, "crt", es=es2)
        rt2 = Rot(kb, 2, [128, 16], F32, "crt2", es=es2)
        t8 = Rot(kb, 2, [128, 4, 8], F32, "ct8", es=es2)
        sm4 = Rot(kb, 4, [128, 4], F32, "csm4", es=es2)
        sm1 = Rot(kb, 4, [128, 1], F32, "csm1", es=es2)
        t8b = Rot(kb, 2, [128, 8], F32, "ct8b", es=es2)
        es2a = contextlib.ExitStack()
        wst_box[0] = Rot.__new__(Rot); wst_box[0].i = 0
        wst_box[0].t = [(kb.sb([128, 8, 512], BF16, "cw2", es=es2a), Res()) for _ in range(2)]
        mixed = kb.sb([128, 8, TOK], BF16, "cmixed", es=es2a); mixr = [Res() for _ in range(8)]
        pb_ = kb.sb([128, 2, TOK], BF16, "cpb", es=es2a); pbr = [Res() for _ in range(2)]
        for c in range(8):
            kb.dma("sp", mixed[:, c, :], mixD[c * 128:(c + 1) * 128, :], reads=[mixDr[c]], writes=[mixr[c]])
        if CDBG == "mixed":
            for c in range(8):
                kb.op("dve", lambda e, c=c: e.tensor_copy(out=r[:, c, :], in_=mixed[:, c, :]), reads=[mixr[c], rr[c]], writes=[rr[c]])
                kb.dma("sp", x2T[c * 128:(c + 1) * 128, :], r[:, c, :], reads=[rr[c]], final=True)
            kb.barrier(); es2a.close()
            return
        for half in range(2):
            wo, wor = load_w(w_out[:, half * 512:(half + 1) * 512], 8, 512)
            for fc in range(4):
                oc = half * 4 + fc
                for tt in range(NT):
                    ts = slice(tt * 512, (tt + 1) * 512)
                    pu, pur = gen.next()
                    for c in range(8):
                        kb.op("pe", lambda e, c=c: e.matmul(pu[:, :], lhsT=wo[:, c, fc * 128:(fc + 1) * 128], rhs=mixed[:, c, ts], start=(c == 0), stop=(c == 7)),
                              reads=[wor, mixr[c]], writes=[pur])
                    kb.op("dve", lambda e: e.scalar_tensor_tensor(out=r[:, oc, ts], in0=r[:, oc, ts], scalar=ALPHA, in1=pu[:, :], op0=ALU.mult, op1=ALU.add),
                          reads=[pur, rr[oc]], writes=[rr[oc]])

        def layer_norm(src, srcr, gcol, bcol, dst_b, dst_br):
            for tt in range(NT):
                ts = slice(tt * 512, (tt + 1) * 512)
                mean, meanr, pq_, pqr, rstd, rstdr = ln_stats(src, srcr, 8, tt, ones1k, 1e-5)
                m2, m2r = stat.next()
                kb.op("pool", lambda e: e.tensor_tensor(out=m2[:], in0=mean[:], in1=mean[:], op=ALU.mult), reads=[meanr], writes=[m2r])
                kb.op("dve", lambda e: e.scalar_tensor_tensor(out=rstd[:], in0=pq_[:, :], scalar=1e-5, in1=m2[:], op0=ALU.add, op1=ALU.subtract), reads=[pqr, m2r], writes=[rstdr])
                kb.op("act", lambda e: e.activation(out=rstd[:], in_=rstd[:], func=AF.Sqrt), reads=[rstdr], writes=[rstdr])
                kb.op("dve", lambda e: e.reciprocal(out=rstd[:], in_=rstd[:]), reads=[rstdr], writes=[rstdr])
                for c in range(8):
                    kb.op("dve", lambda e, c=c: e.tensor_tensor(out=src[:, c, ts], in0=src[:, c, ts], in1=mean[:], op=ALU.subtract), reads=[meanr, srcr[c]], writes=[srcr[c]])
                    kb.op("pool", lambda e, c=c: e.tensor_tensor(out=src[:, c, ts], in0=src[:, c, ts], in1=rstd[:], op=ALU.mult), reads=[rstdr, srcr[c]], writes=[srcr[c]])
                    kb.op("dve", lambda e, c=c: e.tensor_scalar(out=src[:, c, ts], in0=src[:, c, ts], scalar1=cv[:, gcol + c:gcol + c + 1], scalar2=cv[:, bcol + c:bcol + c + 1],
                                                               op0=ALU.mult, op1=ALU.add), reads=[cvr, srcr[c]], writes=[srcr[c]])
                    if dst_b is not None:
                        kb.op("act", lambda e, c=c: e.copy(out=dst_b[:, c, ts], in_=src[:, c, ts]), reads=[srcr[c]], writes=[dst_br[c]])

        layer_norm(r, rr, 0, 8, x1b, x1br)
        if CDBG == "x1":
            for c in range(8):
                kb.dma("sp", x2T[c * 128:(c + 1) * 128, :], r[:, c, :], reads=[rr[c]], final=True)
            kb.barrier(); es2a.close()
            return

        for s_ in range(TOK // 128):
            ss = slice(s_ * 128, (s_ + 1) * 128)
            pl, plr = gen.next()
            for c in range(8):
                kb.op("pe", lambda e, c=c: e.matmul(pl[:, 0:16], lhsT=r[:, c, ss], rhs=rwt[:, c, :], start=(c == 0), stop=(c == 7)), reads=[rr[c], rwr], writes=[plr])
            aff, affr = rt.next()
            kb.op("act", lambda e: e.activation(out=aff[:], in_=pl[:, 0:16], func=AF.Sigmoid), reads=[plr], writes=[affr])
            sel, selr = rt2.next()
            kb.op("dve", lambda e: e.tensor_tensor(out=sel[:], in0=aff[:], in1=rb, op=ALU.add), reads=[affr, cvr], writes=[selr])
            kb.op("dve", lambda e: e.tensor_copy(out=pad[:, :, 0:4], in_=sel[:].rearrange("p (g k) -> p g k", g=4)), reads=[selr, padr], writes=[padr])
            tp, tpr = t8.next()
            for g in range(4):
                kb.op("dve", lambda e, g=g: e.max(out=tp[:, g, :], in_=pad[:, g, :]), reads=[padr], writes=[tpr])
            gs, gsr = sm4.next()
            kb.op("dve", lambda e: e.tensor_tensor(out=gs[:], in0=tp[:, :, 0], in1=tp[:, :, 1], op=ALU.add), reads=[tpr], writes=[gsr])
            gm, gmr = sm1.next()
            kb.op("dve", lambda e: e.reduce_max(out=gm[:], in_=gs[:], axis=AX.X), reads=[gsr], writes=[gmr])
            isb, isbr = sm4.next()
            kb.op("dve", lambda e: e.tensor_scalar(out=isb[:], in0=gs[:], scalar1=gm[:, 0:1], scalar2=None, op0=ALU.is_ge), reads=[gsr, gmr], writes=[isbr])
            off, offr = sm4.next()
            kb.op("dve", lambda e: e.tensor_scalar(out=off[:], in0=isb[:], scalar1=1e9, scalar2=-1e9, op0=ALU.mult, op1=ALU.add), reads=[isbr], writes=[offr])
            msk, mskr = rt2.next()
            for g in range(4):
                kb.op("dve", lambda e, g=g: e.tensor_scalar(out=msk[:, g * 4:(g + 1) * 4], in0=sel[:, g * 4:(g + 1) * 4], scalar1=isb[:, g:g + 1], scalar2=off[:, g:g + 1],
                                                            op0=ALU.mult, op1=ALU.add), reads=[selr, isbr, offr], writes=[mskr])
            tb, tbr = t8b.next()
            kb.op("dve", lambda e: e.max(out=tb[:], in_=msk[:]), reads=[mskr], writes=[tbr])
            kb.op("dve", lambda e: e.tensor_scalar(out=msk[:], in0=msk[:], scalar1=tb[:, 1:2], scalar2=None, op0=ALU.is_ge), reads=[mskr, tbr], writes=[mskr])
            kb.op("dve", lambda e: e.tensor_tensor(out=msk[:], in0=msk[:], in1=aff[:], op=ALU.mult), reads=[mskr, affr], writes=[mskr])
            ws, wsr = sm1.next()
            kb.op("dve", lambda e: e.reduce_sum(out=ws[:], in_=msk[:], axis=AX.X), reads=[mskr], writes=[wsr])
            kb.op("dve", lambda e: e.reciprocal(out=ws[:], in_=ws[:]), reads=[wsr], writes=[wsr])
            kb.op("dve", lambda e: e.tensor_scalar(out=msk[:], in0=msk[:], scalar1=ws[:, 0:1], scalar2=None, op0=ALU.mult), reads=[mskr, wsr], writes=[mskr])
            pt, ptr_ = gen.next()
            kb.op("pe", lambda e: e.transpose(pt[0:16, 0:128], msk[:], ident), reads=[mskr, cvr], writes=[ptr_])
            kb.op("act", lambda e: e.copy(out=gatesT[:, ss], in_=pt[0:16, 0:128]), reads=[ptr_], writes=[gTr])

        for c in range(2):
            kb.dma("pool", pb_[:, c, :], pT[c * 128:(c + 1) * 128, :], writes=[pbr[c]])
        for half in range(2):
            fs = slice(half * 512, (half + 1) * 512)
            wg_, wgr = load_w(pwg[:, fs], 8, 512)
            wp_, wpr = load_w(pwp[:, fs], 2, 512)
            for fc in range(4):
                oc = half * 4 + fc
                for tt in range(NT):
                    ts = slice(tt * 512, (tt + 1) * 512)
                    pg, pgr = gen.next()
                    for c in range(8):
                        kb.op("pe", lambda e, c=c: e.matmul(pg[:, :], lhsT=wg_[:, c, fc * 128:(fc + 1) * 128], rhs=x1b[:, c, ts], start=(c == 0), stop=(c == 7)),
                              reads=[wgr, x1br[c]], writes=[pgr])
                    pp, ppr = gen.next()
                    for c in range(2):
                        kb.op("pe", lambda e, c=c: e.matmul(pp[:, :], lhsT=wp_[:, c, fc * 128:(fc + 1) * 128], rhs=pb_[:, c, ts], start=(c == 0), stop=(c == 1)),
                              reads=[wpr, pbr[c]], writes=[ppr])
                    sg, sgr = tmpf.next()
                    kb.op("act", lambda e: e.activation(out=sg[:], in_=pg[:, :], func=AF.Sigmoid), reads=[pgr], writes=[sgr])
                    kb.op("dve", lambda e: e.tensor_tensor(out=sg[:], in0=pp[:, :], in1=sg[:], op=ALU.mult), reads=[ppr, sgr], writes=[sgr])
                    kb.op("dve", lambda e: e.scalar_tensor_tensor(out=r[:, oc, ts], in0=r[:, oc, ts], scalar=ALPHA, in1=sg[:], op0=ALU.mult, op1=ALU.add),
                          reads=[sgr, rr[oc]], writes=[rr[oc]])

        kb.barrier()
        es2a.close()
        if CDBG == "ple":
            for c in range(8):
                kb.dma("sp", x2T[c * 128:(c + 1) * 128, :], r[:, c, :], reads=[rr[c]], final=True)
            return
        ew = Rot(kb, 2, [128, 2, 3, 8, 256], BF16, "cew", es=es2)
        hb = Rot(kb, 2, [128, 4, 512], BF16, "chb", es=es2)
        steps = [(ep, tt) for ep in range(8) for tt in range(NT)]
        live = {}
        wts = {}

        def load_pair(ep):
            wt, wr = ew.next()
            for m in range(2):
                e_ = ep * 2 + m
                kb.dma("pool", wt[:, m, 0, :, :], ewg[e_, :, :].rearrange("(c p) f -> p c f", p=128), writes=[wr])
                kb.dma("pool", wt[:, m, 1, :, :], ewu[e_, :, :].rearrange("(c p) f -> p c f", p=128), writes=[wr])
                kb.dma("pool", wt[:, m, 2, :, :].rearrange("p (c a) f -> p c (a f)", c=2), ewd[e_, :, :].rearrange("(c p) f -> p c f", p=128), writes=[wr])
            return wt, wr

        for idx in range(len(steps) + 1):
            if idx < len(steps):
                ep, tt = steps[idx]
                if tt == 0:
                    if ep == 0:
                        wts[0] = load_pair(0)
                    wt, wr = wts[ep]
                ts = slice(tt * 512, (tt + 1) * 512)
                h, hr = hb.next()
                for m in range(2):
                    e_ = ep * 2 + m
                    pgb, pgbr = gen.next()
                    kb.op("pe", lambda e, e_=e_, pgb=pgb, ts=ts: e.matmul(pgb[:, :], lhsT=selE(e_), rhs=gatesT[:, ts], start=True, stop=True), reads=[cvr, gTr], writes=[pgbr])
                    gb, gbr = tmpf.next()
                    kb.op("act", lambda e, gb=gb, pgb=pgb: e.copy(out=gb[:], in_=pgb[:, :]), reads=[pgbr], writes=[gbr])
                    for fc in range(2):
                        pg, pgr = gen.next()
                        for c in range(8):
                            kb.op("pe", lambda e, c=c, m=m, fc=fc, pg=pg, wt=wt, ts=ts: e.matmul(pg[:, :], lhsT=wt[:, m, 0, c, fc * 128:(fc + 1) * 128], rhs=x1b[:, c, ts], start=(c == 0), stop=(c == 7)),
                                  reads=[wr, x1br[c]], writes=[pgr])
                        pu, pur = gen.next()
                        for c in range(8):
                            kb.op("pe", lambda e, c=c, m=m, fc=fc, pu=pu, wt=wt, ts=ts: e.matmul(pu[:, :], lhsT=wt[:, m, 1, c, fc * 128:(fc + 1) * 128], rhs=x1b[:, c, ts], start=(c == 0), stop=(c == 7)),
                                  reads=[wr, x1br[c]], writes=[pur])
                        sg, sgr = tmpf.next()
                        kb.op("act", lambda e, sg=sg, pg=pg: e.activation(out=sg[:], in_=pg[:, :], func=AF.Silu), reads=[pgr], writes=[sgr])
                        kb.op("dve", lambda e, sg=sg, pu=pu: e.tensor_tensor(out=sg[:], in0=pu[:, :], in1=sg[:], op=ALU.mult), reads=[pur, sgr], writes=[sgr])
                        kb.op("pool", lambda e, m=m, fc=fc, h=h, sg=sg, gb=gb: e.tensor_tensor(out=h[:, m * 2 + fc, :], in0=sg[:], in1=gb[:], op=ALU.mult), reads=[sgr, gbr], writes=[hr])
                live[idx] = (wt, wr, h, hr, ts)
            if idx >= 1:
                wt_, wr_, h_, hr_, ts_ = live.pop(idx - 1)
                for dc in range(8):
                    py, pyr = gen.next()
                    for m in range(2):
                        wd = wt_[:, m, 2, :, :].rearrange("p (c a) f -> p c (a f)", c=2)
                        for fc in range(2):
                            kb.op("pe", lambda e, wd=wd, m=m, fc=fc, py=py, h_=h_, dc=dc: e.matmul(py[:, :], lhsT=wd[:, fc, dc * 128:(dc + 1) * 128], rhs=h_[:, m * 2 + fc, :],
                                                                                                  start=(m == 0 and fc == 0), stop=(m == 1 and fc == 1)), reads=[wr_, hr_], writes=[pyr])
                    kb.op("dve", lambda e, dc=dc, py=py, ts_=ts_: e.tensor_tensor(out=r[:, dc, ts_], in0=r[:, dc, ts_], in1=py[:, :], op=ALU.add), reads=[pyr, rr[dc]], writes=[rr[dc]])
            if idx < len(steps) and steps[idx][1] == 0 and steps[idx][0] + 1 < 8:
                wts[steps[idx][0] + 1] = load_pair(steps[idx][0] + 1)

        if CDBG != "moe":
            layer_norm(r, rr, 16, 24, None, None)
        for c in range(8):
            kb.dma("sp", x2T[c * 128:(c + 1) * 128, :], r[:, c, :], reads=[rr[c]], writes=[x2r[c]], final=final_out)
        kb.barrier()


NF = 3104
NOM_TILES = [0, 2, 4, 6, 8, 10, 12, 14]
BATCH = 2
T_ALL = BATCH * S
NCORES = 8
U32 = mybir.dt.uint32


def idx_cols():
    names = []
    for nm in ("kcmp", "vcmp", "ksel", "vsel", "kwin", "vwin", "sx", "sB", "sC", "dt", "lx", "ly"):
        names += [nm + str(rp) for rp in range(4)]
    for k in range(8):
        names += ["q%d_%d" % (k, j) for j in range(4)]
        names.append("gate%d" % k)
    for c in range(4):
        names += ["on%d_%d" % (c, q4) for q4 in range(4)]
        names.append("y%d" % c)
        names.append("l%d" % c)
    return {n: i for i, n in enumerate(names)}


IX = idx_cols()
NIDX = len(IX)


def make_gidx(r):
    g, par = r // 2, r % 2
    t = np.zeros((128, NIDX), np.int64)
    p = np.arange(128)
    def rowF(rp, f):
        f = np.asarray(f)
        k = f // 256
        rows_k = np.where(k < 12, 256, 32)
        return k * 1024 + rp * rows_k + (f - 256 * k)
    rowN = lambda rank, f256: (f256 // 128) * 512 + rank * 128 + f256 % 128
    rowY = lambda rank, ch: (ch // 64) * 256 + rank * 64 + ch % 64
    for rp in range(4):
        for nm, f0 in (("kcmp", 512), ("vcmp", 640), ("ksel", 768), ("vsel", 896), ("kwin", 1024), ("vwin", 1152)):
            t[:, IX[nm + str(rp)]] = rowF(rp, f0 + 64 * g + p)
        t[:, IX["sx%d" % rp]] = rowF(rp, 1304 + 128 * r + p)
        t[:, IX["sB%d" % rp]] = rowF(rp, 1304 + 512 + 64 * g + p)
        t[:, IX["sC%d" % rp]] = rowF(rp, 1304 + 640 + 64 * g + p)
        t[:, IX["dt%d" % rp]] = rp * 8 + 2 * r + p
        t[:, IX["lx%d" % rp]] = rowF(rp, 2080 + 128 * r + p)
        t[:, IX["ly%d" % rp]] = rowF(rp, 2080 + 512 + 128 * r + p)
    for k in range(8):
        i = 2 * k + par
        rp, q4 = i // 4, i % 4
        for j in range(4):
            t[:, IX["q%d_%d" % (k, j)]] = rowF(rp, g * 256 + j * 64 + p) * 4 + q4
        t[:, IX["gate%d" % k]] = rowF(rp, 1280 + 12 * g + p) * 4 + q4
    for c in range(4):
        gg, f256 = c // 2, (c % 2) * 128 + p
        for q4 in range(4):
            i = 4 * r + q4
            t[:, IX["on%d_%d" % (c, q4)]] = rowN(2 * gg + i % 2, f256) * 8 + i // 2
        t[:, IX["y%d" % c]] = rowY(c, p) * 4 + r
        t[:, IX["l%d" % c]] = rowY(c, p) * 4 + r
    t = np.clip(t, 0, None)
    return t.astype(np.uint32)


def emit_A2(kb, xT, xTr, w, agF, agFr, agD, agDr, banks, es):
    xs = kb.sb([128, 8, TOK], BF16, "xs", es=es)
    xs_r = [Res("xs") for _ in range(8)]
    for c in range(8):
        kb.dma("pool", xs[:, c, :], xT[c * 128:(c + 1) * 128, :], reads=([xTr[c]] if xTr else []), writes=[xs_r[c]])
    FB = 512
    wbuf = [(kb.sb([128, 8, FB], BF16, "wb", es=es), Res("wb")) for _ in range(2)]
    obuf = [(kb.sb([128, 512], BF16, "ob", es=es), Res("ob")) for _ in range(4)]
    obf = [(kb.sb([8, 512], F32, "obd", es=es), Res("obd")) for _ in range(2)]
    wv = w.rearrange("(c p) f -> p c f", p=128)
    it = 0
    nb = 0
    for (c0, ncols, r0) in ((0, 1304, 0), (1816, 1800, 1304)):
        for fb in range((ncols + FB - 1) // FB):
            f0 = fb * FB
            fw_ = min(FB, ncols - f0)
            wt, wr = wbuf[nb % 2]
            nb += 1
            kb.dma("pool", wt[:, :, :fw_], wv[:, :, c0 + f0:c0 + f0 + fw_], writes=[wr])
            for fc in range((fw_ + 127) // 128):
                m = min(128, fw_ - fc * 128)
                for tt in range(TOK // 512):
                    pt, pr = banks[it % 4]
                    ot, orr = obuf[it % 4]
                    for c in range(8):
                        kb.op("pe", lambda e, c=c: e.matmul(pt[:m, :], lhsT=wt[:, c, fc * 128:fc * 128 + m], rhs=xs[:, c, tt * 512:(tt + 1) * 512],
                                                             start=(c == 0), stop=(c == 7)), reads=[wr, xs_r[c]], writes=[pr])
                    if it % 2 == 0:
                        kb.op("act", lambda e: e.copy(out=ot[:m, :], in_=pt[:m, :]), reads=[pr], writes=[orr])
                    else:
                        kb.op("dve", lambda e: e.tensor_copy(out=ot[:m, :], in_=pt[:m, :]), reads=[pr], writes=[orr])
                    row = r0 + f0 + fc * 128
                    kb.dma("sp", agF[row:row + m, tt * 512:(tt + 1) * 512], ot[:m, :], reads=[orr, agFr])
                    it += 1
    wt, wr = wbuf[nb % 2]
    kb.dma("pool", wt[:, :, 0:8], wv[:, :, 2584:2592], writes=[wr])
    for tt in range(TOK // 512):
        pt, pr = banks[it % 4]
        it += 1
        for c in range(8):
            kb.op("pe", lambda e, c=c: e.matmul(pt[0:8, :], lhsT=wt[:, c, 0:8], rhs=xs[:, c, tt * 512:(tt + 1) * 512], start=(c == 0), stop=(c == 7)),
                  reads=[wr, xs_r[c]], writes=[pr])
        od, odr = obf[tt % 2]
        kb.op("act", lambda e: e.copy(out=od[:, :], in_=pt[0:8, :]), reads=[pr], writes=[odr])
        kb.dma("sp", agD[:, tt * 512:(tt + 1) * 512], od[:, :], reads=[odr, agDr])


def build_fused(nlayers=2, groups=None, ncores=8, fstop=9, ntiles=8):
    groups = groups or [[0, 1, 2, 3], [4, 5, 6, 7]]
    nc = bass.Bass("TRN2", target_bir_lowering=False)
    es = contextlib.ExitStack()
    with es:
        kb = KB(nc, es)
        I = lambda n, s, dt=F32: kb.dram(n, s, dt, "ExternalInput")
        xT0 = I("xT0", [D, TOK]); pT = I("pT", [2, 256, TOK]); gidx = I("gidx", [128, NIDX], U32)
        w_in = I("w_in", [2, D, 6688])
        peT = I("npeT", [2, 2, 64, 32]); w1 = I("nw1", [2, 2, 2048, 256]); w2 = I("nw2", [2, 2, 256, 64])
        nmask = I("nmask", [128, NMASK, 512]); nE0 = I("nE0", [128, S]); nmisc = I("nmisc", [128, MISC_W])
        svec = I("svec", [2, 128, 16]); srep = I("srep", [2, 128, 8]); scst = I("scst", [128, 4, 128])
        lvec = I("lvec", [2, 128, 8]); lwab = I("lwab", [2, 128, 128]); lwxb = I("lwxb", [2, 128, 128])
        pnsa = I("proj_nsa", [2, 512, D]); pssd = I("proj_ssd", [2, 512, D]); plru = I("proj_lru", [2, 512, D])
        w_out = I("w_out", [2, D, D]); pwg = I("ple_w_gate", [2, D, D]); pwp = I("ple_w_proj", [2, 256, D]); rw = I("router_w", [D, 16])
        ewg = I("exp_w_gate", [2, 16, D, 256]); ewu = I("exp_w_up", [2, 16, D, 256]); ewd = I("exp_w_down", [2, 16, 256, D])
        ccst = I("ccst", [2, 128, CW])
        x2T = kb.dram("x2T", [D, TOK], F32, "ExternalOutput")
        N_ = lambda n, s, dt: kb.dram(n, s, dt, "Internal")
        agF_in = N_("agF_in", [NF, TOK], BF16); agF_out = N_("agF_out", [4 * NF, TOK], BF16)
        agD_in = N_("agD_in", [8, TOK], F32); agD_out = N_("agD_out", [32, TOK], F32)
        nq = 8 * QT
        agN_in = N_("agN_in", [256, nq], BF16); agN_out = N_("agN_out", [1024, nq], BF16)
        agY_in = N_("agY_in", [128, S], BF16); agY_out = N_("agY_out", [512, S], BF16)
        agL_in = N_("agL_in", [128, S], BF16); agL_out = N_("agL_out", [512, S], BF16)
        xcur = N_("xcur", [D, TOK], F32)
        rF_in, rF_out, rD_in, rD_out = Res(), Res(), Res(), Res()
        rN_in, rN_out, rY_in, rY_out, rL_in, rL_out = Res(), Res(), Res(), Res(), Res(), Res()
        xcur_r = [Res() for _ in range(8)]
        agF512 = agF_out.rearrange("r (q t) -> (r q) t", t=512)
        agN512 = agN_out.rearrange("r (q t) -> (r q) t", t=512)
        agY2k = agY_out.rearrange("r (q t) -> (r q) t", t=2048)
        agL2k = agL_out.rearrange("r (q t) -> (r q) t", t=2048)
        gx = kb.sb([128, NIDX], U32, "gx"); gxr = Res()
        kb.dma("sp", gx[:], gidx[:, :], writes=[gxr])
        banks = [(kb.ps([128, 512], F32, "bank"), Res("bank", excl=True)) for _ in range(8)]
        rot = lambda items: _rot(items)
        for L in range(nlayers):
            xin = xT0 if L == 0 else xcur
            xin_r = None if L == 0 else xcur_r
            with contextlib.ExitStack() as sa:
                emit_A2(kb, xin, xin_r, w_in[L], agF_in, rF_in, agD_in, rD_in, banks, sa)
                kb.barrier()
            for k in range(13):
                rows = 256 if k < 12 else 32
                kb.allgather(agF_in[k * 256:k * 256 + rows, :], agF_out[k * 1024:k * 1024 + 4 * rows, :], groups, writes=[rF_in, rF_out])
            kb.allgather(agD_in[:, :], agD_out[:, :], groups, writes=[rD_in, rD_out])
            if fstop <= 1:
                break
            with contextlib.ExitStack() as s1:
                emit_nsa2(kb, NOM_TILES[:ntiles], agF_out, agF512, rF_out, gx, gxr, IX, peT[L], w1[L], w2[L], nmask, nE0, nmisc, agN_in, rN_in,
                          banks[0:4], banks[4], rot(banks[5:8]), s1)
                kb.barrier()
            for k in range(2):
                kb.allgather(agN_in[k * 128:(k + 1) * 128, :], agN_out[k * 512:(k + 1) * 512, :], groups, writes=[rN_in, rN_out])
            if fstop <= 2:
                break
            with contextlib.ExitStack() as s2:
                pbf = [(banks[6][0][:, 0:32].bitcast(BF16), banks[6][1]), (banks[7][0][:, 0:32].bitcast(BF16), banks[7][1])]
                emit_ssd2(kb, agF_out, rF_out, agD_out, rD_out, gx, gxr, IX, svec[L], srep[L], scst, agY_in, rY_in, rot(banks[0:6]), pbf, es=s2)
                kb.barrier()
            for k in range(2):
                kb.allgather(agY_in[k * 64:(k + 1) * 64, :], agY_out[k * 256:(k + 1) * 256, :], groups, writes=[rY_in, rY_out])
            if fstop <= 3:
                break
            with contextlib.ExitStack() as s3:
                emit_lru2(kb, agF_out, rF_out, gx, gxr, IX, lvec[L], lwab[L], lwxb[L], agL_in, rL_in, rot(banks[0:4]), es=s3)
                kb.barrier()
            for k in range(2):
                kb.allgather(agL_in[k * 64:(k + 1) * 64, :], agL_out[k * 256:(k + 1) * 256, :], groups, writes=[rL_in, rL_out])
            if fstop <= 4:
                break
            last = (L == nlayers - 1)
            with contextlib.ExitStack() as s4:
                emit_C2(kb, xin, agN512, rN_out, agY2k, rY_out, agL2k, rL_out, gx, gxr, IX, pT[L], w_in[L], pnsa[L], pssd[L], plru[L], w_out[L],
                        pwg[L], pwp[L], rw, ewg[L], ewu[L], ewd[L], ccst[L], x2T if last else xcur, xcur_r, last, banks, s4, xTr=xin_r)
                kb.barrier()
        kb.finish(())
        print("fused instructions", kb.ninst, "sems", kb.nsem)
    return nc


def _rot(items):
    r = Rot.__new__(Rot)
    r.t = list(items)
    r.i = 0
    return r


def fused_inputs(d, core):
    b, r = core // 4, core % 4
    g, par = r // 2, r % 2
    tok = slice(core * TOK, (core + 1) * TOK)
    x = d["x"].reshape(T_ALL, D)
    im = {"xT0": np.ascontiguousarray(x[tok].T),
          "pT": np.ascontiguousarray(np.stack([d["p"][L].reshape(T_ALL, 256)[tok].T for L in range(2)])),
          "gidx": make_gidx(r), "w_in": d["w_in"],
          "npeT": np.ascontiguousarray(np.stack([np.stack([d["nsa_pe_k"][L].T, d["nsa_pe_v"][L].T]) for L in range(2)])),
          "nw1": np.stack([np.stack([d["nsa_w1_k"][L], d["nsa_w1_v"][L]]) for L in range(2)]),
          "nw2": np.stack([np.stack([d["nsa_w2_k"][L], d["nsa_w2_v"][L]]) for L in range(2)]),
          "proj_nsa": d["proj_nsa"], "proj_ssd": d["proj_ssd"], "proj_lru": d["proj_lru"], "w_out": d["w_out"],
          "ple_w_gate": d["ple_w_gate"], "ple_w_proj": d["ple_w_proj"], "router_w": d["router_w"],
          "exp_w_gate": d["exp_w_gate"], "exp_w_up": d["exp_w_up"], "exp_w_down": d["exp_w_down"],
          "ccst": np.stack([c_consts(d, L) for L in range(2)]), "scst": ssd_consts()}
    im.update(nsa_consts(par))
    sv, sr, lv, la, lx_ = [], [], [], [], []
    for L in range(2):
        a_, b_ = ssd_vecs(d, L, r)
        sv.append(a_); sr.append(b_)
        a_, b_, c_ = lru_vecs(d, L, r)
        lv.append(a_); la.append(b_); lx_.append(c_)
    im["svec"] = np.stack(sv); im["srep"] = np.stack(sr); im["lvec"] = np.stack(lv); im["lwab"] = np.stack(la); im["lwxb"] = np.stack(lx_)
    return im


def ssd_vecs(d, layer, r):
    g = r // 2
    cw = d["ssd_conv_w"][layer]; cb = d["ssd_conv_b"][layer]
    v = np.zeros((128, 16), np.float32)
    xs_ = slice(128 * r, 128 * r + 128); Bs_ = slice(512 + 64 * g, 512 + 64 * g + 64); Cs_ = slice(640 + 64 * g, 640 + 64 * g + 64)
    v[:, 0:4] = cw[:, xs_].T; v[:, 4] = cb[xs_]
    v[:64, 5:9] = cw[:, Bs_].T; v[:64, 9] = cb[Bs_]
    v[:64, 10:14] = cw[:, Cs_].T; v[:64, 14] = cb[Cs_]
    rp = np.zeros((128, 8), np.float32)
    hh = slice(2 * r, 2 * r + 2)
    rp[:, 0:2] = d["ssd_dt_bias"][layer][hh][None, :]
    rp[:, 2:4] = d["ssd_a_log"][layer][hh][None, :]
    rp[:, 4:6] = d["ssd_d"][layer][hh][None, :]
    return v, rp


def lru_vecs(d, layer, r):
    ch = slice(128 * r, 128 * r + 128)
    v = np.zeros((128, 8), np.float32)
    v[:, 0:4] = d["lru_conv_w"][layer][:, ch].T
    v[:, 4] = d["lru_conv_b"][layer][ch]
    v[:, 5] = d["lru_ba"][layer][ch]
    v[:, 6] = d["lru_bx"][layer][ch]
    v[:, 7] = d["lru_lambda"][layer][ch]
    wab_ = np.zeros((128, 128), np.float32)
    wxb_ = np.zeros((128, 128), np.float32)
    for k in range(2):
        wab_[64 * k:64 * k + 64, 64 * k:64 * k + 64] = d["lru_wa"][layer][2 * r + k]
        wxb_[64 * k:64 * k + 64, 64 * k:64 * k + 64] = d["lru_wx"][layer][2 * r + k]
    return v, wab_, wxb_


def kernel(**inputs):
    d = {k: np.asarray(v) for k, v in inputs.items()}
    nc = build_fused()
    in_maps = [fused_inputs(d, c) for c in range(NCORES)]
    res = run_bass_kernel_spmd(nc, in_maps, core_ids=list(range(NCORES))).results
    x = np.concatenate([res[c]["x2T"].T for c in range(NCORES)], axis=0)
    return np.ascontiguousarray(x.reshape(BATCH, S, D).astype(np.float32))
```

```python
import numpy as np
import contextlib
import concourse.bass as bass
import concourse.mybir as mybir
from concourse.bass_utils import run_bass_kernel_spmd

F32 = mybir.dt.float32
BF16 = mybir.dt.bfloat16
AF = mybir.ActivationFunctionType
ALU = mybir.AluOpType
AX = mybir.AxisListType

SAME_ENGINE_SYNC = True
SEM_ROT = 20000
NDMA = 6


class Res:
    __slots__ = ("w", "r", "name", "excl")

    def __init__(self, name="", excl=False):
        self.name = name
        self.w = None
        self.r = {}
        self.excl = excl


class MultiRes:
    def __init__(self, items):
        self.items = list(items)


def _flat(rs):
    out = []
    for r in rs:
        if isinstance(r, MultiRes):
            out.extend(r.items)
        else:
            out.append(r)
    return out


class KB:
    def __init__(self, nc, es):
        self.nc = nc
        self.es = es
        self.eng = dict(pe=nc.tensor, act=nc.scalar, dve=nc.vector, pool=nc.gpsimd, sp=nc.sync)
        self.sems = {}
        self.cnt = {}
        self.cur = {}
        self.seen = {e: {} for e in self.eng}
        self.nsem = 0
        self.ninst = 0
        for e in self.eng:
            self.cur[e] = self._new_sem("e_" + e)
        self.dma_pool = {q: [self._new_sem("d_%s%d" % (q, i)) for i in range(NDMA)] for q in ("sp", "pool", "act")}
        self.dma_idx = {q: 0 for q in self.dma_pool}
        self.uid = 0
        self.out_marks = []

    def _new_sem(self, name):
        self.nsem += 1
        key = "%s_%d" % (name, self.nsem)
        self.sems[key] = self.es.enter_context(self.nc.semaphore(key))
        self.cnt[key] = 0
        return key

    def sb(self, shape, dtype, name=None, es=None):
        self.uid += 1
        return (es or self.es).enter_context(self.nc.sbuf_tensor("%s_%d" % (name or "sb", self.uid), list(shape), dtype))

    def ps(self, shape, dtype=F32, name=None):
        self.uid += 1
        return self.es.enter_context(self.nc.psum_tensor("%s_%d" % (name or "ps", self.uid), list(shape), dtype))

    def dram(self, name, shape, dtype, kind):
        return self.nc.dram_tensor(name, list(shape), dtype, kind=kind).ap()

    def _deps(self, reads, writes):
        reads = _flat(reads)
        writes = _flat(writes)
        deps = {}
        for r in reads:
            if r.w is not None:
                k, v = r.w
                if deps.get(k, 0) < v:
                    deps[k] = v
            if r.excl:
                for k, v in r.r.items():
                    if deps.get(k, 0) < v:
                        deps[k] = v
        for w in writes:
            if w.w is not None:
                k, v = w.w
                if deps.get(k, 0) < v:
                    deps[k] = v
            for k, v in w.r.items():
                if deps.get(k, 0) < v:
                    deps[k] = v
        return deps

    def _wait(self, e, deps, skip_key=None):
        seen = self.seen[e]
        for k, v in deps.items():
            if k == skip_key:
                continue
            if seen.get(k, 0) < v:
                self.eng[e].wait_ge(self.sems[k], v)
                seen[k] = v
                self.ninst += 1

    def _mark(self, key, v, reads, writes):
        reads = _flat(reads)
        writes = _flat(writes)
        for r in reads:
            r.r[key] = v
        for w in writes:
            w.w = (key, v)
            w.r = {}

    def op(self, e, fn, reads=(), writes=()):
        deps = self._deps(reads, writes)
        key = self.cur[e]
        skip = key if (e == "pe" or not SAME_ENGINE_SYNC) else None
        self._wait(e, deps, skip)
        inst = fn(self.eng[e])
        self.cnt[key] += 1
        v = self.cnt[key]
        inst.then_inc(self.sems[key], 1)
        self.ninst += 1
        self._mark(key, v, reads, writes)
        if v >= SEM_ROT:
            self.cur[e] = self._new_sem("e_" + e)
        return inst

    def dma(self, q, out, in_, reads=(), writes=(), final=False, **kw):
        pool = self.dma_pool[q]
        key = pool[self.dma_idx[q] % len(pool)]
        self.dma_idx[q] += 1
        deps = self._deps(reads, writes)
        if self.cnt[key] > 0:
            deps[key] = max(deps.get(key, 0), self.cnt[key])
        self._wait(q, deps)
        inst = self.eng[q].dma_start(out=out, in_=in_, **kw)
        self.cnt[key] += 16
        v = self.cnt[key]
        inst.then_inc(self.sems[key], 16)
        self.ninst += 1
        self._mark(key, v, reads, writes)
        if final:
            self.out_marks.append((key, v))
        if v >= SEM_ROT:
            i = pool.index(key)
            pool[i] = self._new_sem("d_" + q)
        return inst

    def barrier(self):
        allc = {k: v for k, v in self.cnt.items() if v > 0}
        for e in self.eng:
            self._wait(e, dict(allc))

    def finish(self, out_res):
        deps = self._deps(out_res, ())
        for k, v in self.out_marks:
            if deps.get(k, 0) < v:
                deps[k] = v
        self._wait("sp", deps)


def _kb_gather(self, out, in2d, idx_ap, reads=(), writes=()):
    pool = self.dma_pool["pool"]
    key = pool[self.dma_idx["pool"] % len(pool)]
    self.dma_idx["pool"] += 1
    deps = self._deps(reads, writes)
    if self.cnt[key] > 0:
        deps[key] = max(deps.get(key, 0), self.cnt[key])
    self._wait("pool", deps)
    inst = self.nc.gpsimd.indirect_dma_start(out=out, out_offset=None, in_=in2d,
                                             in_offset=bass.IndirectOffsetOnAxis(ap=idx_ap, axis=0))
    self.cnt[key] += 16
    v = self.cnt[key]
    inst.then_inc(self.sems[key], 16)
    self.ninst += 1
    self._mark(key, v, reads, writes)
    if v >= SEM_ROT:
        pool[pool.index(key)] = self._new_sem("d_pool")
    return inst


def _kb_allgather(self, in_ap, out_ap, groups, writes=()):
    if not hasattr(self, "cc_key"):
        self.cc_key = self._new_sem("cc")
    deps = self._deps((), writes)
    self._wait("pool", deps)
    inst = self.nc.gpsimd.collective_compute("AllGather", ALU.bypass, replica_groups=groups, ins=[in_ap], outs=[out_ap])
    key = self.cc_key
    self.cnt[key] += 1
    v = self.cnt[key]
    inst.then_inc(self.sems[key], 1)
    self.ninst += 1
    self._mark(key, v, (), writes)
    return inst


KB.gather = _kb_gather
KB.allgather = _kb_allgather


S = 8192


class Rot:
    def __init__(self, kb, n, shape, dtype, name, psum=False, es=None):
        if psum:
            self.t = [(kb.ps(shape, dtype, name), Res(name, excl=True)) for _ in range(n)]
        else:
            self.t = [(kb.sb(shape, dtype, name, es=es), Res(name)) for _ in range(n)]
        self.i = 0

    def next(self):
        x = self.t[self.i % len(self.t)]
        self.i += 1
        return x


LEVEL = 9

S = 8192


def emit_conv(kb, eng, out, xin, vt, vr, c0, N, P, xr, outr):
    kb.op(eng, lambda e: e.tensor_scalar(out=out[:P, :], in0=xin[:P, 0:N], scalar1=vt[:P, c0:c0 + 1], scalar2=vt[:P, c0 + 4:c0 + 5],
                                         op0=ALU.mult, op1=ALU.add), reads=[xr, vr], writes=[outr])
    for k in range(1, 4):
        kb.op("dve", lambda e, k=k: e.scalar_tensor_tensor(out=out[:P, :], in0=xin[:P, k:k + N], scalar=vt[:P, c0 + k:c0 + k + 1], in1=out[:P, :],
                                                          op0=ALU.mult, op1=ALU.add), reads=[xr, vr, outr], writes=[outr])


def ssd_consts():
    k = np.arange(128)
    cst = np.zeros((128, 4, 128), np.float32)
    cst[:, 0, :] = (k[:, None] <= k[None, :])
    cst[:, 1, :] = (k[:, None] > k[None, :])
    cst[:, 2, :] = np.eye(128)
    cst[:, 3, :] = 1.0
    return cst


DBG = ''

S = 8192
QT = 512
NCMP = 511
BIGM = 1024.0
f32 = np.float32


def nsa_consts(par=0):
    kk = np.arange(128)
    tq = np.arange(512)
    c = {}
    zeros = np.zeros((128, 512), f32); onesm = np.ones((128, 512), f32)

    def cm_full(dl):
        if dl < 0:
            return zeros
        if dl >= 5:
            return onesm
        return ((16 * kk[:, None] + 31 - tq[None, :]) <= 512 * dl).astype(f32)

    def cneg_full(dk):
        if dk < 0:
            return zeros
        if dk > 3:
            return -onesm
        return np.where(128 * dk + kk[:, None] <= tq[None, :], 0.0, -1.0).astype(f32)

    def wm_full(dk):
        if dk < -4 or dk > 3:
            return zeros
        diff = tq[None, :] - (128 * dk + kk[:, None])
        return ((diff >= 0) & (diff < 512)).astype(f32)

    cm = [cm_full(dl + par) for dl in range(-1, 5)]
    cneg = [cneg_full(dk - 4 * par) for dk in range(0, 8)]
    wm = [wm_full(dk - 4 * par) for dk in range(-4, 8)]
    c["nmask"] = np.ascontiguousarray(np.stack(cm + cneg + wm, axis=1))
    key = np.arange(S)
    c["nE0"] = (kk[:, None] == (key[None, :] // 64)).astype(f32)
    n = np.arange(512)
    ov = ((n[:, None] // 4) == kk[None, :]).astype(f32) + (((n[:, None] + 1) // 4) == kk[None, :]).astype(f32)
    ov[511] = 0
    ovl = ov.reshape(4, 128, 128).transpose(1, 0, 2)
    mm = np.arange(256) - 8 * par
    hi = (kk >= 64).astype(np.int64)
    C0 = (mm[None, :] <= 128 + hi[:, None]).astype(f32)
    Cm1 = C0 - 1.0
    F = np.where((mm[None, :] == 128 + hi[:, None]) | (mm[None, :] == 127 + hi[:, None]), 1e4, -1e30).astype(f32)
    ident = np.eye(128, dtype=f32)
    ones = np.ones((128, 128), f32)
    sel64 = np.zeros((128, 64), f32); sel64[64] = 1.0
    gsel = np.zeros((128, 12, 64), f32)
    for r in range(12):
        gsel[r, r, :] = 1.0
    c["nmisc"] = np.ascontiguousarray(np.concatenate(
        [ovl.reshape(128, 512), C0, Cm1, F, ident, ones, sel64, gsel.reshape(128, 768)], axis=1))
    return c


NMASK = 26
MISC_W = 512 + 768 + 128 + 128 + 64 + 768


D = 1024
TOK = 2048


CDBG = ''

D = 1024
TOK = 2048
NT = TOK // 512
ALPHA = 4 ** 0.25
OFF_Z = 512 + 768 + 24
OFF_MERGE = 6688 - 3072
f32 = np.float32
CW = 36 + 16 + 128 + 128 + 128 + 2048


def c_consts(d, layer):
    v = np.zeros((128, CW), f32)
    col = lambda a: a.reshape(-1, 128).T
    v[:, 0:8] = col(d["ln1_g"][layer]); v[:, 8:16] = col(d["ln1_b"][layer])
    v[:, 16:24] = col(d["ln2_g"][layer]); v[:, 24:32] = col(d["ln2_b"][layer])
    v[:, 32:36] = col(d["ssd_norm_w"][layer])
    v[:, 36:52] = d["router_b"][None, :]
    v[:, 52:180] = 1.0 / 1024
    v[:, 180:308] = 1.0 / 512
    v[:, 308:436] = np.eye(128)
    sel = np.zeros((128, 16, 128), f32)
    for e in range(16):
        sel[e, e, :] = 1.0
    v[:, 436:436 + 2048] = sel.reshape(128, 2048)
    return v


def emit_nsa2(kb, tiles, agF2k, agF512, agFr, gx, gxr, IX, peT, w1, w2, nmask, nE0, nmisc, onsaT, onsar, acc, pmb, gen, es):
    sb = lambda shape, dt, name: kb.sb(shape, dt, name, es=es)
    mk = sb([128, NMASK, 512], BF16, "nmask"); mkr = Res()
    kb.dma("pool", mk[:], nmask[:, :, :], writes=[mkr])
    E0 = sb([128, S], BF16, "nE0"); E0r = Res()
    kb.dma("pool", E0[:], nE0[:, :], writes=[E0r])
    mf = sb([128, MISC_W], F32, "nmiscf"); mfr = Res()
    kb.dma("sp", mf[:], nmisc[:, :], writes=[mfr])
    o = 0
    ovl_f = mf[:, 0:512]; o = 512
    C0 = mf[:, o:o + 256]; Cm1 = mf[:, o + 256:o + 512]; F4 = mf[:, o + 512:o + 768]; o += 768
    ident = mf[:, o:o + 128]; o += 128
    ones_f = mf[:, o:o + 128]; o += 128
    sel64 = mf[:, o:o + 64]; o += 64
    gsel = mf[:, o:o + 768]
    cb_ = sb([128, 512 + 128 + 128], BF16, "ncb"); cbr = Res()
    kb.op("dve", lambda e: e.tensor_copy(out=cb_[:, 0:512], in_=ovl_f), reads=[mfr], writes=[cbr])
    kb.op("dve", lambda e: e.tensor_copy(out=cb_[:, 512:640], in_=ident), reads=[mfr], writes=[cbr])
    kb.op("dve", lambda e: e.tensor_copy(out=cb_[:, 640:768], in_=ones_f), reads=[mfr], writes=[cbr])
    ovl = cb_[:, 0:512]; identb = cb_[:, 512:640]; onesb = cb_[:, 640:768]

    kall = sb([128, 2, S], BF16, "nk"); kr = [None, None, Res(), Res()]
    kb.op("pool", lambda e: e.memset(kall[64:128, :, :], 0.0), writes=[kr[2], kr[3]])
    for i, nm in ((2, "ksel"), (3, "kwin")):
        for rp in range(4):
            kb.gather(kall[0:64, i - 2, rp * 2048:(rp + 1) * 2048], agF2k, gx[0:64, IX[nm + str(rp)]:IX[nm + str(rp)] + 1], reads=[gxr, agFr], writes=[kr[i]])
    va = sb([128, 2, 64, 128], BF16, "nva"); var = [Res() for _ in range(2)]
    for i in range(2):
        kb.op("pool", lambda e, i=i: e.memset(va[:, i, :, 64:128], 0.0), writes=[var[i]])
        kb.op("pool", lambda e, i=i: e.memset(va[:, i, :, 64:65], 1.0), writes=[var[i]])

    kcT = sb([64, 512], BF16, "nkcT"); kcr = Res()
    vct = sb([128, 4, 65], BF16, "nvct"); vcr = Res()
    kb.op("pool", lambda e: e.memset(kcT[:], 0.0), writes=[kcr])
    kb.op("pool", lambda e: e.memset(vct[:], 0.0), writes=[vcr])
    kb.op("pool", lambda e: e.memset(vct[:, :, 64:65], 1.0), writes=[vcr])
    with contextlib.ExitStack() as esv:
        vT = kb.sb([64, 2, S], BF16, "nvT", es=esv); vTr = [Res(), Res()]
        for i, nm in ((0, "vsel"), (1, "vwin")):
            for rp in range(4):
                kb.gather(vT[:, i, rp * 2048:(rp + 1) * 2048], agF2k, gx[0:64, IX[nm + str(rp)]:IX[nm + str(rp)] + 1], reads=[gxr, agFr], writes=[vTr[i]])
            for k8 in range(8):
                pv8, pv8r = gen.next()
                pvb = pv8[:, 0:256].bitcast(BF16)
                for kk in range(8):
                    kt = k8 * 8 + kk
                    kb.op("pe", lambda e, kt=kt, kk=kk, i=i: e.transpose(pvb[:, kk * 64:(kk + 1) * 64], vT[:, i, kt * 128:(kt + 1) * 128], identb[0:64, 0:64]),
                          reads=[vTr[i], cbr], writes=[pv8r])
                kb.op("act", lambda e, k8=k8, i=i: e.copy(out=va[:, i, k8 * 8:(k8 + 1) * 8, 0:64], in_=pvb.rearrange("p (k d) -> p k d", k=8)), reads=[pv8r], writes=[var[i]])
        kb.barrier()
    with contextlib.ExitStack() as es2:
        sb2 = lambda shape, dt, name: kb.sb(shape, dt, name, es=es2)
        kcv = sb2([64, 2, S], BF16, "nkcv"); kr[0] = Res(); kr[1] = Res()
        for i, nm in ((0, "kcmp"), (1, "vcmp")):
            for rp in range(4):
                kb.gather(kcv[:, i, rp * 2048:(rp + 1) * 2048], agF2k, gx[0:64, IX[nm + str(rp)]:IX[nm + str(rp)] + 1], reads=[gxr, agFr], writes=[kr[i]])
        w1t = sb2([64, 32, 256], BF16, "nw1"); w1r = Res()
        w2t = sb2([128, 2, 64], BF16, "nw2"); w2r = Res()
        pet = sb2([64, 32], BF16, "npe"); per = Res()
        bias = sb2([128, 1], F32, "nbias"); biasr = Res()
        tmp3 = [(sb2([128, 512], F32, "nt"), Res(), sb2([128, 512], F32, "nu"), Res(), sb2([128, 512], BF16, "ngt"), Res()) for _ in range(2)]
        for which in range(2):
            for q4 in range(4):
                kb.dma("pool", w1t[:, q4 * 8:(q4 + 1) * 8, :], w1[which, q4 * 512:(q4 + 1) * 512, :].rearrange("(l d) j -> d l j", d=64), writes=[w1r])
            kb.dma("pool", w2t[:], w2[which, :, :].rearrange("(c p) d -> p c d", p=128), writes=[w2r])
            kb.dma("pool", pet[:], peT[which, :, :], writes=[per])
            src = kcv[:, which, :]
            gts = []
            for jc in range(2):
                ph, phr = gen.next()
                for l in range(32):
                    kb.op("pe", lambda e, l=l: e.matmul(ph[:, 0:NCMP], lhsT=w1t[:, l, jc * 128:(jc + 1) * 128],
                                                         rhs=src[:, l:l + 16 * (NCMP - 1) + 1:16], start=(l == 0), stop=(l == 31)),
                          reads=[w1r, kr[which]], writes=[phr])
                pbias, pbr = gen.next()
                for l in range(32):
                    kb.op("pe", lambda e, l=l: e.matmul(pbias[:, 0:1], lhsT=w1t[:, l, jc * 128:(jc + 1) * 128], rhs=pet[:, l:l + 1],
                                                         start=(l == 0), stop=(l == 31)), reads=[w1r, per], writes=[pbr])
                kb.op("dve", lambda e: e.tensor_copy(out=bias[:], in_=pbias[:, 0:1]), reads=[pbr], writes=[biasr])
                t, tr, u, ur, gt, gr = tmp3[jc]
                kb.op("act", lambda e: e.activation(out=t[:, 0:NCMP], in_=ph[:, 0:NCMP], func=AF.Identity, bias=bias[:, 0:1]), reads=[phr, biasr], writes=[tr])
                kb.op("dve", lambda e: e.tensor_tensor(out=u[:, 0:NCMP], in0=t[:, 0:NCMP], in1=t[:, 0:NCMP], op=ALU.mult), reads=[tr], writes=[ur])
                kb.op("dve", lambda e: e.tensor_scalar(out=u[:, 0:NCMP], in0=u[:, 0:NCMP], scalar1=0.044715, scalar2=1.0, op0=ALU.mult, op1=ALU.add), reads=[ur], writes=[ur])
                kb.op("dve", lambda e: e.tensor_tensor(out=u[:, 0:NCMP], in0=u[:, 0:NCMP], in1=t[:, 0:NCMP], op=ALU.mult), reads=[ur, tr], writes=[ur])
                kb.op("act", lambda e: e.activation(out=u[:, 0:NCMP], in_=u[:, 0:NCMP], func=AF.Sigmoid, scale=1.5957691216057308), reads=[ur], writes=[ur])
                kb.op("pool", lambda e: e.memset(gt[:, NCMP:512], 0.0), writes=[gr])
                kb.op("dve", lambda e: e.tensor_tensor(out=gt[:, 0:NCMP], in0=u[:, 0:NCMP], in1=t[:, 0:NCMP], op=ALU.mult), reads=[ur, tr], writes=[gr])
                gts.append((gt, gr))
            if which == 0:
                pk, pkr = gen.next()
                for jc in range(2):
                    kb.op("pe", lambda e, jc=jc: e.matmul(pk[0:64, 0:NCMP], lhsT=w2t[:, jc, :], rhs=gts[jc][0][:, 0:NCMP], start=(jc == 0), stop=(jc == 1)),
                          reads=[w2r, gts[jc][1]], writes=[pkr])
                kb.op("act", lambda e: e.copy(out=kcT[:, 0:NCMP], in_=pk[0:64, 0:NCMP]), reads=[pkr], writes=[kcr])
            else:
                for m in range(4):
                    pv, pvr = gen.next()
                    for jc in range(2):
                        kb.op("pe", lambda e, jc=jc, m=m: e.matmul(pv[:, 0:64], lhsT=gts[jc][0][:, m * 128:(m + 1) * 128], rhs=w2t[:, jc, :], start=(jc == 0), stop=(jc == 1)),
                              reads=[w2r, gts[jc][1]], writes=[pvr])
                    kb.op("act", lambda e, m=m: e.copy(out=vct[:, m, 0:64], in_=pv[:, 0:64]), reads=[pvr], writes=[vcr])
        kb.barrier()
    kmax = sb([128, 1], F32, "nkmax"); kmr = Res()
    kb.op("pool", lambda e: e.memset(kmax[:], 0.0), writes=[kmr])
    sq = Rot(kb, 2, [64, 512], BF16, "nsq", es=es)
    red = Rot(kb, 2, [128, 1], F32, "nred", es=es)

    def norm_max(src_ap, n, src_res, dst, dstr):
        s_, sr_ = sq.next()
        kb.op("pool", lambda e: e.tensor_tensor(out=s_[:, 0:n], in0=src_ap, in1=src_ap, op=ALU.mult), reads=src_res, writes=[sr_])
        pn, pnr = gen.next()
        kb.op("pe", lambda e: e.matmul(pn[:, 0:n], lhsT=onesb[0:64, :], rhs=s_[:, 0:n], start=True, stop=True), reads=[cbr, sr_], writes=[pnr])
        r_, rr_ = red.next()
        kb.op("dve", lambda e: e.reduce_max(out=r_[:], in_=pn[:, 0:n], axis=AX.X), reads=[pnr], writes=[rr_])
        kb.op("dve", lambda e: e.tensor_tensor(out=dst[:], in0=dst[:], in1=r_[:], op=ALU.max), reads=[rr_, dstr], writes=[dstr])

    norm_max(kcT[:, 0:512], 512, [kcr], kmax, kmr)
    for which in (2, 3):
        for tt in range(S // 512):
            norm_max(kall[0:64, which - 2, tt * 512:(tt + 1) * 512], 512, [kr[which]], kmax, kmr)

    qb = Rot(kb, 2, [128, 4, 512], BF16, "nq", es=es)
    for q_, qr_ in qb.t:
        kb.op("pool", lambda e, q_=q_: e.memset(q_[64:128, :, :], 0.0), writes=[qr_])
    gtl = Rot(kb, 2, [12, 512], F32, "ngate", es=es)
    gtbl = Rot(kb, 2, [12, 512], BF16, "ngateb", es=es)
    ebuf = Rot(kb, 4, [128, 512], BF16, "ne", es=es)
    pbuf = Rot(kb, 4, [128, 512], BF16, "np", es=es)
    mskb = Rot(kb, 2, [128, 512], BF16, "nmsk", es=es)
    pnbuf = Rot(kb, 4, [128, 512], BF16, "npn", es=es)
    rinv = Rot(kb, 2, [128, 512], F32, "nrinv", es=es)
    ocmp = Rot(kb, 1, [64, 4, 512], F32, "nocmp", es=es)
    impT = Rot(kb, 2, [128, 512], F32, "nimpT", es=es)
    selT = Rot(kb, 2, [128, 512], BF16, "nselT", es=es)
    v1b = Rot(kb, 2, [128, 128], F32, "nv1", es=es)
    v2b = Rot(kb, 2, [128, 128], F32, "nv2", es=es)
    m8a = Rot(kb, 2, [128, 8], F32, "nm8a", es=es)
    m8b = Rot(kb, 2, [128, 8], F32, "nm8b", es=es)
    smk = Rot(kb, 2, [128, 128], F32, "nsmk", es=es)
    accs = Rot(kb, 1, [65, 8, 512], F32, "naccs", es=es)
    rec = Rot(kb, 2, [64, 512], F32, "nrec", es=es)
    osb = Rot(kb, 2, [64, 512], F32, "nosb", es=es)
    negc = Rot(kb, 2, [128, 4], F32, "nnegc", es=es)
    qm = Rot(kb, 2, [128, 1], F32, "nqm", es=es)

    for ti, i in enumerate(tiles):
        t0 = i * QT
        q, qr = qb.next()
        for j in range(4):
            cq = IX["q%d_%d" % (ti, j)]
            kb.gather(q[0:64, j, :], agF512, gx[0:64, cq:cq + 1], reads=[gxr, agFr], writes=[qr])
        gtb_, gtbr = gtbl.next()
        cg = IX["gate%d" % ti]
        kb.gather(gtb_[:], agF512, gx[0:12, cg:cg + 1], reads=[gxr, agFr], writes=[gtbr])
        gt_, gtr = gtl.next()
        kb.op("act", lambda e: e.activation(out=gt_[:], in_=gtb_[:], func=AF.Sigmoid), reads=[gtbr], writes=[gtr])
        nc_, ncr = negc.next()
        for j in range(4):
            qm_, qmr = qm.next()
            kb.op("pool", lambda e: e.memset(qm_[:], 0.0), writes=[qmr])
            norm_max(q[0:64, j, :], 512, [qr], qm_, qmr)
            kb.op("dve", lambda e, j=j: e.tensor_tensor(out=nc_[:, j:j + 1], in0=qm_[:], in1=kmax[:], op=ALU.mult), reads=[qmr, kmr], writes=[ncr])
        kb.op("act", lambda e: e.activation(out=nc_[:], in_=nc_[:], func=AF.Sqrt, scale=1.05), reads=[ncr], writes=[ncr])
        kb.op("dve", lambda e: e.tensor_scalar(out=nc_[:], in0=nc_[:], scalar1=-0.125, scalar2=None, op0=ALU.mult), reads=[ncr], writes=[ncr])

        nch = min(4, (32 * (i + 1) + 31 + 127) // 128)
        oc, ocr = ocmp.next()
        pimp, pimpr = acc[0]
        for j in range(4):
            es_ = []
            psum_, psumr = acc[1]
            for m in range(nch):
                ps_, psr = gen.next()
                kb.op("pe", lambda e, m=m, j=j: e.matmul(ps_[:, :], lhsT=kcT[:, m * 128:(m + 1) * 128], rhs=q[0:64, j, :], start=True, stop=True),
                      reads=[kcr, qr], writes=[psr])
                e_, er = ebuf.next()
                kb.op("act", lambda e, j=j: e.activation(out=e_[:], in_=ps_[:, :], func=AF.Exp, scale=0.125, bias=nc_[:, j:j + 1]), reads=[psr, ncr], writes=[er])
                dl = i - 4 * m
                if dl <= 4:
                    kb.op("pool", lambda e, dl=dl: e.tensor_tensor(out=e_[:], in0=e_[:], in1=mk[:, dl + 1, :], op=ALU.mult), reads=[er, mkr], writes=[er])
                kb.op("pe", lambda e, m=m: e.matmul(psum_[:, :], lhsT=onesb, rhs=e_[:], start=(m == 0), stop=(m == nch - 1)), reads=[cbr, er], writes=[psumr])
                es_.append((e_, er))
            ri, rir = rinv.next()
            kb.op("dve", lambda e: e.tensor_scalar(out=ri[:], in0=psum_[:, :], scalar1=1e-30, scalar2=None, op0=ALU.max), reads=[psumr], writes=[rir])
            kb.op("dve", lambda e: e.reciprocal(out=ri[:], in_=ri[:]), reads=[rir], writes=[rir])
            po, por = acc[2]
            for m in range(nch):
                e_, er = es_[m]
                pn_, pnr_ = pnbuf.next()
                kb.op("dve", lambda e: e.tensor_tensor(out=pn_[:], in0=e_[:], in1=ri[:], op=ALU.mult), reads=[er, rir], writes=[pnr_])
                kb.op("pe", lambda e, m=m: e.matmul(po[0:64, :], lhsT=vct[:, m, 0:64], rhs=pn_[:], start=(m == 0), stop=(m == nch - 1)), reads=[vcr, pnr_], writes=[por])
                kb.op("pe", lambda e, m=m, j=j: e.matmul(pimp[:, :], lhsT=ovl[:, m * 128:(m + 1) * 128], rhs=pn_[:], start=(j == 0 and m == 0), stop=(j == 3 and m == nch - 1)),
                      reads=[cbr, pnr_], writes=[pimpr])
            kb.op("act", lambda e, j=j: e.copy(out=oc[:, j, :], in_=po[0:64, :]), reads=[por], writes=[ocr])
        it_, itr = impT.next()
        kb.op("act", lambda e: e.copy(out=it_[:], in_=pimp[:, :]), reads=[pimpr], writes=[itr])
        st_, str_ = selT.next()
        for k4 in range(4):
            ksub = 4 * i + k4
            ptr_, ptrr = gen.next()
            kb.op("pe", lambda e, k4=k4: e.transpose(ptr_[:, 0:128], it_[:, k4 * 128:(k4 + 1) * 128], ident), reads=[itr, mfr], writes=[ptrr])
            co = 128 - 2 * ksub
            v1, v1r = v1b.next()
            kb.op("dve", lambda e, co=co: e.tensor_tensor(out=v1[:], in0=ptr_[:, 0:128], in1=C0[:, co:co + 128], op=ALU.mult), reads=[ptrr, mfr], writes=[v1r])
            kb.op("dve", lambda e, co=co: e.tensor_tensor(out=v1[:], in0=v1[:], in1=Cm1[:, co:co + 128], op=ALU.add), reads=[v1r, mfr], writes=[v1r])
            kb.op("dve", lambda e, co=co: e.tensor_tensor(out=v1[:], in0=v1[:], in1=F4[:, co:co + 128], op=ALU.max), reads=[v1r, mfr], writes=[v1r])
            kb.op("dve", lambda e: e.memset(v1[:, 0:1], 1e4), reads=[], writes=[v1r])
            a8, a8r = m8a.next()
            kb.op("dve", lambda e: e.max(out=a8[:], in_=v1[:]), reads=[v1r], writes=[a8r])
            v2, v2r = v2b.next()
            kb.op("dve", lambda e: e.match_replace(out=v2[:], in_to_replace=a8[:], in_values=v1[:], imm_value=-1e30), reads=[a8r, v1r], writes=[v2r])
            b8, b8r = m8b.next()
            kb.op("dve", lambda e: e.max(out=b8[:], in_=v2[:]), reads=[v2r], writes=[b8r])
            sm, smr = smk.next()
            kb.op("dve", lambda e: e.tensor_scalar(out=sm[:], in0=v1[:], scalar1=b8[:, 7:8], scalar2=None, op0=ALU.is_ge), reads=[v1r, b8r], writes=[smr])
            if DBG == "sm" and k4 == 0 and ti == 0:
                kb.dma("sp", onsaT[0:128, 0:128], sm[:], reads=[smr], final=True)
                kb.dma("sp", onsaT[128:256, 0:128], v1[:], reads=[v1r], final=True)
                kb.dma("sp", onsaT[0:128, 128:136], a8[:], reads=[a8r], final=True)
                kb.dma("sp", onsaT[0:128, 136:144], b8[:], reads=[b8r], final=True)
                kb.dma("sp", onsaT[128:256, 128:256], v2[:], reads=[v2r], final=True)
            pt2, pt2r = gen.next()
            kb.op("pe", lambda e: e.transpose(pt2[:, 0:128], sm[:], ident), reads=[smr, mfr], writes=[pt2r])
            kb.op("act", lambda e, k4=k4: e.copy(out=st_[:, k4 * 128:(k4 + 1) * 128], in_=pt2[:, 0:128]), reads=[pt2r], writes=[str_])

        as_, asr = accs.next()
        LA = 2
        for br in range(2):
            if br == 0:
                kts = list(range(0, min(64, 4 * i + 8)))
            else:
                kts = [kt for kt in range(4 * i - 4, min(64, 4 * i + 8)) if kt >= 0]
            ksrc = 2 + br
            pairs = [(n_, kt, j) for n_, kt in enumerate(kts) for j in range(4)]
            pend = []
            msk = None
            for idx in range(len(pairs) + LA):
                if idx < len(pairs):
                    n_, kt, j = pairs[idx]
                    dk = kt - 4 * i
                    if br == 0 and j == 0:
                        pm, pmr = pmb
                        kb.op("pe", lambda e, kt=kt, dk=dk: e.matmul(pm[:, :], lhsT=E0[:, kt * 128:(kt + 1) * 128], rhs=st_[:], start=True, stop=(dk < 0)),
                              reads=[E0r, str_], writes=[pmr])
                        if dk >= 0:
                            kb.op("pe", lambda e, dk=dk: e.matmul(pm[:, :], lhsT=identb, rhs=mk[:, 6 + dk, :], start=False, stop=True), reads=[cbr, mkr], writes=[pmr])
                        msk = mskb.next()
                        kb.op("act", lambda e, msk=msk: e.activation(out=msk[0][:], in_=pm[:, :], func=AF.Relu), reads=[pmr], writes=[msk[1]])
                    ps_, psr = gen.next()
                    kb.op("pe", lambda e, kt=kt, j=j, ps_=ps_: e.matmul(ps_[:, :], lhsT=kall[:, br, kt * 128:(kt + 1) * 128], rhs=q[:, j, :], start=True, stop=True),
                          reads=[kr[ksrc], qr], writes=[psr])
                    e_, er = ebuf.next()
                    kb.op("act", lambda e, j=j, e_=e_, ps_=ps_: e.activation(out=e_[:], in_=ps_[:, :], func=AF.Exp, scale=0.125, bias=nc_[:, j:j + 1]), reads=[psr, ncr], writes=[er])
                    p_, pr_ = pbuf.next()
                    eng = "dve"
                    if br == 0:
                        kb.op(eng, lambda e, p_=p_, e_=e_, msk=msk: e.tensor_tensor(out=p_[:], in0=e_[:], in1=msk[0][:], op=ALU.mult), reads=[er, msk[1]], writes=[pr_])
                    else:
                        kb.op(eng, lambda e, dk=dk, p_=p_, e_=e_: e.tensor_tensor(out=p_[:], in0=e_[:], in1=mk[:, 14 + dk + 4, :], op=ALU.mult), reads=[er, mkr], writes=[pr_])
                    pend.append((p_, pr_, n_, kt, j))
                if idx >= LA:
                    p_, pr_, n_, kt, j = pend[idx - LA]
                    pa, par = acc[j]
                    kb.op("pe", lambda e, kt=kt, n_=n_, p_=p_, pa=pa: e.matmul(pa[:, :], lhsT=va[:, br, kt, :], rhs=p_[:], start=(n_ == 0), stop=(n_ == len(kts) - 1)),
                          reads=[var[br], pr_], writes=[par])
            for j in range(4):
                pa, par = acc[j]
                kb.op("act", lambda e, j=j, br=br: e.copy(out=as_[:, br * 4 + j, :], in_=pa[0:65, :]), reads=[par], writes=[asr])
        for j in range(4 if DBG not in ("sm", "pm") else 0):
            o_, or_ = osb.next()
            pg, pgr = gen.next()
            kb.op("pe", lambda e, j=j: e.matmul(pg[0:64, :], lhsT=gsel[0:12, (j * 3) * 64:(j * 3 + 1) * 64], rhs=gt_[:], start=True, stop=True), reads=[mfr, gtr], writes=[pgr])
            if DBG == "cmp":
                kb.op("dve", lambda e, j=j: e.tensor_copy(out=o_[:], in_=oc[:, j, :]), reads=[pgr, ocr], writes=[or_])
            elif DBG:
                kb.op("dve", lambda e, j=j: e.memset(o_[:], 0.0), reads=[pgr, ocr], writes=[or_])
            else:
                kb.op("dve", lambda e, j=j: e.tensor_tensor(out=o_[:], in0=pg[0:64, :], in1=oc[:, j, :], op=ALU.mult), reads=[pgr, ocr], writes=[or_])
            for br in range(2):
                if DBG == "cmp" or (DBG == "sel" and br == 1) or (DBG == "win" and br == 0):
                    continue
                psm, psmr = gen.next()
                kb.op("pe", lambda e, j=j, br=br: e.matmul(psm[0:64, :], lhsT=sel64[0:65, :], rhs=as_[:, br * 4 + j, :], start=True, stop=True), reads=[mfr, asr], writes=[psmr])
                rc, rcr = rec.next()
                kb.op("dve", lambda e: e.tensor_scalar(out=rc[:], in0=psm[0:64, :], scalar1=1e-30, scalar2=None, op0=ALU.max), reads=[psmr], writes=[rcr])
                kb.op("dve", lambda e: e.reciprocal(out=rc[:], in_=rc[:]), reads=[rcr], writes=[rcr])
                pg2, pg2r = gen.next()
                kb.op("pe", lambda e, j=j, br=br: e.matmul(pg2[0:64, :], lhsT=gsel[0:12, (j * 3 + 1 + br) * 64:(j * 3 + 2 + br) * 64], rhs=gt_[:], start=True, stop=True),
                      reads=[mfr, gtr], writes=[pg2r])
                if not DBG:
                    kb.op("dve", lambda e: e.tensor_tensor(out=rc[:], in0=pg2[0:64, :], in1=rc[:], op=ALU.mult), reads=[pg2r, rcr], writes=[rcr])
                kb.op("pool", lambda e, j=j, br=br: e.tensor_tensor(out=rc[:], in0=rc[:], in1=as_[0:64, br * 4 + j, :], op=ALU.mult), reads=[rcr, asr], writes=[rcr])
                kb.op("pool", lambda e: e.tensor_tensor(out=o_[:], in0=o_[:], in1=rc[:], op=ALU.add), reads=[rcr, or_], writes=[or_])
            kb.dma("pool", onsaT[j * 64:(j + 1) * 64, ti * QT:(ti + 1) * QT], o_[:], reads=[or_, onsar])


def emit_ssd2(kb, agF2k, agFr, agD2k, agDr, gx, gxr, IX, svec, srep, cst, yT_dst, yT_r, pbanks, pbf, es=None):
    N = 1024
    NCH = S // 128
    ct = kb.sb([128, 4, 128], F32, "scst", es=es); cr = Res()
    kb.dma("sp", ct[:], cst[:, :, :], writes=[cr])
    tri = ct[:, 0, :]; U = ct[:, 1, :]; ident = ct[:, 2, :]; ones = ct[:, 3, :]
    identb = kb.sb([128, 128], BF16, "sidb", es=es); idbr = Res()
    kb.op("dve", lambda e: e.tensor_copy(out=identb[:], in_=ident), reads=[cr], writes=[idbr])
    vt = kb.sb([128, 16], F32, "svec", es=es); vr = Res()
    kb.dma("sp", vt[:], svec[:, :], writes=[vr])
    rp = kb.sb([128, 8], F32, "srep", es=es); rpr = Res()
    kb.dma("sp", rp[:], srep[:, :], writes=[rpr])
    if LEVEL == -1: return
    dt = kb.sb([128, NCH, 2], F32, "sdt", es=es); dtr = Res()
    dtT = kb.sb([2, S], F32, "sdtT", es=es); dtTr = Res()
    for rq in range(4):
        cd = IX["dt%d" % rq]
        kb.gather(dtT[0:2, rq * 2048:(rq + 1) * 2048], agD2k, gx[0:2, cd:cd + 1], reads=[gxr, agDr], writes=[dtTr])
    pdt, pdtr = pbanks.next()
    for c in range(NCH):
        kb.op("pe", lambda e, c=c: e.transpose(pdt[:, c * 2:(c + 1) * 2], dtT[0:2, c * 128:(c + 1) * 128], ident[0:2, 0:2]), reads=[dtTr, cr], writes=[pdtr])
    kb.op("dve", lambda e: e.tensor_copy(out=dt[:].rearrange("p c h -> p (c h)"), in_=pdt[:, 0:NCH * 2]), reads=[pdtr], writes=[dtr])
    aa = kb.sb([128, NCH, 2], F32, "sa", es=es); aar = Res()
    An = kb.sb([128, 2], F32, "sAn", es=es); Anr = Res()
    kb.op("act", lambda e: e.activation(out=An[:], in_=rp[:, 2:4], func=AF.Exp), reads=[rpr], writes=[Anr])
    kb.op("dve", lambda e: e.tensor_scalar(out=An[:], in0=An[:], scalar1=-1.0, scalar2=None, op0=ALU.mult), reads=[Anr], writes=[Anr])
    for h in range(2):
        kb.op("dve", lambda e, h=h: e.tensor_scalar(out=dt[:, :, h], in0=dt[:, :, h], scalar1=rp[:, h:h + 1], scalar2=None, op0=ALU.add),
              reads=[dtr, rpr], writes=[dtr])
    kb.op("act", lambda e: e.activation(out=dt[:], in_=dt[:], func=AF.Exp), reads=[dtr], writes=[dtr])
    kb.op("act", lambda e: e.activation(out=dt[:], in_=dt[:], func=AF.Ln, bias=1.0), reads=[dtr], writes=[dtr])
    for h in range(2):
        kb.op("dve", lambda e, h=h: e.tensor_scalar(out=aa[:, :, h], in0=dt[:, :, h], scalar1=An[:, h:h + 1], scalar2=None, op0=ALU.mult),
              reads=[dtr, Anr], writes=[aar])
    if LEVEL == -2: return
    acum = kb.sb([128, NCH, 2], F32, "sacum", es=es); acr = Res()
    dout = kb.sb([128, NCH, 2], F32, "sdout", es=es); dor = Res()
    dst = kb.sb([128, NCH, 2], F32, "sdst", es=es); dsr = Res()
    dtot = kb.sb([128, NCH, 2], F32, "sdtot", es=es); dtor = Res()
    aflat = aa[:].rearrange("p c h -> p (c h)")
    p1, p1r = pbanks.next()
    kb.op("pe", lambda e: e.matmul(p1[:, 0:NCH * 2], lhsT=tri, rhs=aflat, start=True, stop=True), reads=[cr, aar], writes=[p1r])
    p2, p2r = pbanks.next()
    kb.op("pe", lambda e: e.matmul(p2[:, 0:NCH * 2], lhsT=ones, rhs=aflat, start=True, stop=True), reads=[cr, aar], writes=[p2r])
    fl = lambda t: t[:].rearrange("p c h -> p (c h)")
    kb.op("dve", lambda e: e.tensor_copy(out=fl(acum), in_=p1[:, 0:NCH * 2]), reads=[p1r], writes=[acr])
    kb.op("act", lambda e: e.activation(out=fl(dout), in_=p1[:, 0:NCH * 2], func=AF.Exp), reads=[p1r], writes=[dor])
    kb.op("act", lambda e: e.activation(out=fl(dtot), in_=p2[:, 0:NCH * 2], func=AF.Exp), reads=[p2r], writes=[dtor])
    kb.op("dve", lambda e: e.tensor_tensor(out=fl(dst), in0=p2[:, 0:NCH * 2], in1=fl(acum), op=ALU.subtract), reads=[p2r, acr], writes=[dsr])
    kb.op("act", lambda e: e.activation(out=fl(dst), in_=fl(dst), func=AF.Exp), reads=[dsr], writes=[dsr])

    if LEVEL == -3: return
    prev = [kb.sb([64, 64], F32, "sprev", es=es) for _ in range(2)]
    prevr = [Res() for _ in range(2)]
    prevb = [kb.sb([64, 64], BF16, "sprevb", es=es) for _ in range(2)]
    prevbr = [Res() for _ in range(2)]
    for h in range(2):
        kb.op("pool", lambda e, h=h: e.memset(prev[h][:], 0.0), writes=[prevr[h]])
        kb.op("pool", lambda e, h=h: e.memset(prevb[h][:], 0.0), writes=[prevbr[h]])

    xfull = kb.sb([128, S + 3], BF16, "sxfull", es=es); xfr = Res()
    bfull = kb.sb([64, S + 3], BF16, "sbfull", es=es); bfr = Res()
    cfull = kb.sb([64, S + 3], BF16, "scfull", es=es); cfr = Res()
    for (tl, rs, nm, P) in ((xfull, xfr, "sx", 128), (bfull, bfr, "sB", 64), (cfull, cfr, "sC", 64)):
        kb.op("pool", lambda e, tl=tl, P=P: e.memset(tl[:P, 0:3], 0.0), writes=[rs])
        for rq in range(4):
            cc_ = IX[nm + str(rq)]
            kb.gather(tl[:P, 3 + rq * 2048:3 + (rq + 1) * 2048], agF2k, gx[0:P, cc_:cc_ + 1], reads=[gxr, agFr], writes=[rs])
    yTb = Rot(kb, 2, [128, N], BF16, "syTb", es=es)
    xcv = Rot(kb, 2, [128, N], F32, "sxcv", es=es)
    bcv = Rot(kb, 2, [64, N], F32, "sbcv", es=es)
    ccv = Rot(kb, 2, [64, N], F32, "sccv", es=es)
    xs = Rot(kb, 2, [128, N], F32, "sxs", es=es)
    bs = Rot(kb, 2, [64, N], BF16, "sbs", es=es)
    cs = Rot(kb, 2, [64, N], BF16, "scs", es=es)
    xtm = Rot(kb, 2, [128, 128], F32, "sxtm", es=es)
    Xb = Rot(kb, 2, [128, 128], BF16, "sXb", es=es)
    Xd = Rot(kb, 2, [128, 128], BF16, "sXd", es=es)
    Btm = Rot(kb, 2, [128, 64], BF16, "sBtm", es=es)
    CBm = Rot(kb, 2, [128, 128], F32, "sCBm", es=es)
    lh = Rot(kb, 2, [128, 128], F32, "slh", es=es)
    EE = Rot(kb, 2, [128, 128], F32, "sE", es=es)
    MT = Rot(kb, 2, [128, 128], BF16, "sMT", es=es)
    yt = Rot(kb, 2, [128, 8, 128], F32, "syt", es=es)
    pbi = 0
    for j in range(S // N if LEVEL > 0 else 0):
        t0 = j * N
        xt, xr = xfull[:, t0:t0 + N + 3], xfr
        bt, br = bfull[:, t0:t0 + N + 3], bfr
        ctt, ctr = cfull[:, t0:t0 + N + 3], cfr
        xc, xcr = xcv.next(); bc, bcr = bcv.next(); cc, ccr = ccv.next()
        emit_conv(kb, "dve", xc, xt, vt, vr, 0, N, 128, xr, xcr)
        emit_conv(kb, "dve", bc, bt, vt, vr, 5, N, 64, br, bcr)
        emit_conv(kb, "dve", cc, ctt, vt, vr, 10, N, 64, ctr, ccr)
        xst, xsr = xs.next(); bst, bsr = bs.next(); cst_, csr = cs.next()
        kb.op("act", lambda e: e.activation(out=xst[:], in_=xc[:], func=AF.Silu), reads=[xcr], writes=[xsr])
        kb.op("act", lambda e: e.activation(out=bst[:], in_=bc[:], func=AF.Silu), reads=[bcr], writes=[bsr])
        kb.op("act", lambda e: e.activation(out=cst_[:], in_=cc[:], func=AF.Silu), reads=[ccr], writes=[csr])
        ytile, ytr = yt.next()
        for c in range(N // 128 if LEVEL > 1 else 0):
            gc = j * (N // 128) + c
            ts = slice(c * 128, (c + 1) * 128)
            pT, pTr = pbanks.next()
            kb.op("pe", lambda e: e.transpose(pT[:, 0:128], xst[:, ts], ident), reads=[xsr, cr], writes=[pTr])
            xm, xmr = xtm.next()
            kb.op("act", lambda e: e.copy(out=xm[:], in_=pT[:, 0:128]), reads=[pTr], writes=[xmr])
            xb, xbr = Xb.next()
            for h in range(2):
                hs = slice(h * 64, (h + 1) * 64)
                kb.op("dve", lambda e, hs=hs, h=h: e.tensor_scalar(out=xb[:, hs], in0=pT[:, hs], scalar1=dt[:, gc, h:h + 1], scalar2=None, op0=ALU.mult),
                      reads=[pTr, dtr], writes=[xbr])
            xd, xdr = Xd.next()
            for h in range(2):
                hs = slice(h * 64, (h + 1) * 64)
                kb.op("pool", lambda e, hs=hs, h=h: e.tensor_scalar(out=xd[:, hs], in0=xb[:, hs], scalar1=dst[:, gc, h:h + 1], scalar2=None, op0=ALU.mult),
                      reads=[xbr, dsr], writes=[xdr])
            if LEVEL < 3: continue
            pb_t, pb_r = pbf[pbi % len(pbf)]; pbi += 1
            kb.op("pe", lambda e: e.transpose(pb_t[:, 0:64], bst[:, ts], identb[0:64, 0:64]), reads=[bsr, idbr], writes=[pb_r])
            btm, btmr = Btm.next()
            kb.op("act", lambda e: e.copy(out=btm[:], in_=pb_t[:, 0:64]), reads=[pb_r], writes=[btmr])
            if LEVEL < 4: continue
            pcb, pcbr = pbanks.next()
            kb.op("pe", lambda e: e.matmul(pcb[:, 0:128], lhsT=bst[:, ts], rhs=cst_[:, ts], start=True, stop=True), reads=[bsr, csr], writes=[pcbr])
            cbm, cbmr = CBm.next()
            kb.op("dve", lambda e: e.tensor_tensor(out=cbm[:], in0=pcb[:, 0:128], in1=tri, op=ALU.mult), reads=[pcbr, cr], writes=[cbmr])
            for h in range(2 if LEVEL > 4 else 0):
                hs = slice(h * 64, (h + 1) * 64)
                l_, lr_ = lh.next()
                kb.op("dve", lambda e, h=h: e.tensor_scalar(out=l_[:], in0=U, scalar1=aa[:, gc, h:h + 1], scalar2=None, op0=ALU.mult),
                      reads=[cr, aar], writes=[lr_])
                pseg, psegr = pbanks.next()
                kb.op("pe", lambda e: e.matmul(pseg[:, 0:128], lhsT=l_[:], rhs=tri, start=True, stop=True), reads=[lr_, cr], writes=[psegr])
                E, Er = EE.next()
                kb.op("act", lambda e: e.activation(out=E[:], in_=pseg[:, 0:128], func=AF.Exp), reads=[psegr], writes=[Er])
                mt, mtr = MT.next()
                kb.op("dve", lambda e: e.tensor_tensor(out=mt[:], in0=E[:], in1=cbm[:], op=ALU.mult), reads=[Er, cbmr], writes=[mtr])
                py, pyr = pbanks.next()
                kb.op("pe", lambda e, hs=hs: e.matmul(py[:, 0:64], lhsT=mt[:], rhs=xb[:, hs], start=True, stop=True), reads=[mtr, xbr], writes=[pyr])
                po, por = pbanks.next()
                kb.op("pe", lambda e, h=h: e.matmul(po[:, 0:64], lhsT=cst_[:, ts], rhs=prevb[h][:], start=True, stop=True), reads=[csr, prevbr[h]], writes=[por])
                kb.op("act", lambda e, hs=hs: e.copy(out=ytile[:, c, hs], in_=py[:, 0:64]), reads=[pyr], writes=[ytr])
                kb.op("dve", lambda e, hs=hs, h=h: e.scalar_tensor_tensor(out=ytile[:, c, hs], in0=po[:, 0:64], scalar=dout[:, gc, h:h + 1], in1=ytile[:, c, hs],
                                                                          op0=ALU.mult, op1=ALU.add), reads=[por, dor, ytr], writes=[ytr])
                kb.op("dve", lambda e, hs=hs, h=h: e.scalar_tensor_tensor(out=ytile[:, c, hs], in0=xm[:, hs], scalar=rp[:, 4 + h:5 + h], in1=ytile[:, c, hs],
                                                                          op0=ALU.mult, op1=ALU.add), reads=[xmr, rpr, ytr], writes=[ytr])
                pst, pstr = pbanks.next()
                kb.op("pe", lambda e, hs=hs: e.matmul(pst[0:64, 0:64], lhsT=btm[:], rhs=xd[:, hs], start=True, stop=True), reads=[btmr, xdr], writes=[pstr])
                kb.op("dve", lambda e, h=h: e.scalar_tensor_tensor(out=prev[h][:], in0=prev[h][:], scalar=dtot[0:64, gc, h:h + 1], in1=pst[0:64, 0:64],
                                                                  op0=ALU.mult, op1=ALU.add), reads=[pstr, dtor, prevr[h]], writes=[prevr[h]])
                kb.op("act", lambda e, h=h: e.copy(out=prevb[h][:], in_=prev[h][:]), reads=[prevr[h]], writes=[prevbr[h]])
        yb_, ybr = yTb.next()
        for q2 in range(N // 512):
            pyt, pytr = pbanks.next()
            for c4 in range(4):
                c = q2 * 4 + c4
                kb.op("pe", lambda e, c=c, c4=c4: e.transpose(pyt[:, c4 * 128:(c4 + 1) * 128], ytile[:, c, :], ident), reads=[ytr, cr], writes=[pytr])
            kb.op("act", lambda e, q2=q2: e.copy(out=yb_[:, q2 * 512:(q2 + 1) * 512], in_=pyt[:, :]), reads=[pytr], writes=[ybr])
        kb.dma("sp", yT_dst[:, t0:t0 + N], yb_[:], reads=[ybr, yT_r])


def emit_lru2(kb, agF2k, agFr, gx, gxr, IX, vec, wab, wxb, outT, outr, pbanks, es=None):
    N = 1024
    vt = kb.sb([128, 8], F32, "lvec", es=es); vr = Res()
    wa = kb.sb([128, 128], F32, "lwa", es=es); war = Res()
    wx = kb.sb([128, 128], F32, "lwx", es=es); wxr = Res()
    kb.dma("sp", vt[:], vec[:, :], writes=[vr])
    kb.dma("sp", wa[:], wab[:, :], writes=[war])
    kb.dma("sp", wx[:], wxb[:, :], writes=[wxr])
    c1 = kb.sb([128, 1], F32, "lc1", es=es); c1r = Res()
    kb.op("act", lambda e: e.activation(out=c1[:], in_=vt[:, 7:8], func=AF.Exp, scale=-1.0), reads=[vr], writes=[c1r])
    kb.op("act", lambda e: e.activation(out=c1[:], in_=c1[:], func=AF.Ln, bias=1.0), reads=[c1r], writes=[c1r])
    kb.op("dve", lambda e: e.tensor_scalar(out=c1[:], in0=c1[:], scalar1=-8.0, scalar2=None, op0=ALU.mult), reads=[c1r], writes=[c1r])

    xfull = kb.sb([128, S + 3], BF16, "lxfull", es=es); xfr = Res()
    yfull = kb.sb([128, S], BF16, "lyfull", es=es); yfr = Res()
    kb.op("pool", lambda e: e.memset(xfull[:, 0:3], 0.0), writes=[xfr])
    for rp in range(4):
        c1_ = IX["lx%d" % rp]; c2_ = IX["ly%d" % rp]
        kb.gather(xfull[:, 3 + rp * 2048:3 + (rp + 1) * 2048], agF2k, gx[:, c1_:c1_ + 1], reads=[gxr, agFr], writes=[xfr])
        kb.gather(yfull[:, rp * 2048:(rp + 1) * 2048], agF2k, gx[:, c2_:c2_ + 1], reads=[gxr, agFr], writes=[yfr])
    obf = Rot(kb, 2, [128, N], BF16, "lob", es=es)
    xc = Rot(kb, 2, [128, N], F32, "lxc", es=es)
    rr = Rot(kb, 2, [128, N], F32, "lr", es=es)
    ii = Rot(kb, 2, [128, N], F32, "li", es=es)
    aa = Rot(kb, 2, [128, N], F32, "la", es=es)
    qq = Rot(kb, 2, [128, N], F32, "lq", es=es)
    hh = Rot(kb, 2, [128, N], F32, "lh", es=es)
    uu = Rot(kb, 2, [128, N], F32, "lu", es=es)
    hprev = None
    for j in range(S // N):
        t0 = j * N
        xt, xr = xfull[:, t0:t0 + N + 3], xfr
        yt, yr = yfull[:, t0:t0 + N], yfr
        ct, cr = xc.next()
        kb.op("dve", lambda e: e.tensor_scalar(out=ct[:], in0=xt[:, 0:N], scalar1=vt[:, 0:1], scalar2=vt[:, 4:5],
                                               op0=ALU.mult, op1=ALU.add), reads=[xr, vr], writes=[cr])
        for k in range(1, 4):
            kb.op("dve", lambda e, k=k: e.scalar_tensor_tensor(out=ct[:], in0=xt[:, k:k + N], scalar=vt[:, k:k + 1], in1=ct[:],
                                                              op0=ALU.mult, op1=ALU.add), reads=[xr, vr, cr], writes=[cr])
        rt, rres = rr.next()
        it, ires = ii.next()
        for hf in range(N // 512):
            sl = slice(hf * 512, (hf + 1) * 512)
            pa, par = pbanks.next()
            kb.op("pe", lambda e: e.matmul(pa[:, :], lhsT=wa[:], rhs=ct[:, sl], start=True, stop=True), reads=[war, cr], writes=[par])
            kb.op("act", lambda e: e.activation(out=rt[:, sl], in_=pa[:, :], func=AF.Sigmoid, bias=vt[:, 5:6]), reads=[par, vr], writes=[rres])
            px, pxr = pbanks.next()
            kb.op("pe", lambda e: e.matmul(px[:, :], lhsT=wx[:], rhs=ct[:, sl], start=True, stop=True), reads=[wxr, cr], writes=[pxr])
            kb.op("act", lambda e: e.activation(out=it[:, sl], in_=px[:, :], func=AF.Sigmoid, bias=vt[:, 6:7]), reads=[pxr, vr], writes=[ires])
        at, ar = aa.next()
        kb.op("act", lambda e: e.activation(out=at[:], in_=rt[:], func=AF.Exp, scale=c1[:, 0:1]), reads=[rres, c1r], writes=[ar])
        qt, qr = qq.next()
        kb.op("pool", lambda e: e.tensor_tensor(out=qt[:], in0=at[:], in1=at[:], op=ALU.mult), reads=[ar], writes=[qr])
        kb.op("pool", lambda e: e.tensor_scalar(out=qt[:], in0=qt[:], scalar1=-1.0, scalar2=1.0, op0=ALU.mult, op1=ALU.add), reads=[qr], writes=[qr])
        kb.op("act", lambda e: e.activation(out=qt[:], in_=qt[:], func=AF.Sqrt), reads=[qr], writes=[qr])
        kb.op("dve", lambda e: e.tensor_tensor(out=it[:], in0=it[:], in1=ct[:], op=ALU.mult), reads=[ires, cr], writes=[ires])
        kb.op("dve", lambda e: e.tensor_tensor(out=it[:], in0=it[:], in1=qt[:], op=ALU.mult), reads=[ires, qr], writes=[ires])
        ht, hr = hh.next()
        if hprev is None:
            kb.op("dve", lambda e: e.tensor_tensor_scan(out=ht[:], data0=at[:], data1=it[:], initial=0.0, op0=ALU.mult, op1=ALU.add),
                  reads=[ar, ires], writes=[hr])
        else:
            hp, hpr = hprev
            kb.op("dve", lambda e: e.tensor_tensor_scan(out=ht[:], data0=at[:], data1=it[:], initial=hp[:, N - 1:N], op0=ALU.mult, op1=ALU.add),
                  reads=[ar, ires, hpr], writes=[hr])
        hprev = (ht, hr)
        ut, ur = uu.next()
        kb.op("pool", lambda e: e.tensor_tensor(out=ut[:], in0=yt[:], in1=yt[:], op=ALU.mult), reads=[yr], writes=[ur])
        kb.op("pool", lambda e: e.tensor_scalar(out=ut[:], in0=ut[:], scalar1=0.044715, scalar2=1.0, op0=ALU.mult, op1=ALU.add), reads=[ur], writes=[ur])
        kb.op("pool", lambda e: e.tensor_tensor(out=ut[:], in0=ut[:], in1=yt[:], op=ALU.mult), reads=[ur, yr], writes=[ur])
        kb.op("act", lambda e: e.activation(out=ut[:], in_=ut[:], func=AF.Sigmoid, scale=1.5957691216057308), reads=[ur], writes=[ur])
        kb.op("pool", lambda e: e.tensor_tensor(out=ut[:], in0=ut[:], in1=yt[:], op=ALU.mult), reads=[ur, yr], writes=[ur])
        ob_, obr_ = obf.next()
        kb.op("pool", lambda e: e.tensor_tensor(out=ob_[:], in0=ut[:], in1=ht[:], op=ALU.mult), reads=[ur, hr], writes=[obr_])
        kb.dma("sp", outT[:, t0:t0 + N], ob_[:], reads=[obr_, outr])


def emit_C2(kb, xT, agN512, agNr, agY2k, agYr, agL2k, agLr, gx, gxr, IX, pT, w_in, pnsa, pssd, plru, w_out, pwg, pwp, rw, ewg, ewu, ewd, cst, x2T, x2r, final_out, banks, es, xTr=None):
    gen = Rot.__new__(Rot); gen.t = banks; gen.i = 0
    sb = lambda shape, dt, name: kb.sb(shape, dt, name, es=es)
    cv = sb([128, CW], F32, "cc"); cvr = Res()
    kb.dma("sp", cv[:], cst[:, :], writes=[cvr])
    ones1k = cv[:, 52:180]; ones512 = cv[:, 180:308]; ident = cv[:, 308:436]
    selE = lambda e: cv[0:16, 436 + e * 128:436 + (e + 1) * 128]
    rb = cv[:, 36:52]

    tmpf = Rot(kb, 4, [128, 512], F32, "ctf", es=es)
    stat = Rot(kb, 3, [128, 512], F32, "cstat", es=es)
    kb.uid += 1
    mixD = kb.dram("mixD%d" % kb.uid, [D, TOK], BF16, "Internal")
    mixDr = [Res() for _ in range(8)]
    wst_box = [None]

    def load_w(src_rows, nk, ncols):
        wt, wr = wst_box[0].next()
        kb.dma("pool", wt[:, 0:nk, 0:ncols], src_rows.rearrange("(c p) f -> p c f", p=128), writes=[wr])
        return wt, wr

    def ln_stats(src, srcr, nchunk, tt, ones_ap, eps, sq_eng="dve"):
        ts = slice(tt * 512, (tt + 1) * 512)
        pm_, pmr = gen.next()
        for c in range(nchunk):
            kb.op("pe", lambda e, c=c: e.matmul(pm_[:, :], lhsT=ones_ap, rhs=src[:, c, ts], start=(c == 0), stop=(c == nchunk - 1)), reads=[cvr, srcr[c]], writes=[pmr])
        pq_, pqr = gen.next()
        for c in range(nchunk):
            sq, sqr = tmpf.next()
            kb.op(sq_eng, lambda e, c=c: e.tensor_tensor(out=sq[:], in0=src[:, c, ts], in1=src[:, c, ts], op=ALU.mult), reads=[srcr[c]], writes=[sqr])
            kb.op("pe", lambda e, c=c: e.matmul(pq_[:, :], lhsT=ones_ap, rhs=sq[:], start=(c == 0), stop=(c == nchunk - 1)), reads=[cvr, sqr], writes=[pqr])
        mean, meanr = stat.next()
        kb.op("act", lambda e: e.copy(out=mean[:], in_=pm_[:, :]), reads=[pmr], writes=[meanr])
        rstd, rstdr = stat.next()
        return mean, meanr, pq_, pqr, rstd, rstdr

    with contextlib.ExitStack() as es1:
        sb1 = lambda shape, dt, name: kb.sb(shape, dt, name, es=es1)
        wst_box[0] = Rot.__new__(Rot); wst_box[0].i = 0
        wst_box[0].t = [(sb1([128, 8, 512], BF16, "cw"), Res()) for _ in range(3)]
        xbf = sb1([128, 8, TOK], BF16, "cxbf"); xbr = [Res() for _ in range(8)]
        for c in range(8):
            kb.dma("pool", xbf[:, c, :], xT[c * 128:(c + 1) * 128, :], reads=([xTr[c]] if xTr else []), writes=[xbr[c]])
        ob = [sb1([128, 4, TOK], BF16, "cob%d" % i) for i in range(3)]
        obr = [[Res() for _ in range(4)] for _ in range(3)]
        for c in range(4):
            for q4 in range(4):
                cn = IX["on%d_%d" % (c, q4)]
                kb.gather(ob[0][:, c, q4 * 512:(q4 + 1) * 512], agN512, gx[:, cn:cn + 1], reads=[gxr, agNr], writes=[obr[0][c]])
            cl = IX["l%d" % c]
            kb.gather(ob[2][:, c, :], agL2k, gx[:, cl:cl + 1], reads=[gxr, agLr], writes=[obr[2][c]])
        with contextlib.ExitStack() as es1b:
            yg = kb.sb([128, 4, TOK], F32, "cyg", es=es1b); ygr = [Res() for _ in range(4)]
            ygb = ob[1]; ygbr = obr[1]
            for c in range(4):
                cy = IX["y%d" % c]
                kb.gather(ygb[:, c, :], agY2k, gx[:, cy:cy + 1], reads=[gxr, agYr], writes=[ygbr[c]])
            wz, wzr = load_w(w_in[:, OFF_Z:OFF_Z + 512], 8, 512)
            for tt in range(NT):
                ts = slice(tt * 512, (tt + 1) * 512)
                for fc in range(4):
                    pz, pzr = gen.next()
                    for c in range(8):
                        kb.op("pe", lambda e, c=c: e.matmul(pz[:, :], lhsT=wz[:, c, fc * 128:(fc + 1) * 128], rhs=xbf[:, c, ts], start=(c == 0), stop=(c == 7)),
                              reads=[wzr, xbr[c]], writes=[pzr])
                    sz, szr = tmpf.next()
                    kb.op("act", lambda e: e.activation(out=sz[:], in_=pz[:, :], func=AF.Silu), reads=[pzr], writes=[szr])
                    kb.op("dve", lambda e, fc=fc: e.tensor_tensor(out=yg[:, fc, ts], in0=ygb[:, fc, ts], in1=sz[:], op=ALU.mult), reads=[szr, ygbr[fc]], writes=[ygr[fc]])
                pq_, pqr = gen.next()
                for c in range(4):
                    sq, sqr = tmpf.next()
                    kb.op("dve", lambda e, c=c: e.tensor_tensor(out=sq[:], in0=yg[:, c, ts], in1=yg[:, c, ts], op=ALU.mult), reads=[ygr[c]], writes=[sqr])
                    kb.op("pe", lambda e, c=c: e.matmul(pq_[:, :], lhsT=ones512, rhs=sq[:], start=(c == 0), stop=(c == 3)), reads=[cvr, sqr], writes=[pqr])
                rstd, rstdr = stat.next()
                kb.op("dve", lambda e: e.tensor_scalar(out=rstd[:], in0=pq_[:, :], scalar1=1e-5, scalar2=None, op0=ALU.add), reads=[pqr], writes=[rstdr])
                kb.op("act", lambda e: e.activation(out=rstd[:], in_=rstd[:], func=AF.Sqrt), reads=[rstdr], writes=[rstdr])
                kb.op("dve", lambda e: e.reciprocal(out=rstd[:], in_=rstd[:]), reads=[rstdr], writes=[rstdr])
                for c in range(4):
                    t1, t1r = tmpf.next()
                    kb.op("dve", lambda e, c=c: e.tensor_tensor(out=t1[:], in0=yg[:, c, ts], in1=rstd[:], op=ALU.mult), reads=[ygr[c], rstdr], writes=[t1r])
                    kb.op("act", lambda e, c=c: e.activation(out=ob[1][:, c, ts], in_=t1[:], func=AF.Copy, scale=cv[:, 32 + c:33 + c]), reads=[t1r, cvr], writes=[obr[1][c]])
            kb.barrier()
        if CDBG == "ossd":
            dbg = sb1([128, 4, TOK], F32, "cdbg"); dbgr = Res()
            for c in range(4):
                kb.op("dve", lambda e, c=c: e.tensor_copy(out=dbg[:, c, :], in_=ob[1][:, c, :]), reads=[obr[1][c]], writes=[dbgr])
                kb.dma("sp", x2T[c * 128:(c + 1) * 128, :], dbg[:, c, :], reads=[dbgr], final=True)
            kb.barrier()
            return
        projs = [pnsa, pssd, plru]
        mixh = sb1([128, 4, TOK], BF16, "cmixh"); mixhr = [Res() for _ in range(4)]
        for half in range(2):
            fs = slice(half * 512, (half + 1) * 512)
            for br in range(3):
                wp, wpr = load_w(projs[br][:, fs], 4, 512)
                wm, wmr = load_w(w_in[:, OFF_MERGE + br * 1024 + half * 512: OFF_MERGE + br * 1024 + (half + 1) * 512], 8, 512)
                for fc in range(4):
                    oc = half * 4 + fc
                    for tt in range(NT):
                        ts = slice(tt * 512, (tt + 1) * 512)
                        pg, pgr = gen.next()
                        for c in range(8):
                            kb.op("pe", lambda e, c=c: e.matmul(pg[:, :], lhsT=wm[:, c, fc * 128:(fc + 1) * 128], rhs=xbf[:, c, ts], start=(c == 0), stop=(c == 7)),
                                  reads=[wmr, xbr[c]], writes=[pgr])
                        pp, ppr = gen.next()
                        for c in range(4):
                            kb.op("pe", lambda e, c=c: e.matmul(pp[:, :], lhsT=wp[:, c, fc * 128:(fc + 1) * 128], rhs=ob[br][:, c, ts], start=(c == 0), stop=(c == 3)),
                                  reads=[wpr, obr[br][c]], writes=[ppr])
                        sg, sgr = tmpf.next()
                        kb.op("act", lambda e: e.activation(out=sg[:], in_=pg[:, :], func=AF.Sigmoid), reads=[pgr], writes=[sgr])
                        if br == 0:
                            kb.op("dve", lambda e: e.tensor_tensor(out=mixh[:, fc, ts], in0=pp[:, :], in1=sg[:], op=ALU.mult), reads=[ppr, sgr], writes=[mixhr[fc]])
                        else:
                            kb.op("dve", lambda e: e.tensor_tensor(out=sg[:], in0=pp[:, :], in1=sg[:], op=ALU.mult), reads=[ppr, sgr], writes=[sgr])
                            kb.op("dve", lambda e: e.tensor_tensor(out=mixh[:, fc, ts], in0=mixh[:, fc, ts], in1=sg[:], op=ALU.add), reads=[sgr, mixhr[fc]], writes=[mixhr[fc]])
            for fc in range(4):
                kb.dma("sp", mixD[(half * 4 + fc) * 128:(half * 4 + fc + 1) * 128, :], mixh[:, fc, :], reads=[mixhr[fc]], writes=[mixDr[half * 4 + fc]])
        kb.barrier()

    with contextlib.ExitStack() as es2:
        sb2 = lambda shape, dt, name: kb.sb(shape, dt, name, es=es2)
        r = sb2([128, 8, TOK], F32, "cr"); rr = [Res() for _ in range(8)]
        x1b = sb2([128, 8, TOK], BF16, "cx1b"); x1br = [Res() for _ in range(8)]
        for c in range(8):
            kb.dma("sp", r[:, c, :], xT[c * 128:(c + 1) * 128, :], reads=([xTr[c]] if xTr else []), writes=[rr[c]])
        gatesT = sb2([16, TOK], F32, "cgT"); gTr = Res()
        rwt = sb2([128, 8, 16], F32, "crw"); rwr = Res()
        kb.dma("sp", rwt[:], rw.rearrange("(c p) e -> p c e", p=128), writes=[rwr])
        pad = sb2([128, 4, 8], F32, "cpad"); padr = Res()
        kb.op("pool", lambda e: e.memset(pad[:], -1e30), writes=[padr])
        rt = Rot(kb, 2, [128, 16], F32, "crt", es=es2)
        rt2 = Rot(kb, 2, [128, 16], F32, "crt2", es=es2)
        t8 = Rot(kb, 2, [128, 4, 8], F32, "ct8", es=es2)
        sm4 = Rot(kb, 4, [128, 4], F32, "csm4", es=es2)
        sm1 = Rot(kb, 4, [128, 1], F32, "csm1", es=es2)
        t8b = Rot(kb, 2, [128, 8], F32, "ct8b", es=es2)
        es2a = contextlib.ExitStack()
        wst_box[0] = Rot.__new__(Rot); wst_box[0].i = 0
        wst_box[0].t = [(kb.sb([128, 8, 512], BF16, "cw2", es=es2a), Res()) for _ in range(2)]
        mixed = kb.sb([128, 8, TOK], BF16, "cmixed", es=es2a); mixr = [Res() for _ in range(8)]
        pb_ = kb.sb([128, 2, TOK], BF16, "cpb", es=es2a); pbr = [Res() for _ in range(2)]
        for c in range(8):
            kb.dma("sp", mixed[:, c, :], mixD[c * 128:(c + 1) * 128, :], reads=[mixDr[c]], writes=[mixr[c]])
        if CDBG == "mixed":
            for c in range(8):
                kb.op("dve", lambda e, c=c: e.tensor_copy(out=r[:, c, :], in_=mixed[:, c, :]), reads=[mixr[c], rr[c]], writes=[rr[c]])
                kb.dma("sp", x2T[c * 128:(c + 1) * 128, :], r[:, c, :], reads=[rr[c]], final=True)
            kb.barrier(); es2a.close()
            return
        for half in range(2):
            wo, wor = load_w(w_out[:, half * 512:(half + 1) * 512], 8, 512)
            for fc in range(4):
                oc = half * 4 + fc
                for tt in range(NT):
                    ts = slice(tt * 512, (tt + 1) * 512)
                    pu, pur = gen.next()
                    for c in range(8):
                        kb.op("pe", lambda e, c=c: e.matmul(pu[:, :], lhsT=wo[:, c, fc * 128:(fc + 1) * 128], rhs=mixed[:, c, ts], start=(c == 0), stop=(c == 7)),
                              reads=[wor, mixr[c]], writes=[pur])
                    kb.op("dve", lambda e: e.scalar_tensor_tensor(out=r[:, oc, ts], in0=r[:, oc, ts], scalar=ALPHA, in1=pu[:, :], op0=ALU.mult, op1=ALU.add),
                          reads=[pur, rr[oc]], writes=[rr[oc]])

        def layer_norm(src, srcr, gcol, bcol, dst_b, dst_br):
            for tt in range(NT):
                ts = slice(tt * 512, (tt + 1) * 512)
                mean, meanr, pq_, pqr, rstd, rstdr = ln_stats(src, srcr, 8, tt, ones1k, 1e-5)
                m2, m2r = stat.next()
                kb.op("dve", lambda e: e.tensor_tensor(out=m2[:], in0=mean[:], in1=mean[:], op=ALU.mult), reads=[meanr], writes=[m2r])
                kb.op("dve", lambda e: e.scalar_tensor_tensor(out=rstd[:], in0=pq_[:, :], scalar=1e-5, in1=m2[:], op0=ALU.add, op1=ALU.subtract), reads=[pqr, m2r], writes=[rstdr])
                kb.op("act", lambda e: e.activation(out=rstd[:], in_=rstd[:], func=AF.Sqrt), reads=[rstdr], writes=[rstdr])
                kb.op("dve", lambda e: e.reciprocal(out=rstd[:], in_=rstd[:]), reads=[rstdr], writes=[rstdr])
                for c in range(8):
                    kb.op("dve", lambda e, c=c: e.tensor_tensor(out=src[:, c, ts], in0=src[:, c, ts], in1=mean[:], op=ALU.subtract), reads=[meanr, srcr[c]], writes=[srcr[c]])
                    kb.op("dve", lambda e, c=c: e.tensor_tensor(out=src[:, c, ts], in0=src[:, c, ts], in1=rstd[:], op=ALU.mult), reads=[rstdr, srcr[c]], writes=[srcr[c]])
                    kb.op("dve", lambda e, c=c: e.tensor_scalar(out=src[:, c, ts], in0=src[:, c, ts], scalar1=cv[:, gcol + c:gcol + c + 1], scalar2=cv[:, bcol + c:bcol + c + 1],
                                                               op0=ALU.mult, op1=ALU.add), reads=[cvr, srcr[c]], writes=[srcr[c]])
                    if dst_b is not None:
                        kb.op("act", lambda e, c=c: e.copy(out=dst_b[:, c, ts], in_=src[:, c, ts]), reads=[srcr[c]], writes=[dst_br[c]])

        layer_norm(r, rr, 0, 8, x1b, x1br)
        if CDBG == "x1":
            for c in range(8):
                kb.dma("sp", x2T[c * 128:(c + 1) * 128, :], r[:, c, :], reads=[rr[c]], final=True)
            kb.barrier(); es2a.close()
            return

        for s_ in range(TOK // 128):
            ss = slice(s_ * 128, (s_ + 1) * 128)
            pl, plr = gen.next()
            for c in range(8):
                kb.op("pe", lambda e, c=c: e.matmul(pl[:, 0:16], lhsT=r[:, c, ss], rhs=rwt[:, c, :], start=(c == 0), stop=(c == 7)), reads=[rr[c], rwr], writes=[plr])
            aff, affr = rt.next()
            kb.op("act", lambda e: e.activation(out=aff[:], in_=pl[:, 0:16], func=AF.Sigmoid), reads=[plr], writes=[affr])
            sel, selr = rt2.next()
            kb.op("dve", lambda e: e.tensor_tensor(out=sel[:], in0=aff[:], in1=rb, op=ALU.add), reads=[affr, cvr], writes=[selr])
            kb.op("dve", lambda e: e.tensor_copy(out=pad[:, :, 0:4], in_=sel[:].rearrange("p (g k) -> p g k", g=4)), reads=[selr, padr], writes=[padr])
            tp, tpr = t8.next()
            for g in range(4):
                kb.op("dve", lambda e, g=g: e.max(out=tp[:, g, :], in_=pad[:, g, :]), reads=[padr], writes=[tpr])
            gs, gsr = sm4.next()
            kb.op("dve", lambda e: e.tensor_tensor(out=gs[:], in0=tp[:, :, 0], in1=tp[:, :, 1], op=ALU.add), reads=[tpr], writes=[gsr])
            gm, gmr = sm1.next()
            kb.op("dve", lambda e: e.reduce_max(out=gm[:], in_=gs[:], axis=AX.X), reads=[gsr], writes=[gmr])
            isb, isbr = sm4.next()
            kb.op("dve", lambda e: e.tensor_scalar(out=isb[:], in0=gs[:], scalar1=gm[:, 0:1], scalar2=None, op0=ALU.is_ge), reads=[gsr, gmr], writes=[isbr])
            off, offr = sm4.next()
            kb.op("dve", lambda e: e.tensor_scalar(out=off[:], in0=isb[:], scalar1=1e9, scalar2=-1e9, op0=ALU.mult, op1=ALU.add), reads=[isbr], writes=[offr])
            msk, mskr = rt2.next()
            for g in range(4):
                kb.op("dve", lambda e, g=g: e.tensor_scalar(out=msk[:, g * 4:(g + 1) * 4], in0=sel[:, g * 4:(g + 1) * 4], scalar1=isb[:, g:g + 1], scalar2=off[:, g:g + 1],
                                                            op0=ALU.mult, op1=ALU.add), reads=[selr, isbr, offr], writes=[mskr])
            tb, tbr = t8b.next()
            kb.op("dve", lambda e: e.max(out=tb[:], in_=msk[:]), reads=[mskr], writes=[tbr])
            kb.op("dve", lambda e: e.tensor_scalar(out=msk[:], in0=msk[:], scalar1=tb[:, 1:2], scalar2=None, op0=ALU.is_ge), reads=[mskr, tbr], writes=[mskr])
            kb.op("dve", lambda e: e.tensor_tensor(out=msk[:], in0=msk[:], in1=aff[:], op=ALU.mult), reads=[mskr, affr], writes=[mskr])
            ws, wsr = sm1.next()
            kb.op("dve", lambda e: e.reduce_sum(out=ws[:], in_=msk[:], axis=AX.X), reads=[mskr], writes=[wsr])
            kb.op("dve", lambda e: e.reciprocal(out=ws[:], in_=ws[:]), reads=[wsr], writes=[wsr])
            kb.op("dve", lambda e: e.tensor_scalar(out=msk[:], in0=msk[:], scalar1=ws[:, 0:1], scalar2=None, op0=ALU.mult), reads=[mskr, wsr], writes=[mskr])
            pt, ptr_ = gen.next()
            kb.op("pe", lambda e: e.transpose(pt[0:16, 0:128], msk[:], ident), reads=[mskr, cvr], writes=[ptr_])
            kb.op("act", lambda e: e.copy(out=gatesT[:, ss], in_=pt[0:16, 0:128]), reads=[ptr_], writes=[gTr])

        for c in range(2):
            kb.dma("pool", pb_[:, c, :], pT[c * 128:(c + 1) * 128, :], writes=[pbr[c]])
        for half in range(2):
            fs = slice(half * 512, (half + 1) * 512)
            wg_, wgr = load_w(pwg[:, fs], 8, 512)
            wp_, wpr = load_w(pwp[:, fs], 2, 512)
            for fc in range(4):
                oc = half * 4 + fc
                for tt in range(NT):
                    ts = slice(tt * 512, (tt + 1) * 512)
                    pg, pgr = gen.next()
                    for c in range(8):
                        kb.op("pe", lambda e, c=c: e.matmul(pg[:, :], lhsT=wg_[:, c, fc * 128:(fc + 1) * 128], rhs=x1b[:, c, ts], start=(c == 0), stop=(c == 7)),
                              reads=[wgr, x1br[c]], writes=[pgr])
                    pp, ppr = gen.next()
                    for c in range(2):
                        kb.op("pe", lambda e, c=c: e.matmul(pp[:, :], lhsT=wp_[:, c, fc * 128:(fc + 1) * 128], rhs=pb_[:, c, ts], start=(c == 0), stop=(c == 1)),
                              reads=[wpr, pbr[c]], writes=[ppr])
                    sg, sgr = tmpf.next()
                    kb.op("act", lambda e: e.activation(out=sg[:], in_=pg[:, :], func=AF.Sigmoid), reads=[pgr], writes=[sgr])
                    kb.op("dve", lambda e: e.tensor_tensor(out=sg[:], in0=pp[:, :], in1=sg[:], op=ALU.mult), reads=[ppr, sgr], writes=[sgr])
                    kb.op("dve", lambda e: e.scalar_tensor_tensor(out=r[:, oc, ts], in0=r[:, oc, ts], scalar=ALPHA, in1=sg[:], op0=ALU.mult, op1=ALU.add),
                          reads=[sgr, rr[oc]], writes=[rr[oc]])

        kb.barrier()
        es2a.close()
        if CDBG == "ple":
            for c in range(8):
                kb.dma("sp", x2T[c * 128:(c + 1) * 128, :], r[:, c, :], reads=[rr[c]], final=True)
            return
        ew = Rot(kb, 2, [128, 2, 3, 8, 256], BF16, "cew", es=es2)
        hb = Rot(kb, 2, [128, 4, 512], BF16, "chb", es=es2)
        steps = [(ep, tt) for ep in range(8) for tt in range(NT)]
        live = {}
        wts = {}

        def load_pair(ep):
            wt, wr = ew.next()
            for m in range(2):
                e_ = ep * 2 + m
                kb.dma("pool", wt[:, m, 0, :, :], ewg[e_, :, :].rearrange("(c p) f -> p c f", p=128), writes=[wr])
                kb.dma("pool", wt[:, m, 1, :, :], ewu[e_, :, :].rearrange("(c p) f -> p c f", p=128), writes=[wr])
                kb.dma("pool", wt[:, m, 2, :, :].rearrange("p (c a) f -> p c (a f)", c=2), ewd[e_, :, :].rearrange("(c p) f -> p c f", p=128), writes=[wr])
            return wt, wr

        for idx in range(len(steps) + 1):
            if idx < len(steps):
                ep, tt = steps[idx]
                if tt == 0:
                    if ep == 0:
                        wts[0] = load_pair(0)
                    wt, wr = wts[ep]
                ts = slice(tt * 512, (tt + 1) * 512)
                h, hr = hb.next()
                for m in range(2):
                    e_ = ep * 2 + m
                    pgb, pgbr = gen.next()
                    kb.op("pe", lambda e, e_=e_, pgb=pgb, ts=ts: e.matmul(pgb[:, :], lhsT=selE(e_), rhs=gatesT[:, ts], start=True, stop=True), reads=[cvr, gTr], writes=[pgbr])
                    gb, gbr = tmpf.next()
                    kb.op("act", lambda e, gb=gb, pgb=pgb: e.copy(out=gb[:], in_=pgb[:, :]), reads=[pgbr], writes=[gbr])
                    for fc in range(2):
                        pg, pgr = gen.next()
                        for c in range(8):
                            kb.op("pe", lambda e, c=c, m=m, fc=fc, pg=pg, wt=wt, ts=ts: e.matmul(pg[:, :], lhsT=wt[:, m, 0, c, fc * 128:(fc + 1) * 128], rhs=x1b[:, c, ts], start=(c == 0), stop=(c == 7)),
                                  reads=[wr, x1br[c]], writes=[pgr])
                        pu, pur = gen.next()
                        for c in range(8):
                            kb.op("pe", lambda e, c=c, m=m, fc=fc, pu=pu, wt=wt, ts=ts: e.matmul(pu[:, :], lhsT=wt[:, m, 1, c, fc * 128:(fc + 1) * 128], rhs=x1b[:, c, ts], start=(c == 0), stop=(c == 7)),
                                  reads=[wr, x1br[c]], writes=[pur])
                        sg, sgr = tmpf.next()
                        kb.op("act", lambda e, sg=sg, pg=pg: e.activation(out=sg[:], in_=pg[:, :], func=AF.Silu), reads=[pgr], writes=[sgr])
                        kb.op("dve", lambda e, sg=sg, pu=pu: e.tensor_tensor(out=sg[:], in0=pu[:, :], in1=sg[:], op=ALU.mult), reads=[pur, sgr], writes=[sgr])
                        kb.op("dve", lambda e, m=m, fc=fc, h=h, sg=sg, gb=gb: e.tensor_tensor(out=h[:, m * 2 + fc, :], in0=sg[:], in1=gb[:], op=ALU.mult), reads=[sgr, gbr], writes=[hr])
                live[idx] = (wt, wr, h, hr, ts)
            if idx >= 1:
                wt_, wr_, h_, hr_, ts_ = live.pop(idx - 1)
                for dc in range(8):
                    py, pyr = gen.next()
                    for m in range(2):
                        wd = wt_[:, m, 2, :, :].rearrange("p (c a) f -> p c (a f)", c=2)
                        for fc in range(2):
                            kb.op("pe", lambda e, wd=wd, m=m, fc=fc, py=py, h_=h_, dc=dc: e.matmul(py[:, :], lhsT=wd[:, fc, dc * 128:(dc + 1) * 128], rhs=h_[:, m * 2 + fc, :],
                                                                                                  start=(m == 0 and fc == 0), stop=(m == 1 and fc == 1)), reads=[wr_, hr_], writes=[pyr])
                    kb.op("dve", lambda e, dc=dc, py=py, ts_=ts_: e.tensor_tensor(out=r[:, dc, ts_], in0=r[:, dc, ts_], in1=py[:, :], op=ALU.add), reads=[pyr, rr[dc]], writes=[rr[dc]])
            if idx < len(steps) and steps[idx][1] == 0 and steps[idx][0] + 1 < 8:
                wts[steps[idx][0] + 1] = load_pair(steps[idx][0] + 1)

        if CDBG != "moe":
            layer_norm(r, rr, 16, 24, None, None)
        for c in range(8):
            kb.dma("sp", x2T[c * 128:(c + 1) * 128, :], r[:, c, :], reads=[rr[c]], writes=[x2r[c]], final=final_out)
        kb.barrier()


NF = 3104
NOM_TILES = [0, 2, 4, 6, 8, 10, 12, 14]
BATCH = 2
T_ALL = BATCH * S
NCORES = 8
U32 = mybir.dt.uint32


def idx_cols():
    names = []
    for nm in ("kcmp", "vcmp", "ksel", "vsel", "kwin", "vwin", "sx", "sB", "sC", "dt", "lx", "ly"):
        names += [nm + str(rp) for rp in range(4)]
    for k in range(8):
        names += ["q%d_%d" % (k, j) for j in range(4)]
        names.append("gate%d" % k)
    for c in range(4):
        names += ["on%d_%d" % (c, q4) for q4 in range(4)]
        names.append("y%d" % c)
        names.append("l%d" % c)
    return {n: i for i, n in enumerate(names)}


IX = idx_cols()
NIDX = len(IX)


def make_gidx(r):
    g, par = r // 2, r % 2
    t = np.zeros((128, NIDX), np.int64)
    p = np.arange(128)
    def rowF(rp, f):
        f = np.asarray(f)
        k = f // 256
        rows_k = np.where(k < 12, 256, 32)
        return k * 1024 + rp * rows_k + (f - 256 * k)
    rowN = lambda rank, f256: (f256 // 128) * 512 + rank * 128 + f256 % 128
    rowY = lambda rank, ch: (ch // 64) * 256 + rank * 64 + ch % 64
    for rp in range(4):
        for nm, f0 in (("kcmp", 512), ("vcmp", 640), ("ksel", 768), ("vsel", 896), ("kwin", 1024), ("vwin", 1152)):
            t[:, IX[nm + str(rp)]] = rowF(rp, f0 + 64 * g + p)
        t[:, IX["sx%d" % rp]] = rowF(rp, 1304 + 128 * r + p)
        t[:, IX["sB%d" % rp]] = rowF(rp, 1304 + 512 + 64 * g + p)
        t[:, IX["sC%d" % rp]] = rowF(rp, 1304 + 640 + 64 * g + p)
        t[:, IX["dt%d" % rp]] = rp * 8 + 2 * r + p
        t[:, IX["lx%d" % rp]] = rowF(rp, 2080 + 128 * r + p)
        t[:, IX["ly%d" % rp]] = rowF(rp, 2080 + 512 + 128 * r + p)
    for k in range(8):
        i = 2 * k + par
        rp, q4 = i // 4, i % 4
        for j in range(4):
            t[:, IX["q%d_%d" % (k, j)]] = rowF(rp, g * 256 + j * 64 + p) * 4 + q4
        t[:, IX["gate%d" % k]] = rowF(rp, 1280 + 12 * g + p) * 4 + q4
    for c in range(4):
        gg, f256 = c // 2, (c % 2) * 128 + p
        for q4 in range(4):
            i = 4 * r + q4
            t[:, IX["on%d_%d" % (c, q4)]] = rowN(2 * gg + i % 2, f256) * 8 + i // 2
        t[:, IX["y%d" % c]] = rowY(c, p) * 4 + r
        t[:, IX["l%d" % c]] = rowY(c, p) * 4 + r
    t = np.clip(t, 0, None)
    return t.astype(np.uint32)


def emit_A2(kb, xT, xTr, w, agF, agFr, agD, agDr, banks, es):
    xs = kb.sb([128, 8, TOK], BF16, "xs", es=es)
    xs_r = [Res("xs") for _ in range(8)]
    for c in range(8):
        kb.dma("pool", xs[:, c, :], xT[c * 128:(c + 1) * 128, :], reads=([xTr[c]] if xTr else []), writes=[xs_r[c]])
    FB = 512
    wbuf = [(kb.sb([128, 8, FB], BF16, "wb", es=es), Res("wb")) for _ in range(2)]
    obuf = [(kb.sb([128, 512], BF16, "ob", es=es), Res("ob")) for _ in range(4)]
    obf = [(kb.sb([8, 512], F32, "obd", es=es), Res("obd")) for _ in range(2)]
    wv = w.rearrange("(c p) f -> p c f", p=128)
    it = 0
    nb = 0
    for (c0, ncols, r0) in ((0, 1304, 0), (1816, 1800, 1304)):
        for fb in range((ncols + FB - 1) // FB):
            f0 = fb * FB
            fw_ = min(FB, ncols - f0)
            wt, wr = wbuf[nb % 2]
            nb += 1
            kb.dma("pool", wt[:, :, :fw_], wv[:, :, c0 + f0:c0 + f0 + fw_], writes=[wr])
            for fc in range((fw_ + 127) // 128):
                m = min(128, fw_ - fc * 128)
                for tt in range(TOK // 512):
                    pt, pr = banks[it % 4]
                    ot, orr = obuf[it % 4]
                    for c in range(8):
                        kb.op("pe", lambda e, c=c: e.matmul(pt[:m, :], lhsT=wt[:, c, fc * 128:fc * 128 + m], rhs=xs[:, c, tt * 512:(tt + 1) * 512],
                                                             start=(c == 0), stop=(c == 7)), reads=[wr, xs_r[c]], writes=[pr])
                    if it % 2 == 0:
                        kb.op("act", lambda e: e.copy(out=ot[:m, :], in_=pt[:m, :]), reads=[pr], writes=[orr])
                    else:
                        kb.op("dve", lambda e: e.tensor_copy(out=ot[:m, :], in_=pt[:m, :]), reads=[pr], writes=[orr])
                    row = r0 + f0 + fc * 128
                    kb.dma("sp", agF[row:row + m, tt * 512:(tt + 1) * 512], ot[:m, :], reads=[orr, agFr])
                    it += 1
    wt, wr = wbuf[nb % 2]
    kb.dma("pool", wt[:, :, 0:8], wv[:, :, 2584:2592], writes=[wr])
    for tt in range(TOK // 512):
        pt, pr = banks[it % 4]
        it += 1
        for c in range(8):
            kb.op("pe", lambda e, c=c: e.matmul(pt[0:8, :], lhsT=wt[:, c, 0:8], rhs=xs[:, c, tt * 512:(tt + 1) * 512], start=(c == 0), stop=(c == 7)),
                  reads=[wr, xs_r[c]], writes=[pr])
        od, odr = obf[tt % 2]
        kb.op("act", lambda e: e.copy(out=od[:, :], in_=pt[0:8, :]), reads=[pr], writes=[odr])
        kb.dma("sp", agD[:, tt * 512:(tt + 1) * 512], od[:, :], reads=[odr, agDr])


def build_fused(nlayers=2, groups=None, ncores=8, fstop=9, ntiles=8):
    groups = groups or [[0, 1, 2, 3], [4, 5, 6, 7]]
    nc = bass.Bass("TRN2", target_bir_lowering=False)
    es = contextlib.ExitStack()
    with es:
        kb = KB(nc, es)
        I = lambda n, s, dt=F32: kb.dram(n, s, dt, "ExternalInput")
        xT0 = I("xT0", [D, TOK]); pT = I("pT", [2, 256, TOK]); gidx = I("gidx", [128, NIDX], U32)
        w_in = I("w_in", [2, D, 6688])
        peT = I("npeT", [2, 2, 64, 32]); w1 = I("nw1", [2, 2, 2048, 256]); w2 = I("nw2", [2, 2, 256, 64])
        nmask = I("nmask", [128, NMASK, 512]); nE0 = I("nE0", [128, S]); nmisc = I("nmisc", [128, MISC_W])
        svec = I("svec", [2, 128, 16]); srep = I("srep", [2, 128, 8]); scst = I("scst", [128, 4, 128])
        lvec = I("lvec", [2, 128, 8]); lwab = I("lwab", [2, 128, 128]); lwxb = I("lwxb", [2, 128, 128])
        pnsa = I("proj_nsa", [2, 512, D]); pssd = I("proj_ssd", [2, 512, D]); plru = I("proj_lru", [2, 512, D])
        w_out = I("w_out", [2, D, D]); pwg = I("ple_w_gate", [2, D, D]); pwp = I("ple_w_proj", [2, 256, D]); rw = I("router_w", [D, 16])
        ewg = I("exp_w_gate", [2, 16, D, 256]); ewu = I("exp_w_up", [2, 16, D, 256]); ewd = I("exp_w_down", [2, 16, 256, D])
        ccst = I("ccst", [2, 128, CW])
        x2T = kb.dram("x2T", [D, TOK], F32, "ExternalOutput")
        N_ = lambda n, s, dt: kb.dram(n, s, dt, "Internal")
        agF_in = N_("agF_in", [NF, TOK], BF16); agF_out = N_("agF_out", [4 * NF, TOK], BF16)
        agD_in = N_("agD_in", [8, TOK], F32); agD_out = N_("agD_out", [32, TOK], F32)
        nq = 8 * QT
        agN_in = N_("agN_in", [256, nq], BF16); agN_out = N_("agN_out", [1024, nq], BF16)
        agY_in = N_("agY_in", [128, S], BF16); agY_out = N_("agY_out", [512, S], BF16)
        agL_in = N_("agL_in", [128, S], BF16); agL_out = N_("agL_out", [512, S], BF16)
        xcur = N_("xcur", [D, TOK], F32)
        rD_in, rD_out = Res(), Res()
        rF_in_ch = [Res() for _ in range(13)]
        rF_in = MultiRes(rF_in_ch)
        rF_ch = [Res() for _ in range(13)]
        rF_nsa, rF_ssd, rF_lru = MultiRes(rF_ch[0:6]), MultiRes(rF_ch[5:9]), MultiRes(rF_ch[8:13])
        rN_in, rN_out, rY_in, rY_out, rL_in, rL_out = Res(), Res(), Res(), Res(), Res(), Res()
        xcur_r = [Res() for _ in range(8)]
        agF512 = agF_out.rearrange("r (q t) -> (r q) t", t=512)
        agN512 = agN_out.rearrange("r (q t) -> (r q) t", t=512)
        agY2k = agY_out.rearrange("r (q t) -> (r q) t", t=2048)
        agL2k = agL_out.rearrange("r (q t) -> (r q) t", t=2048)
        gx = kb.sb([128, NIDX], U32, "gx"); gxr = Res()
        kb.dma("sp", gx[:], gidx[:, :], writes=[gxr])
        banks = [(kb.ps([128, 512], F32, "bank"), Res("bank", excl=True)) for _ in range(8)]
        rot = lambda items: _rot(items)
        for L in range(nlayers):
            xin = xT0 if L == 0 else xcur
            xin_r = None if L == 0 else xcur_r
            with contextlib.ExitStack() as sa:
                emit_A2(kb, xin, xin_r, w_in[L], agF_in, rF_in, agD_in, rD_in, banks, sa)
                kb.barrier()
            for k in range(13):
                rows = 256 if k < 12 else 32
                kb.allgather(agF_in[k * 256:k * 256 + rows, :], agF_out[k * 1024:k * 1024 + 4 * rows, :], groups, writes=[rF_in_ch[k], rF_ch[k]])
            kb.allgather(agD_in[:, :], agD_out[:, :], groups, writes=[rD_in, rD_out])
            if fstop <= 1:
                break
            with contextlib.ExitStack() as s1:
                emit_nsa2(kb, NOM_TILES[:ntiles], agF_out, agF512, rF_nsa, gx, gxr, IX, peT[L], w1[L], w2[L], nmask, nE0, nmisc, agN_in, rN_in,
                          banks[0:4], banks[4], rot(banks[5:8]), s1)
                kb.barrier()
            for k in range(2):
                kb.allgather(agN_in[k * 128:(k + 1) * 128, :], agN_out[k * 512:(k + 1) * 512, :], groups, writes=[rN_in, rN_out])
            if fstop <= 2:
                break
            with contextlib.ExitStack() as s2:
                pbf = [(banks[6][0][:, 0:32].bitcast(BF16), banks[6][1]), (banks[7][0][:, 0:32].bitcast(BF16), banks[7][1])]
                emit_ssd2(kb, agF_out, rF_ssd, agD_out, rD_out, gx, gxr, IX, svec[L], srep[L], scst, agY_in, rY_in, rot(banks[0:6]), pbf, es=s2)
                kb.barrier()
            for k in range(2):
                kb.allgather(agY_in[k * 64:(k + 1) * 64, :], agY_out[k * 256:(k + 1) * 256, :], groups, writes=[rY_in, rY_out])
            if fstop <= 3:
                break
            with contextlib.ExitStack() as s3:
                emit_lru2(kb, agF_out, rF_lru, gx, gxr, IX, lvec[L], lwab[L], lwxb[L], agL_in, rL_in, rot(banks[0:4]), es=s3)
                kb.barrier()
            for k in range(2):
                kb.allgather(agL_in[k * 64:(k + 1) * 64, :], agL_out[k * 256:(k + 1) * 256, :], groups, writes=[rL_in, rL_out])
            if fstop <= 4:
                break
            last = (L == nlayers - 1)
            with contextlib.ExitStack() as s4:
                emit_C2(kb, xin, agN512, rN_out, agY2k, rY_out, agL2k, rL_out, gx, gxr, IX, pT[L], w_in[L], pnsa[L], pssd[L], plru[L], w_out[L],
                        pwg[L], pwp[L], rw, ewg[L], ewu[L], ewd[L], ccst[L], x2T if last else xcur, xcur_r, last, banks, s4, xTr=xin_r)
                kb.barrier()
        kb.finish(())
        print("fused instructions", kb.ninst, "sems", kb.nsem)
    return nc


def _rot(items):
    r = Rot.__new__(Rot)
    r.t = list(items)
    r.i = 0
    return r


def fused_inputs(d, core):
    b, r = core // 4, core % 4
    g, par = r // 2, r % 2
    tok = slice(core * TOK, (core + 1) * TOK)
    x = d["x"].reshape(T_ALL, D)
    im = {"xT0": np.ascontiguousarray(x[tok].T),
          "pT": np.ascontiguousarray(np.stack([d["p"][L].reshape(T_ALL, 256)[tok].T for L in range(2)])),
          "gidx": make_gidx(r), "w_in": d["w_in"],
          "npeT": np.ascontiguousarray(np.stack([np.stack([d["nsa_pe_k"][L].T, d["nsa_pe_v"][L].T]) for L in range(2)])),
          "nw1": np.stack([np.stack([d["nsa_w1_k"][L], d["nsa_w1_v"][L]]) for L in range(2)]),
          "nw2": np.stack([np.stack([d["nsa_w2_k"][L], d["nsa_w2_v"][L]]) for L in range(2)]),
          "proj_nsa": d["proj_nsa"], "proj_ssd": d["proj_ssd"], "proj_lru": d["proj_lru"], "w_out": d["w_out"],
          "ple_w_gate": d["ple_w_gate"], "ple_w_proj": d["ple_w_proj"], "router_w": d["router_w"],
          "exp_w_gate": d["exp_w_gate"], "exp_w_up": d["exp_w_up"], "exp_w_down": d["exp_w_down"],
          "ccst": np.stack([c_consts(d, L) for L in range(2)]), "scst": ssd_consts()}
    im.update(nsa_consts(par))
    sv, sr, lv, la, lx_ = [], [], [], [], []
    for L in range(2):
        a_, b_ = ssd_vecs(d, L, r)
        sv.append(a_); sr.append(b_)
        a_, b_, c_ = lru_vecs(d, L, r)
        lv.append(a_); la.append(b_); lx_.append(c_)
    im["svec"] = np.stack(sv); im["srep"] = np.stack(sr); im["lvec"] = np.stack(lv); im["lwab"] = np.stack(la); im["lwxb"] = np.stack(lx_)
    return im


def ssd_vecs(d, layer, r):
    g = r // 2
    cw = d["ssd_conv_w"][layer]; cb = d["ssd_conv_b"][layer]
    v = np.zeros((128, 16), np.float32)
    xs_ = slice(128 * r, 128 * r + 128); Bs_ = slice(512 + 64 * g, 512 + 64 * g + 64); Cs_ = slice(640 + 64 * g, 640 + 64 * g + 64)
    v[:, 0:4] = cw[:, xs_].T; v[:, 4] = cb[xs_]
    v[:64, 5:9] = cw[:, Bs_].T; v[:64, 9] = cb[Bs_]
    v[:64, 10:14] = cw[:, Cs_].T; v[:64, 14] = cb[Cs_]
    rp = np.zeros((128, 8), np.float32)
    hh = slice(2 * r, 2 * r + 2)
    rp[:, 0:2] = d["ssd_dt_bias"][layer][hh][None, :]
    rp[:, 2:4] = d["ssd_a_log"][layer][hh][None, :]
    rp[:, 4:6] = d["ssd_d"][layer][hh][None, :]
    return v, rp


def lru_vecs(d, layer, r):
    ch = slice(128 * r, 128 * r + 128)
    v = np.zeros((128, 8), np.float32)
    v[:, 0:4] = d["lru_conv_w"][layer][:, ch].T
    v[:, 4] = d["lru_conv_b"][layer][ch]
    v[:, 5] = d["lru_ba"][layer][ch]
    v[:, 6] = d["lru_bx"][layer][ch]
    v[:, 7] = d["lru_lambda"][layer][ch]
    wab_ = np.zeros((128, 128), np.float32)
    wxb_ = np.zeros((128, 128), np.float32)
    for k in range(2):
        wab_[64 * k:64 * k + 64, 64 * k:64 * k + 64] = d["lru_wa"][layer][2 * r + k]
        wxb_[64 * k:64 * k + 64, 64 * k:64 * k + 64] = d["lru_wx"][layer][2 * r + k]
    return v, wab_, wxb_


def kernel(**inputs):
    d = {k: np.asarray(v) for k, v in inputs.items()}
    nc = build_fused()
    in_maps = [fused_inputs(d, c) for c in range(NCORES)]
    res = run_bass_kernel_spmd(nc, in_maps, core_ids=list(range(NCORES))).results
    x = np.concatenate([res[c]["x2T"].T for c in range(NCORES)], axis=0)
    return np.ascontiguousarray(x.reshape(BATCH, S, D).astype(np.float32))
```

```python
import numpy as np
import contextlib
import concourse.bass as bass
import concourse.mybir as mybir
from concourse.bass_utils import run_bass_kernel_spmd

F32 = mybir.dt.float32
BF16 = mybir.dt.bfloat16
AF = mybir.ActivationFunctionType
ALU = mybir.AluOpType
AX = mybir.AxisListType

SAME_ENGINE_SYNC = True
SEM_ROT = 20000
NDMA = 6


class Res:
    __slots__ = ("w", "r", "name", "excl")

    def __init__(self, name="", excl=False):
        self.name = name
        self.w = None
        self.r = {}
        self.excl = excl


class MultiRes:
    def __init__(self, items):
        self.items = list(items)


def _flat(rs):
    out = []
    for r in rs:
        if isinstance(r, MultiRes):
            out.extend(r.items)
        else:
            out.append(r)
    return out


class KB:
    def __init__(self, nc, es):
        self.nc = nc
        self.es = es
        self.eng = dict(pe=nc.tensor, act=nc.scalar, dve=nc.vector, pool=nc.gpsimd, sp=nc.sync)
        self.sems = {}
        self.cnt = {}
        self.cur = {}
        self.seen = {e: {} for e in self.eng}
        self.nsem = 0
        self.ninst = 0
        for e in self.eng:
            self.cur[e] = self._new_sem("e_" + e)
        self.dma_pool = {q: [self._new_sem("d_%s%d" % (q, i)) for i in range(NDMA)] for q in ("sp", "pool", "act")}
        self.dma_idx = {q: 0 for q in self.dma_pool}
        self.uid = 0
        self.out_marks = []

    def _new_sem(self, name):
        self.nsem += 1
        key = "%s_%d" % (name, self.nsem)
        self.sems[key] = self.es.enter_context(self.nc.semaphore(key))
        self.cnt[key] = 0
        return key

    def sb(self, shape, dtype, name=None, es=None):
        self.uid += 1
        return (es or self.es).enter_context(self.nc.sbuf_tensor("%s_%d" % (name or "sb", self.uid), list(shape), dtype))

    def ps(self, shape, dtype=F32, name=None):
        self.uid += 1
        return self.es.enter_context(self.nc.psum_tensor("%s_%d" % (name or "ps", self.uid), list(shape), dtype))

    def dram(self, name, shape, dtype, kind):
        return self.nc.dram_tensor(name, list(shape), dtype, kind=kind).ap()

    def _deps(self, reads, writes):
        reads = _flat(reads)
        writes = _flat(writes)
        deps = {}
        for r in reads:
            if r.w is not None:
                k, v = r.w
                if deps.get(k, 0) < v:
                    deps[k] = v
            if r.excl:
                for k, v in r.r.items():
                    if deps.get(k, 0) < v:
                        deps[k] = v
        for w in writes:
            if w.w is not None:
                k, v = w.w
                if deps.get(k, 0) < v:
                    deps[k] = v
            for k, v in w.r.items():
                if deps.get(k, 0) < v:
                    deps[k] = v
        return deps

    def _wait(self, e, deps, skip_key=None):
        seen = self.seen[e]
        for k, v in deps.items():
            if k == skip_key:
                continue
            if seen.get(k, 0) < v:
                self.eng[e].wait_ge(self.sems[k], v)
                seen[k] = v
                self.ninst += 1

    def _mark(self, key, v, reads, writes):
        reads = _flat(reads)
        writes = _flat(writes)
        for r in reads:
            r.r[key] = v
        for w in writes:
            w.w = (key, v)
            w.r = {}

    def op(self, e, fn, reads=(), writes=()):
        deps = self._deps(reads, writes)
        key = self.cur[e]
        skip = key if (e == "pe" or not SAME_ENGINE_SYNC) else None
        self._wait(e, deps, skip)
        inst = fn(self.eng[e])
        self.cnt[key] += 1
        v = self.cnt[key]
        inst.then_inc(self.sems[key], 1)
        self.ninst += 1
        self._mark(key, v, reads, writes)
        if v >= SEM_ROT:
            self.cur[e] = self._new_sem("e_" + e)
        return inst

    def dma(self, q, out, in_, reads=(), writes=(), final=False, **kw):
        pool = self.dma_pool[q]
        key = pool[self.dma_idx[q] % len(pool)]
        self.dma_idx[q] += 1
        deps = self._deps(reads, writes)
        if self.cnt[key] > 0:
            deps[key] = max(deps.get(key, 0), self.cnt[key])
        self._wait(q, deps)
        inst = self.eng[q].dma_start(out=out, in_=in_, **kw)
        self.cnt[key] += 16
        v = self.cnt[key]
        inst.then_inc(self.sems[key], 16)
        self.ninst += 1
        self._mark(key, v, reads, writes)
        if final:
            self.out_marks.append((key, v))
        if v >= SEM_ROT:
            i = pool.index(key)
            pool[i] = self._new_sem("d_" + q)
        return inst

    def barrier(self):
        cck = getattr(self, "cc_key", None)
        allc = {k: v for k, v in self.cnt.items() if v > 0 and k != cck}
        for e in self.eng:
            self._wait(e, dict(allc))

    def finish(self, out_res):
        deps = self._deps(out_res, ())
        for k, v in self.out_marks:
            if deps.get(k, 0) < v:
                deps[k] = v
        self._wait("sp", deps)


def _kb_gather(self, out, in2d, idx_ap, reads=(), writes=()):
    pool = self.dma_pool["pool"]
    key = pool[self.dma_idx["pool"] % len(pool)]
    self.dma_idx["pool"] += 1
    deps = self._deps(reads, writes)
    if self.cnt[key] > 0:
        deps[key] = max(deps.get(key, 0), self.cnt[key])
    self._wait("pool", deps)
    inst = self.nc.gpsimd.indirect_dma_start(out=out, out_offset=None, in_=in2d,
                                             in_offset=bass.IndirectOffsetOnAxis(ap=idx_ap, axis=0))
    self.cnt[key] += 16
    v = self.cnt[key]
    inst.then_inc(self.sems[key], 16)
    self.ninst += 1
    self._mark(key, v, reads, writes)
    if v >= SEM_ROT:
        pool[pool.index(key)] = self._new_sem("d_pool")
    return inst


def _kb_allgather(self, in_ap, out_ap, groups, writes=()):
    if not hasattr(self, "cc_key"):
        self.cc_key = self._new_sem("cc")
    deps = self._deps((), writes)
    self._wait("pool", deps)
    inst = self.nc.gpsimd.collective_compute("AllGather", ALU.bypass, replica_groups=groups, ins=[in_ap], outs=[out_ap])
    key = self.cc_key
    self.cnt[key] += 1
    v = self.cnt[key]
    inst.then_inc(self.sems[key], 1)
    self.ninst += 1
    self._mark(key, v, (), writes)
    return inst


KB.gather = _kb_gather
KB.allgather = _kb_allgather


S = 8192


class Rot:
    def __init__(self, kb, n, shape, dtype, name, psum=False, es=None):
        if psum:
            self.t = [(kb.ps(shape, dtype, name), Res(name, excl=True)) for _ in range(n)]
        else:
            self.t = [(kb.sb(shape, dtype, name, es=es), Res(name)) for _ in range(n)]
        self.i = 0

    def next(self):
        x = self.t[self.i % len(self.t)]
        self.i += 1
        return x


LEVEL = 9

S = 8192


def emit_conv(kb, eng, out, xin, vt, vr, c0, N, P, xr, outr):
    kb.op(eng, lambda e: e.tensor_scalar(out=out[:P, :], in0=xin[:P, 0:N], scalar1=vt[:P, c0:c0 + 1], scalar2=vt[:P, c0 + 4:c0 + 5],
                                         op0=ALU.mult, op1=ALU.add), reads=[xr, vr], writes=[outr])
    for k in range(1, 4):
        kb.op("dve", lambda e, k=k: e.scalar_tensor_tensor(out=out[:P, :], in0=xin[:P, k:k + N], scalar=vt[:P, c0 + k:c0 + k + 1], in1=out[:P, :],
                                                          op0=ALU.mult, op1=ALU.add), reads=[xr, vr, outr], writes=[outr])


def ssd_consts():
    k = np.arange(128)
    cst = np.zeros((128, 4, 128), np.float32)
    cst[:, 0, :] = (k[:, None] <= k[None, :])
    cst[:, 1, :] = (k[:, None] > k[None, :])
    cst[:, 2, :] = np.eye(128)
    cst[:, 3, :] = 1.0
    return cst


DBG = ''

S = 8192
QT = 512
NCMP = 511
BIGM = 1024.0
f32 = np.float32


def nsa_consts(par=0):
    kk = np.arange(128)
    tq = np.arange(512)
    c = {}
    zeros = np.zeros((128, 512), f32); onesm = np.ones((128, 512), f32)

    def cm_full(dl):
        if dl < 0:
            return zeros
        if dl >= 5:
            return onesm
        return ((16 * kk[:, None] + 31 - tq[None, :]) <= 512 * dl).astype(f32)

    def cneg_full(dk):
        if dk < 0:
            return zeros
        if dk > 3:
            return -onesm
        return np.where(128 * dk + kk[:, None] <= tq[None, :], 0.0, -1.0).astype(f32)

    def wm_full(dk):
        if dk < -4 or dk > 3:
            return zeros
        diff = tq[None, :] - (128 * dk + kk[:, None])
        return ((diff >= 0) & (diff < 512)).astype(f32)

    cm = [cm_full(dl + par) for dl in range(-1, 5)]
    cneg = [cneg_full(dk - 4 * par) for dk in range(0, 8)]
    wm = [wm_full(dk - 4 * par) for dk in range(-4, 8)]
    c["nmask"] = np.ascontiguousarray(np.stack(cm + cneg + wm, axis=1))
    key = np.arange(S)
    c["nE0"] = (kk[:, None] == (key[None, :] // 64)).astype(f32)
    n = np.arange(512)
    ov = ((n[:, None] // 4) == kk[None, :]).astype(f32) + (((n[:, None] + 1) // 4) == kk[None, :]).astype(f32)
    ov[511] = 0
    ovl = ov.reshape(4, 128, 128).transpose(1, 0, 2)
    mm = np.arange(256) - 8 * par
    hi = (kk >= 64).astype(np.int64)
    C0 = (mm[None, :] <= 128 + hi[:, None]).astype(f32)
    Cm1 = C0 - 1.0
    F = np.where((mm[None, :] == 128 + hi[:, None]) | (mm[None, :] == 127 + hi[:, None]), 1e4, -1e30).astype(f32)
    ident = np.eye(128, dtype=f32)
    ones = np.ones((128, 128), f32)
    sel64 = np.zeros((128, 64), f32); sel64[64] = 1.0
    gsel = np.zeros((128, 12, 64), f32)
    for r in range(12):
        gsel[r, r, :] = 1.0
    c["nmisc"] = np.ascontiguousarray(np.concatenate(
        [ovl.reshape(128, 512), C0, Cm1, F, ident, ones, sel64, gsel.reshape(128, 768)], axis=1))
    return c


NMASK = 26
MISC_W = 512 + 768 + 128 + 128 + 64 + 768


D = 1024
TOK = 2048


CDBG = ''

D = 1024
TOK = 2048
NT = TOK // 512
ALPHA = 4 ** 0.25
OFF_Z = 512 + 768 + 24
OFF_MERGE = 6688 - 3072
f32 = np.float32
CW = 36 + 16 + 128 + 128 + 128 + 2048


def c_consts(d, layer):
    v = np.zeros((128, CW), f32)
    col = lambda a: a.reshape(-1, 128).T
    v[:, 0:8] = col(d["ln1_g"][layer]); v[:, 8:16] = col(d["ln1_b"][layer])
    v[:, 16:24] = col(d["ln2_g"][layer]); v[:, 24:32] = col(d["ln2_b"][layer])
    v[:, 32:36] = col(d["ssd_norm_w"][layer])
    v[:, 36:52] = d["router_b"][None, :]
    v[:, 52:180] = 1.0 / 1024
    v[:, 180:308] = 1.0 / 512
    v[:, 308:436] = np.eye(128)
    sel = np.zeros((128, 16, 128), f32)
    for e in range(16):
        sel[e, e, :] = 1.0
    v[:, 436:436 + 2048] = sel.reshape(128, 2048)
    return v


def emit_nsa2(kb, tiles, agF2k, agF512, agFr, gx, gxr, IX, peT, w1, w2, nmask, nE0, nmisc, onsaT, onsar, acc, pmb, gen, es):
    sb = lambda shape, dt, name: kb.sb(shape, dt, name, es=es)
    mk = sb([128, NMASK, 512], BF16, "nmask"); mkr = Res()
    kb.dma("pool", mk[:], nmask[:, :, :], writes=[mkr])
    E0 = sb([128, S], BF16, "nE0"); E0r = Res()
    kb.dma("pool", E0[:], nE0[:, :], writes=[E0r])
    mf = sb([128, MISC_W], F32, "nmiscf"); mfr = Res()
    kb.dma("sp", mf[:], nmisc[:, :], writes=[mfr])
    o = 0
    ovl_f = mf[:, 0:512]; o = 512
    C0 = mf[:, o:o + 256]; Cm1 = mf[:, o + 256:o + 512]; F4 = mf[:, o + 512:o + 768]; o += 768
    ident = mf[:, o:o + 128]; o += 128
    ones_f = mf[:, o:o + 128]; o += 128
    sel64 = mf[:, o:o + 64]; o += 64
    gsel = mf[:, o:o + 768]
    cb_ = sb([128, 512 + 128 + 128], BF16, "ncb"); cbr = Res()
    kb.op("dve", lambda e: e.tensor_copy(out=cb_[:, 0:512], in_=ovl_f), reads=[mfr], writes=[cbr])
    kb.op("dve", lambda e: e.tensor_copy(out=cb_[:, 512:640], in_=ident), reads=[mfr], writes=[cbr])
    kb.op("dve", lambda e: e.tensor_copy(out=cb_[:, 640:768], in_=ones_f), reads=[mfr], writes=[cbr])
    ovl = cb_[:, 0:512]; identb = cb_[:, 512:640]; onesb = cb_[:, 640:768]

    kall = sb([128, 2, S], BF16, "nk"); kr = [None, None, Res(), Res()]
    kb.op("pool", lambda e: e.memset(kall[64:128, :, :], 0.0), writes=[kr[2], kr[3]])
    for i, nm in ((2, "ksel"), (3, "kwin")):
        for rp in range(4):
            kb.gather(kall[0:64, i - 2, rp * 2048:(rp + 1) * 2048], agF2k, gx[0:64, IX[nm + str(rp)]:IX[nm + str(rp)] + 1], reads=[gxr, agFr], writes=[kr[i]])
    va = sb([128, 2, 64, 128], BF16, "nva"); var = [Res() for _ in range(2)]
    for i in range(2):
        kb.op("pool", lambda e, i=i: e.memset(va[:, i, :, 64:128], 0.0), writes=[var[i]])
        kb.op("pool", lambda e, i=i: e.memset(va[:, i, :, 64:65], 1.0), writes=[var[i]])

    kcT = sb([64, 512], BF16, "nkcT"); kcr = Res()
    vct = sb([128, 4, 65], BF16, "nvct"); vcr = Res()
    kb.op("pool", lambda e: e.memset(kcT[:], 0.0), writes=[kcr])
    kb.op("pool", lambda e: e.memset(vct[:], 0.0), writes=[vcr])
    kb.op("pool", lambda e: e.memset(vct[:, :, 64:65], 1.0), writes=[vcr])
    with contextlib.ExitStack() as esv:
        vT = kb.sb([64, 2, S], BF16, "nvT", es=esv); vTr = [Res(), Res()]
        for i, nm in ((0, "vsel"), (1, "vwin")):
            for rp in range(4):
                kb.gather(vT[:, i, rp * 2048:(rp + 1) * 2048], agF2k, gx[0:64, IX[nm + str(rp)]:IX[nm + str(rp)] + 1], reads=[gxr, agFr], writes=[vTr[i]])
            for k8 in range(8):
                pv8, pv8r = gen.next()
                pvb = pv8[:, 0:256].bitcast(BF16)
                for kk in range(8):
                    kt = k8 * 8 + kk
                    kb.op("pe", lambda e, kt=kt, kk=kk, i=i: e.transpose(pvb[:, kk * 64:(kk + 1) * 64], vT[:, i, kt * 128:(kt + 1) * 128], identb[0:64, 0:64]),
                          reads=[vTr[i], cbr], writes=[pv8r])
                kb.op("act", lambda e, k8=k8, i=i: e.copy(out=va[:, i, k8 * 8:(k8 + 1) * 8, 0:64], in_=pvb.rearrange("p (k d) -> p k d", k=8)), reads=[pv8r], writes=[var[i]])
        kb.barrier()
    with contextlib.ExitStack() as es2:
        sb2 = lambda shape, dt, name: kb.sb(shape, dt, name, es=es2)
        kcv = sb2([64, 2, S], BF16, "nkcv"); kr[0] = Res(); kr[1] = Res()
        for i, nm in ((0, "kcmp"), (1, "vcmp")):
            for rp in range(4):
                kb.gather(kcv[:, i, rp * 2048:(rp + 1) * 2048], agF2k, gx[0:64, IX[nm + str(rp)]:IX[nm + str(rp)] + 1], reads=[gxr, agFr], writes=[kr[i]])
        w1t = sb2([64, 32, 256], BF16, "nw1"); w1r = Res()
        w2t = sb2([128, 2, 64], BF16, "nw2"); w2r = Res()
        pet = sb2([64, 32], BF16, "npe"); per = Res()
        bias = sb2([128, 1], F32, "nbias"); biasr = Res()
        tmp3 = [(sb2([128, 512], F32, "nt"), Res(), sb2([128, 512], F32, "nu"), Res(), sb2([128, 512], BF16, "ngt"), Res()) for _ in range(2)]
        for which in range(2):
            for q4 in range(4):
                kb.dma("pool", w1t[:, q4 * 8:(q4 + 1) * 8, :], w1[which, q4 * 512:(q4 + 1) * 512, :].rearrange("(l d) j -> d l j", d=64), writes=[w1r])
            kb.dma("pool", w2t[:], w2[which, :, :].rearrange("(c p) d -> p c d", p=128), writes=[w2r])
            kb.dma("pool", pet[:], peT[which, :, :], writes=[per])
            src = kcv[:, which, :]
            gts = []
            for jc in range(2):
                ph, phr = gen.next()
                for l in range(32):
                    kb.op("pe", lambda e, l=l: e.matmul(ph[:, 0:NCMP], lhsT=w1t[:, l, jc * 128:(jc + 1) * 128],
                                                         rhs=src[:, l:l + 16 * (NCMP - 1) + 1:16], start=(l == 0), stop=(l == 31)),
                          reads=[w1r, kr[which]], writes=[phr])
                pbias, pbr = gen.next()
                for l in range(32):
                    kb.op("pe", lambda e, l=l: e.matmul(pbias[:, 0:1], lhsT=w1t[:, l, jc * 128:(jc + 1) * 128], rhs=pet[:, l:l + 1],
                                                         start=(l == 0), stop=(l == 31)), reads=[w1r, per], writes=[pbr])
                kb.op("dve", lambda e: e.tensor_copy(out=bias[:], in_=pbias[:, 0:1]), reads=[pbr], writes=[biasr])
                t, tr, u, ur, gt, gr = tmp3[jc]
                kb.op("act", lambda e: e.activation(out=t[:, 0:NCMP], in_=ph[:, 0:NCMP], func=AF.Identity, bias=bias[:, 0:1]), reads=[phr, biasr], writes=[tr])
                kb.op("dve", lambda e: e.tensor_tensor(out=u[:, 0:NCMP], in0=t[:, 0:NCMP], in1=t[:, 0:NCMP], op=ALU.mult), reads=[tr], writes=[ur])
                kb.op("dve", lambda e: e.tensor_scalar(out=u[:, 0:NCMP], in0=u[:, 0:NCMP], scalar1=0.044715, scalar2=1.0, op0=ALU.mult, op1=ALU.add), reads=[ur], writes=[ur])
                kb.op("dve", lambda e: e.tensor_tensor(out=u[:, 0:NCMP], in0=u[:, 0:NCMP], in1=t[:, 0:NCMP], op=ALU.mult), reads=[ur, tr], writes=[ur])
                kb.op("act", lambda e: e.activation(out=u[:, 0:NCMP], in_=u[:, 0:NCMP], func=AF.Sigmoid, scale=1.5957691216057308), reads=[ur], writes=[ur])
                kb.op("pool", lambda e: e.memset(gt[:, NCMP:512], 0.0), writes=[gr])
                kb.op("dve", lambda e: e.tensor_tensor(out=gt[:, 0:NCMP], in0=u[:, 0:NCMP], in1=t[:, 0:NCMP], op=ALU.mult), reads=[ur, tr], writes=[gr])
                gts.append((gt, gr))
            if which == 0:
                pk, pkr = gen.next()
                for jc in range(2):
                    kb.op("pe", lambda e, jc=jc: e.matmul(pk[0:64, 0:NCMP], lhsT=w2t[:, jc, :], rhs=gts[jc][0][:, 0:NCMP], start=(jc == 0), stop=(jc == 1)),
                          reads=[w2r, gts[jc][1]], writes=[pkr])
                kb.op("act", lambda e: e.copy(out=kcT[:, 0:NCMP], in_=pk[0:64, 0:NCMP]), reads=[pkr], writes=[kcr])
            else:
                for m in range(4):
                    pv, pvr = gen.next()
                    for jc in range(2):
                        kb.op("pe", lambda e, jc=jc, m=m: e.matmul(pv[:, 0:64], lhsT=gts[jc][0][:, m * 128:(m + 1) * 128], rhs=w2t[:, jc, :], start=(jc == 0), stop=(jc == 1)),
                              reads=[w2r, gts[jc][1]], writes=[pvr])
                    kb.op("act", lambda e, m=m: e.copy(out=vct[:, m, 0:64], in_=pv[:, 0:64]), reads=[pvr], writes=[vcr])
        kb.barrier()
    kmax = sb([128, 1], F32, "nkmax"); kmr = Res()
    kb.op("pool", lambda e: e.memset(kmax[:], 0.0), writes=[kmr])
    sq = Rot(kb, 2, [64, 512], BF16, "nsq", es=es)
    red = Rot(kb, 2, [128, 1], F32, "nred", es=es)

    def norm_max(src_ap, n, src_res, dst, dstr):
        s_, sr_ = sq.next()
        kb.op("pool", lambda e: e.tensor_tensor(out=s_[:, 0:n], in0=src_ap, in1=src_ap, op=ALU.mult), reads=src_res, writes=[sr_])
        pn, pnr = gen.next()
        kb.op("pe", lambda e: e.matmul(pn[:, 0:n], lhsT=onesb[0:64, :], rhs=s_[:, 0:n], start=True, stop=True), reads=[cbr, sr_], writes=[pnr])
        r_, rr_ = red.next()
        kb.op("dve", lambda e: e.reduce_max(out=r_[:], in_=pn[:, 0:n], axis=AX.X), reads=[pnr], writes=[rr_])
        kb.op("dve", lambda e: e.tensor_tensor(out=dst[:], in0=dst[:], in1=r_[:], op=ALU.max), reads=[rr_, dstr], writes=[dstr])

    norm_max(kcT[:, 0:512], 512, [kcr], kmax, kmr)
    for which in (2, 3):
        for tt in range(S // 512):
            norm_max(kall[0:64, which - 2, tt * 512:(tt + 1) * 512], 512, [kr[which]], kmax, kmr)

    qb = Rot(kb, 2, [128, 4, 512], BF16, "nq", es=es)
    for q_, qr_ in qb.t:
        kb.op("pool", lambda e, q_=q_: e.memset(q_[64:128, :, :], 0.0), writes=[qr_])
    gtl = Rot(kb, 2, [12, 512], F32, "ngate", es=es)
    gtbl = Rot(kb, 2, [12, 512], BF16, "ngateb", es=es)
    ebuf = Rot(kb, 4, [128, 512], BF16, "ne", es=es)
    pbuf = Rot(kb, 4, [128, 512], BF16, "np", es=es)
    mskb = Rot(kb, 2, [128, 512], BF16, "nmsk", es=es)
    pnbuf = Rot(kb, 4, [128, 512], BF16, "npn", es=es)
    rinv = Rot(kb, 2, [128, 512], F32, "nrinv", es=es)
    ocmp = Rot(kb, 1, [64, 4, 512], F32, "nocmp", es=es)
    impT = Rot(kb, 2, [128, 512], F32, "nimpT", es=es)
    selT = Rot(kb, 2, [128, 512], BF16, "nselT", es=es)
    v1b = Rot(kb, 2, [128, 128], F32, "nv1", es=es)
    v2b = Rot(kb, 2, [128, 128], F32, "nv2", es=es)
    m8a = Rot(kb, 2, [128, 8], F32, "nm8a", es=es)
    m8b = Rot(kb, 2, [128, 8], F32, "nm8b", es=es)
    smk = Rot(kb, 2, [128, 128], F32, "nsmk", es=es)
    accs = Rot(kb, 1, [65, 8, 512], F32, "naccs", es=es)
    rec = Rot(kb, 2, [64, 512], F32, "nrec", es=es)
    osb = Rot(kb, 2, [64, 512], F32, "nosb", es=es)
    negc = Rot(kb, 2, [128, 4], F32, "nnegc", es=es)
    qm = Rot(kb, 2, [128, 1], F32, "nqm", es=es)

    for ti, i in enumerate(tiles):
        t0 = i * QT
        q, qr = qb.next()
        for j in range(4):
            cq = IX["q%d_%d" % (ti, j)]
            kb.gather(q[0:64, j, :], agF512, gx[0:64, cq:cq + 1], reads=[gxr, agFr], writes=[qr])
        gtb_, gtbr = gtbl.next()
        cg = IX["gate%d" % ti]
        kb.gather(gtb_[:], agF512, gx[0:12, cg:cg + 1], reads=[gxr, agFr], writes=[gtbr])
        gt_, gtr = gtl.next()
        kb.op("act", lambda e: e.activation(out=gt_[:], in_=gtb_[:], func=AF.Sigmoid), reads=[gtbr], writes=[gtr])
        nc_, ncr = negc.next()
        for j in range(4):
            qm_, qmr = qm.next()
            kb.op("pool", lambda e: e.memset(qm_[:], 0.0), writes=[qmr])
            norm_max(q[0:64, j, :], 512, [qr], qm_, qmr)
            kb.op("dve", lambda e, j=j: e.tensor_tensor(out=nc_[:, j:j + 1], in0=qm_[:], in1=kmax[:], op=ALU.mult), reads=[qmr, kmr], writes=[ncr])
        kb.op("act", lambda e: e.activation(out=nc_[:], in_=nc_[:], func=AF.Sqrt, scale=1.05), reads=[ncr], writes=[ncr])
        kb.op("dve", lambda e: e.tensor_scalar(out=nc_[:], in0=nc_[:], scalar1=-0.125, scalar2=None, op0=ALU.mult), reads=[ncr], writes=[ncr])

        nch = min(4, (32 * (i + 1) + 31 + 127) // 128)
        oc, ocr = ocmp.next()
        pimp, pimpr = acc[0]
        for j in range(4):
            es_ = []
            psum_, psumr = acc[1]
            for m in range(nch):
                ps_, psr = gen.next()
                kb.op("pe", lambda e, m=m, j=j: e.matmul(ps_[:, :], lhsT=kcT[:, m * 128:(m + 1) * 128], rhs=q[0:64, j, :], start=True, stop=True),
                      reads=[kcr, qr], writes=[psr])
                e_, er = ebuf.next()
                kb.op("act", lambda e, j=j: e.activation(out=e_[:], in_=ps_[:, :], func=AF.Exp, scale=0.125, bias=nc_[:, j:j + 1]), reads=[psr, ncr], writes=[er])
                dl = i - 4 * m
                if dl <= 4:
                    kb.op("pool", lambda e, dl=dl: e.tensor_tensor(out=e_[:], in0=e_[:], in1=mk[:, dl + 1, :], op=ALU.mult), reads=[er, mkr], writes=[er])
                kb.op("pe", lambda e, m=m: e.matmul(psum_[:, :], lhsT=onesb, rhs=e_[:], start=(m == 0), stop=(m == nch - 1)), reads=[cbr, er], writes=[psumr])
                es_.append((e_, er))
            ri, rir = rinv.next()
            kb.op("dve", lambda e: e.tensor_scalar(out=ri[:], in0=psum_[:, :], scalar1=1e-30, scalar2=None, op0=ALU.max), reads=[psumr], writes=[rir])
            kb.op("dve", lambda e: e.reciprocal(out=ri[:], in_=ri[:]), reads=[rir], writes=[rir])
            po, por = acc[2]
            for m in range(nch):
                e_, er = es_[m]
                pn_, pnr_ = pnbuf.next()
                kb.op("dve", lambda e: e.tensor_tensor(out=pn_[:], in0=e_[:], in1=ri[:], op=ALU.mult), reads=[er, rir], writes=[pnr_])
                kb.op("pe", lambda e, m=m: e.matmul(po[0:64, :], lhsT=vct[:, m, 0:64], rhs=pn_[:], start=(m == 0), stop=(m == nch - 1)), reads=[vcr, pnr_], writes=[por])
                kb.op("pe", lambda e, m=m, j=j: e.matmul(pimp[:, :], lhsT=ovl[:, m * 128:(m + 1) * 128], rhs=pn_[:], start=(j == 0 and m == 0), stop=(j == 3 and m == nch - 1)),
                      reads=[cbr, pnr_], writes=[pimpr])
            kb.op("act", lambda e, j=j: e.copy(out=oc[:, j, :], in_=po[0:64, :]), reads=[por], writes=[ocr])
        it_, itr = impT.next()
        kb.op("act", lambda e: e.copy(out=it_[:], in_=pimp[:, :]), reads=[pimpr], writes=[itr])
        st_, str_ = selT.next()
        for k4 in range(4):
            ksub = 4 * i + k4
            ptr_, ptrr = gen.next()
            kb.op("pe", lambda e, k4=k4: e.transpose(ptr_[:, 0:128], it_[:, k4 * 128:(k4 + 1) * 128], ident), reads=[itr, mfr], writes=[ptrr])
            co = 128 - 2 * ksub
            v1, v1r = v1b.next()
            kb.op("dve", lambda e, co=co: e.tensor_tensor(out=v1[:], in0=ptr_[:, 0:128], in1=C0[:, co:co + 128], op=ALU.mult), reads=[ptrr, mfr], writes=[v1r])
            kb.op("dve", lambda e, co=co: e.tensor_tensor(out=v1[:], in0=v1[:], in1=Cm1[:, co:co + 128], op=ALU.add), reads=[v1r, mfr], writes=[v1r])
            kb.op("dve", lambda e, co=co: e.tensor_tensor(out=v1[:], in0=v1[:], in1=F4[:, co:co + 128], op=ALU.max), reads=[v1r, mfr], writes=[v1r])
            kb.op("dve", lambda e: e.memset(v1[:, 0:1], 1e4), reads=[], writes=[v1r])
            a8, a8r = m8a.next()
            kb.op("dve", lambda e: e.max(out=a8[:], in_=v1[:]), reads=[v1r], writes=[a8r])
            v2, v2r = v2b.next()
            kb.op("dve", lambda e: e.match_replace(out=v2[:], in_to_replace=a8[:], in_values=v1[:], imm_value=-1e30), reads=[a8r, v1r], writes=[v2r])
            b8, b8r = m8b.next()
            kb.op("dve", lambda e: e.max(out=b8[:], in_=v2[:]), reads=[v2r], writes=[b8r])
            sm, smr = smk.next()
            kb.op("dve", lambda e: e.tensor_scalar(out=sm[:], in0=v1[:], scalar1=b8[:, 7:8], scalar2=None, op0=ALU.is_ge), reads=[v1r, b8r], writes=[smr])
            if DBG == "sm" and k4 == 0 and ti == 0:
                kb.dma("sp", onsaT[0:128, 0:128], sm[:], reads=[smr], final=True)
                kb.dma("sp", onsaT[128:256, 0:128], v1[:], reads=[v1r], final=True)
                kb.dma("sp", onsaT[0:128, 128:136], a8[:], reads=[a8r], final=True)
                kb.dma("sp", onsaT[0:128, 136:144], b8[:], reads=[b8r], final=True)
                kb.dma("sp", onsaT[128:256, 128:256], v2[:], reads=[v2r], final=True)
            pt2, pt2r = gen.next()
            kb.op("pe", lambda e: e.transpose(pt2[:, 0:128], sm[:], ident), reads=[smr, mfr], writes=[pt2r])
            kb.op("act", lambda e, k4=k4: e.copy(out=st_[:, k4 * 128:(k4 + 1) * 128], in_=pt2[:, 0:128]), reads=[pt2r], writes=[str_])

        as_, asr = accs.next()
        LA = 2
        for br in range(2):
            if br == 0:
                kts = list(range(0, min(64, 4 * i + 8)))
            else:
                kts = [kt for kt in range(4 * i - 4, min(64, 4 * i + 8)) if kt >= 0]
            ksrc = 2 + br
            pairs = [(n_, kt, j) for n_, kt in enumerate(kts) for j in range(4)]
            pend = []
            msk = None
            for idx in range(len(pairs) + LA):
                if idx < len(pairs):
                    n_, kt, j = pairs[idx]
                    dk = kt - 4 * i
                    if br == 0 and j == 0:
                        pm, pmr = pmb
                        kb.op("pe", lambda e, kt=kt, dk=dk: e.matmul(pm[:, :], lhsT=E0[:, kt * 128:(kt + 1) * 128], rhs=st_[:], start=True, stop=(dk < 0)),
                              reads=[E0r, str_], writes=[pmr])
                        if dk >= 0:
                            kb.op("pe", lambda e, dk=dk: e.matmul(pm[:, :], lhsT=identb, rhs=mk[:, 6 + dk, :], start=False, stop=True), reads=[cbr, mkr], writes=[pmr])
                        msk = mskb.next()
                        kb.op("act", lambda e, msk=msk: e.activation(out=msk[0][:], in_=pm[:, :], func=AF.Relu), reads=[pmr], writes=[msk[1]])
                    ps_, psr = gen.next()
                    kb.op("pe", lambda e, kt=kt, j=j, ps_=ps_: e.matmul(ps_[:, :], lhsT=kall[:, br, kt * 128:(kt + 1) * 128], rhs=q[:, j, :], start=True, stop=True),
                          reads=[kr[ksrc], qr], writes=[psr])
                    e_, er = ebuf.next()
                    kb.op("act", lambda e, j=j, e_=e_, ps_=ps_: e.activation(out=e_[:], in_=ps_[:, :], func=AF.Exp, scale=0.125, bias=nc_[:, j:j + 1]), reads=[psr, ncr], writes=[er])
                    p_, pr_ = pbuf.next()
                    eng = "dve"
                    if br == 0:
                        kb.op(eng, lambda e, p_=p_, e_=e_, msk=msk: e.tensor_tensor(out=p_[:], in0=e_[:], in1=msk[0][:], op=ALU.mult), reads=[er, msk[1]], writes=[pr_])
                    else:
                        kb.op(eng, lambda e, dk=dk, p_=p_, e_=e_: e.tensor_tensor(out=p_[:], in0=e_[:], in1=mk[:, 14 + dk + 4, :], op=ALU.mult), reads=[er, mkr], writes=[pr_])
                    pend.append((p_, pr_, n_, kt, j))
                if idx >= LA:
                    p_, pr_, n_, kt, j = pend[idx - LA]
                    pa, par = acc[j]
                    kb.op("pe", lambda e, kt=kt, n_=n_, p_=p_, pa=pa: e.matmul(pa[:, :], lhsT=va[:, br, kt, :], rhs=p_[:], start=(n_ == 0), stop=(n_ == len(kts) - 1)),
                          reads=[var[br], pr_], writes=[par])
            for j in range(4):
                pa, par = acc[j]
                kb.op("act", lambda e, j=j, br=br: e.copy(out=as_[:, br * 4 + j, :], in_=pa[0:65, :]), reads=[par], writes=[asr])
        for j in range(4 if DBG not in ("sm", "pm") else 0):
            o_, or_ = osb.next()
            pg, pgr = gen.next()
            kb.op("pe", lambda e, j=j: e.matmul(pg[0:64, :], lhsT=gsel[0:12, (j * 3) * 64:(j * 3 + 1) * 64], rhs=gt_[:], start=True, stop=True), reads=[mfr, gtr], writes=[pgr])
            if DBG == "cmp":
                kb.op("dve", lambda e, j=j: e.tensor_copy(out=o_[:], in_=oc[:, j, :]), reads=[pgr, ocr], writes=[or_])
            elif DBG:
                kb.op("dve", lambda e, j=j: e.memset(o_[:], 0.0), reads=[pgr, ocr], writes=[or_])
            else:
                kb.op("dve", lambda e, j=j: e.tensor_tensor(out=o_[:], in0=pg[0:64, :], in1=oc[:, j, :], op=ALU.mult), reads=[pgr, ocr], writes=[or_])
            for br in range(2):
                if DBG == "cmp" or (DBG == "sel" and br == 1) or (DBG == "win" and br == 0):
                    continue
                psm, psmr = gen.next()
                kb.op("pe", lambda e, j=j, br=br: e.matmul(psm[0:64, :], lhsT=sel64[0:65, :], rhs=as_[:, br * 4 + j, :], start=True, stop=True), reads=[mfr, asr], writes=[psmr])
                rc, rcr = rec.next()
                kb.op("dve", lambda e: e.tensor_scalar(out=rc[:], in0=psm[0:64, :], scalar1=1e-30, scalar2=None, op0=ALU.max), reads=[psmr], writes=[rcr])
                kb.op("dve", lambda e: e.reciprocal(out=rc[:], in_=rc[:]), reads=[rcr], writes=[rcr])
                pg2, pg2r = gen.next()
                kb.op("pe", lambda e, j=j, br=br: e.matmul(pg2[0:64, :], lhsT=gsel[0:12, (j * 3 + 1 + br) * 64:(j * 3 + 2 + br) * 64], rhs=gt_[:], start=True, stop=True),
                      reads=[mfr, gtr], writes=[pg2r])
                if not DBG:
                    kb.op("dve", lambda e: e.tensor_tensor(out=rc[:], in0=pg2[0:64, :], in1=rc[:], op=ALU.mult), reads=[pg2r, rcr], writes=[rcr])
                kb.op("pool", lambda e, j=j, br=br: e.tensor_tensor(out=rc[:], in0=rc[:], in1=as_[0:64, br * 4 + j, :], op=ALU.mult), reads=[rcr, asr], writes=[rcr])
                kb.op("pool", lambda e: e.tensor_tensor(out=o_[:], in0=o_[:], in1=rc[:], op=ALU.add), reads=[rcr, or_], writes=[or_])
            kb.dma("pool", onsaT[j * 64:(j + 1) * 64, ti * QT:(ti + 1) * QT], o_[:], reads=[or_, onsar])


def emit_ssd2(kb, agF2k, agFr, agD2k, agDr, gx, gxr, IX, svec, srep, cst, yT_dst, yT_r, pbanks, pbf, es=None):
    N = 1024
    NCH = S // 128
    ct = kb.sb([128, 4, 128], F32, "scst", es=es); cr = Res()
    kb.dma("sp", ct[:], cst[:, :, :], writes=[cr])
    tri = ct[:, 0, :]; U = ct[:, 1, :]; ident = ct[:, 2, :]; ones = ct[:, 3, :]
    identb = kb.sb([128, 128], BF16, "sidb", es=es); idbr = Res()
    kb.op("dve", lambda e: e.tensor_copy(out=identb[:], in_=ident), reads=[cr], writes=[idbr])
    vt = kb.sb([128, 16], F32, "svec", es=es); vr = Res()
    kb.dma("sp", vt[:], svec[:, :], writes=[vr])
    rp = kb.sb([128, 8], F32, "srep", es=es); rpr = Res()
    kb.dma("sp", rp[:], srep[:, :], writes=[rpr])
    if LEVEL == -1: return
    dt = kb.sb([128, NCH, 2], F32, "sdt", es=es); dtr = Res()
    dtT = kb.sb([2, S], F32, "sdtT", es=es); dtTr = Res()
    for rq in range(4):
        cd = IX["dt%d" % rq]
        kb.gather(dtT[0:2, rq * 2048:(rq + 1) * 2048], agD2k, gx[0:2, cd:cd + 1], reads=[gxr, agDr], writes=[dtTr])
    pdt, pdtr = pbanks.next()
    for c in range(NCH):
        kb.op("pe", lambda e, c=c: e.transpose(pdt[:, c * 2:(c + 1) * 2], dtT[0:2, c * 128:(c + 1) * 128], ident[0:2, 0:2]), reads=[dtTr, cr], writes=[pdtr])
    kb.op("dve", lambda e: e.tensor_copy(out=dt[:].rearrange("p c h -> p (c h)"), in_=pdt[:, 0:NCH * 2]), reads=[pdtr], writes=[dtr])
    aa = kb.sb([128, NCH, 2], F32, "sa", es=es); aar = Res()
    An = kb.sb([128, 2], F32, "sAn", es=es); Anr = Res()
    kb.op("act", lambda e: e.activation(out=An[:], in_=rp[:, 2:4], func=AF.Exp), reads=[rpr], writes=[Anr])
    kb.op("dve", lambda e: e.tensor_scalar(out=An[:], in0=An[:], scalar1=-1.0, scalar2=None, op0=ALU.mult), reads=[Anr], writes=[Anr])
    for h in range(2):
        kb.op("dve", lambda e, h=h: e.tensor_scalar(out=dt[:, :, h], in0=dt[:, :, h], scalar1=rp[:, h:h + 1], scalar2=None, op0=ALU.add),
              reads=[dtr, rpr], writes=[dtr])
    kb.op("act", lambda e: e.activation(out=dt[:], in_=dt[:], func=AF.Exp), reads=[dtr], writes=[dtr])
    kb.op("act", lambda e: e.activation(out=dt[:], in_=dt[:], func=AF.Ln, bias=1.0), reads=[dtr], writes=[dtr])
    for h in range(2):
        kb.op("dve", lambda e, h=h: e.tensor_scalar(out=aa[:, :, h], in0=dt[:, :, h], scalar1=An[:, h:h + 1], scalar2=None, op0=ALU.mult),
              reads=[dtr, Anr], writes=[aar])
    if LEVEL == -2: return
    acum = kb.sb([128, NCH, 2], F32, "sacum", es=es); acr = Res()
    dout = kb.sb([128, NCH, 2], F32, "sdout", es=es); dor = Res()
    dst = kb.sb([128, NCH, 2], F32, "sdst", es=es); dsr = Res()
    dtot = kb.sb([128, NCH, 2], F32, "sdtot", es=es); dtor = Res()
    aflat = aa[:].rearrange("p c h -> p (c h)")
    p1, p1r = pbanks.next()
    kb.op("pe", lambda e: e.matmul(p1[:, 0:NCH * 2], lhsT=tri, rhs=aflat, start=True, stop=True), reads=[cr, aar], writes=[p1r])
    p2, p2r = pbanks.next()
    kb.op("pe", lambda e: e.matmul(p2[:, 0:NCH * 2], lhsT=ones, rhs=aflat, start=True, stop=True), reads=[cr, aar], writes=[p2r])
    fl = lambda t: t[:].rearrange("p c h -> p (c h)")
    kb.op("dve", lambda e: e.tensor_copy(out=fl(acum), in_=p1[:, 0:NCH * 2]), reads=[p1r], writes=[acr])
    kb.op("act", lambda e: e.activation(out=fl(dout), in_=p1[:, 0:NCH * 2], func=AF.Exp), reads=[p1r], writes=[dor])
    kb.op("act", lambda e: e.activation(out=fl(dtot), in_=p2[:, 0:NCH * 2], func=AF.Exp), reads=[p2r], writes=[dtor])
    kb.op("dve", lambda e: e.tensor_tensor(out=fl(dst), in0=p2[:, 0:NCH * 2], in1=fl(acum), op=ALU.subtract), reads=[p2r, acr], writes=[dsr])
    kb.op("act", lambda e: e.activation(out=fl(dst), in_=fl(dst), func=AF.Exp), reads=[dsr], writes=[dsr])

    if LEVEL == -3: return
    prev = [kb.sb([64, 64], F32, "sprev", es=es) for _ in range(2)]
    prevr = [Res() for _ in range(2)]
    prevb = [kb.sb([64, 64], BF16, "sprevb", es=es) for _ in range(2)]
    prevbr = [Res() for _ in range(2)]
    for h in range(2):
        kb.op("pool", lambda e, h=h: e.memset(prev[h][:], 0.0), writes=[prevr[h]])
        kb.op("pool", lambda e, h=h: e.memset(prevb[h][:], 0.0), writes=[prevbr[h]])

    xfull = kb.sb([128, S + 3], BF16, "sxfull", es=es); xfr = Res()
    bfull = kb.sb([64, S + 3], BF16, "sbfull", es=es); bfr = Res()
    cfull = kb.sb([64, S + 3], BF16, "scfull", es=es); cfr = Res()
    for (tl, rs, nm, P) in ((xfull, xfr, "sx", 128), (bfull, bfr, "sB", 64), (cfull, cfr, "sC", 64)):
        kb.op("pool", lambda e, tl=tl, P=P: e.memset(tl[:P, 0:3], 0.0), writes=[rs])
        for rq in range(4):
            cc_ = IX[nm + str(rq)]
            kb.gather(tl[:P, 3 + rq * 2048:3 + (rq + 1) * 2048], agF2k, gx[0:P, cc_:cc_ + 1], reads=[gxr, agFr], writes=[rs])
    yTb = Rot(kb, 2, [128, N], BF16, "syTb", es=es)
    xcv = Rot(kb, 2, [128, N], F32, "sxcv", es=es)
    bcv = Rot(kb, 2, [64, N], F32, "sbcv", es=es)
    ccv = Rot(kb, 2, [64, N], F32, "sccv", es=es)
    xs = Rot(kb, 2, [128, N], F32, "sxs", es=es)
    bs = Rot(kb, 2, [64, N], BF16, "sbs", es=es)
    cs = Rot(kb, 2, [64, N], BF16, "scs", es=es)
    xtm = Rot(kb, 2, [128, 128], F32, "sxtm", es=es)
    Xb = Rot(kb, 2, [128, 128], BF16, "sXb", es=es)
    Xd = Rot(kb, 2, [128, 128], BF16, "sXd", es=es)
    Btm = Rot(kb, 2, [128, 64], BF16, "sBtm", es=es)
    CBm = Rot(kb, 2, [128, 128], F32, "sCBm", es=es)
    lh = Rot(kb, 2, [128, 128], F32, "slh", es=es)
    EE = Rot(kb, 2, [128, 128], F32, "sE", es=es)
    MT = Rot(kb, 2, [128, 128], BF16, "sMT", es=es)
    yt = Rot(kb, 2, [128, 8, 128], F32, "syt", es=es)
    pbi = 0
    for j in range(S // N if LEVEL > 0 else 0):
        t0 = j * N
        xt, xr = xfull[:, t0:t0 + N + 3], xfr
        bt, br = bfull[:, t0:t0 + N + 3], bfr
        ctt, ctr = cfull[:, t0:t0 + N + 3], cfr
        xc, xcr = xcv.next(); bc, bcr = bcv.next(); cc, ccr = ccv.next()
        emit_conv(kb, "dve", xc, xt, vt, vr, 0, N, 128, xr, xcr)
        emit_conv(kb, "dve", bc, bt, vt, vr, 5, N, 64, br, bcr)
        emit_conv(kb, "dve", cc, ctt, vt, vr, 10, N, 64, ctr, ccr)
        xst, xsr = xs.next(); bst, bsr = bs.next(); cst_, csr = cs.next()
        kb.op("act", lambda e: e.activation(out=xst[:], in_=xc[:], func=AF.Silu), reads=[xcr], writes=[xsr])
        kb.op("act", lambda e: e.activation(out=bst[:], in_=bc[:], func=AF.Silu), reads=[bcr], writes=[bsr])
        kb.op("act", lambda e: e.activation(out=cst_[:], in_=cc[:], func=AF.Silu), reads=[ccr], writes=[csr])
        ytile, ytr = yt.next()
        for c in range(N // 128 if LEVEL > 1 else 0):
            gc = j * (N // 128) + c
            ts = slice(c * 128, (c + 1) * 128)
            pT, pTr = pbanks.next()
            kb.op("pe", lambda e: e.transpose(pT[:, 0:128], xst[:, ts], ident), reads=[xsr, cr], writes=[pTr])
            xm, xmr = xtm.next()
            kb.op("act", lambda e: e.copy(out=xm[:], in_=pT[:, 0:128]), reads=[pTr], writes=[xmr])
            xb, xbr = Xb.next()
            for h in range(2):
                hs = slice(h * 64, (h + 1) * 64)
                kb.op("dve", lambda e, hs=hs, h=h: e.tensor_scalar(out=xb[:, hs], in0=pT[:, hs], scalar1=dt[:, gc, h:h + 1], scalar2=None, op0=ALU.mult),
                      reads=[pTr, dtr], writes=[xbr])
            xd, xdr = Xd.next()
            for h in range(2):
                hs = slice(h * 64, (h + 1) * 64)
                kb.op("pool", lambda e, hs=hs, h=h: e.tensor_scalar(out=xd[:, hs], in0=xb[:, hs], scalar1=dst[:, gc, h:h + 1], scalar2=None, op0=ALU.mult),
                      reads=[xbr, dsr], writes=[xdr])
            if LEVEL < 3: continue
            pb_t, pb_r = pbf[pbi % len(pbf)]; pbi += 1
            kb.op("pe", lambda e: e.transpose(pb_t[:, 0:64], bst[:, ts], identb[0:64, 0:64]), reads=[bsr, idbr], writes=[pb_r])
            btm, btmr = Btm.next()
            kb.op("act", lambda e: e.copy(out=btm[:], in_=pb_t[:, 0:64]), reads=[pb_r], writes=[btmr])
            if LEVEL < 4: continue
            pcb, pcbr = pbanks.next()
            kb.op("pe", lambda e: e.matmul(pcb[:, 0:128], lhsT=bst[:, ts], rhs=cst_[:, ts], start=True, stop=True), reads=[bsr, csr], writes=[pcbr])
            cbm, cbmr = CBm.next()
            kb.op("dve", lambda e: e.tensor_tensor(out=cbm[:], in0=pcb[:, 0:128], in1=tri, op=ALU.mult), reads=[pcbr, cr], writes=[cbmr])
            for h in range(2 if LEVEL > 4 else 0):
                hs = slice(h * 64, (h + 1) * 64)
                l_, lr_ = lh.next()
                kb.op("dve", lambda e, h=h: e.tensor_scalar(out=l_[:], in0=U, scalar1=aa[:, gc, h:h + 1], scalar2=None, op0=ALU.mult),
                      reads=[cr, aar], writes=[lr_])
                pseg, psegr = pbanks.next()
                kb.op("pe", lambda e: e.matmul(pseg[:, 0:128], lhsT=l_[:], rhs=tri, start=True, stop=True), reads=[lr_, cr], writes=[psegr])
                E, Er = EE.next()
                kb.op("act", lambda e: e.activation(out=E[:], in_=pseg[:, 0:128], func=AF.Exp), reads=[psegr], writes=[Er])
                mt, mtr = MT.next()
                kb.op("dve", lambda e: e.tensor_tensor(out=mt[:], in0=E[:], in1=cbm[:], op=ALU.mult), reads=[Er, cbmr], writes=[mtr])
                py, pyr = pbanks.next()
                kb.op("pe", lambda e, hs=hs: e.matmul(py[:, 0:64], lhsT=mt[:], rhs=xb[:, hs], start=True, stop=True), reads=[mtr, xbr], writes=[pyr])
                po, por = pbanks.next()
                kb.op("pe", lambda e, h=h: e.matmul(po[:, 0:64], lhsT=cst_[:, ts], rhs=prevb[h][:], start=True, stop=True), reads=[csr, prevbr[h]], writes=[por])
                kb.op("act", lambda e, hs=hs: e.copy(out=ytile[:, c, hs], in_=py[:, 0:64]), reads=[pyr], writes=[ytr])
                kb.op("dve", lambda e, hs=hs, h=h: e.scalar_tensor_tensor(out=ytile[:, c, hs], in0=po[:, 0:64], scalar=dout[:, gc, h:h + 1], in1=ytile[:, c, hs],
                                                                          op0=ALU.mult, op1=ALU.add), reads=[por, dor, ytr], writes=[ytr])
                kb.op("dve", lambda e, hs=hs, h=h: e.scalar_tensor_tensor(out=ytile[:, c, hs], in0=xm[:, hs], scalar=rp[:, 4 + h:5 + h], in1=ytile[:, c, hs],
                                                                          op0=ALU.mult, op1=ALU.add), reads=[xmr, rpr, ytr], writes=[ytr])
                pst, pstr = pbanks.next()
                kb.op("pe", lambda e, hs=hs: e.matmul(pst[0:64, 0:64], lhsT=btm[:], rhs=xd[:, hs], start=True, stop=True), reads=[btmr, xdr], writes=[pstr])
                kb.op("dve", lambda e, h=h: e.scalar_tensor_tensor(out=prev[h][:], in0=prev[h][:], scalar=dtot[0:64, gc, h:h + 1], in1=pst[0:64, 0:64],
                                                                  op0=ALU.mult, op1=ALU.add), reads=[pstr, dtor, prevr[h]], writes=[prevr[h]])
                kb.op("act", lambda e, h=h: e.copy(out=prevb[h][:], in_=prev[h][:]), reads=[prevr[h]], writes=[prevbr[h]])
        yb_, ybr = yTb.next()
        for q2 in range(N // 512):
            pyt, pytr = pbanks.next()
            for c4 in range(4):
                c = q2 * 4 + c4
                kb.op("pe", lambda e, c=c, c4=c4: e.transpose(pyt[:, c4 * 128:(c4 + 1) * 128], ytile[:, c, :], ident), reads=[ytr, cr], writes=[pytr])
            kb.op("act", lambda e, q2=q2: e.copy(out=yb_[:, q2 * 512:(q2 + 1) * 512], in_=pyt[:, :]), reads=[pytr], writes=[ybr])
        kb.dma("sp", yT_dst[:, t0:t0 + N], yb_[:], reads=[ybr, yT_r])


def emit_lru2(kb, agF2k, agFr, gx, gxr, IX, vec, wab, wxb, outT, outr, pbanks, es=None):
    N = 1024
    vt = kb.sb([128, 8], F32, "lvec", es=es); vr = Res()
    wa = kb.sb([128, 128], F32, "lwa", es=es); war = Res()
    wx = kb.sb([128, 128], F32, "lwx", es=es); wxr = Res()
    kb.dma("sp", vt[:], vec[:, :], writes=[vr])
    kb.dma("sp", wa[:], wab[:, :], writes=[war])
    kb.dma("sp", wx[:], wxb[:, :], writes=[wxr])
    c1 = kb.sb([128, 1], F32, "lc1", es=es); c1r = Res()
    kb.op("act", lambda e: e.activation(out=c1[:], in_=vt[:, 7:8], func=AF.Exp, scale=-1.0), reads=[vr], writes=[c1r])
    kb.op("act", lambda e: e.activation(out=c1[:], in_=c1[:], func=AF.Ln, bias=1.0), reads=[c1r], writes=[c1r])
    kb.op("dve", lambda e: e.tensor_scalar(out=c1[:], in0=c1[:], scalar1=-8.0, scalar2=None, op0=ALU.mult), reads=[c1r], writes=[c1r])

    xfull = kb.sb([128, S + 3], BF16, "lxfull", es=es); xfr = Res()
    yfull = kb.sb([128, S], BF16, "lyfull", es=es); yfr = Res()
    kb.op("pool", lambda e: e.memset(xfull[:, 0:3], 0.0), writes=[xfr])
    for rp in range(4):
        c1_ = IX["lx%d" % rp]; c2_ = IX["ly%d" % rp]
        kb.gather(xfull[:, 3 + rp * 2048:3 + (rp + 1) * 2048], agF2k, gx[:, c1_:c1_ + 1], reads=[gxr, agFr], writes=[xfr])
        kb.gather(yfull[:, rp * 2048:(rp + 1) * 2048], agF2k, gx[:, c2_:c2_ + 1], reads=[gxr, agFr], writes=[yfr])
    obf = Rot(kb, 2, [128, N], BF16, "lob", es=es)
    xc = Rot(kb, 2, [128, N], F32, "lxc", es=es)
    rr = Rot(kb, 2, [128, N], F32, "lr", es=es)
    ii = Rot(kb, 2, [128, N], F32, "li", es=es)
    aa = Rot(kb, 2, [128, N], F32, "la", es=es)
    qq = Rot(kb, 2, [128, N], F32, "lq", es=es)
    hh = Rot(kb, 2, [128, N], F32, "lh", es=es)
    uu = Rot(kb, 2, [128, N], F32, "lu", es=es)
    hprev = None
    for j in range(S // N):
        t0 = j * N
        xt, xr = xfull[:, t0:t0 + N + 3], xfr
        yt, yr = yfull[:, t0:t0 + N], yfr
        ct, cr = xc.next()
        kb.op("dve", lambda e: e.tensor_scalar(out=ct[:], in0=xt[:, 0:N], scalar1=vt[:, 0:1], scalar2=vt[:, 4:5],
                                               op0=ALU.mult, op1=ALU.add), reads=[xr, vr], writes=[cr])
        for k in range(1, 4):
            kb.op("dve", lambda e, k=k: e.scalar_tensor_tensor(out=ct[:], in0=xt[:, k:k + N], scalar=vt[:, k:k + 1], in1=ct[:],
                                                              op0=ALU.mult, op1=ALU.add), reads=[xr, vr, cr], writes=[cr])
        rt, rres = rr.next()
        it, ires = ii.next()
        for hf in range(N // 512):
            sl = slice(hf * 512, (hf + 1) * 512)
            pa, par = pbanks.next()
            kb.op("pe", lambda e: e.matmul(pa[:, :], lhsT=wa[:], rhs=ct[:, sl], start=True, stop=True), reads=[war, cr], writes=[par])
            kb.op("act", lambda e: e.activation(out=rt[:, sl], in_=pa[:, :], func=AF.Sigmoid, bias=vt[:, 5:6]), reads=[par, vr], writes=[rres])
            px, pxr = pbanks.next()
            kb.op("pe", lambda e: e.matmul(px[:, :], lhsT=wx[:], rhs=ct[:, sl], start=True, stop=True), reads=[wxr, cr], writes=[pxr])
            kb.op("act", lambda e: e.activation(out=it[:, sl], in_=px[:, :], func=AF.Sigmoid, bias=vt[:, 6:7]), reads=[pxr, vr], writes=[ires])
        at, ar = aa.next()
        kb.op("act", lambda e: e.activation(out=at[:], in_=rt[:], func=AF.Exp, scale=c1[:, 0:1]), reads=[rres, c1r], writes=[ar])
        qt, qr = qq.next()
        kb.op("pool", lambda e: e.tensor_tensor(out=qt[:], in0=at[:], in1=at[:], op=ALU.mult), reads=[ar], writes=[qr])
        kb.op("pool", lambda e: e.tensor_scalar(out=qt[:], in0=qt[:], scalar1=-1.0, scalar2=1.0, op0=ALU.mult, op1=ALU.add), reads=[qr], writes=[qr])
        kb.op("act", lambda e: e.activation(out=qt[:], in_=qt[:], func=AF.Sqrt), reads=[qr], writes=[qr])
        kb.op("dve", lambda e: e.tensor_tensor(out=it[:], in0=it[:], in1=ct[:], op=ALU.mult), reads=[ires, cr], writes=[ires])
        kb.op("dve", lambda e: e.tensor_tensor(out=it[:], in0=it[:], in1=qt[:], op=ALU.mult), reads=[ires, qr], writes=[ires])
        ht, hr = hh.next()
        if hprev is None:
            kb.op("dve", lambda e: e.tensor_tensor_scan(out=ht[:], data0=at[:], data1=it[:], initial=0.0, op0=ALU.mult, op1=ALU.add),
                  reads=[ar, ires], writes=[hr])
        else:
            hp, hpr = hprev
            kb.op("dve", lambda e: e.tensor_tensor_scan(out=ht[:], data0=at[:], data1=it[:], initial=hp[:, N - 1:N], op0=ALU.mult, op1=ALU.add),
                  reads=[ar, ires, hpr], writes=[hr])
        hprev = (ht, hr)
        ut, ur = uu.next()
        kb.op("pool", lambda e: e.tensor_tensor(out=ut[:], in0=yt[:], in1=yt[:], op=ALU.mult), reads=[yr], writes=[ur])
        kb.op("pool", lambda e: e.tensor_scalar(out=ut[:], in0=ut[:], scalar1=0.044715, scalar2=1.0, op0=ALU.mult, op1=ALU.add), reads=[ur], writes=[ur])
        kb.op("pool", lambda e: e.tensor_tensor(out=ut[:], in0=ut[:], in1=yt[:], op=ALU.mult), reads=[ur, yr], writes=[ur])
        kb.op("act", lambda e: e.activation(out=ut[:], in_=ut[:], func=AF.Sigmoid, scale=1.5957691216057308), reads=[ur], writes=[ur])
        kb.op("pool", lambda e: e.tensor_tensor(out=ut[:], in0=ut[:], in1=yt[:], op=ALU.mult), reads=[ur, yr], writes=[ur])
        ob_, obr_ = obf.next()
        kb.op("pool", lambda e: e.tensor_tensor(out=ob_[:], in0=ut[:], in1=ht[:], op=ALU.mult), reads=[ur, hr], writes=[obr_])
        kb.dma("sp", outT[:, t0:t0 + N], ob_[:], reads=[obr_, outr])


def emit_C2(kb, xT, agN512, agNr, agY2k, agYr, agL2k, agLr, gx, gxr, IX, pT, w_in, pnsa, pssd, plru, w_out, pwg, pwp, rw, ewg, ewu, ewd, cst, x2T, x2r, final_out, banks, es, xTr=None):
    gen = Rot.__new__(Rot); gen.t = banks; gen.i = 0
    sb = lambda shape, dt, name: kb.sb(shape, dt, name, es=es)
    cv = sb([128, CW], F32, "cc"); cvr = Res()
    kb.dma("sp", cv[:], cst[:, :], writes=[cvr])
    ones1k = cv[:, 52:180]; ones512 = cv[:, 180:308]; ident = cv[:, 308:436]
    selE = lambda e: cv[0:16, 436 + e * 128:436 + (e + 1) * 128]
    rb = cv[:, 36:52]

    tmpf = Rot(kb, 4, [128, 512], F32, "ctf", es=es)
    stat = Rot(kb, 3, [128, 512], F32, "cstat", es=es)
    kb.uid += 1
    mixD = kb.dram("mixD%d" % kb.uid, [D, TOK], BF16, "Internal")
    mixDr = [Res() for _ in range(8)]
    wst_box = [None]

    def load_w(src_rows, nk, ncols):
        wt, wr = wst_box[0].next()
        kb.dma("pool", wt[:, 0:nk, 0:ncols], src_rows.rearrange("(c p) f -> p c f", p=128), writes=[wr])
        return wt, wr

    def ln_stats(src, srcr, nchunk, tt, ones_ap, eps, sq_eng="dve"):
        ts = slice(tt * 512, (tt + 1) * 512)
        pm_, pmr = gen.next()
        for c in range(nchunk):
            kb.op("pe", lambda e, c=c: e.matmul(pm_[:, :], lhsT=ones_ap, rhs=src[:, c, ts], start=(c == 0), stop=(c == nchunk - 1)), reads=[cvr, srcr[c]], writes=[pmr])
        pq_, pqr = gen.next()
        for c in range(nchunk):
            sq, sqr = tmpf.next()
            kb.op(sq_eng, lambda e, c=c: e.tensor_tensor(out=sq[:], in0=src[:, c, ts], in1=src[:, c, ts], op=ALU.mult), reads=[srcr[c]], writes=[sqr])
            kb.op("pe", lambda e, c=c: e.matmul(pq_[:, :], lhsT=ones_ap, rhs=sq[:], start=(c == 0), stop=(c == nchunk - 1)), reads=[cvr, sqr], writes=[pqr])
        mean, meanr = stat.next()
        kb.op("act", lambda e: e.copy(out=mean[:], in_=pm_[:, :]), reads=[pmr], writes=[meanr])
        rstd, rstdr = stat.next()
        return mean, meanr, pq_, pqr, rstd, rstdr

    with contextlib.ExitStack() as es1:
        sb1 = lambda shape, dt, name: kb.sb(shape, dt, name, es=es1)
        wst_box[0] = Rot.__new__(Rot); wst_box[0].i = 0
        wst_box[0].t = [(sb1([128, 8, 512], BF16, "cw"), Res()) for _ in range(3)]
        xbf = sb1([128, 8, TOK], BF16, "cxbf"); xbr = [Res() for _ in range(8)]
        for c in range(8):
            kb.dma("pool", xbf[:, c, :], xT[c * 128:(c + 1) * 128, :], reads=([xTr[c]] if xTr else []), writes=[xbr[c]])
        ob = [sb1([128, 4, TOK], BF16, "cob%d" % i) for i in range(3)]
        obr = [[Res() for _ in range(4)] for _ in range(3)]
        for c in range(4):
            for q4 in range(4):
                cn = IX["on%d_%d" % (c, q4)]
                kb.gather(ob[0][:, c, q4 * 512:(q4 + 1) * 512], agN512, gx[:, cn:cn + 1], reads=[gxr, agNr], writes=[obr[0][c]])
            cl = IX["l%d" % c]
            kb.gather(ob[2][:, c, :], agL2k, gx[:, cl:cl + 1], reads=[gxr, agLr], writes=[obr[2][c]])
        with contextlib.ExitStack() as es1b:
            yg = kb.sb([128, 4, TOK], F32, "cyg", es=es1b); ygr = [Res() for _ in range(4)]
            ygb = ob[1]; ygbr = obr[1]
            for c in range(4):
                cy = IX["y%d" % c]
                kb.gather(ygb[:, c, :], agY2k, gx[:, cy:cy + 1], reads=[gxr, agYr], writes=[ygbr[c]])
            wz, wzr = load_w(w_in[:, OFF_Z:OFF_Z + 512], 8, 512)
            for tt in range(NT):
                ts = slice(tt * 512, (tt + 1) * 512)
                for fc in range(4):
                    pz, pzr = gen.next()
                    for c in range(8):
                        kb.op("pe", lambda e, c=c: e.matmul(pz[:, :], lhsT=wz[:, c, fc * 128:(fc + 1) * 128], rhs=xbf[:, c, ts], start=(c == 0), stop=(c == 7)),
                              reads=[wzr, xbr[c]], writes=[pzr])
                    sz, szr = tmpf.next()
                    kb.op("act", lambda e: e.activation(out=sz[:], in_=pz[:, :], func=AF.Silu), reads=[pzr], writes=[szr])
                    kb.op("dve", lambda e, fc=fc: e.tensor_tensor(out=yg[:, fc, ts], in0=ygb[:, fc, ts], in1=sz[:], op=ALU.mult), reads=[szr, ygbr[fc]], writes=[ygr[fc]])
                pq_, pqr = gen.next()
                for c in range(4):
                    sq, sqr = tmpf.next()
                    kb.op("dve", lambda e, c=c: e.tensor_tensor(out=sq[:], in0=yg[:, c, ts], in1=yg[:, c, ts], op=ALU.mult), reads=[ygr[c]], writes=[sqr])
                    kb.op("pe", lambda e, c=c: e.matmul(pq_[:, :], lhsT=ones512, rhs=sq[:], start=(c == 0), stop=(c == 3)), reads=[cvr, sqr], writes=[pqr])
                rstd, rstdr = stat.next()
                kb.op("dve", lambda e: e.tensor_scalar(out=rstd[:], in0=pq_[:, :], scalar1=1e-5, scalar2=None, op0=ALU.add), reads=[pqr], writes=[rstdr])
                kb.op("act", lambda e: e.activation(out=rstd[:], in_=rstd[:], func=AF.Sqrt), reads=[rstdr], writes=[rstdr])
                kb.op("dve", lambda e: e.reciprocal(out=rstd[:], in_=rstd[:]), reads=[rstdr], writes=[rstdr])
                for c in range(4):
                    t1, t1r = tmpf.next()
                    kb.op("dve", lambda e, c=c: e.tensor_tensor(out=t1[:], in0=yg[:, c, ts], in1=rstd[:], op=ALU.mult), reads=[ygr[c], rstdr], writes=[t1r])
                    kb.op("act", lambda e, c=c: e.activation(out=ob[1][:, c, ts], in_=t1[:], func=AF.Copy, scale=cv[:, 32 + c:33 + c]), reads=[t1r, cvr], writes=[obr[1][c]])
            kb.barrier()
        if CDBG == "ossd":
            dbg = sb1([128, 4, TOK], F32, "cdbg"); dbgr = Res()
            for c in range(4):
                kb.op("dve", lambda e, c=c: e.tensor_copy(out=dbg[:, c, :], in_=ob[1][:, c, :]), reads=[obr[1][c]], writes=[dbgr])
                kb.dma("sp", x2T[c * 128:(c + 1) * 128, :], dbg[:, c, :], reads=[dbgr], final=True)
            kb.barrier()
            return
        projs = [pnsa, pssd, plru]
        mixh = sb1([128, 4, TOK], BF16, "cmixh"); mixhr = [Res() for _ in range(4)]
        for half in range(2):
            fs = slice(half * 512, (half + 1) * 512)
            for br in range(3):
                wp, wpr = load_w(projs[br][:, fs], 4, 512)
                wm, wmr = load_w(w_in[:, OFF_MERGE + br * 1024 + half * 512: OFF_MERGE + br * 1024 + (half + 1) * 512], 8, 512)
                for fc in range(4):
                    oc = half * 4 + fc
                    for tt in range(NT):
                        ts = slice(tt * 512, (tt + 1) * 512)
                        pg, pgr = gen.next()
                        for c in range(8):
                            kb.op("pe", lambda e, c=c: e.matmul(pg[:, :], lhsT=wm[:, c, fc * 128:(fc + 1) * 128], rhs=xbf[:, c, ts], start=(c == 0), stop=(c == 7)),
                                  reads=[wmr, xbr[c]], writes=[pgr])
                        pp, ppr = gen.next()
                        for c in range(4):
                            kb.op("pe", lambda e, c=c: e.matmul(pp[:, :], lhsT=wp[:, c, fc * 128:(fc + 1) * 128], rhs=ob[br][:, c, ts], start=(c == 0), stop=(c == 3)),
                                  reads=[wpr, obr[br][c]], writes=[ppr])
                        sg, sgr = tmpf.next()
                        kb.op("act", lambda e: e.activation(out=sg[:], in_=pg[:, :], func=AF.Sigmoid), reads=[pgr], writes=[sgr])
                        if br == 0:
                            kb.op("dve", lambda e: e.tensor_tensor(out=mixh[:, fc, ts], in0=pp[:, :], in1=sg[:], op=ALU.mult), reads=[ppr, sgr], writes=[mixhr[fc]])
                        else:
                            kb.op("dve", lambda e: e.tensor_tensor(out=sg[:], in0=pp[:, :], in1=sg[:], op=ALU.mult), reads=[ppr, sgr], writes=[sgr])
                            kb.op("dve", lambda e: e.tensor_tensor(out=mixh[:, fc, ts], in0=mixh[:, fc, ts], in1=sg[:], op=ALU.add), reads=[sgr, mixhr[fc]], writes=[mixhr[fc]])
            for fc in range(4):
                kb.dma("sp", mixD[(half * 4 + fc) * 128:(half * 4 + fc + 1) * 128, :], mixh[:, fc, :], reads=[mixhr[fc]], writes=[mixDr[half * 4 + fc]])
        kb.barrier()

    with contextlib.ExitStack() as es2:
        sb2 = lambda shape, dt, name: kb.sb(shape, dt, name, es=es2)
        r = sb2([128, 8, TOK], F32, "cr"); rr = [Res() for _ in range(8)]
        x1b = sb2([128, 8, TOK], BF16, "cx1b"); x1br = [Res() for _ in range(8)]
        for c in range(8):
            kb.dma("sp", r[:, c, :], xT[c * 128:(c + 1) * 128, :], reads=([xTr[c]] if xTr else []), writes=[rr[c]])
        gatesT = sb2([16, TOK], F32, "cgT"); gTr = Res()
        rwt = sb2([128, 8, 16], F32, "crw"); rwr = Res()
        kb.dma("sp", rwt[:], rw.rearrange("(c p) e -> p c e", p=128), writes=[rwr])
        pad = sb2([128, 4, 8], F32, "cpad"); padr = Res()
        kb.op("pool", lambda e: e.memset(pad[:], -1e30), writes=[padr])
        rt = Rot(kb, 2, [128, 16], F32, "crt", es=es2)
        rt2 = Rot(kb, 2, [128, 16], F32, "crt2", es=es2)
        t8 = Rot(kb, 2, [128, 4, 8], F32, "ct8", es=es2)
        sm4 = Rot(kb, 4, [128, 4], F32, "csm4", es=es2)
        sm1 = Rot(kb, 4, [128, 1], F32, "csm1", es=es2)
        t8b = Rot(kb, 2, [128, 8], F32, "ct8b", es=es2)
        es2a = contextlib.ExitStack()
        wst_box[0] = Rot.__new__(Rot); wst_box[0].i = 0
        wst_box[0].t = [(kb.sb([128, 8, 512], BF16, "cw2", es=es2a), Res()) for _ in range(2)]
        mixed = kb.sb([128, 8, TOK], BF16, "cmixed", es=es2a); mixr = [Res() for _ in range(8)]
        pb_ = kb.sb([128, 2, TOK], BF16, "cpb", es=es2a); pbr = [Res() for _ in range(2)]
        for c in range(8):
            kb.dma("sp", mixed[:, c, :], mixD[c * 128:(c + 1) * 128, :], reads=[mixDr[c]], writes=[mixr[c]])
        if CDBG == "mixed":
            for c in range(8):
                kb.op("dve", lambda e, c=c: e.tensor_copy(out=r[:, c, :], in_=mixed[:, c, :]), reads=[mixr[c], rr[c]], writes=[rr[c]])
                kb.dma("sp", x2T[c * 128:(c + 1) * 128, :], r[:, c, :], reads=[rr[c]], final=True)
            kb.barrier(); es2a.close()
            return
        for half in range(2):
            wo, wor = load_w(w_out[:, half * 512:(half + 1) * 512], 8, 512)
            for fc in range(4):
                oc = half * 4 + fc
                for tt in range(NT):
                    ts = slice(tt * 512, (tt + 1) * 512)
                    pu, pur = gen.next()
                    for c in range(8):
                        kb.op("pe", lambda e, c=c: e.matmul(pu[:, :], lhsT=wo[:, c, fc * 128:(fc + 1) * 128], rhs=mixed[:, c, ts], start=(c == 0), stop=(c == 7)),
                              reads=[wor, mixr[c]], writes=[pur])
                    kb.op("dve", lambda e: e.scalar_tensor_tensor(out=r[:, oc, ts], in0=r[:, oc, ts], scalar=ALPHA, in1=pu[:, :], op0=ALU.mult, op1=ALU.add),
                          reads=[pur, rr[oc]], writes=[rr[oc]])

        def layer_norm(src, srcr, gcol, bcol, dst_b, dst_br):
            for tt in range(NT):
                ts = slice(tt * 512, (tt + 1) * 512)
                mean, meanr, pq_, pqr, rstd, rstdr = ln_stats(src, srcr, 8, tt, ones1k, 1e-5)
                m2, m2r = stat.next()
                kb.op("dve", lambda e: e.tensor_tensor(out=m2[:], in0=mean[:], in1=mean[:], op=ALU.mult), reads=[meanr], writes=[m2r])
                kb.op("dve", lambda e: e.scalar_tensor_tensor(out=rstd[:], in0=pq_[:, :], scalar=1e-5, in1=m2[:], op0=ALU.add, op1=ALU.subtract), reads=[pqr, m2r], writes=[rstdr])
                kb.op("act", lambda e: e.activation(out=rstd[:], in_=rstd[:], func=AF.Sqrt), reads=[rstdr], writes=[rstdr])
                kb.op("dve", lambda e: e.reciprocal(out=rstd[:], in_=rstd[:]), reads=[rstdr], writes=[rstdr])
                for c in range(8):
                    kb.op("dve", lambda e, c=c: e.tensor_tensor(out=src[:, c, ts], in0=src[:, c, ts], in1=mean[:], op=ALU.subtract), reads=[meanr, srcr[c]], writes=[srcr[c]])
                    kb.op("dve", lambda e, c=c: e.tensor_tensor(out=src[:, c, ts], in0=src[:, c, ts], in1=rstd[:], op=ALU.mult), reads=[rstdr, srcr[c]], writes=[srcr[c]])
                    kb.op("dve", lambda e, c=c: e.tensor_scalar(out=src[:, c, ts], in0=src[:, c, ts], scalar1=cv[:, gcol + c:gcol + c + 1], scalar2=cv[:, bcol + c:bcol + c + 1],
                                                               op0=ALU.mult, op1=ALU.add), reads=[cvr, srcr[c]], writes=[srcr[c]])
                    if dst_b is not None:
                        kb.op("act", lambda e, c=c: e.copy(out=dst_b[:, c, ts], in_=src[:, c, ts]), reads=[srcr[c]], writes=[dst_br[c]])

        layer_norm(r, rr, 0, 8, x1b, x1br)
        if CDBG == "x1":
            for c in range(8):
                kb.dma("sp", x2T[c * 128:(c + 1) * 128, :], r[:, c, :], reads=[rr[c]], final=True)
            kb.barrier(); es2a.close()
            return

        for s_ in range(TOK // 128):
            ss = slice(s_ * 128, (s_ + 1) * 128)
            pl, plr = gen.next()
            for c in range(8):
                kb.op("pe", lambda e, c=c: e.matmul(pl[:, 0:16], lhsT=r[:, c, ss], rhs=rwt[:, c, :], start=(c == 0), stop=(c == 7)), reads=[rr[c], rwr], writes=[plr])
            aff, affr = rt.next()
            kb.op("act", lambda e: e.activation(out=aff[:], in_=pl[:, 0:16], func=AF.Sigmoid), reads=[plr], writes=[affr])
            sel, selr = rt2.next()
            kb.op("dve", lambda e: e.tensor_tensor(out=sel[:], in0=aff[:], in1=rb, op=ALU.add), reads=[affr, cvr], writes=[selr])
            kb.op("dve", lambda e: e.tensor_copy(out=pad[:, :, 0:4], in_=sel[:].rearrange("p (g k) -> p g k", g=4)), reads=[selr, padr], writes=[padr])
            tp, tpr = t8.next()
            for g in range(4):
                kb.op("dve", lambda e, g=g: e.max(out=tp[:, g, :], in_=pad[:, g, :]), reads=[padr], writes=[tpr])
            gs, gsr = sm4.next()
            kb.op("dve", lambda e: e.tensor_tensor(out=gs[:], in0=tp[:, :, 0], in1=tp[:, :, 1], op=ALU.add), reads=[tpr], writes=[gsr])
            gm, gmr = sm1.next()
            kb.op("dve", lambda e: e.reduce_max(out=gm[:], in_=gs[:], axis=AX.X), reads=[gsr], writes=[gmr])
            isb, isbr = sm4.next()
            kb.op("dve", lambda e: e.tensor_scalar(out=isb[:], in0=gs[:], scalar1=gm[:, 0:1], scalar2=None, op0=ALU.is_ge), reads=[gsr, gmr], writes=[isbr])
            off, offr = sm4.next()
            kb.op("dve", lambda e: e.tensor_scalar(out=off[:], in0=isb[:], scalar1=1e9, scalar2=-1e9, op0=ALU.mult, op1=ALU.add), reads=[isbr], writes=[offr])
            msk, mskr = rt2.next()
            for g in range(4):
                kb.op("dve", lambda e, g=g: e.tensor_scalar(out=msk[:, g * 4:(g + 1) * 4], in0=sel[:, g * 4:(g + 1) * 4], scalar1=isb[:, g:g + 1], scalar2=off[:, g:g + 1],
                                                            op0=ALU.mult, op1=ALU.add), reads=[selr, isbr, offr], writes=[mskr])
            tb, tbr = t8b.next()
            kb.op("dve", lambda e: e.max(out=tb[:], in_=msk[:]), reads=[mskr], writes=[tbr])
            kb.op("dve", lambda e: e.tensor_scalar(out=msk[:], in0=msk[:], scalar1=tb[:, 1:2], scalar2=None, op0=ALU.is_ge), reads=[mskr, tbr], writes=[mskr])
            kb.op("dve", lambda e: e.tensor_tensor(out=msk[:], in0=msk[:], in1=aff[:], op=ALU.mult), reads=[mskr, affr], writes=[mskr])
            ws, wsr = sm1.next()
            kb.op("dve", lambda e: e.reduce_sum(out=ws[:], in_=msk[:], axis=AX.X), reads=[mskr], writes=[wsr])
            kb.op("dve", lambda e: e.reciprocal(out=ws[:], in_=ws[:]), reads=[wsr], writes=[wsr])
            kb.op("dve", lambda e: e.tensor_scalar(out=msk[:], in0=msk[:], scalar1=ws[:, 0:1], scalar2=None, op0=ALU.mult), reads=[mskr, wsr], writes=[mskr])
            pt, ptr_ = gen.next()
            kb.op("pe", lambda e: e.transpose(pt[0:16, 0:128], msk[:], ident), reads=[mskr, cvr], writes=[ptr_])
            kb.op("act", lambda e: e.copy(out=gatesT[:, ss], in_=pt[0:16, 0:128]), reads=[ptr_], writes=[gTr])

        for c in range(2):
            kb.dma("pool", pb_[:, c, :], pT[c * 128:(c + 1) * 128, :], writes=[pbr[c]])
        for half in range(2):
            fs = slice(half * 512, (half + 1) * 512)
            wg_, wgr = load_w(pwg[:, fs], 8, 512)
            wp_, wpr = load_w(pwp[:, fs], 2, 512)
            for fc in range(4):
                oc = half * 4 + fc
                for tt in range(NT):
                    ts = slice(tt * 512, (tt + 1) * 512)
                    pg, pgr = gen.next()
                    for c in range(8):
                        kb.op("pe", lambda e, c=c: e.matmul(pg[:, :], lhsT=wg_[:, c, fc * 128:(fc + 1) * 128], rhs=x1b[:, c, ts], start=(c == 0), stop=(c == 7)),
                              reads=[wgr, x1br[c]], writes=[pgr])
                    pp, ppr = gen.next()
                    for c in range(2):
                        kb.op("pe", lambda e, c=c: e.matmul(pp[:, :], lhsT=wp_[:, c, fc * 128:(fc + 1) * 128], rhs=pb_[:, c, ts], start=(c == 0), stop=(c == 1)),
                              reads=[wpr, pbr[c]], writes=[ppr])
                    sg, sgr = tmpf.next()
                    kb.op("act", lambda e: e.activation(out=sg[:], in_=pg[:, :], func=AF.Sigmoid), reads=[pgr], writes=[sgr])
                    kb.op("dve", lambda e: e.tensor_tensor(out=sg[:], in0=pp[:, :], in1=sg[:], op=ALU.mult), reads=[ppr, sgr], writes=[sgr])
                    kb.op("dve", lambda e: e.scalar_tensor_tensor(out=r[:, oc, ts], in0=r[:, oc, ts], scalar=ALPHA, in1=sg[:], op0=ALU.mult, op1=ALU.add),
                          reads=[sgr, rr[oc]], writes=[rr[oc]])

        kb.barrier()
        es2a.close()
        if CDBG == "ple":
            for c in range(8):
                kb.dma("sp", x2T[c * 128:(c + 1) * 128, :], r[:, c, :], reads=[rr[c]], final=True)
            return
        ew = Rot(kb, 2, [128, 2, 3, 8, 256], BF16, "cew", es=es2)
        hb = Rot(kb, 2, [128, 4, 512], BF16, "chb", es=es2)
        steps = [(ep, tt) for ep in range(8) for tt in range(NT)]
        live = {}
        wts = {}

        def load_pair(ep):
            wt, wr = ew.next()
            for m in range(2):
                e_ = ep * 2 + m
                kb.dma("pool", wt[:, m, 0, :, :], ewg[e_, :, :].rearrange("(c p) f -> p c f", p=128), writes=[wr])
                kb.dma("pool", wt[:, m, 1, :, :], ewu[e_, :, :].rearrange("(c p) f -> p c f", p=128), writes=[wr])
                kb.dma("pool", wt[:, m, 2, :, :].rearrange("p (c a) f -> p c (a f)", c=2), ewd[e_, :, :].rearrange("(c p) f -> p c f", p=128), writes=[wr])
            return wt, wr

        for idx in range(len(steps) + 1):
            if idx < len(steps):
                ep, tt = steps[idx]
                if tt == 0:
                    if ep == 0:
                        wts[0] = load_pair(0)
                    wt, wr = wts[ep]
                ts = slice(tt * 512, (tt + 1) * 512)
                h, hr = hb.next()
                for m in range(2):
                    e_ = ep * 2 + m
                    pgb, pgbr = gen.next()
                    kb.op("pe", lambda e, e_=e_, pgb=pgb, ts=ts: e.matmul(pgb[:, :], lhsT=selE(e_), rhs=gatesT[:, ts], start=True, stop=True), reads=[cvr, gTr], writes=[pgbr])
                    gb, gbr = tmpf.next()
                    kb.op("act", lambda e, gb=gb, pgb=pgb: e.copy(out=gb[:], in_=pgb[:, :]), reads=[pgbr], writes=[gbr])
                    for fc in range(2):
                        pg, pgr = gen.next()
                        for c in range(8):
                            kb.op("pe", lambda e, c=c, m=m, fc=fc, pg=pg, wt=wt, ts=ts: e.matmul(pg[:, :], lhsT=wt[:, m, 0, c, fc * 128:(fc + 1) * 128], rhs=x1b[:, c, ts], start=(c == 0), stop=(c == 7)),
                                  reads=[wr, x1br[c]], writes=[pgr])
                        pu, pur = gen.next()
                        for c in range(8):
                            kb.op("pe", lambda e, c=c, m=m, fc=fc, pu=pu, wt=wt, ts=ts: e.matmul(pu[:, :], lhsT=wt[:, m, 1, c, fc * 128:(fc + 1) * 128], rhs=x1b[:, c, ts], start=(c == 0), stop=(c == 7)),
                                  reads=[wr, x1br[c]], writes=[pur])
                        sg, sgr = tmpf.next()
                        kb.op("act", lambda e, sg=sg, pg=pg: e.activation(out=sg[:], in_=pg[:, :], func=AF.Silu), reads=[pgr], writes=[sgr])
                        kb.op("dve", lambda e, sg=sg, pu=pu: e.tensor_tensor(out=sg[:], in0=pu[:, :], in1=sg[:], op=ALU.mult), reads=[pur, sgr], writes=[sgr])
                        kb.op("dve", lambda e, m=m, fc=fc, h=h, sg=sg, gb=gb: e.tensor_tensor(out=h[:, m * 2 + fc, :], in0=sg[:], in1=gb[:], op=ALU.mult), reads=[sgr, gbr], writes=[hr])
                live[idx] = (wt, wr, h, hr, ts)
            if idx >= 1:
                wt_, wr_, h_, hr_, ts_ = live.pop(idx - 1)
                for dc in range(8):
                    py, pyr = gen.next()
                    for m in range(2):
                        wd = wt_[:, m, 2, :, :].rearrange("p (c a) f -> p c (a f)", c=2)
                        for fc in range(2):
                            kb.op("pe", lambda e, wd=wd, m=m, fc=fc, py=py, h_=h_, dc=dc: e.matmul(py[:, :], lhsT=wd[:, fc, dc * 128:(dc + 1) * 128], rhs=h_[:, m * 2 + fc, :],
                                                                                                  start=(m == 0 and fc == 0), stop=(m == 1 and fc == 1)), reads=[wr_, hr_], writes=[pyr])
                    kb.op("dve", lambda e, dc=dc, py=py, ts_=ts_: e.tensor_tensor(out=r[:, dc, ts_], in0=r[:, dc, ts_], in1=py[:, :], op=ALU.add), reads=[pyr, rr[dc]], writes=[rr[dc]])
            if idx < len(steps) and steps[idx][1] == 0 and steps[idx][0] + 1 < 8:
                wts[steps[idx][0] + 1] = load_pair(steps[idx][0] + 1)

        if CDBG != "moe":
            layer_norm(r, rr, 16, 24, None, None)
        for c in range(8):
            kb.dma("sp", x2T[c * 128:(c + 1) * 128, :], r[:, c, :], reads=[rr[c]], writes=[x2r[c]], final=final_out)
        kb.barrier()


NF = 3104
NOM_TILES = [0, 2, 4, 6, 8, 10, 12, 14]
BATCH = 2
T_ALL = BATCH * S
NCORES = 8
U32 = mybir.dt.uint32


def idx_cols():
    names = []
    for nm in ("kcmp", "vcmp", "ksel", "vsel", "kwin", "vwin", "sx", "sB", "sC", "dt", "lx", "ly"):
        names += [nm + str(rp) for rp in range(4)]
    for k in range(8):
        names += ["q%d_%d" % (k, j) for j in range(4)]
        names.append("gate%d" % k)
    for c in range(4):
        names += ["on%d_%d" % (c, q4) for q4 in range(4)]
        names.append("y%d" % c)
        names.append("l%d" % c)
    return {n: i for i, n in enumerate(names)}


IX = idx_cols()
NIDX = len(IX)


def make_gidx(r):
    g, par = r // 2, r % 2
    t = np.zeros((128, NIDX), np.int64)
    p = np.arange(128)
    def rowF(rp, f):
        f = np.asarray(f)
        k = f // 256
        rows_k = np.where(k < 12, 256, 32)
        return k * 1024 + rp * rows_k + (f - 256 * k)
    rowN = lambda rank, f256: (f256 // 128) * 512 + rank * 128 + f256 % 128
    rowY = lambda rank, ch: (ch // 64) * 256 + rank * 64 + ch % 64
    for rp in range(4):
        for nm, f0 in (("kcmp", 512), ("vcmp", 640), ("ksel", 768), ("vsel", 896), ("kwin", 1024), ("vwin", 1152)):
            t[:, IX[nm + str(rp)]] = rowF(rp, f0 + 64 * g + p)
        t[:, IX["sx%d" % rp]] = rowF(rp, 1304 + 128 * r + p)
        t[:, IX["sB%d" % rp]] = rowF(rp, 1304 + 512 + 64 * g + p)
        t[:, IX["sC%d" % rp]] = rowF(rp, 1304 + 640 + 64 * g + p)
        t[:, IX["dt%d" % rp]] = rp * 8 + 2 * r + p
        t[:, IX["lx%d" % rp]] = rowF(rp, 2080 + 128 * r + p)
        t[:, IX["ly%d" % rp]] = rowF(rp, 2080 + 512 + 128 * r + p)
    for k in range(8):
        i = 2 * k + par
        rp, q4 = i // 4, i % 4
        for j in range(4):
            t[:, IX["q%d_%d" % (k, j)]] = rowF(rp, g * 256 + j * 64 + p) * 4 + q4
        t[:, IX["gate%d" % k]] = rowF(rp, 1280 + 12 * g + p) * 4 + q4
    for c in range(4):
        gg, f256 = c // 2, (c % 2) * 128 + p
        for q4 in range(4):
            i = 4 * r + q4
            t[:, IX["on%d_%d" % (c, q4)]] = rowN(2 * gg + i % 2, f256) * 8 + i // 2
        t[:, IX["y%d" % c]] = rowY(c, p) * 4 + r
        t[:, IX["l%d" % c]] = rowY(c, p) * 4 + r
    t = np.clip(t, 0, None)
    return t.astype(np.uint32)


def emit_A2(kb, xT, xTr, w, agF, agFr, agD, agDr, banks, es, agF_ch=None, rows_cb=None):
    xs = kb.sb([128, 8, TOK], BF16, "xs", es=es)
    xs_r = [Res("xs") for _ in range(8)]
    for c in range(8):
        kb.dma("pool", xs[:, c, :], xT[c * 128:(c + 1) * 128, :], reads=([xTr[c]] if xTr else []), writes=[xs_r[c]])
    FB = 512
    wbuf = [(kb.sb([128, 8, FB], BF16, "wb", es=es), Res("wb")) for _ in range(2)]
    obuf = [(kb.sb([128, 512], BF16, "ob", es=es), Res("ob")) for _ in range(4)]
    obf = [(kb.sb([8, 512], F32, "obd", es=es), Res("obd")) for _ in range(2)]
    wv = w.rearrange("(c p) f -> p c f", p=128)
    it = 0
    nb = 0
    for (c0, ncols, r0) in ((0, 1304, 0), (1816, 1800, 1304)):
        for fb in range((ncols + FB - 1) // FB):
            f0 = fb * FB
            fw_ = min(FB, ncols - f0)
            wt, wr = wbuf[nb % 2]
            nb += 1
            kb.dma("pool", wt[:, :, :fw_], wv[:, :, c0 + f0:c0 + f0 + fw_], writes=[wr])
            if rows_cb is not None:
                rows_cb(r0 + f0)
            for fc in range((fw_ + 127) // 128):
                m = min(128, fw_ - fc * 128)
                for tt in range(TOK // 512):
                    pt, pr = banks[it % 4]
                    ot, orr = obuf[it % 4]
                    for c in range(8):
                        kb.op("pe", lambda e, c=c: e.matmul(pt[:m, :], lhsT=wt[:, c, fc * 128:fc * 128 + m], rhs=xs[:, c, tt * 512:(tt + 1) * 512],
                                                             start=(c == 0), stop=(c == 7)), reads=[wr, xs_r[c]], writes=[pr])
                    if it % 2 == 0:
                        kb.op("act", lambda e: e.copy(out=ot[:m, :], in_=pt[:m, :]), reads=[pr], writes=[orr])
                    else:
                        kb.op("dve", lambda e: e.tensor_copy(out=ot[:m, :], in_=pt[:m, :]), reads=[pr], writes=[orr])
                    row = r0 + f0 + fc * 128
                    agw = [agFr] if agF_ch is None else agF_ch[row // 256:(row + m - 1) // 256 + 1]
                    kb.dma("sp", agF[row:row + m, tt * 512:(tt + 1) * 512], ot[:m, :], reads=[orr] + agw)
                    it += 1
    wt, wr = wbuf[nb % 2]
    kb.dma("pool", wt[:, :, 0:8], wv[:, :, 2584:2592], writes=[wr])
    for tt in range(TOK // 512):
        pt, pr = banks[it % 4]
        it += 1
        for c in range(8):
            kb.op("pe", lambda e, c=c: e.matmul(pt[0:8, :], lhsT=wt[:, c, 0:8], rhs=xs[:, c, tt * 512:(tt + 1) * 512], start=(c == 0), stop=(c == 7)),
                  reads=[wr, xs_r[c]], writes=[pr])
        od, odr = obf[tt % 2]
        kb.op("act", lambda e: e.copy(out=od[:, :], in_=pt[0:8, :]), reads=[pr], writes=[odr])
        kb.dma("sp", agD[:, tt * 512:(tt + 1) * 512], od[:, :], reads=[odr, agDr])


def build_fused(nlayers=2, groups=None, ncores=8, fstop=9, ntiles=8):
    groups = groups or [[0, 1, 2, 3], [4, 5, 6, 7]]
    nc = bass.Bass("TRN2", target_bir_lowering=False)
    es = contextlib.ExitStack()
    with es:
        kb = KB(nc, es)
        I = lambda n, s, dt=F32: kb.dram(n, s, dt, "ExternalInput")
        xT0 = I("xT0", [D, TOK]); pT = I("pT", [2, 256, TOK]); gidx = I("gidx", [128, NIDX], U32)
        w_in = I("w_in", [2, D, 6688])
        peT = I("npeT", [2, 2, 64, 32]); w1 = I("nw1", [2, 2, 2048, 256]); w2 = I("nw2", [2, 2, 256, 64])
        nmask = I("nmask", [128, NMASK, 512]); nE0 = I("nE0", [128, S]); nmisc = I("nmisc", [128, MISC_W])
        svec = I("svec", [2, 128, 16]); srep = I("srep", [2, 128, 8]); scst = I("scst", [128, 4, 128])
        lvec = I("lvec", [2, 128, 8]); lwab = I("lwab", [2, 128, 128]); lwxb = I("lwxb", [2, 128, 128])
        pnsa = I("proj_nsa", [2, 512, D]); pssd = I("proj_ssd", [2, 512, D]); plru = I("proj_lru", [2, 512, D])
        w_out = I("w_out", [2, D, D]); pwg = I("ple_w_gate", [2, D, D]); pwp = I("ple_w_proj", [2, 256, D]); rw = I("router_w", [D, 16])
        ewg = I("exp_w_gate", [2, 16, D, 256]); ewu = I("exp_w_up", [2, 16, D, 256]); ewd = I("exp_w_down", [2, 16, 256, D])
        ccst = I("ccst", [2, 128, CW])
        x2T = kb.dram("x2T", [D, TOK], F32, "ExternalOutput")
        N_ = lambda n, s, dt: kb.dram(n, s, dt, "Internal")
        agF_in = N_("agF_in", [NF, TOK], BF16); agF_out = N_("agF_out", [4 * NF, TOK], BF16)
        agD_in = N_("agD_in", [8, TOK], F32); agD_out = N_("agD_out", [32, TOK], F32)
        nq = 8 * QT
        agN_in = N_("agN_in", [256, nq], BF16); agN_out = N_("agN_out", [1024, nq], BF16)
        agY_in = N_("agY_in", [128, S], BF16); agY_out = N_("agY_out", [512, S], BF16)
        agL_in = N_("agL_in", [128, S], BF16); agL_out = N_("agL_out", [512, S], BF16)
        xcur = N_("xcur", [D, TOK], F32)
        rD_in, rD_out = Res(), Res()
        rF_in_ch = [Res() for _ in range(13)]
        rF_in = MultiRes(rF_in_ch)
        rF_ch = [Res() for _ in range(13)]
        rF_nsa, rF_ssd, rF_lru = MultiRes(rF_ch[0:6]), MultiRes(rF_ch[5:9]), MultiRes(rF_ch[8:13])
        rN_in, rN_out, rY_in, rY_out, rL_in, rL_out = Res(), Res(), Res(), Res(), Res(), Res()
        xcur_r = [Res() for _ in range(8)]
        agF512 = agF_out.rearrange("r (q t) -> (r q) t", t=512)
        agN512 = agN_out.rearrange("r (q t) -> (r q) t", t=512)
        agY2k = agY_out.rearrange("r (q t) -> (r q) t", t=2048)
        agL2k = agL_out.rearrange("r (q t) -> (r q) t", t=2048)
        gx = kb.sb([128, NIDX], U32, "gx"); gxr = Res()
        kb.dma("sp", gx[:], gidx[:, :], writes=[gxr])
        banks = [(kb.ps([128, 512], F32, "bank"), Res("bank", excl=True)) for _ in range(8)]
        rot = lambda items: _rot(items)
        for L in range(nlayers):
            xin = xT0 if L == 0 else xcur
            xin_r = None if L == 0 else xcur_r
            with contextlib.ExitStack() as sa:
                agk = [0]

                def ag_rows(done):
                    while agk[0] < 13 and min((agk[0] + 1) * 256, NF) <= done:
                        k = agk[0]
                        rows = 256 if k < 12 else 32
                        kb.allgather(agF_in[k * 256:k * 256 + rows, :], agF_out[k * 1024:k * 1024 + 4 * rows, :], groups, writes=[rF_in_ch[k], rF_ch[k]])
                        agk[0] += 1

                emit_A2(kb, xin, xin_r, w_in[L], agF_in, rF_in, agD_in, rD_in, banks, sa, agF_ch=rF_in_ch, rows_cb=ag_rows)
                kb.barrier()
            for k in range(agk[0], 13):
                rows = 256 if k < 12 else 32
                kb.allgather(agF_in[k * 256:k * 256 + rows, :], agF_out[k * 1024:k * 1024 + 4 * rows, :], groups, writes=[rF_in_ch[k], rF_ch[k]])
            kb.allgather(agD_in[:, :], agD_out[:, :], groups, writes=[rD_in, rD_out])
            if fstop <= 1:
                break
            with contextlib.ExitStack() as s1:
                emit_nsa2(kb, NOM_TILES[:ntiles], agF_out, agF512, rF_nsa, gx, gxr, IX, peT[L], w1[L], w2[L], nmask, nE0, nmisc, agN_in, rN_in,
                          banks[0:4], banks[4], rot(banks[5:8]), s1)
                kb.barrier()
            for k in range(2):
                kb.allgather(agN_in[k * 128:(k + 1) * 128, :], agN_out[k * 512:(k + 1) * 512, :], groups, writes=[rN_in, rN_out])
            if fstop <= 2:
                break
            with contextlib.ExitStack() as s2:
                pbf = [(banks[6][0][:, 0:32].bitcast(BF16), banks[6][1]), (banks[7][0][:, 0:32].bitcast(BF16), banks[7][1])]
                emit_ssd2(kb, agF_out, rF_ssd, agD_out, rD_out, gx, gxr, IX, svec[L], srep[L], scst, agY_in, rY_in, rot(banks[0:6]), pbf, es=s2)
                kb.barrier()
            for k in range(2):
                kb.allgather(agY_in[k * 64:(k + 1) * 64, :], agY_out[k * 256:(k + 1) * 256, :], groups, writes=[rY_in, rY_out])
            if fstop <= 3:
                break
            with contextlib.ExitStack() as s3:
                emit_lru2(kb, agF_out, rF_lru, gx, gxr, IX, lvec[L], lwab[L], lwxb[L], agL_in, rL_in, rot(banks[0:4]), es=s3)
                kb.barrier()
            for k in range(2):
                kb.allgather(agL_in[k * 64:(k + 1) * 64, :], agL_out[k * 256:(k + 1) * 256, :], groups, writes=[rL_in, rL_out])
            if fstop <= 4:
                break
            last = (L == nlayers - 1)
            with contextlib.ExitStack() as s4:
                emit_C2(kb, xin, agN512, rN_out, agY2k, rY_out, agL2k, rL_out, gx, gxr, IX, pT[L], w_in[L], pnsa[L], pssd[L], plru[L], w_out[L],
                        pwg[L], pwp[L], rw, ewg[L], ewu[L], ewd[L], ccst[L], x2T if last else xcur, xcur_r, last, banks, s4, xTr=xin_r)
                kb.barrier()
        kb.finish(())
        print("fused instructions", kb.ninst, "sems", kb.nsem)
    return nc


def _rot(items):
    r = Rot.__new__(Rot)
    r.t = list(items)
    r.i = 0
    return r


def fused_inputs(d, core):
    b, r = core // 4, core % 4
    g, par = r // 2, r % 2
    tok = slice(core * TOK, (core + 1) * TOK)
    x = d["x"].reshape(T_ALL, D)
    im = {"xT0": np.ascontiguousarray(x[tok].T),
          "pT": np.ascontiguousarray(np.stack([d["p"][L].reshape(T_ALL, 256)[tok].T for L in range(2)])),
          "gidx": make_gidx(r), "w_in": d["w_in"],
          "npeT": np.ascontiguousarray(np.stack([np.stack([d["nsa_pe_k"][L].T, d["nsa_pe_v"][L].T]) for L in range(2)])),
          "nw1": np.stack([np.stack([d["nsa_w1_k"][L], d["nsa_w1_v"][L]]) for L in range(2)]),
          "nw2": np.stack([np.stack([d["nsa_w2_k"][L], d["nsa_w2_v"][L]]) for L in range(2)]),
          "proj_nsa": d["proj_nsa"], "proj_ssd": d["proj_ssd"], "proj_lru": d["proj_lru"], "w_out": d["w_out"],
          "ple_w_gate": d["ple_w_gate"], "ple_w_proj": d["ple_w_proj"], "router_w": d["router_w"],
          "exp_w_gate": d["exp_w_gate"], "exp_w_up": d["exp_w_up"], "exp_w_down": d["exp_w_down"],
          "ccst": np.stack([c_consts(d, L) for L in range(2)]), "scst": ssd_consts()}
    im.update(nsa_consts(par))
    sv, sr, lv, la, lx_ = [], [], [], [], []
    for L in range(2):
        a_, b_ = ssd_vecs(d, L, r)
        sv.append(a_); sr.append(b_)
        a_, b_, c_ = lru_vecs(d, L, r)
        lv.append(a_); la.append(b_); lx_.append(c_)
    im["svec"] = np.stack(sv); im["srep"] = np.stack(sr); im["lvec"] = np.stack(lv); im["lwab"] = np.stack(la); im["lwxb"] = np.stack(lx_)
    return im


def ssd_vecs(d, layer, r):
    g = r // 2
    cw = d["ssd_conv_w"][layer]; cb = d["ssd_conv_b"][layer]
    v = np.zeros((128, 16), np.float32)
    xs_ = slice(128 * r, 128 * r + 128); Bs_ = slice(512 + 64 * g, 512 + 64 * g + 64); Cs_ = slice(640 + 64 * g, 640 + 64 * g + 64)
    v[:, 0:4] = cw[:, xs_].T; v[:, 4] = cb[xs_]
    v[:64, 5:9] = cw[:, Bs_].T; v[:64, 9] = cb[Bs_]
    v[:64, 10:14] = cw[:, Cs_].T; v[:64, 14] = cb[Cs_]
    rp = np.zeros((128, 8), np.float32)
    hh = slice(2 * r, 2 * r + 2)
    rp[:, 0:2] = d["ssd_dt_bias"][layer][hh][None, :]
    rp[:, 2:4] = d["ssd_a_log"][layer][hh][None, :]
    rp[:, 4:6] = d["ssd_d"][layer][hh][None, :]
    return v, rp


def lru_vecs(d, layer, r):
    ch = slice(128 * r, 128 * r + 128)
    v = np.zeros((128, 8), np.float32)
    v[:, 0:4] = d["lru_conv_w"][layer][:, ch].T
    v[:, 4] = d["lru_conv_b"][layer][ch]
    v[:, 5] = d["lru_ba"][layer][ch]
    v[:, 6] = d["lru_bx"][layer][ch]
    v[:, 7] = d["lru_lambda"][layer][ch]
    wab_ = np.zeros((128, 128), np.float32)
    wxb_ = np.zeros((128, 128), np.float32)
    for k in range(2):
        wab_[64 * k:64 * k + 64, 64 * k:64 * k + 64] = d["lru_wa"][layer][2 * r + k]
        wxb_[64 * k:64 * k + 64, 64 * k:64 * k + 64] = d["lru_wx"][layer][2 * r + k]
    return v, wab_, wxb_


def kernel(**inputs):
    d = {k: np.asarray(v) for k, v in inputs.items()}
    nc = build_fused()
    in_maps = [fused_inputs(d, c) for c in range(NCORES)]
    res = run_bass_kernel_spmd(nc, in_maps, core_ids=list(range(NCORES))).results
    x = np.concatenate([res[c]["x2T"].T for c in range(NCORES)], axis=0)
    return np.ascontiguousarray(x.reshape(BATCH, S, D).astype(np.float32))
```
